# Optimizing a Trainium2 kernel written in Bass

```python
import math
import jax, jax.numpy as jnp
from jax import lax
import numpy as np

D_MODEL = 1024
BATCH = 8
SEQ = 4096
DEPTH = 1

MIX_WIDTH = D_MODEL
ATTN_WIDTH = MIX_WIDTH // 2
SSM_WIDTH = MIX_WIDTH - ATTN_WIDTH
HEAD_DIM = 64
N_HEADS = ATTN_WIDTH // HEAD_DIM
N_KV = 2
GQA_R = N_HEADS // N_KV
CMP_LEN = 32
CMP_STRIDE = 16
CMP_HIDDEN = 256
SEL_BLOCK = 64
SEL_TOPK = 16
WINDOW = 512
Q_BLOCK = 128
SSM_GROUP = 16
SSM_GROUPS = SSM_WIDTH // SSM_GROUP
SSM_STATE = 64
D_FF = 2816
EPS = 1e-6

Q_COLS = N_HEADS * HEAD_DIM
KV_COLS = N_KV * HEAD_DIM
GATE_COLS = N_HEADS * 3
IN_SIZES = [Q_COLS] + [KV_COLS] * 6 + [GATE_COLS, SSM_WIDTH]
IN_COLS = sum(IN_SIZES)

kernel_name = "hymba_nsa_s5_macaron"


def rmsnorm(x, g):
    xf = x.astype(jnp.float32)
    r = lax.rsqrt(jnp.mean(xf * xf, axis=-1, keepdims=True) + EPS)
    return (xf * r).astype(x.dtype) * g


def swiglu(x, w_gate, w_up, w_down):
    return (jax.nn.silu(x @ w_gate) * (x @ w_up)) @ w_down


def masked_softmax(s, mask):
    s = jnp.where(mask, s.astype(jnp.float32), -jnp.inf)
    m = jnp.max(s, axis=-1, keepdims=True)
    m = jnp.where(jnp.isfinite(m), m, 0.0)
    e = jnp.where(mask, jnp.exp(s - m), 0.0)
    return e / jnp.maximum(jnp.sum(e, axis=-1, keepdims=True), 1e-30)


def compress_blocks(kv, pos, w1, b1, w2):
    b, t, g, d = kv.shape
    n_half = CMP_LEN // CMP_STRIDE
    nc = t // CMP_STRIDE - n_half + 1
    halves = kv.reshape(b, t // CMP_STRIDE, CMP_STRIDE, g, d)
    blocks = jnp.concatenate([halves[:, j:j + nc] for j in range(n_half)], axis=2)
    blocks = blocks + pos[:, None, :]
    blocks = blocks.transpose(0, 3, 1, 2, 4).reshape(b, g, nc, CMP_LEN * d)
    return jax.nn.gelu(blocks @ w1 + b1) @ w2


def nsa_attention(q, kc, vc, ks, vs, kw, vw, gates):
    b, t = q.shape[:2]
    d = HEAD_DIM
    nq = t // Q_BLOCK
    nb = t // SEL_BLOCK
    nc = kc.shape[2]
    topk = min(SEL_TOPK, nb)
    scale = HEAD_DIM ** -0.5

    q_blocks = q.reshape(b, nq, Q_BLOCK, N_KV, GQA_R, d).transpose(1, 0, 3, 4, 2, 5)
    g_blocks = gates.reshape(b, nq, Q_BLOCK, N_KV, GQA_R, 3).transpose(1, 0, 3, 4, 2, 5)
    ks_blocks = ks.reshape(b, nb, SEL_BLOCK, N_KV, d).transpose(0, 3, 1, 2, 4)
    vs_blocks = vs.reshape(b, nb, SEL_BLOCK, N_KV, d).transpose(0, 3, 1, 2, 4)
    pad = ((0, 0), (0, 0), (WINDOW, 0), (0, 0))
    kw_pad = jnp.pad(kw.transpose(0, 2, 1, 3), pad)
    vw_pad = jnp.pad(vw.transpose(0, 2, 1, 3), pad)

    cmp_start = jnp.arange(nc) * CMP_STRIDE
    cmp_end = cmp_start + CMP_LEN - 1
    sel_start = jnp.arange(nb) * SEL_BLOCK
    overlap = ((cmp_start[:, None] < sel_start[None, :] + SEL_BLOCK)
               & (cmp_start[:, None] + CMP_LEN > sel_start[None, :])).astype(jnp.float32)
    blk = jnp.arange(nb)
    b_idx = jnp.arange(b)[:, None, None, None]
    g_idx = jnp.arange(N_KV)[None, :, None, None]

    def one_block(args):
        c, qb, gb = args
        tq = c * Q_BLOCK + jnp.arange(Q_BLOCK)
        s_c = jnp.einsum('bgrqd,bgnd->bgrqn', qb, kc) * scale
        p_c = masked_softmax(s_c, cmp_end[None, :] <= tq[:, None])
        o_cmp = jnp.einsum('bgrqn,bgnd->bgrqd', p_c.astype(vc.dtype), vc)
        imp = jnp.einsum('bgrqn,nj->bgqj', p_c, overlap)
        cur = tq // SEL_BLOCK
        forced = (blk[None, :] == 0) | (blk[None, :] == cur[:, None]) | (blk[None, :] == cur[:, None] - 1)
        valid = sel_start[None, :] <= tq[:, None]
        score = jnp.where(forced, jnp.inf, jnp.where(valid, imp, -jnp.inf))
        _, idx = lax.top_k(score, topk)
        k_sel = ks_blocks[b_idx, g_idx, idx]
        v_sel = vs_blocks[b_idx, g_idx, idx]
        kpos = idx[..., None] * SEL_BLOCK + jnp.arange(SEL_BLOCK)
        sel_mask = (kpos <= tq[None, None, :, None, None]).reshape(b, N_KV, 1, Q_BLOCK, topk * SEL_BLOCK)
        s_s = jnp.einsum('bgrqd,bgqksd->bgrqks', qb, k_sel) * scale
        p_s = masked_softmax(s_s.reshape(b, N_KV, GQA_R, Q_BLOCK, topk * SEL_BLOCK), sel_mask)
        o_slc = jnp.einsum('bgrqn,bgqnd->bgrqd', p_s.astype(v_sel.dtype),
                           v_sel.reshape(b, N_KV, Q_BLOCK, topk * SEL_BLOCK, d))
        kw_b = lax.dynamic_slice_in_dim(kw_pad, c * Q_BLOCK, WINDOW + Q_BLOCK, axis=2)
        vw_b = lax.dynamic_slice_in_dim(vw_pad, c * Q_BLOCK, WINDOW + Q_BLOCK, axis=2)
        wpos = c * Q_BLOCK - WINDOW + jnp.arange(WINDOW + Q_BLOCK)
        w_mask = ((wpos[None, :] <= tq[:, None]) & (wpos[None, :] > tq[:, None] - WINDOW)
                  & (wpos[None, :] >= 0))
        s_w = jnp.einsum('bgrqd,bgkd->bgrqk', qb, kw_b) * scale
        p_w = masked_softmax(s_w, w_mask)
        o_win = jnp.einsum('bgrqk,bgkd->bgrqd', p_w.astype(vw_b.dtype), vw_b)
        return gb[..., 0:1] * o_cmp + gb[..., 1:2] * o_slc + gb[..., 2:3] * o_win

    out = lax.map(one_block, (jnp.arange(nq), q_blocks, g_blocks))
    return out.transpose(1, 0, 4, 2, 3, 5).reshape(b, t, N_HEADS * d)


def s5_mixer(u, lam_re, lam_im, log_dt, b_re, b_im, c_re, c_im, d_skip, w_glu, b_glu):
    bsz, t, _ = u.shape
    uf = u.astype(jnp.float32).reshape(bsz, t, SSM_GROUPS, SSM_GROUP)
    lr = lam_re.astype(jnp.float32)
    li = lam_im.astype(jnp.float32)
    dt = jnp.exp(log_dt.astype(jnp.float32))[:, None]
    mag = jnp.exp(lr * dt)
    ab_re = mag * jnp.cos(li * dt)
    ab_im = mag * jnp.sin(li * dt)
    nr = ab_re - 1.0
    den = lr * lr + li * li
    f_re = (nr * lr + ab_im * li) / den
    f_im = (ab_im * lr - nr * li) / den
    br = b_re.astype(jnp.float32)
    bi = b_im.astype(jnp.float32)
    bb_re = f_re[..., None] * br - f_im[..., None] * bi
    bb_im = f_re[..., None] * bi + f_im[..., None] * br
    bu_re = jnp.einsum('btgh,gph->btgp', uf, bb_re)
    bu_im = jnp.einsum('btgh,gph->btgp', uf, bb_im)
    a_re = jnp.broadcast_to(ab_re, (1, t, SSM_GROUPS, SSM_STATE))
    a_im = jnp.broadcast_to(ab_im, (1, t, SSM_GROUPS, SSM_STATE))

    def combine(e1, e2):
        a1r, a1i, b1r, b1i = e1
        a2r, a2i, b2r, b2i = e2
        return (a2r * a1r - a2i * a1i, a2r * a1i + a2i * a1r,
                a2r * b1r - a2i * b1i + b2r, a2r * b1i + a2i * b1r + b2i)

    _, _, xr, xi = lax.associative_scan(combine, (a_re, a_im, bu_re, bu_im), axis=1)
    y = (jnp.einsum('btgp,ghp->btgh', xr, c_re.astype(jnp.float32))
         - jnp.einsum('btgp,ghp->btgh', xi, c_im.astype(jnp.float32))
         + d_skip.astype(jnp.float32) * uf)
    y = jax.nn.gelu(y.reshape(bsz, t, SSM_WIDTH).astype(u.dtype))
    return y * jax.nn.sigmoid(y @ w_glu + b_glu)


def setup_inputs(seed: int = 0) -> dict:
    key = jax.random.key(seed)
    ks = jax.random.split(key, 40)
    L = DEPTH

    def nrm(k, shape, scale):
        return jax.random.normal(k, shape, jnp.float32) * scale

    def gain(k, shape):
        return 1.0 + 0.01 * jax.random.normal(k, shape, jnp.float32)

    n_idx = jnp.arange(SSM_STATE, dtype=jnp.float32)
    lam_re = -0.5 + 0.01 * jax.random.normal(ks[17], (L, SSM_GROUPS, SSM_STATE), jnp.float32)
    lam_im = math.pi * n_idx + 0.01 * jax.random.normal(ks[18], (L, SSM_GROUPS, SSM_STATE), jnp.float32)
    log_dt = jax.random.uniform(ks[19], (L, SSM_GROUPS), jnp.float32, math.log(1e-3), math.log(1e-1))
    return {
        "x": nrm(ks[0], (BATCH, SEQ, D_MODEL), 1.0),
        "ffn1_norm": gain(ks[1], (L, D_MODEL)),
        "ffn1_w_gate": nrm(ks[2], (L, D_MODEL, D_FF), D_MODEL ** -0.5),
        "ffn1_w_up": nrm(ks[3], (L, D_MODEL, D_FF), D_MODEL ** -0.5),
        "ffn1_w_down": nrm(ks[4], (L, D_FF, D_MODEL), D_FF ** -0.5),
        "mix_norm": gain(ks[5], (L, D_MODEL)),
        "w_in": nrm(ks[6], (L, D_MODEL, IN_COLS), D_MODEL ** -0.5),
        "gate_bias": nrm(ks[7], (L, N_HEADS, 3), 0.01),
        "cmp_pos_k": nrm(ks[8], (L, CMP_LEN, HEAD_DIM), 0.02),
        "cmp_pos_v": nrm(ks[9], (L, CMP_LEN, HEAD_DIM), 0.02),
        "cmp_k_w1": nrm(ks[10], (L, CMP_LEN * HEAD_DIM, CMP_HIDDEN), (CMP_LEN * HEAD_DIM) ** -0.5),
        "cmp_k_b1": nrm(ks[11], (L, CMP_HIDDEN), 0.01),
        "cmp_k_w2": nrm(ks[12], (L, CMP_HIDDEN, HEAD_DIM), CMP_HIDDEN ** -0.5),
        "cmp_v_w1": nrm(ks[13], (L, CMP_LEN * HEAD_DIM, CMP_HIDDEN), (CMP_LEN * HEAD_DIM) ** -0.5),
        "cmp_v_b1": nrm(ks[14], (L, CMP_HIDDEN), 0.01),
        "cmp_v_w2": nrm(ks[15], (L, CMP_HIDDEN, HEAD_DIM), CMP_HIDDEN ** -0.5),
        "s5_lambda_re": lam_re,
        "s5_lambda_im": lam_im,
        "s5_log_dt": log_dt,
        "s5_b_re": nrm(ks[20], (L, SSM_GROUPS, SSM_STATE, SSM_GROUP), (2 * SSM_GROUP) ** -0.5),
        "s5_b_im": nrm(ks[21], (L, SSM_GROUPS, SSM_STATE, SSM_GROUP), (2 * SSM_GROUP) ** -0.5),
        "s5_c_re": nrm(ks[22], (L, SSM_GROUPS, SSM_GROUP, SSM_STATE), (2 * SSM_STATE) ** -0.5),
        "s5_c_im": nrm(ks[23], (L, SSM_GROUPS, SSM_GROUP, SSM_STATE), (2 * SSM_STATE) ** -0.5),
        "s5_d": nrm(ks[24], (L, SSM_GROUPS, SSM_GROUP), 1.0),
        "s5_w_glu": nrm(ks[25], (L, SSM_WIDTH, SSM_WIDTH), SSM_WIDTH ** -0.5),
        "s5_b_glu": nrm(ks[26], (L, SSM_WIDTH), 0.01),
        "attn_out_norm": gain(ks[27], (L, ATTN_WIDTH)),
        "ssm_out_norm": gain(ks[28], (L, SSM_WIDTH)),
        "w_out": nrm(ks[29], (L, MIX_WIDTH, D_MODEL), MIX_WIDTH ** -0.5),
        "ffn2_norm": gain(ks[30], (L, D_MODEL)),
        "ffn2_w_gate": nrm(ks[31], (L, D_MODEL, D_FF), D_MODEL ** -0.5),
        "ffn2_w_up": nrm(ks[32], (L, D_MODEL, D_FF), D_MODEL ** -0.5),
        "ffn2_w_down": nrm(ks[33], (L, D_FF, D_MODEL), D_FF ** -0.5),
        "final_norm": gain(ks[34], (D_MODEL,)),
    }


def reference(x, ffn1_norm, ffn1_w_gate, ffn1_w_up, ffn1_w_down, mix_norm, w_in, gate_bias,
              cmp_pos_k, cmp_pos_v, cmp_k_w1, cmp_k_b1, cmp_k_w2, cmp_v_w1, cmp_v_b1, cmp_v_w2,
              s5_lambda_re, s5_lambda_im, s5_log_dt, s5_b_re, s5_b_im, s5_c_re, s5_c_im, s5_d,
              s5_w_glu, s5_b_glu, attn_out_norm, ssm_out_norm, w_out,
              ffn2_norm, ffn2_w_gate, ffn2_w_up, ffn2_w_down, final_norm):
    b, t, _ = x.shape
    split_at = [int(v) for v in np.cumsum(IN_SIZES)[:-1]]
    for l in range(DEPTH):
        x = x + 0.5 * swiglu(rmsnorm(x, ffn1_norm[l]), ffn1_w_gate[l], ffn1_w_up[l], ffn1_w_down[l])
        h = rmsnorm(x, mix_norm[l])
        proj = h @ w_in[l]
        pq, pkc, pvc, pks, pvs, pkw, pvw, pg, pu = jnp.split(proj, split_at, axis=-1)
        q = pq.reshape(b, t, N_HEADS, HEAD_DIM)
        kv_shape = (b, t, N_KV, HEAD_DIM)
        kc = compress_blocks(pkc.reshape(kv_shape), cmp_pos_k[l], cmp_k_w1[l], cmp_k_b1[l], cmp_k_w2[l])
        vc = compress_blocks(pvc.reshape(kv_shape), cmp_pos_v[l], cmp_v_w1[l], cmp_v_b1[l], cmp_v_w2[l])
        gates = jax.nn.sigmoid(pg.reshape(b, t, N_HEADS, 3) + gate_bias[l])
        attn = nsa_attention(q, kc, vc, pks.reshape(kv_shape), pvs.reshape(kv_shape),
                             pkw.reshape(kv_shape), pvw.reshape(kv_shape), gates)
        ssm = s5_mixer(pu, s5_lambda_re[l], s5_lambda_im[l], s5_log_dt[l], s5_b_re[l], s5_b_im[l],
                       s5_c_re[l], s5_c_im[l], s5_d[l], s5_w_glu[l], s5_b_glu[l])
        mixed = jnp.concatenate([rmsnorm(attn, attn_out_norm[l]), rmsnorm(ssm, ssm_out_norm[l])], axis=-1)
        x = x + mixed @ w_out[l]
        x = x + 0.5 * swiglu(rmsnorm(x, ffn2_norm[l]), ffn2_w_gate[l], ffn2_w_up[l], ffn2_w_down[l])
    return rmsnorm(x, final_norm)
```

```python
import contextlib
import numpy as np
import concourse.bass as bass
import concourse.mybir as mybir
from concourse.bass_utils import run_bass_kernel_spmd

F32 = mybir.dt.float32
BF16 = mybir.dt.bfloat16
ALU = mybir.AluOpType
AF = mybir.ActivationFunctionType

T = 4096
D = 1024
FF = 2816
NF = FF // 128
EPS = 1e-6
BIGM = 30000.0
NCORES = 8
GELU_C = 1.5957691216057308


class TR:
    def __init__(self, nc, es):
        self.nc, self.es = nc, es
        self.eng = dict(pe=nc.tensor, dve=nc.vector, act=nc.scalar, pool=nc.gpsimd, sp=nc.sync)
        self.sem, self.cnt = {}, {}
        self.seen = {e: {} for e in self.eng}
        for e in self.eng:
            self.sem[e] = es.enter_context(nc.semaphore("s_" + e))
            self.cnt[e] = 0
        self.bs = {}

    def _st(self, k):
        s = self.bs.get(k)
        if s is None:
            s = self.bs[k] = ({}, {})
        return s

    def _deps(self, r, w):
        d = {}
        for k in r:
            for sk, v in self._st(k)[0].items():
                if d.get(sk, 0) < v:
                    d[sk] = v
        for k in w:
            s = self._st(k)
            for dd in s:
                for sk, v in dd.items():
                    if d.get(sk, 0) < v:
                        d[sk] = v
        return d

    def _wait(self, e, d):
        for k, v in d.items():
            if k == e and e == 'pe':
                continue
            if self.seen[e].get(k, 0) >= v:
                continue
            self.eng[e].wait_ge(self.sem[k], v)
            self.seen[e][k] = v

    def _mark(self, src, v, r, w):
        for k in r:
            self._st(k)[1][src] = v
        for k in w:
            self._st(k)[0][src] = v

    def op(self, e, fn, r=(), w=()):
        self._wait(e, self._deps(r, w))
        ins = fn(self.eng[e])
        self.cnt[e] += 1
        ins.then_inc(self.sem[e], 1)
        self._mark(e, self.cnt[e], r, w)

    def dma(self, q, ch, out, in_, r=(), w=()):
        if ch not in self.sem:
            self.sem[ch] = self.es.enter_context(self.nc.semaphore("c_" + ch))
            self.cnt[ch] = 0
        self._wait(q, self._deps(r, w))
        ins = self.eng[q].dma_start(out=out, in_=in_)
        self.cnt[ch] += 16
        ins.then_inc(self.sem[ch], 16)
        self._mark(ch, self.cnt[ch], r, w)

    def barrier(self):
        allk = {k: v for k, v in self.cnt.items() if v > 0}
        for e in self.eng:
            self._wait(e, allk)


def build(dbg=False, stop=9):
    nc = bass.Bass("TRN2", target_bir_lowering=False)
    es = contextlib.ExitStack()
    with es:
        tr = TR(nc, es)

        def din(name, shape):
            return nc.dram_tensor(name, list(shape), F32, kind="ExternalInput").ap()

        def dscr(name, shape, dt):
            kind = "ExternalOutput" if dbg else "Internal"
            return nc.dram_tensor(name, list(shape), dt, kind=kind).ap()

        def sb(name, shape, dt):
            return es.enter_context(nc.sbuf_tensor(name, list(shape), dt))

        xT = din("xT", [D, T])
        wgs = [din("wg1", [D, FF]), din("wg2", [D, FF])]
        wus = [din("wu1", [D, FF]), din("wu2", [D, FF])]
        wds = [din("wd1", [FF, D]), din("wd2", [FF, D])]
        w_in = din("w_in", [D, 1816])
        w_out = din("w_out", [D, D])
        w_glu = din("w_glu", [512, 512])
        gcols = din("gcols", [128, 36])
        grow_attn = din("grow_attn", [1, 512])
        gate_bias = din("gate_bias", [1, 24])
        cw1 = [din("cw1k", [2048, 256]), din("cw1v", [2048, 256])]
        cw2 = [din("cw2k", [256, 64]), din("cw2v", [256, 64])]
        cb1 = din("cb1", [128, 4])
        posT = din("posT", [64, 64])
        s5_rows = din("s5_rows", [3, 2048])
        s5_cols = din("s5_cols", [128, 48])
        s5_rep = din("s5_rep", [128, 4 * 3 * 64])
        s5_bT = din("s5_bT", [128, 4 * 2 * 64])
        s5_c = din("s5_c", [128, 16 * 2 * 16])
        s5_dcol = din("s5_dcol", [128, 4])
        bglu_col = din("bglu_col", [128, 4])
        c_ident = din("c_ident", [128, 128])
        c_tri = din("c_tri", [128, 128])
        c_E0 = din("c_E0", [64, T])
        c_CB = din("c_CB", [128, 512])
        c_WB = din("c_WB", [128, 512])
        c_CM = din("c_CM", [128, 512])
        c_BD = din("c_BD", [128, 384])
        c_U0 = din("c_U0", [128, 128])
        c_L0 = din("c_L0", [128, 128])
        c_ovl = din("c_ovl", [128, 128])
        c_maskB = din("c_maskB", [128, 8])
        c_maskC = din("c_maskC", [128, 32])
        c_iota = din("c_iota", [128, 128])
        c_jcol = din("c_jcol", [128, 1])

        yT = nc.dram_tensor("yT", [D, T], F32, kind="ExternalOutput").ap()
        x1T = dscr("x1T", [D, T], F32)
        h2T = dscr("h2T", [D, T], BF16)
        qTd = dscr("qTd", [64, 32 * 8 * 128], BF16)
        uTd = dscr("uTd", [128, 4 * T], BF16)
        mixT = dscr("mixT", [D, T], BF16)

        GC = sb("GC", [128, 36], F32)
        ONES = sb("ONES", [128, 128], BF16)
        IDN = sb("IDN", [128, 128], BF16)
        EPSC = sb("EPSC", [128, 1], F32)
        tr.dma('sp', 'const', GC[:], gcols[:, :], w=['c'])
        tr.dma('pool', 'const', IDN[:], c_ident[:, :], w=['c'])
        tr.op('dve', lambda e: e.memset(ONES[:], 1.0), w=['c'])
        tr.op('dve', lambda e: e.memset(EPSC[:], 0.0), w=['c'])

        pb = [es.enter_context(nc.psum_tensor("pb%d" % i, [128, 512], F32)) for i in range(8)]

        def xview(dram, t0, n):
            return dram.rearrange("(k p) t -> p k t", p=128)[:, :, t0:t0 + n]

        def ffn_phase(ph):
            with contextlib.ExitStack() as fs:
                def fsb(name, shape, dt):
                    return fs.enter_context(nc.sbuf_tensor("%s_%d" % (name, ph), list(shape), dt))
                WG = fsb("WG", [128, 8, FF], BF16)
                WU = fsb("WU", [128, 8, FF], BF16)
                WD = fsb("WD", [128, NF, D], BF16)
                XT = [fsb("XT%d" % i, [128, 8, 512], F32) for i in range(2)]
                H = fsb("H", [128, 8, 512], BF16)
                AT = fsb("AT", [128, NF, 512], BF16)
                SG = [fsb("SG%d" % i, [128, 512], BF16) for i in range(2)]
                RS = [fsb("RS%d" % i, [128, 512], F32) for i in range(2)]
                wgd, wud, wdd = wgs[ph - 1], wus[ph - 1], wds[ph - 1]
                for k in range(8):
                    tr.dma('pool', 'wg', WG[:, k, :], wgd[k * 128:(k + 1) * 128, :], w=['WG'])
                    tr.dma('pool', 'wu', WU[:, k, :], wud[k * 128:(k + 1) * 128, :], w=['WU'])
                for f in range(NF):
                    tr.dma('pool', 'wd', WD[:, f, :], wdd[f * 128:(f + 1) * 128, :], w=['WD'])
                src = xT if ph == 1 else x1T
                g0 = 0 if ph == 1 else 16
                hkeys = ['h%d' % k for k in range(8)]
                atk2 = ['at%d' % f for f in range(14, 22)]

                def xs(tt):
                    return XT[tt % 2], 'xt%d' % (tt % 2)

                def load(tt):
                    X, xk = xs(tt)
                    tr.dma('sp', 'xin%d' % (tt % 2), X[:], xview(src, tt * 512, 512), w=[xk])

                def norm_a(X, xk, HB, hk):
                    tr.op('act', lambda e: e.activation(out=HB, in_=X[:], func=AF.Square), r=[xk], w=hk)

                def norm_b(HBk, hk, PN, pnk):
                    for k in range(8):
                        tr.op('pe', lambda e, k=k: e.matmul(out=PN[:], lhsT=ONES[:], rhs=HBk(k),
                                                           start=(k == 0), stop=(k == 7)), r=[hk[k], 'c'], w=[pnk])

                def norm_c(X, xk, HBk, hk, PN, pnk, R, rk, goff, inplace=False):
                    tr.op('dve', lambda e: e.tensor_scalar(out=R[:], in0=PN[:], scalar1=1.0 / D, scalar2=EPS,
                                                          op0=ALU.mult, op1=ALU.add), r=[pnk], w=[rk])
                    tr.op('act', lambda e: e.activation(out=R[:], in_=R[:], func=AF.Sqrt), r=[rk], w=[rk])
                    tr.op('dve', lambda e: e.reciprocal(out=R[:], in_=R[:]), r=[rk], w=[rk])
                    for k in range(8):
                        dst = X[:, k, :] if inplace else HBk(k)
                        tr.op('dve', lambda e, k=k, dst=dst: e.scalar_tensor_tensor(
                            out=dst, in0=X[:, k, :], scalar=GC[:, goff + k:goff + k + 1], in1=R[:],
                            op0=ALU.mult, op1=ALU.mult), r=[xk, rk, 'c'], w=[xk if inplace else hk[k]])

                Hk = lambda k: H[:, k, :]
                A2k = lambda k: AT[:, 14 + k, :]

                def post_a(tt):
                    X, xk = xs(tt)
                    if ph == 1:
                        tr.dma('sp', 'xout%d' % (tt % 2), xview(x1T, tt * 512, 512), X[:], r=[xk], w=['x1T'])
                    norm_a(X, xk, AT[:, 14:22, :], atk2)

                def post_bc(tt):
                    X, xk = xs(tt)
                    norm_b(A2k, atk2, pb[7], 'pn2')
                    if ph == 1:
                        norm_c(X, xk, A2k, atk2, pb[7], 'pn2', RS[1], 'rs1', 8)
                        tr.dma('sp', 'hout', xview(h2T, tt * 512, 512), AT[:, 14:22, :], r=atk2, w=['h2T'])
                    else:
                        norm_c(X, xk, A2k, atk2, pb[7], 'pn2', RS[1], 'rs1', 24, inplace=True)
                        tr.dma('sp', 'yout%d' % (tt % 2), xview(yT, tt * 512, 512), X[:], r=[xk], w=['yT'])

                load(0)
                X0, xk0 = xs(0)
                norm_a(X0, xk0, H[:], hkeys)
                norm_b(Hk, hkeys, pb[6], 'pn')
                norm_c(X0, xk0, Hk, hkeys, pb[6], 'pn', RS[0], 'rs0', g0)
                for tt in range(8):
                    X, xk = xs(tt)
                    for f in range(NF):
                        b = f % 2
                        for k in range(8):
                            tr.op('pe', lambda e, k=k, f=f, b=b: e.matmul(
                                out=pb[b][:], lhsT=WG[:, k, f * 128:(f + 1) * 128], rhs=H[:, k, :],
                                start=(k == 0), stop=(k == 7)), r=['WG', hkeys[k]], w=['pg%d' % b])
                        for k in range(8):
                            tr.op('pe', lambda e, k=k, f=f, b=b: e.matmul(
                                out=pb[2 + b][:], lhsT=WU[:, k, f * 128:(f + 1) * 128], rhs=H[:, k, :],
                                start=(k == 0), stop=(k == 7)), r=['WU', hkeys[k]], w=['pu%d' % b])
                        tr.op('act', lambda e, b=b: e.activation(out=SG[b][:], in_=pb[b][:], func=AF.Silu),
                              r=['pg%d' % b], w=['sg%d' % b])
                        tr.op('dve', lambda e, b=b, f=f: e.tensor_tensor(out=AT[:, f, :], in0=SG[b][:],
                                                                         in1=pb[2 + b][:], op=ALU.mult),
                              r=['sg%d' % b, 'pu%d' % b], w=['at%d' % f])
                        if f == 2 and tt > 0:
                            post_bc(tt - 1)
                    if tt + 1 < 8:
                        load(tt + 1)
                        Xn, xkn = xs(tt + 1)
                        norm_a(Xn, xkn, H[:], hkeys)
                    for dk in range(8):
                        b = dk % 2
                        for f in range(NF):
                            tr.op('pe', lambda e, f=f, dk=dk, b=b: e.matmul(
                                out=pb[4 + b][:], lhsT=WD[:, f, dk * 128:(dk + 1) * 128], rhs=AT[:, f, :],
                                start=(f == 0), stop=(f == NF - 1)), r=['WD', 'at%d' % f], w=['pd%d' % b])
                        tr.op('dve', lambda e, dk=dk, b=b, X=X: e.scalar_tensor_tensor(
                            out=X[:, dk, :], in0=pb[4 + b][:], scalar=0.5, in1=X[:, dk, :],
                            op0=ALU.mult, op1=ALU.add), r=['pd%d' % b, xk], w=[xk])
                        if dk == 2 and tt + 1 < 8:
                            norm_b(Hk, hkeys, pb[6], 'pn')
                            norm_c(Xn, xkn, Hk, hkeys, pb[6], 'pn', RS[0], 'rs0', g0)
                    post_a(tt)
                post_bc(7)
            tr.barrier()

        def attention_phase():
            with contextlib.ExitStack() as a_:
                def asb(name, shape, dt):
                    return a_.enter_context(nc.sbuf_tensor(name, list(shape), dt))
                CBt = asb("CBt", [128, 512], BF16)
                WBt = asb("WBt", [128, 512], BF16)
                CMt = asb("CMt", [128, 512], BF16)
                BDt = asb("BDt", [128, 384], BF16)
                U0t = asb("U0t", [128, 128], F32)
                L0t = asb("L0t", [128, 128], F32)
                GAT = asb("GAT", [128, 512], F32)
                QN = [asb("QN%d" % i, [128, 512], BF16) for i in range(2)]
                ET = [asb("ET%d" % i, [128, 512], BF16) for i in range(3)]
                SCO = asb("SCO", [128, 64], F32)
                M8 = asb("M8", [128, 16], F32)
                WK = asb("WK", [128, 64], F32)
                SELW = asb("SELW", [128, 128], BF16)
                RZ = asb("RZ", [128, 4], F32)
                DEN = asb("DEN", [128, 12], F32)
                ATT = asb("ATT", [128, 512], F32)
                ATN = asb("ATN", [128, 512], BF16)
                JUNK = asb("JUNK", [128, 512], F32)
                SSQ = asb("SSQ", [128, 1], F32)
                MST = asb("MST", [128, 4, 128], BF16)
                tr.dma('pool', 'aconst', CBt[:], c_CB[:, :], w=['ac'])
                tr.dma('pool', 'aconst', WBt[:], c_WB[:, :], w=['ac'])
                tr.dma('pool', 'aconst', CMt[:], c_CM[:, :], w=['ac'])
                tr.dma('pool', 'aconst', BDt[:], c_BD[:, :], w=['ac'])
                tr.dma('sp', 'aconst', U0t[:], c_U0[:, :], w=['ac'])
                tr.dma('sp', 'aconst', L0t[:], c_L0[:, :], w=['ac'])
                tr.dma('sp', 'aconst', GAT[:], grow_attn.partition_broadcast(128), w=['ac'])
                tr.op('dve', lambda e: e.memset(SELW[:], 0.0), w=['selw'])
                OS = pb[3][:, 0:260].rearrange("p (r x) -> p r x", x=65)
                OW = pb[4][:, 0:260].rearrange("p (r x) -> p r x", x=65)
                OC = pb[5][:, 0:260].rearrange("p (r x) -> p r x", x=65)
                IM = pb[6][:, 0:256].rearrange("p (r x) -> p r x", x=64)
                PT = pb[7][:, 0:64].bitcast(BF16)
                PT4 = pb[7][:, 64:320].bitcast(BF16)
                sring = [0]
                ering = [0]

                def nexts():
                    b = sring[0] % 3
                    sring[0] += 1
                    return pb[b], 'ps%d' % b

                def nexte():
                    b = ering[0] % 3
                    ering[0] += 1
                    return ET[b], 'et%d' % b

                qview = qTd.rearrange("p (c x) -> p c x", c=32)
                QN3 = [QN[0], QN[1], asb("QN2", [128, 512], BF16)]
                OCS = [asb("OCS%d" % i, [128, 4, 65], F32) for i in range(2)]
                OSS = [asb("OSS%d" % i, [128, 4, 65], F32) for i in range(2)]
                OWS = [asb("OWS%d" % i, [128, 4, 65], F32) for i in range(2)]
                ATN2 = [ATN, asb("ATN1", [128, 512], BF16)]
                NIT = 64
                mixv = mixT.rearrange("(k p) t -> p k t", p=128)
                pending = []

                def qload(it):
                    c, g = divmod(it, 2)
                    sl = it % 3
                    tr.dma('sp', 'qin%d' % sl, QN3[sl][0:64, :], qview[:, c, g * 512:(g + 1) * 512], r=['qTd'],
                           w=['qnq%d' % sl])

                def stageA(it):
                    c, g = divmod(it, 2)
                    sl = it % 3
                    Q = QN3[sl]
                    qq, qs = 'qnq%d' % sl, 'qns%d' % sl
                    if it + 1 < NIT:
                        qload(it + 1)
                    nts = 1 if c < 16 else 2
                    tl = []
                    for nt in range(nts):
                        Mn = min(128, 8 * c + 7 - 128 * nt)
                        m = 8 * c - 128 * nt
                        need = m <= 192
                        PS, psk = nexts()
                        tr.op('pe', lambda e, PS=PS, Mn=Mn, nt=nt, need=need: e.matmul(
                            out=PS[0:Mn, :], lhsT=KC[g][:, 128 * nt:128 * nt + Mn], rhs=Q[0:64, :],
                            start=True, stop=not need), r=['KC%d' % g, qq], w=[psk])
                        if need:
                            tr.op('pe', lambda e, PS=PS, Mn=Mn, m=m: e.matmul(
                                out=PS[0:Mn, :], lhsT=BDt[:, 192 - m:192 - m + Mn], rhs=CMt[:],
                                start=False, stop=True), r=['ac'], w=[psk])
                        Et, ek = nexte()
                        tr.op('act', lambda e, PS=PS, Et=Et, Mn=Mn: e.activation(
                            out=Et[0:Mn, :], in_=PS[0:Mn, :], func=AF.Exp, scale=0.125), r=[psk], w=[ek])
                        tl.append((nt, Mn, Et, ek))
                    first = True
                    for (nt, Mn, Et, ek) in tl:
                        for r in range(4):
                            tr.op('pe', lambda e, Et=Et, Mn=Mn, nt=nt, r=r, first=first: e.matmul(
                                out=OC[:, r, :], lhsT=Et[0:Mn, r * 128:(r + 1) * 128], rhs=VCX[g][0:Mn, nt, 0:65],
                                start=first, stop=(nt == nts - 1), skip_group_check=True),
                                r=[ek, 'VCX%d' % g], w=['oc'])
                            tr.op('pe', lambda e, Et=Et, Mn=Mn, nt=nt, r=r, first=first: e.matmul(
                                out=IM[:, r, :], lhsT=Et[0:Mn, r * 128:(r + 1) * 128], rhs=VCX[g][0:Mn, nt, 65:129],
                                start=first, stop=(nt == nts - 1), skip_group_check=True),
                                r=[ek, 'VCX%d' % g], w=['im'])
                            first = False
                    OCb, ock = OCS[it % 2], 'ocs%d' % (it % 2)
                    tr.op('act', lambda e, OCb=OCb: e.activation(out=OCb[:], in_=OC, func=AF.Copy), r=['oc'], w=[ock])
                    tr.op('dve', lambda e, OCb=OCb: e.tensor_scalar(out=RZ[:], in0=OCb[:, :, 64], scalar1=1e-30,
                                                                   scalar2=None, op0=ALU.max), r=[ock], w=['rz'])
                    tr.op('dve', lambda e: e.reciprocal(out=RZ[:], in_=RZ[:]), r=['rz'], w=['rz'])
                    tr.op('dve', lambda e: e.tensor_scalar(out=SCO[:], in0=IM[:, 0, :], scalar1=RZ[:, 0:1],
                                                          scalar2=None, op0=ALU.mult), r=['im', 'rz'], w=['sco'])
                    for r in range(1, 4):
                        tr.op('dve', lambda e, r=r: e.scalar_tensor_tensor(
                            out=SCO[:], in0=IM[:, r, :], scalar=RZ[:, r:r + 1], in1=SCO[:],
                            op0=ALU.mult, op1=ALU.add), r=['im', 'rz', 'sco'], w=['sco'])
                    lo = 62 - 2 * c
                    tr.op('dve', lambda e, lo=lo: e.tensor_tensor(out=SCO[:], in0=SCO[:], in1=L0t[:, lo:lo + 64],
                                                                 op=ALU.max), r=['sco', 'ac'], w=['sco'])
                    tr.op('dve', lambda e, lo=lo: e.tensor_tensor(out=SCO[:], in0=SCO[:], in1=U0t[:, lo:lo + 64],
                                                                 op=ALU.min), r=['sco', 'ac'], w=['sco'])
                    tr.op('dve', lambda e: e.memset(SCO[:, 0:1], 3e30), r=['sco'], w=['sco'])
                    tr.op('dve', lambda e: e.max(out=M8[:, 0:8], in_=SCO[:]), r=['sco'], w=['m8'])
                    tr.op('dve', lambda e: e.match_replace(out=WK[:], in_to_replace=M8[:, 0:8], in_values=SCO[:],
                                                          imm_value=-3e38), r=['sco', 'm8'], w=['wk'])
                    tr.op('dve', lambda e: e.max(out=M8[:, 8:16], in_=WK[:]), r=['wk'], w=['m8'])
                    tr.op('dve', lambda e: e.tensor_scalar(out=SELW[:, 64:128], in0=SCO[:], scalar1=M8[:, 15:16],
                                                          scalar2=None, op0=ALU.is_ge), r=['sco', 'm8'], w=['selw'])
                    tr.op('pe', lambda e: e.transpose(out=PT, in_=SELW[:], identity=IDN[:]), r=['selw', 'c'], w=['pt'])
                    tr.op('dve', lambda e, Q=Q: e.tensor_scalar(
                        out=Q[64:128, :].rearrange("p (r q) -> p r q", r=4),
                        in0=PT[64:128, :].unsqueeze(1).to_broadcast([64, 4, 128]),
                        scalar1=-1.0, scalar2=BIGM, op0=ALU.add, op1=ALU.mult), r=['pt'], w=[qs])

                def finish_pe(c):
                    AN = ATN2[c % 2]
                    for j in range(4):
                        tr.op('pe', lambda e, j=j, AN=AN: e.transpose(out=PT4[:, j * 128:(j + 1) * 128],
                                                                     in_=AN[:, j * 128:(j + 1) * 128], identity=IDN[:]),
                              r=['atn%d' % (c % 2), 'c'], w=['pt'])
                    tr.op('dve', lambda e: e.tensor_copy(out=MST[:].rearrange("p a b -> p (a b)"), in_=PT4),
                          r=['pt'], w=['mst'])
                    tr.dma('sp', 'mout', mixv[:, 0:4, c * 128:(c + 1) * 128], MST[:], r=['mst'], w=['mixT'])

                def stageB(it):
                    c, g = divmod(it, 2)
                    sl = it % 3
                    Q = QN3[sl]
                    qq, qs = 'qnq%d' % sl, 'qns%d' % sl
                    k0 = max(0, c - 4)
                    tiles = [('w', kt) for kt in range(k0, c + 1)] + [('s', kt) for kt in range(c + 1)]
                    n = len(tiles)
                    info = {}

                    def qk(i):
                        br, kt = tiles[i]
                        PS, psk = nexts()
                        extra = []
                        if kt == c:
                            extra.append(CBt)
                        if br == 'w' and kt == c - 4:
                            extra.append(WBt)
                        if br == 'w':
                            tr.op('pe', lambda e, PS=PS, kt=kt, ne=len(extra): e.matmul(
                                out=PS[:], lhsT=KW[g][:, kt * 128:(kt + 1) * 128], rhs=Q[0:64, :],
                                start=True, stop=(ne == 0)), r=['KW%d' % g, qq], w=[psk])
                        else:
                            tr.op('pe', lambda e, PS=PS, kt=kt, ne=len(extra): e.matmul(
                                out=PS[:], lhsT=KE[g][:, kt * 128:(kt + 1) * 128], rhs=Q[:, :],
                                start=True, stop=(ne == 0)), r=['KE%d' % g, qq, qs], w=[psk])
                        for j, bt in enumerate(extra):
                            tr.op('pe', lambda e, PS=PS, bt=bt, last=(j == len(extra) - 1): e.matmul(
                                out=PS[:], lhsT=IDN[:], rhs=bt[:], start=False, stop=last), r=['ac', 'c'], w=[psk])
                        Et, ek = nexte()
                        tr.op('act', lambda e, PS=PS, Et=Et: e.activation(
                            out=Et[:], in_=PS[:], func=AF.Exp, scale=0.125), r=[psk], w=[ek])
                        info[i] = (Et, ek)

                    started = {'w': False, 's': False}
                    lastw = max(i for i in range(n) if tiles[i][0] == 'w')

                    def pv(i):
                        br, kt = tiles[i]
                        Et, ek = info[i]
                        O, ok, V, vk = (OW, 'ow', VW1[g], 'VW1%d' % g) if br == 'w' else (OS, 'os', VS1[g], 'VS1%d' % g)
                        last = (i == lastw) if br == 'w' else (i == n - 1)
                        for r in range(4):
                            st = not started[br]
                            started[br] = True
                            tr.op('pe', lambda e, Et=Et, kt=kt, r=r, O=O, V=V, st=st, last=last: e.matmul(
                                out=O[:, r, :], lhsT=Et[:, r * 128:(r + 1) * 128], rhs=V[:, kt, :],
                                start=st, stop=last, skip_group_check=True), r=[ek, vk], w=[ok])

                    for i in range(min(2, n)):
                        qk(i)
                    if pending:
                        finish_pe(pending.pop(0))
                    for i in range(n):
                        if i + 2 < n:
                            qk(i + 2)
                        pv(i)
                    b2 = it % 2
                    tr.op('act', lambda e: e.activation(out=OWS[b2][:], in_=OW, func=AF.Copy), r=['ow'], w=['ows%d' % b2])
                    tr.op('act', lambda e: e.activation(out=OSS[b2][:], in_=OS, func=AF.Copy), r=['os'], w=['oss%d' % b2])
                    srcs = ((OCS[b2], 'ocs%d' % b2), (OSS[b2], 'oss%d' % b2), (OWS[b2], 'ows%d' % b2))
                    for bi, (O, ok) in enumerate(srcs):
                        tr.op('dve', lambda e, O=O, bi=bi: e.tensor_scalar(
                            out=DEN[:, bi * 4:bi * 4 + 4], in0=O[:, :, 64], scalar1=1e-30, scalar2=None,
                            op0=ALU.max), r=[ok], w=['den'])
                    tr.op('dve', lambda e: e.reciprocal(out=DEN[:], in_=DEN[:]), r=['den'], w=['den'])
                    gv = GT[:, c, 12 * g:12 * g + 12].rearrange("p (r b) -> p b r", b=3)
                    tr.op('dve', lambda e, gv=gv: e.tensor_tensor(
                        out=DEN[:].rearrange("p (b r) -> p b r", b=3), in0=DEN[:].rearrange("p (b r) -> p b r", b=3),
                        in1=gv, op=ALU.mult), r=['den', 'GT'], w=['den'])
                    for r in range(4):
                        dst = ATT[:, (4 * g + r) * 64:(4 * g + r + 1) * 64]
                        tr.op('dve', lambda e, r=r, dst=dst: e.tensor_scalar(
                            out=dst, in0=OCS[b2][:, r, 0:64], scalar1=DEN[:, r:r + 1], scalar2=None, op0=ALU.mult),
                            r=['ocs%d' % b2, 'den'], w=['att'])
                        tr.op('dve', lambda e, r=r, dst=dst: e.scalar_tensor_tensor(
                            out=dst, in0=OSS[b2][:, r, 0:64], scalar=DEN[:, 4 + r:5 + r], in1=dst,
                            op0=ALU.mult, op1=ALU.add), r=['oss%d' % b2, 'den', 'att'], w=['att'])
                        tr.op('dve', lambda e, r=r, dst=dst: e.scalar_tensor_tensor(
                            out=dst, in0=OWS[b2][:, r, 0:64], scalar=DEN[:, 8 + r:9 + r], in1=dst,
                            op0=ALU.mult, op1=ALU.add), r=['ows%d' % b2, 'den', 'att'], w=['att'])
                    if g == 1:
                        AN, ank = ATN2[c % 2], 'atn%d' % (c % 2)
                        tr.op('act', lambda e: e.activation(out=JUNK[:], in_=ATT[:], func=AF.Square, accum_out=SSQ[:]),
                              r=['att'], w=['junk', 'ssq'])
                        tr.op('dve', lambda e: e.tensor_scalar(out=SSQ[:], in0=SSQ[:], scalar1=1.0 / 512, scalar2=EPS,
                                                              op0=ALU.mult, op1=ALU.add), r=['ssq'], w=['ssq'])
                        tr.op('act', lambda e: e.activation(out=SSQ[:], in_=SSQ[:], func=AF.Sqrt), r=['ssq'], w=['ssq'])
                        tr.op('dve', lambda e: e.reciprocal(out=SSQ[:], in_=SSQ[:]), r=['ssq'], w=['ssq'])
                        tr.op('dve', lambda e, AN=AN: e.scalar_tensor_tensor(out=AN[:], in0=ATT[:], scalar=SSQ[:, 0:1],
                                                                            in1=GAT[:], op0=ALU.mult, op1=ALU.mult),
                              r=['att', 'ssq', 'ac'], w=[ank])
                        pending.append(c)

                qload(0)
                stageA(0)
                for it in range(NIT):
                    if it + 1 < NIT:
                        stageA(it + 1)
                    stageB(it)
                while pending:
                    finish_pe(pending.pop(0))
            tr.barrier()

        def s5_phase():
            PI = float(np.pi)
            with contextlib.ExitStack() as s_:
                def ssb(name, shape, dt):
                    return s_.enter_context(nc.sbuf_tensor(name, list(shape), dt))
                ROW = [ssb("ROW%d" % i, [128, 2048], F32) for i in range(3)]
                PRE_RE = ssb("PRE_RE", [128, 2048], F32)
                PRE_IM = ssb("PRE_IM", [128, 2048], F32)
                TH = ssb("TH", [128, 2048], F32)
                TMPI = ssb("TMPI", [128, 2048], mybir.dt.int32)
                JC = ssb("JC", [128, 1], F32)
                NJC = ssb("NJC", [128, 1], F32)
                NPI = ssb("NPI", [128, 1], F32)
                COL = ssb("COL", [128, 48], F32)
                IOT = ssb("IOT", [128, 128], F32)
                POST_RE = ssb("POST_RE", [128, 16, 128], F32)
                POST_IM = ssb("POST_IM", [128, 16, 128], F32)
                A128 = ssb("A128", [128, 2, 16], F32)
                SM = [ssb("SM%d" % i, [128, 16], F32) for i in range(3)]
                REP = ssb("REP", [128, 4, 3, 64], F32)
                BT = ssb("BT", [128, 4, 2, 64], F32)
                BB = ssb("BB", [128, 4, 2, 64], F32)
                RT = [ssb("RT%d" % i, [128, 4, 64], F32) for i in range(6)]
                BBD = ssb("BBD", [128, 4, 1024], BF16)
                MASKB = ssb("MASKB", [128, 8], F32)
                CC = ssb("CC", [128, 16, 2, 16], F32)
                MASKC = ssb("MASKC", [128, 4, 8], F32)
                CMAT = ssb("CMAT", [128, 16, 2, 128], BF16)
                DCOL = ssb("DCOL", [128, 4], F32)
                BGL = ssb("BGL", [128, 4], F32)
                WGL = ssb("WGL", [128, 4, 512], BF16)
                TRI = ssb("TRI", [128, 128], BF16)
                UT = [ssb("UT%d" % i, [128, 4, 512], BF16) for i in range(2)]
                W = ssb("W", [128, 16, 2, 128], BF16)
                T1 = [ssb("T1_%d" % i, [128, 512], F32) for i in range(2)]
                T2 = [ssb("T2_%d" % i, [128, 512], F32) for i in range(2)]
                ZCF = ssb("ZCF", [128, 16, 2, 128], F32)
                CRE = ssb("CRE", [128, 16], F32)
                CIM = ssb("CIM", [128, 16], F32)
                TQ = [ssb("TQ%d" % i, [128, 16], F32) for i in range(4)]
                U1 = [ssb("U1_%d" % i, [128, 512], F32) for i in range(2)]
                U2 = [ssb("U2_%d" % i, [128, 512], F32) for i in range(2)]
                X = ssb("X", [128, 16, 2, 128], BF16)
                CARRY = ssb("CARRY", [128, 16, 2], F32)
                CT = [ssb("CT%d" % i, [128, 2, 2], F32) for i in range(2)]
                Y = ROW[0][:].rearrange("p (a b) -> p a b", a=4)
                YB = ROW[1][:].rearrange("p (a b) -> p a b", a=4)
                YG = ROW[2][:].rearrange("p (a b) -> p a b", a=4)
                YGB = ssb("YGB", [128, 4, 512], BF16)
                SGL = [ssb("SGL%d" % i, [128, 512], F32) for i in range(2)]
                S = TH[:].rearrange("p (a b) -> p a b", a=4)
                SQ = ssb("SQ", [128, 4, 512], BF16)
                RS5 = ssb("RS5", [128, 512], F32)
                OUT = ssb("OUT", [128, 4, 512], BF16)
                ck = ['s5c']
                for i in range(3):
                    tr.dma('sp', 's5const', ROW[i][:], s5_rows[i:i + 1, :].partition_broadcast(128), w=ck)
                tr.dma('sp', 's5const', JC[:], c_jcol[:, :], w=ck)
                tr.dma('sp', 's5const', COL[:], s5_cols[:, :], w=ck)
                tr.dma('sp', 's5const', IOT[:], c_iota[:, :], w=ck)
                tr.dma('sp', 's5const', REP[:].rearrange("p a b c -> p (a b c)"), s5_rep[:, :], w=ck)
                tr.dma('sp', 's5const', BT[:].rearrange("p a b c -> p (a b c)"), s5_bT[:, :], w=ck)
                tr.dma('sp', 's5const', MASKB[:], c_maskB[:, :], w=ck)
                tr.dma('sp', 's5const', CC[:].rearrange("p a b c -> p (a b c)"), s5_c[:, :], w=ck)
                tr.dma('sp', 's5const', MASKC[:].rearrange("p a b -> p (a b)"), c_maskC[:, :], w=ck)
                tr.dma('sp', 's5const', DCOL[:], s5_dcol[:, :], w=ck)
                tr.dma('sp', 's5const', BGL[:], bglu_col[:, :], w=ck)
                tr.dma('pool', 's5const', TRI[:], c_tri[:, :], w=ck)
                for k in range(4):
                    tr.dma('pool', 's5const', WGL[:, k, :], w_glu[k * 128:(k + 1) * 128, :], w=ck)
                V = lambda fn, r, w: tr.op('dve', fn, r=r, w=w)
                A = lambda fn, r, w: tr.op('act', fn, r=r, w=w)
                V(lambda e: e.memset(NPI[:], -PI), [], ck)
                V(lambda e: e.tensor_scalar(out=NJC[:], in0=JC[:], scalar1=-1.0, scalar2=None, op0=ALU.mult), ck, ck)
                V(lambda e: e.memset(CRE[:], 0.0), [], ['carry'])
                V(lambda e: e.memset(CIM[:], 0.0), [], ['carry'])

                def sincos(out_sin, out_cos, theta, tmpf, tmpi):
                    for out, shift in ((out_sin, 0.0), (out_cos, 0.5 * PI)):
                        V(lambda e, shift=shift: e.tensor_scalar(out=tmpf, in0=theta, scalar1=shift, scalar2=1.0 / (2 * PI),
                                                                 op0=ALU.add, op1=ALU.mult), ck, ck)
                        V(lambda e: e.tensor_copy(out=tmpi, in_=tmpf), ck, ck)
                        V(lambda e: e.tensor_copy(out=tmpf, in_=tmpi), ck, ck)
                        V(lambda e, out=out: e.scalar_tensor_tensor(out=out, in0=tmpf, scalar=-2 * PI, in1=theta,
                                                                    op0=ALU.mult, op1=ALU.add), ck, ck)
                        if shift:
                            V(lambda e, out=out, shift=shift: e.tensor_scalar(out=out, in0=out, scalar1=shift, scalar2=None,
                                                                              op0=ALU.add), ck, ck)
                        V(lambda e, out=out: e.tensor_scalar(out=tmpf, in0=out, scalar1=PI, scalar2=-2 * PI,
                                                             op0=ALU.is_gt, op1=ALU.mult), ck, ck)
                        V(lambda e, out=out: e.tensor_tensor(out=out, in0=out, in1=tmpf, op=ALU.add), ck, ck)
                        A(lambda e, out=out: e.activation(out=out, in_=out, func=AF.Sin), ck, ck)

                A(lambda e: e.activation(out=ROW[2][:], in_=ROW[2][:], func=AF.Exp), ck, ck)
                V(lambda e: e.tensor_tensor(out=ROW[0][:], in0=ROW[0][:], in1=ROW[2][:], op=ALU.mult), ck, ck)
                V(lambda e: e.tensor_tensor(out=ROW[1][:], in0=ROW[1][:], in1=ROW[2][:], op=ALU.mult), ck, ck)
                A(lambda e: e.activation(out=ROW[2][:], in_=ROW[0][:], func=AF.Exp, scale=NJC[:, 0:1]), ck, ck)
                V(lambda e: e.tensor_scalar(out=TH[:], in0=ROW[1][:], scalar1=JC[:, 0:1], scalar2=None, op0=ALU.mult),
                  ck, ck)
                sincos(PRE_IM[:], PRE_RE[:], TH[:], ROW[0][:], TMPI[:])
                V(lambda e: e.tensor_tensor(out=PRE_RE[:], in0=PRE_RE[:], in1=ROW[2][:], op=ALU.mult), ck, ck)
                V(lambda e: e.scalar_tensor_tensor(out=PRE_IM[:], in0=PRE_IM[:], scalar=-1.0, in1=ROW[2][:],
                                                   op0=ALU.mult, op1=ALU.mult), ck, ck)
                LRc, LIc, DTc = COL[:, 0:16], COL[:, 16:32], COL[:, 32:48]
                A(lambda e: e.activation(out=DTc, in_=DTc, func=AF.Exp), ck, ck)
                V(lambda e: e.tensor_tensor(out=LRc, in0=LRc, in1=DTc, op=ALU.mult), ck, ck)
                V(lambda e: e.tensor_tensor(out=LIc, in0=LIc, in1=DTc, op=ALU.mult), ck, ck)
                iob = IOT[:].unsqueeze(1).to_broadcast([128, 16, 128])
                THv = TH[:].rearrange("p (a b) -> p a b", a=16)
                R0v = ROW[0][:].rearrange("p (a b) -> p a b", a=16)
                R2v = ROW[2][:].rearrange("p (a b) -> p a b", a=16)
                V(lambda e: e.tensor_tensor(out=R2v, in0=iob, in1=LRc.unsqueeze(2).to_broadcast([128, 16, 128]),
                                            op=ALU.mult), ck, ck)
                A(lambda e: e.activation(out=ROW[2][:], in_=ROW[2][:], func=AF.Exp), ck, ck)
                V(lambda e: e.tensor_tensor(out=THv, in0=iob, in1=LIc.unsqueeze(2).to_broadcast([128, 16, 128]),
                                            op=ALU.mult), ck, ck)
                sincos(POST_IM[:].rearrange("p a b -> p (a b)"), POST_RE[:].rearrange("p a b -> p (a b)"), TH[:],
                       ROW[0][:], TMPI[:])
                V(lambda e: e.tensor_tensor(out=POST_RE[:], in0=POST_RE[:], in1=R2v, op=ALU.mult), ck, ck)
                V(lambda e: e.tensor_tensor(out=POST_IM[:], in0=POST_IM[:], in1=R2v, op=ALU.mult), ck, ck)
                V(lambda e: e.tensor_scalar(out=SM[0][:], in0=LRc, scalar1=128.0, scalar2=None, op0=ALU.mult), ck, ck)
                A(lambda e: e.activation(out=SM[0][:], in_=SM[0][:], func=AF.Exp), ck, ck)
                V(lambda e: e.tensor_scalar(out=SM[1][:], in0=LIc, scalar1=128.0, scalar2=None, op0=ALU.mult), ck, ck)
                sincos(A128[:, 1, :], A128[:, 0, :], SM[1][:], SM[2][:], TMPI[:, 0:16])
                V(lambda e: e.tensor_tensor(out=A128[:, 0, :], in0=A128[:, 0, :], in1=SM[0][:], op=ALU.mult), ck, ck)
                V(lambda e: e.tensor_tensor(out=A128[:, 1, :], in0=A128[:, 1, :], in1=SM[0][:], op=ALU.mult), ck, ck)
                lr, li, ldt = REP[:, :, 0, :], REP[:, :, 1, :], REP[:, :, 2, :]
                dtv, ldr, ldi, mag, sn, cs = [t[:] for t in RT]
                A(lambda e: e.activation(out=dtv, in_=ldt, func=AF.Exp), ck, ck)
                V(lambda e: e.tensor_tensor(out=ldr, in0=lr, in1=dtv, op=ALU.mult), ck, ck)
                V(lambda e: e.tensor_tensor(out=ldi, in0=li, in1=dtv, op=ALU.mult), ck, ck)
                A(lambda e: e.activation(out=mag, in_=ldr, func=AF.Exp), ck, ck)
                sincos(sn, cs, ldi, dtv, TMPI[:, 0:256].rearrange("p (a b) -> p a b", a=4))
                V(lambda e: e.tensor_tensor(out=cs, in0=cs, in1=mag, op=ALU.mult), ck, ck)
                V(lambda e: e.tensor_tensor(out=sn, in0=sn, in1=mag, op=ALU.mult), ck, ck)
                V(lambda e: e.tensor_scalar(out=cs, in0=cs, scalar1=-1.0, scalar2=None, op0=ALU.add), ck, ck)
                V(lambda e: e.tensor_tensor(out=dtv, in0=lr, in1=lr, op=ALU.mult), ck, ck)
                V(lambda e: e.tensor_tensor(out=mag, in0=li, in1=li, op=ALU.mult), ck, ck)
                V(lambda e: e.tensor_tensor(out=dtv, in0=dtv, in1=mag, op=ALU.add), ck, ck)
                V(lambda e: e.reciprocal(out=dtv, in_=dtv), ck, ck)
                V(lambda e: e.tensor_tensor(out=ldr, in0=cs, in1=lr, op=ALU.mult), ck, ck)
                V(lambda e: e.tensor_tensor(out=mag, in0=sn, in1=li, op=ALU.mult), ck, ck)
                V(lambda e: e.tensor_tensor(out=ldr, in0=ldr, in1=mag, op=ALU.add), ck, ck)
                V(lambda e: e.tensor_tensor(out=ldr, in0=ldr, in1=dtv, op=ALU.mult), ck, ck)
                V(lambda e: e.tensor_tensor(out=ldi, in0=sn, in1=lr, op=ALU.mult), ck, ck)
                V(lambda e: e.tensor_tensor(out=mag, in0=cs, in1=li, op=ALU.mult), ck, ck)
                V(lambda e: e.tensor_tensor(out=ldi, in0=ldi, in1=mag, op=ALU.subtract), ck, ck)
                V(lambda e: e.tensor_tensor(out=ldi, in0=ldi, in1=dtv, op=ALU.mult), ck, ck)
                br, bi_ = BT[:, :, 0, :], BT[:, :, 1, :]
                V(lambda e: e.tensor_tensor(out=BB[:, :, 0, :], in0=ldr, in1=br, op=ALU.mult), ck, ck)
                V(lambda e: e.tensor_tensor(out=mag, in0=ldi, in1=bi_, op=ALU.mult), ck, ck)
                V(lambda e: e.tensor_tensor(out=BB[:, :, 0, :], in0=BB[:, :, 0, :], in1=mag, op=ALU.subtract), ck, ck)
                V(lambda e: e.tensor_tensor(out=BB[:, :, 1, :], in0=ldr, in1=bi_, op=ALU.mult), ck, ck)
                V(lambda e: e.tensor_tensor(out=mag, in0=ldi, in1=br, op=ALU.mult), ck, ck)
                V(lambda e: e.tensor_tensor(out=BB[:, :, 1, :], in0=BB[:, :, 1, :], in1=mag, op=ALU.add), ck, ck)
                BBDv = BBD[:].rearrange("p k (a r g x) -> p k a r g x", a=4, r=2, g=2)
                for kt in range(4):
                    for a in range(4):
                        for ri in range(2):
                            V(lambda e, kt=kt, a=a, ri=ri: e.tensor_tensor(
                                out=BBDv[:, kt, a, ri, :, :],
                                in0=BB[:, kt, ri, :].unsqueeze(1).to_broadcast([128, 2, 64]),
                                in1=MASKB[:, 2 * a:2 * a + 2].unsqueeze(2).to_broadcast([128, 2, 64]),
                                op=ALU.mult), ck, ck)
                CMv = CMAT[:].rearrange("p a r (g h) -> p a r g h", g=8)
                for pr in range(16):
                    for ri in range(2):
                        in0 = CC[:, pr, ri, :].unsqueeze(1).to_broadcast([128, 8, 16])
                        in1 = MASKC[:, pr % 4, :].unsqueeze(2).to_broadcast([128, 8, 16])
                        if ri == 0:
                            V(lambda e, pr=pr, in0=in0, in1=in1: e.tensor_tensor(out=CMv[:, pr, 0, :, :], in0=in0, in1=in1,
                                                                               op=ALU.mult), ck, ck)
                        else:
                            V(lambda e, pr=pr, in0=in0, in1=in1: e.scalar_tensor_tensor(
                                out=CMv[:, pr, 1, :, :], in0=in0, scalar=-1.0, in1=in1, op0=ALU.mult, op1=ALU.mult),
                                ck, ck)
                uview = uTd.rearrange("p (k t) -> p k t", k=4)
                mview = mixT.rearrange("(k p) t -> p k t", p=128)
                bring = [0]
                WB2 = [W, TMPI[:].bitcast(BF16).rearrange("p (a r x) -> p a r x", a=16, r=2)]
                def uload(tt):
                    us = tt % 2
                    tr.dma('sp', 'uin%d' % us, UT[us][:], uview[:, :, tt * 512:tt * 512 + 512], r=['uTd'], w=['ut%d' % us])
                def pre(cc, jj):
                    tt, sub = divmod(cc, 4)
                    us = tt % 2; uk = 'ut%d' % us; U = UT[us]; Wc = WB2[cc % 2]; wp = 'w%d_' % (cc % 2)
                    tsl = slice(sub * 128, (sub + 1) * 128)
                    for kt in [jj // 2]:
                        for half in [jj % 2]:
                            i = bring[0] % 2
                            bring[0] += 1
                            PB = pb[i]
                            pk = 'pbu%d' % i
                            pr0 = 4 * kt + 2 * half
                            tr.op('pe', lambda e, PB=PB, kt=kt, half=half, tsl=tsl: e.matmul(
                                out=PB[:], lhsT=U[:, kt, tsl], rhs=BBD[:, kt, half * 512:(half + 1) * 512],
                                start=True, stop=True), r=[uk, 's5c'], w=[pk])
                            PBv = PB[:].rearrange("p (a r x) -> p a r x", a=2, r=2)
                            pre_r = PRE_RE[:, pr0 * 128:pr0 * 128 + 256].rearrange("p (a x) -> p a x", a=2) \
                                .unsqueeze(2).to_broadcast([128, 2, 2, 128])
                            pre_i = PRE_IM[:, pr0 * 128:pr0 * 128 + 256].rearrange("p (a x) -> p a x", a=2) \
                                .unsqueeze(2).to_broadcast([128, 2, 2, 128])
                            t1 = T1[i][:].rearrange("p (a r x) -> p a r x", a=2, r=2)
                            t2 = T2[i][:].rearrange("p (a r x) -> p a r x", a=2, r=2)
                            tr.op('dve', lambda e, PBv=PBv, pre_r=pre_r, t1=t1: e.tensor_tensor(
                                out=t1, in0=PBv, in1=pre_r, op=ALU.mult), r=[pk, 's5c'], w=['t1_%d' % i])
                            tr.op('dve', lambda e, PBv=PBv, pre_i=pre_i, t2=t2: e.tensor_tensor(
                                out=t2, in0=PBv, in1=pre_i, op=ALU.mult), r=[pk, 's5c'], w=['t2_%d' % i])
                            tr.op('dve', lambda e, t1=t1, t2=t2, pr0=pr0: e.tensor_tensor(
                                out=Wc[:, pr0:pr0 + 2, 0, :], in0=t1[:, :, 0, :], in1=t2[:, :, 1, :], op=ALU.subtract),
                                r=['t1_%d' % i, 't2_%d' % i], w=[wp + str(pr0 // 2)])
                            tr.op('pool', lambda e, t1=t1, t2=t2, pr0=pr0: e.tensor_tensor(
                                out=Wc[:, pr0:pr0 + 2, 1, :], in0=t1[:, :, 1, :], in1=t2[:, :, 0, :], op=ALU.add),
                                r=['t1_%d' % i, 't2_%d' % i], w=[wp + str(pr0 // 2)])
                def post(cc, jj):
                    tt, sub = divmod(cc, 4)
                    us = tt % 2; uk = 'ut%d' % us; U = UT[us]; Wc = WB2[cc % 2]; wp = 'w%d_' % (cc % 2)
                    tsl = slice(sub * 128, (sub + 1) * 128)
                    for pg in [jj]:
                        i = pg % 2
                        PZ = pb[2 + i]
                        zk = 'pz%d' % i
                        pr0 = 2 * pg
                        PZv = PZ[:].rearrange("p (a r x) -> p a r x", a=2, r=2)
                        for a in range(2):
                            for ri in range(2):
                                tr.op('pe', lambda e, PZv=PZv, a=a, ri=ri, pr0=pr0: e.matmul(
                                    out=PZv[:, a, ri, :], lhsT=Wc[:, pr0 + a, ri, :], rhs=TRI[:],
                                    start=True, stop=True, skip_group_check=True), r=[wp + str(pg), 's5c'], w=[zk])
                        Z = ZCF[:, pr0:pr0 + 2, :, :]
                        zck = 'zc%d' % pg
                        for a in range(2):
                            for ri in range(2):
                                CB_ = CRE if ri == 0 else CIM
                                tr.op('act', lambda e, PZv=PZv, pr0=pr0, a=a, ri=ri, CB_=CB_: e.activation(
                                    out=ZCF[:, pr0 + a, ri, :], in_=PZv[:, a, ri, :], func=AF.Identity,
                                    bias=CB_[:, pr0 + a:pr0 + a + 1]), r=[zk, 'carry'], w=[zck])
                        po_r = POST_RE[:, pr0:pr0 + 2, :].unsqueeze(2).to_broadcast([128, 2, 2, 128])
                        po_i = POST_IM[:, pr0:pr0 + 2, :].unsqueeze(2).to_broadcast([128, 2, 2, 128])
                        u1 = U1[i][:].rearrange("p (a r x) -> p a r x", a=2, r=2)
                        u2 = U2[i][:].rearrange("p (a r x) -> p a r x", a=2, r=2)
                        tr.op('dve', lambda e, Z=Z, po_r=po_r, u1=u1: e.tensor_tensor(
                            out=u1, in0=Z, in1=po_r, op=ALU.mult), r=[zck, 's5c'], w=['u1_%d' % i])
                        tr.op('pool', lambda e, Z=Z, po_i=po_i, u2=u2: e.tensor_tensor(
                            out=u2, in0=Z, in1=po_i, op=ALU.mult), r=[zck, 's5c'], w=['u2_%d' % i])
                        tr.op('dve', lambda e, u1=u1, u2=u2, pr0=pr0: e.tensor_tensor(
                            out=X[:, pr0:pr0 + 2, 0, :], in0=u1[:, :, 0, :], in1=u2[:, :, 1, :], op=ALU.subtract),
                            r=['u1_%d' % i, 'u2_%d' % i], w=['x%d' % pg])
                        tr.op('pool', lambda e, u1=u1, u2=u2, pr0=pr0: e.tensor_tensor(
                            out=X[:, pr0:pr0 + 2, 1, :], in0=u1[:, :, 1, :], in1=u2[:, :, 0, :], op=ALU.add),
                            r=['u1_%d' % i, 'u2_%d' % i], w=['x%d' % pg])
                def post_end(cc):
                    tt, sub = divmod(cc, 4)
                    us = tt % 2; uk = 'ut%d' % us; U = UT[us]
                    tsl = slice(sub * 128, (sub + 1) * 128)
                    zall = ['zc%d' % pg for pg in range(8)]
                    zr = ZCF[:, :, 0, 127]
                    zi = ZCF[:, :, 1, 127]
                    tr.op('dve', lambda e: e.tensor_tensor(out=TQ[0][:], in0=zr, in1=A128[:, 0, :], op=ALU.mult),
                          r=zall + ['s5c'], w=['tq'])
                    tr.op('dve', lambda e: e.tensor_tensor(out=TQ[1][:], in0=zi, in1=A128[:, 1, :], op=ALU.mult),
                          r=zall + ['s5c'], w=['tq'])
                    tr.op('dve', lambda e: e.tensor_tensor(out=TQ[2][:], in0=zr, in1=A128[:, 1, :], op=ALU.mult),
                          r=zall + ['s5c'], w=['tq'])
                    tr.op('dve', lambda e: e.tensor_tensor(out=TQ[3][:], in0=zi, in1=A128[:, 0, :], op=ALU.mult),
                          r=zall + ['s5c'], w=['tq'])
                    tr.op('dve', lambda e: e.tensor_tensor(out=CRE[:], in0=TQ[0][:], in1=TQ[1][:], op=ALU.subtract),
                          r=['tq'], w=['carry'])
                    tr.op('dve', lambda e: e.tensor_tensor(out=CIM[:], in0=TQ[2][:], in1=TQ[3][:], op=ALU.add),
                          r=['tq'], w=['carry'])
                    for kt in range(4):
                        i = kt % 2
                        PY = pb[4 + i]
                        yk = 'py%d' % i
                        n = 0
                        for a in range(4):
                            pr = 4 * kt + a
                            for ri in range(2):
                                tr.op('pe', lambda e, PY=PY, pr=pr, ri=ri, n=n: e.matmul(
                                    out=PY[:, 0:128], lhsT=CMAT[:, pr, ri, :], rhs=X[:, pr, ri, :],
                                    start=(n == 0), stop=(n == 7)), r=['s5c', 'x%d' % (pr // 2)], w=[yk])
                                n += 1
                        tr.op('dve', lambda e, PY=PY, kt=kt, tsl=tsl: e.scalar_tensor_tensor(
                            out=Y[:, kt, tsl], in0=U[:, kt, tsl], scalar=DCOL[:, kt:kt + 1], in1=PY[:, 0:128],
                            op0=ALU.mult, op1=ALU.add), r=[uk, yk, 's5c'], w=['y'])
                def tail(tt):
                    t0 = tt * 512
                    Yf = ROW[0][:]
                    YBf = ROW[1][:]
                    YGf = ROW[2][:]
                    tr.op('pool', lambda e: e.tensor_tensor(out=YBf, in0=Yf, in1=Yf, op=ALU.mult), r=['y'], w=['yb'])
                    tr.op('pool', lambda e: e.tensor_scalar(out=YBf, in0=YBf, scalar1=0.044715, scalar2=1.0,
                                                           op0=ALU.mult, op1=ALU.add), r=['yb'], w=['yb'])
                    tr.op('pool', lambda e: e.tensor_tensor(out=YBf, in0=YBf, in1=Yf, op=ALU.mult), r=['yb', 'y'], w=['yb'])
                    tr.op('act', lambda e: e.activation(out=YBf, in_=YBf, func=AF.Sigmoid, scale=GELU_C), r=['yb'], w=['yb'])
                    tr.op('dve', lambda e: e.tensor_tensor(out=YGf, in0=Yf, in1=YBf, op=ALU.mult), r=['y', 'yb'], w=['yg'])
                    tr.op('pool', lambda e: e.tensor_copy(out=YGB[:].rearrange("p a b -> p (a b)"), in_=YGf),
                          r=['yg'], w=['ygb'])
                    for mt in range(4):
                        i = mt % 2
                        PG = pb[6 + i]
                        gk = 'pgl%d' % i
                        for kt in range(4):
                            tr.op('pe', lambda e, PG=PG, kt=kt, mt=mt: e.matmul(
                                out=PG[:], lhsT=WGL[:, kt, mt * 128:(mt + 1) * 128], rhs=YGB[:, kt, :],
                                start=(kt == 0), stop=(kt == 3)), r=['s5c', 'ygb'], w=[gk])
                        tr.op('act', lambda e, PG=PG, mt=mt, i=i: e.activation(
                            out=SGL[i][:], in_=PG[:], func=AF.Sigmoid, bias=BGL[:, mt:mt + 1]), r=[gk, 's5c'],
                            w=['sgl%d' % i])
                        tr.op('dve', lambda e, mt=mt, i=i: e.tensor_tensor(out=S[:, mt, :], in0=YG[:, mt, :],
                                                                          in1=SGL[i][:], op=ALU.mult),
                              r=['yg', 'sgl%d' % i], w=['s'])
                    tr.op('act', lambda e: e.activation(out=SQ[:].rearrange("p a b -> p (a b)"),
                                                       in_=TH[:], func=AF.Square),
                          r=['s'], w=['sq'])
                    PN = pb[6]
                    for mt in range(4):
                        tr.op('pe', lambda e, mt=mt: e.matmul(out=PN[:], lhsT=ONES[:], rhs=SQ[:, mt, :],
                                                             start=(mt == 0), stop=(mt == 3)), r=['sq', 'c'], w=['pgl0'])
                    tr.op('dve', lambda e: e.tensor_scalar(out=RS5[:], in0=PN[:], scalar1=1.0 / 512, scalar2=EPS,
                                                          op0=ALU.mult, op1=ALU.add), r=['pgl0'], w=['rs5'])
                    tr.op('act', lambda e: e.activation(out=RS5[:], in_=RS5[:], func=AF.Sqrt), r=['rs5'], w=['rs5'])
                    tr.op('dve', lambda e: e.reciprocal(out=RS5[:], in_=RS5[:]), r=['rs5'], w=['rs5'])
                    for mt in range(4):
                        tr.op('dve', lambda e, mt=mt: e.scalar_tensor_tensor(
                            out=OUT[:, mt, :], in0=S[:, mt, :], scalar=GC[:, 32 + mt:33 + mt], in1=RS5[:],
                            op0=ALU.mult, op1=ALU.mult), r=['s', 'rs5', 'c'], w=['out5'])
                    tr.dma('sp', 'sout', mview[:, 4:8, t0:t0 + 512], OUT[:], r=['out5'], w=['mixT'])
                uload(0)
                uload(1)
                for jj in range(8):
                    pre(0, jj)
                for cc in range(32):
                    for jj in range(8):
                        if cc + 1 < 32:
                            pre(cc + 1, jj)
                        post(cc, jj)
                    post_end(cc)
                    if cc % 4 == 3:
                        tail(cc // 4)
                        if cc // 4 + 2 < 8:
                            uload(cc // 4 + 2)
            tr.barrier()

        def wout_phase():
            with contextlib.ExitStack() as w_:
                def wsb(name, shape, dt):
                    return w_.enter_context(nc.sbuf_tensor(name, list(shape), dt))
                WO = wsb("WO", [128, 8, D], BF16)
                XW = [wsb("XW%d" % i, [128, 8, 512], F32) for i in range(2)]
                MX = [wsb("MX%d" % i, [128, 8, 512], BF16) for i in range(2)]
                for k in range(8):
                    tr.dma('pool', 'wo', WO[:, k, :], w_out[k * 128:(k + 1) * 128, :], w=['WO'])
                for tt in range(8):
                    s = tt % 2
                    t0 = tt * 512
                    xk, mk = 'xw%d' % s, 'mx%d' % s
                    tr.dma('sp', 'xwin%d' % s, XW[s][:], xview(x1T, t0, 512), r=['x1T'], w=[xk])
                    tr.dma('sp', 'mxin%d' % s, MX[s][:], xview(mixT, t0, 512), r=['mixT'], w=[mk])
                    for dk in range(8):
                        b = dk % 4
                        for k in range(8):
                            tr.op('pe', lambda e, k=k, dk=dk, b=b, s=s: e.matmul(
                                out=pb[b][:], lhsT=WO[:, k, dk * 128:(dk + 1) * 128], rhs=MX[s][:, k, :],
                                start=(k == 0), stop=(k == 7)), r=['WO', mk], w=['pw%d' % b])
                        tr.op('dve', lambda e, dk=dk, b=b, s=s: e.tensor_tensor(
                            out=XW[s][:, dk, :], in0=pb[b][:], in1=XW[s][:, dk, :], op=ALU.add),
                            r=['pw%d' % b, xk], w=[xk])
                    tr.dma('sp', 'xwout%d' % s, xview(x1T, t0, 512), XW[s][:], r=[xk], w=['x1T'])
            tr.barrier()

        ffn_phase(1)

        with contextlib.ExitStack() as ms:
            if stop < 1.2:
                return nc
            def msb(name, shape, dt):
                return ms.enter_context(nc.sbuf_tensor(name, list(shape), dt))
            KE = [msb("KE%d" % g, [128, T], BF16) for g in range(2)]
            KW = [msb("KW%d" % g, [64, T], BF16) for g in range(2)]
            VS1 = [msb("VS1%d" % g, [128, 32, 65], BF16) for g in range(2)]
            VW1 = [msb("VW1%d" % g, [128, 32, 65], BF16) for g in range(2)]
            GT = msb("GT", [128, 32, 24], F32)
            KC = [msb("KC%d" % g, [64, 256], BF16) for g in range(2)]
            VCX = [msb("VCX%d" % g, [128, 2, 129], BF16) for g in range(2)]
            for g in range(2):
                tr.dma('pool', 'const', KE[g][64:128, :], c_E0[:, :], w=['KE%d' % g])
                tr.op('dve', lambda e, g=g: e.memset(VS1[g][:, :, 64:65], 1.0), w=['VS1%d' % g])
                tr.op('dve', lambda e, g=g: e.memset(VW1[g][:, :, 64:65], 1.0), w=['VW1%d' % g])
                tr.op('dve', lambda e, g=g: e.memset(VCX[g][:, :, 64:65], 1.0), w=['VCX%d' % g])
                tr.dma('pool', 'const', VCX[g][:, :, 65:129], c_ovl.rearrange("p (a b) -> p a b", a=2),
                       w=['VCX%d' % g])

            with contextlib.ExitStack() as ps_:
                def psb(name, shape, dt):
                    return ps_.enter_context(nc.sbuf_tensor(name, list(shape), dt))
                WIN = psb("WIN", [128, 8, 1816], BF16)
                H2 = [psb("H2_%d" % i, [128, 8, 512], BF16) for i in range(2)]
                QST = psb("QST", [64, 4, 8, 128], BF16)
                UST = psb("UST", [128, 4, 512], BF16)
                KCin = [psb("KCin%d" % g, [64, T], BF16) for g in range(2)]
                VCin = [psb("VCin%d" % g, [64, T], BF16) for g in range(2)]
                GB = psb("GB", [128, 24], F32)
                W1 = [psb("W1_%d" % i, [64, 32, 256], BF16) for i in range(2)]
                W2 = [psb("W2_%d" % i, [128, 2, 64], BF16) for i in range(2)]
                CB1 = psb("CB1", [128, 4], F32)
                POST = psb("POST", [64, 64], BF16)
                HID = psb("HID", [128, 2, 256], BF16)
                GTMP = [psb("GTMP%d" % i, [128, 256], F32) for i in range(3)]
                PBIAS = psb("PBIAS", [128, 1], F32)
                for k in range(8):
                    tr.dma('pool', 'win', WIN[:, k, :], w_in[k * 128:(k + 1) * 128, :], w=['WIN'])
                tr.dma('sp', 'const', GB[:], gate_bias.partition_broadcast(128), w=['GB'])
                tr.dma('sp', 'const', CB1[:], cb1[:, :], w=['cmpw'])
                tr.dma('pool', 'const', POST[:], posT[:, :], w=['cmpw'])
                for i in range(2):
                    w1v = cw1[i].rearrange("(l d) h -> d l h", d=64)
                    for lc in range(8):
                        tr.dma('pool', 'const', W1[i][:, 4 * lc:4 * lc + 4, :], w1v[:, 4 * lc:4 * lc + 4, :], w=['cmpw'])
                    tr.dma('pool', 'const', W2[i][:], cw2[i].rearrange("(t p) d -> p t d", p=128), w=['cmpw'])
                if stop < 1.6:
                    tr.barrier()
                    return nc
                ring = [0]

                def nextbank():
                    b = ring[0] % 6
                    ring[0] += 1
                    return pb[b], 'pp%d' % b

                evq = [0]

                def evac(out, in_, rk, wk, force=None):
                    evq[0] += 1
                    if (evq[0] % 2 == 0 and force is None) or force == 'act':
                        tr.op('act', lambda e: e.activation(out=out, in_=in_, func=AF.Copy), r=[rk], w=[wk])
                    else:
                        tr.op('dve', lambda e: e.tensor_copy(out=out, in_=in_), r=[rk], w=[wk])

                for tt in range(8):
                    s = tt % 2
                    t0 = tt * 512
                    hk = 'h2_%d' % s
                    HH = H2[s]
                    tr.dma('sp', 'h2in%d' % s, HH[:], xview(h2T, t0, 512), r=['h2T'], w=[hk])
                    for h in range(8):
                        P, pk = nextbank()
                        for k in range(8):
                            tr.op('pe', lambda e, k=k, h=h, P=P: e.matmul(
                                out=P[0:64, :], lhsT=WIN[:, k, 64 * h:64 * h + 64], rhs=HH[:, k, :],
                                start=(k == 0), stop=(k == 7)), r=['WIN', hk], w=[pk])
                        evac(QST[:, :, h, :], P[0:64, :].rearrange("p (c q) -> p c q", q=128), pk, 'qst')
                    tr.dma('sp', 'qout', qTd.rearrange("p (c x) -> p c x", c=32)[:, 4 * tt:4 * tt + 4, :],
                           QST[:].rearrange("p c h q -> p c (h q)"), r=['qst'], w=['qTd'])
                    for (c0, dest, dn) in ((512, KCin, 'KCin'), (640, VCin, 'VCin'), (768, KE, 'KE'), (1024, KW, 'KW')):
                        for g in range(2):
                            P, pk = nextbank()
                            for k in range(8):
                                tr.op('pe', lambda e, k=k, P=P, cc=c0 + 64 * g: e.matmul(
                                    out=P[0:64, :], lhsT=WIN[:, k, cc:cc + 64], rhs=HH[:, k, :],
                                    start=(k == 0), stop=(k == 7)), r=['WIN', hk], w=[pk])
                            evac(dest[g][0:64, t0:t0 + 512], P[0:64, :], pk, '%s%d' % (dn, g))
                    for kt in range(4):
                        P, pk = nextbank()
                        for k in range(8):
                            tr.op('pe', lambda e, k=k, P=P, cc=1304 + 128 * kt: e.matmul(
                                out=P[:, :], lhsT=WIN[:, k, cc:cc + 128], rhs=HH[:, k, :],
                                start=(k == 0), stop=(k == 7)), r=['WIN', hk], w=[pk])
                        evac(UST[:, kt, :], P[:, :], pk, 'ust')
                    tr.dma('sp', 'uout', uTd.rearrange("p (k t) -> p k t", k=4)[:, :, t0:t0 + 512], UST[:],
                           r=['ust'], w=['uTd'])
                    for sbk in range(4):
                        blk = tt * 4 + sbk
                        P, pk = nextbank()
                        for k in range(8):
                            tr.op('pe', lambda e, k=k, P=P, sbk=sbk: e.matmul(
                                out=P[:, 0:408], lhsT=HH[:, k, sbk * 128:(sbk + 1) * 128], rhs=WIN[:, k, 896:1304],
                                start=(k == 0), stop=(k == 7)), r=['WIN', hk], w=[pk])
                        for g in range(2):
                            evac(VS1[g][:, blk, 0:64], P[:, 64 * g:64 * g + 64], pk, 'VS1%d' % g, force='dve')
                            evac(VW1[g][:, blk, 0:64], P[:, 256 + 64 * g:256 + 64 * g + 64], pk, 'VW1%d' % g, force='dve')
                        tr.op('dve', lambda e, P=P, blk=blk: e.tensor_tensor(out=GT[:, blk, :], in0=P[:, 384:408],
                                                                            in1=GB[:], op=ALU.add),
                              r=[pk, 'GB'], w=['GT'])
                        tr.op('act', lambda e, blk=blk: e.activation(out=GT[:, blk, :], in_=GT[:, blk, :],
                                                                    func=AF.Sigmoid), r=['GT'], w=['GT'])

                if stop < 1.8:
                    tr.barrier()
                    return nc
                HIDS = [psb("HIDS%d" % g, [128, 2, 256], BF16) for g in range(2)]
                for g in range(2):
                    tr.op('dve', lambda e, g=g: e.memset(HIDS[g][:], 0.0), w=['hid%d_0' % g, 'hid%d_1' % g])
                A, B_, C_ = GTMP
                for kind in range(2):
                    srcs = KCin if kind == 0 else VCin
                    sname = 'KCin' if kind == 0 else 'VCin'
                    for hh in range(2):
                        P, pk = nextbank()
                        for l in range(32):
                            tr.op('pe', lambda e, l=l, P=P, hh=hh: e.matmul(
                                out=P[:, 0:1], lhsT=W1[kind][:, l, hh * 128:(hh + 1) * 128],
                                rhs=POST[:, kind * 32 + l:kind * 32 + l + 1], start=(l == 0), stop=(l == 31)),
                                r=['cmpw'], w=[pk])
                        tr.op('dve', lambda e, P=P, hh=hh: e.tensor_tensor(
                            out=PBIAS[:], in0=P[:, 0:1], in1=CB1[:, kind * 2 + hh:kind * 2 + hh + 1], op=ALU.add),
                            r=[pk, 'cmpw'], w=['pbias'])
                        for g in range(2):
                            P2, pk2 = nextbank()
                            for l in range(32):
                                rhs = bass.AP(srcs[g].tensor if hasattr(srcs[g], 'tensor') else srcs[g], l,
                                              [[T, 64], [16, 255]])
                                tr.op('pe', lambda e, l=l, P2=P2, rhs=rhs, hh=hh: e.matmul(
                                    out=P2[:, 0:255], lhsT=W1[kind][:, l, hh * 128:(hh + 1) * 128], rhs=rhs,
                                    start=(l == 0), stop=(l == 31)), r=['cmpw', '%s%d' % (sname, g)], w=[pk2])
                            tr.op('dve', lambda e, P2=P2: e.tensor_scalar(out=A[:, 0:255], in0=P2[:, 0:255],
                                                                         scalar1=PBIAS[:, 0:1], scalar2=None,
                                                                         op0=ALU.add), r=[pk2, 'pbias'], w=['ga'])
                            tr.op('dve', lambda e: e.tensor_tensor(out=B_[:, 0:255], in0=A[:, 0:255], in1=A[:, 0:255],
                                                                  op=ALU.mult), r=['ga'], w=['gb'])
                            tr.op('dve', lambda e: e.tensor_scalar(out=B_[:, 0:255], in0=B_[:, 0:255], scalar1=0.044715,
                                                                  scalar2=1.0, op0=ALU.mult, op1=ALU.add),
                                  r=['gb'], w=['gb'])
                            tr.op('dve', lambda e: e.tensor_tensor(out=B_[:, 0:255], in0=B_[:, 0:255], in1=A[:, 0:255],
                                                                  op=ALU.mult), r=['gb', 'ga'], w=['gb'])
                            tr.op('act', lambda e: e.activation(out=C_[:, 0:255], in_=B_[:, 0:255], func=AF.Sigmoid,
                                                               scale=GELU_C), r=['gb'], w=['gc'])
                            tr.op('dve', lambda e, g=g, hh=hh: e.tensor_tensor(
                                out=HIDS[g][:, hh, 0:255], in0=A[:, 0:255], in1=C_[:, 0:255], op=ALU.mult),
                                r=['ga', 'gc'], w=['hid%d_%d' % (g, hh)])
                    for g in range(2):
                        hk2 = ['hid%d_0' % g, 'hid%d_1' % g]
                        if kind == 0:
                            P, pk = nextbank()
                            for hh in range(2):
                                tr.op('pe', lambda e, hh=hh, P=P, g=g: e.matmul(
                                    out=P[0:64, 0:256], lhsT=W2[0][:, hh, :], rhs=HIDS[g][:, hh, :],
                                    start=(hh == 0), stop=(hh == 1)), r=['cmpw'] + hk2, w=[pk])
                            evac(KC[g][:, :], P[0:64, 0:256], pk, 'KC%d' % g)
                        else:
                            for nt in range(2):
                                P, pk = nextbank()
                                for hh in range(2):
                                    tr.op('pe', lambda e, hh=hh, P=P, g=g, nt=nt: e.matmul(
                                        out=P[:, 0:64], lhsT=HIDS[g][:, hh, nt * 128:(nt + 1) * 128],
                                        rhs=W2[1][:, hh, :], start=(hh == 0), stop=(hh == 1)),
                                        r=['cmpw'] + hk2, w=[pk])
                                evac(VCX[g][:, nt, 0:64], P[:, 0:64], pk, 'VCX%d' % g)
            tr.barrier()
            if stop >= 3:
                attention_phase()
        if stop >= 4:
            s5_phase()
        if stop >= 5:
            wout_phase()
        if stop >= 6:
            ffn_phase(2)
    return nc


def _consts():
    f = np.float32
    c = {}
    c["c_ident"] = np.eye(128, dtype=f)
    j = np.arange(128)
    c["c_tri"] = (j[:, None] <= j[None, :]).astype(f)
    c["c_E0"] = (np.arange(T)[None, :] // 64 == np.arange(64)[:, None]).astype(f)
    cb = np.where(j[:, None] > j[None, :], -BIGM, 0.0).astype(f)
    wb = np.where(j[:, None] <= j[None, :], -BIGM, 0.0).astype(f)
    cm = np.where(16 * (j[:, None] - 64) > j[None, :] - 31, -BIGM, 0.0).astype(f)
    c["c_CB"] = np.tile(cb, (1, 4))
    c["c_WB"] = np.tile(wb, (1, 4))
    c["c_CM"] = np.tile(cm, (1, 4))
    c["c_BD"] = (np.arange(384)[None, :] == j[:, None] + 128).astype(f)
    hh = (j >= 64).astype(np.int64)[:, None]
    m = np.arange(128)[None, :]
    c["c_U0"] = np.where(m > 62 + hh, -1e30, 3e38).astype(f)
    l0 = np.full((128, 128), -3e38, dtype=f)
    l0[m == 62 + hh] = 2e30
    l0[m == 61 + hh] = 1e30
    c["c_L0"] = l0
    n = np.arange(256)[:, None]
    jj = np.arange(64)[None, :]
    ov = ((16 * n < 64 * jj + 64) & (16 * n + 32 > 64 * jj)).astype(f)
    ov[255] = 0
    c["c_ovl"] = np.ascontiguousarray(ov.reshape(2, 128, 64).transpose(1, 0, 2).reshape(128, 128))
    c["c_maskB"] = (np.arange(8)[None, :] == (j // 16)[:, None]).astype(f)
    gi = (j // 64)[:, None, None]
    p4 = np.arange(4)[None, :, None]
    g8 = np.arange(8)[None, None, :]
    c["c_maskC"] = (g8 == 2 * p4 + gi).astype(f).reshape(128, 32)
    c["c_iota"] = np.tile(np.arange(128, dtype=f)[None, :], (128, 1))
    c["c_jcol"] = np.arange(128, dtype=f)[:, None].copy()
    return c


def _prep_shared(inp):
    f = np.float32
    A = lambda a: np.ascontiguousarray(np.asarray(a, dtype=f))
    d = {}
    d["wg1"], d["wu1"], d["wd1"] = A(inp["ffn1_w_gate"][0]), A(inp["ffn1_w_up"][0]), A(inp["ffn1_w_down"][0])
    d["wg2"], d["wu2"], d["wd2"] = A(inp["ffn2_w_gate"][0]), A(inp["ffn2_w_up"][0]), A(inp["ffn2_w_down"][0])
    d["w_in"], d["w_out"], d["w_glu"] = A(inp["w_in"][0]), A(inp["w_out"][0]), A(inp["s5_w_glu"][0])
    col8 = lambda v: np.asarray(v, dtype=f).reshape(-1, 128).T
    d["gcols"] = A(np.concatenate([col8(inp["ffn1_norm"][0]), col8(inp["mix_norm"][0]), col8(inp["ffn2_norm"][0]),
                                   col8(inp["final_norm"]), col8(inp["ssm_out_norm"][0])], axis=1))
    d["grow_attn"] = A(np.asarray(inp["attn_out_norm"][0]).reshape(1, 512))
    d["gate_bias"] = A(np.asarray(inp["gate_bias"][0]).reshape(1, 24))
    d["cw1k"], d["cw1v"] = A(inp["cmp_k_w1"][0]), A(inp["cmp_v_w1"][0])
    d["cw2k"], d["cw2v"] = A(inp["cmp_k_w2"][0]), A(inp["cmp_v_w2"][0])
    d["cb1"] = A(np.concatenate([col8(inp["cmp_k_b1"][0]), col8(inp["cmp_v_b1"][0])], axis=1))
    d["posT"] = A(np.concatenate([np.asarray(inp["cmp_pos_k"][0]).T, np.asarray(inp["cmp_pos_v"][0]).T], axis=1))
    lr = np.asarray(inp["s5_lambda_re"][0], dtype=f)
    li = np.asarray(inp["s5_lambda_im"][0], dtype=f)
    ldt = np.repeat(np.asarray(inp["s5_log_dt"][0], dtype=f)[:, None], 64, axis=1)
    d["s5_rows"] = A(np.stack([lr.reshape(2048), li.reshape(2048), ldt.reshape(2048)]))
    colify = lambda a: a.reshape(16, 128).T
    d["s5_cols"] = A(np.concatenate([colify(lr), colify(li), colify(ldt)], axis=1))

    def rows_gh(a):
        return np.repeat(a.reshape(4, 8, 1, 64), 16, axis=2).transpose(1, 2, 0, 3).reshape(128, 4, 64)
    d["s5_rep"] = A(np.stack([rows_gh(lr), rows_gh(li), rows_gh(ldt)], axis=2).reshape(128, 4 * 3 * 64))

    def bt(a):
        return np.asarray(a, dtype=f).reshape(4, 8, 64, 16).transpose(1, 3, 0, 2).reshape(128, 4, 64)
    d["s5_bT"] = A(np.stack([bt(inp["s5_b_re"][0]), bt(inp["s5_b_im"][0])], axis=2).reshape(128, 4 * 2 * 64))

    def ct(a):
        return np.asarray(a, dtype=f).reshape(16, 2, 16, 64).transpose(1, 3, 0, 2).reshape(128, 16, 16)
    d["s5_c"] = A(np.stack([ct(inp["s5_c_re"][0]), ct(inp["s5_c_im"][0])], axis=2).reshape(128, 16 * 2 * 16))
    d["s5_dcol"] = A(np.asarray(inp["s5_d"][0], dtype=f).reshape(4, 128).T)
    d["bglu_col"] = A(np.asarray(inp["s5_b_glu"][0], dtype=f).reshape(4, 128).T)
    d.update(_consts())
    return d


def kernel(**inputs):
    x = np.asarray(inputs["x"], dtype=np.float32)
    shared = _prep_shared(inputs)
    nc = build(dbg=False)
    in_maps = []
    for b in range(NCORES):
        m = dict(shared)
        m["xT"] = np.ascontiguousarray(x[b].T)
        in_maps.append(m)
    res = run_bass_kernel_spmd(nc, in_maps, core_ids=list(range(NCORES)))
    out = np.empty((NCORES, T, D), dtype=np.float32)
    for b in range(NCORES):
        out[b] = np.asarray(res.results[b]["yT"], dtype=np.float32).T
    return out
```

```python
import contextlib
import numpy as np
import concourse.bass as bass
import concourse.mybir as mybir
from concourse.bass_utils import run_bass_kernel_spmd

F32 = mybir.dt.float32
BF16 = mybir.dt.bfloat16
ALU = mybir.AluOpType
AF = mybir.ActivationFunctionType

T = 4096
D = 1024
FF = 2816
NF = FF // 128
EPS = 1e-6
BIGM = 30000.0
NCORES = 8
GELU_C = 1.5957691216057308


class TR:
    def __init__(self, nc, es):
        self.nc, self.es = nc, es
        self.eng = dict(pe=nc.tensor, dve=nc.vector, act=nc.scalar, pool=nc.gpsimd, sp=nc.sync)
        self.sem, self.cnt = {}, {}
        self.seen = {e: {} for e in self.eng}
        for e in self.eng:
            self.sem[e] = es.enter_context(nc.semaphore("s_" + e))
            self.cnt[e] = 0
        self.bs = {}
        self.defer = None

    def _st(self, k):
        s = self.bs.get(k)
        if s is None:
            s = self.bs[k] = ({}, {})
        return s

    def _deps(self, r, w):
        d = {}
        for k in r:
            for sk, v in self._st(k)[0].items():
                if d.get(sk, 0) < v:
                    d[sk] = v
        for k in w:
            s = self._st(k)
            for dd in s:
                for sk, v in dd.items():
                    if d.get(sk, 0) < v:
                        d[sk] = v
        return d

    def _wait(self, e, d):
        for k, v in d.items():
            if k == e and e == 'pe':
                continue
            if self.seen[e].get(k, 0) >= v:
                continue
            self.eng[e].wait_ge(self.sem[k], v)
            self.seen[e][k] = v

    def _mark(self, src, v, r, w):
        for k in r:
            self._st(k)[1][src] = v
        for k in w:
            self._st(k)[0][src] = v

    def op(self, e, fn, r=(), w=()):
        if self.defer is not None:
            r, w = list(r), list(w)
            self.defer.append(lambda: self.op(e, fn, r, w))
            return
        self._wait(e, self._deps(r, w))
        ins = fn(self.eng[e])
        self.cnt[e] += 1
        ins.then_inc(self.sem[e], 1)
        self._mark(e, self.cnt[e], r, w)

    def dma(self, q, ch, out, in_, r=(), w=()):
        if self.defer is not None:
            r, w = list(r), list(w)
            self.defer.append(lambda: self.dma(q, ch, out, in_, r, w))
            return
        if ch not in self.sem:
            self.sem[ch] = self.es.enter_context(self.nc.semaphore("c_" + ch))
            self.cnt[ch] = 0
        self._wait(q, self._deps(r, w))
        ins = self.eng[q].dma_start(out=out, in_=in_)
        self.cnt[ch] += 16
        ins.then_inc(self.sem[ch], 16)
        self._mark(ch, self.cnt[ch], r, w)

    def barrier(self):
        allk = {k: v for k, v in self.cnt.items() if v > 0}
        for e in self.eng:
            self._wait(e, allk)


def build(dbg=False, stop=9):
    nc = bass.Bass("TRN2", target_bir_lowering=False)
    es = contextlib.ExitStack()
    with es:
        tr = TR(nc, es)

        def din(name, shape):
            return nc.dram_tensor(name, list(shape), F32, kind="ExternalInput").ap()

        def dscr(name, shape, dt):
            kind = "ExternalOutput" if dbg else "Internal"
            return nc.dram_tensor(name, list(shape), dt, kind=kind).ap()

        def sb(name, shape, dt):
            return es.enter_context(nc.sbuf_tensor(name, list(shape), dt))

        xT = din("xT", [D, T])
        wgs = [din("wg1", [D, FF]), din("wg2", [D, FF])]
        wus = [din("wu1", [D, FF]), din("wu2", [D, FF])]
        wds = [din("wd1", [FF, D]), din("wd2", [FF, D])]
        w_in = din("w_in", [D, 1816])
        w_out = din("w_out", [D, D])
        w_glu = din("w_glu", [512, 512])
        gcols = din("gcols", [128, 36])
        grow_attn = din("grow_attn", [1, 512])
        gate_bias = din("gate_bias", [1, 24])
        cw1 = [din("cw1k", [2048, 256]), din("cw1v", [2048, 256])]
        cw2 = [din("cw2k", [256, 64]), din("cw2v", [256, 64])]
        cb1 = din("cb1", [128, 4])
        posT = din("posT", [64, 64])
        s5_rows = din("s5_rows", [3, 2048])
        s5_cols = din("s5_cols", [128, 48])
        s5_rep = din("s5_rep", [128, 4 * 3 * 64])
        s5_bT = din("s5_bT", [128, 4 * 2 * 64])
        s5_c = din("s5_c", [128, 16 * 2 * 16])
        s5_dcol = din("s5_dcol", [128, 4])
        bglu_col = din("bglu_col", [128, 4])
        c_ident = din("c_ident", [128, 128])
        c_tri = din("c_tri", [128, 128])
        c_E0 = din("c_E0", [64, T])
        c_CB = din("c_CB", [128, 512])
        c_WB = din("c_WB", [128, 512])
        c_CM = din("c_CM", [128, 512])
        c_BD = din("c_BD", [128, 384])
        c_U0 = din("c_U0", [128, 128])
        c_L0 = din("c_L0", [128, 128])
        c_ovl = din("c_ovl", [128, 128])
        c_maskB = din("c_maskB", [128, 8])
        c_maskC = din("c_maskC", [128, 32])
        c_iota = din("c_iota", [128, 128])
        c_jcol = din("c_jcol", [128, 1])

        yT = nc.dram_tensor("yT", [D, T], F32, kind="ExternalOutput").ap()
        x1T = dscr("x1T", [D, T], F32)
        h2T = dscr("h2T", [D, T], BF16)
        qTd = dscr("qTd", [64, 32 * 8 * 128], BF16)
        uTd = dscr("uTd", [128, 4 * T], BF16)
        mixT = dscr("mixT", [D, T], BF16)

        GC = sb("GC", [128, 36], F32)
        ONES = sb("ONES", [128, 128], BF16)
        IDN = sb("IDN", [128, 128], BF16)
        EPSC = sb("EPSC", [128, 1], F32)
        tr.dma('sp', 'const', GC[:], gcols[:, :], w=['c'])
        tr.dma('pool', 'const', IDN[:], c_ident[:, :], w=['c'])
        tr.op('dve', lambda e: e.memset(ONES[:], 1.0), w=['c'])
        tr.op('dve', lambda e: e.memset(EPSC[:], 0.0), w=['c'])

        pb = [es.enter_context(nc.psum_tensor("pb%d" % i, [128, 512], F32)) for i in range(8)]

        def xview(dram, t0, n):
            return dram.rearrange("(k p) t -> p k t", p=128)[:, :, t0:t0 + n]

        def ffn_weights(ph, stk):
            WG = stk.enter_context(nc.sbuf_tensor("WG_%d" % ph, [128, 8, FF], BF16))
            WU = stk.enter_context(nc.sbuf_tensor("WU_%d" % ph, [128, 8, FF], BF16))
            WD = stk.enter_context(nc.sbuf_tensor("WD_%d" % ph, [128, NF, D], BF16))
            wgd, wud, wdd = wgs[ph - 1], wus[ph - 1], wds[ph - 1]
            for k in range(8):
                tr.dma('pool', 'wg', WG[:, k, :], wgd[k * 128:(k + 1) * 128, :], w=['WG'])
                tr.dma('pool', 'wu', WU[:, k, :], wud[k * 128:(k + 1) * 128, :], w=['WU'])
            for f in range(NF):
                tr.dma('pool', 'wd', WD[:, f, :], wdd[f * 128:(f + 1) * 128, :], w=['WD'])
            return WG, WU, WD

        def ffn_phase(ph, wts=None):
            with contextlib.ExitStack() as fs:
                def fsb(name, shape, dt):
                    return fs.enter_context(nc.sbuf_tensor("%s_%d" % (name, ph), list(shape), dt))
                WG, WU, WD = wts if wts is not None else ffn_weights(ph, fs)
                XT = [fsb("XT%d" % i, [128, 8, 512], F32) for i in range(2)]
                H = fsb("H", [128, 8, 512], BF16)
                AT = fsb("AT", [128, NF, 512], BF16)
                SG = [fsb("SG%d" % i, [128, 512], BF16) for i in range(2)]
                RS = [fsb("RS%d" % i, [128, 512], F32) for i in range(2)]
                src = xT if ph == 1 else x1T
                g0 = 0 if ph == 1 else 16
                hkeys = ['h%d' % k for k in range(8)]
                atk2 = ['at%d' % f for f in range(14, 22)]

                def xs(tt):
                    return XT[tt % 2], 'xt%d' % (tt % 2)

                def load(tt):
                    X, xk = xs(tt)
                    tr.dma('sp', 'xin%d' % (tt % 2), X[:], xview(src, tt * 512, 512), w=[xk])

                def norm_a(X, xk, HB, hk):
                    tr.op('act', lambda e: e.activation(out=HB, in_=X[:], func=AF.Square), r=[xk], w=hk)

                def norm_b(HBk, hk, PN, pnk):
                    for k in range(8):
                        tr.op('pe', lambda e, k=k: e.matmul(out=PN[:], lhsT=ONES[:], rhs=HBk(k),
                                                           start=(k == 0), stop=(k == 7)), r=[hk[k], 'c'], w=[pnk])

                def norm_c(X, xk, HBk, hk, PN, pnk, R, rk, goff, inplace=False):
                    tr.op('dve', lambda e: e.tensor_scalar(out=R[:], in0=PN[:], scalar1=1.0 / D, scalar2=EPS,
                                                          op0=ALU.mult, op1=ALU.add), r=[pnk], w=[rk])
                    tr.op('act', lambda e: e.activation(out=R[:], in_=R[:], func=AF.Sqrt), r=[rk], w=[rk])
                    tr.op('dve', lambda e: e.reciprocal(out=R[:], in_=R[:]), r=[rk], w=[rk])
                    for k in range(8):
                        dst = X[:, k, :] if inplace else HBk(k)
                        tr.op('dve', lambda e, k=k, dst=dst: e.scalar_tensor_tensor(
                            out=dst, in0=X[:, k, :], scalar=GC[:, goff + k:goff + k + 1], in1=R[:],
                            op0=ALU.mult, op1=ALU.mult), r=[xk, rk, 'c'], w=[xk if inplace else hk[k]])

                Hk = lambda k: H[:, k, :]
                A2k = lambda k: AT[:, 14 + k, :]

                def post_a(tt):
                    X, xk = xs(tt)
                    if ph == 1:
                        tr.dma('sp', 'xout%d' % (tt % 2), xview(x1T, tt * 512, 512), X[:], r=[xk], w=['x1T'])
                    norm_a(X, xk, AT[:, 14:22, :], atk2)

                def post_bc(tt):
                    X, xk = xs(tt)
                    norm_b(A2k, atk2, pb[7], 'pn2')
                    if ph == 1:
                        norm_c(X, xk, A2k, atk2, pb[7], 'pn2', RS[1], 'rs1', 8)
                        tr.dma('sp', 'hout', xview(h2T, tt * 512, 512), AT[:, 14:22, :], r=atk2, w=['h2T'])
                    else:
                        norm_c(X, xk, A2k, atk2, pb[7], 'pn2', RS[1], 'rs1', 24, inplace=True)
                        tr.dma('sp', 'yout%d' % (tt % 2), xview(yT, tt * 512, 512), X[:], r=[xk], w=['yT'])

                load(0)
                X0, xk0 = xs(0)
                norm_a(X0, xk0, H[:], hkeys)
                norm_b(Hk, hkeys, pb[6], 'pn')
                norm_c(X0, xk0, Hk, hkeys, pb[6], 'pn', RS[0], 'rs0', g0)
                for tt in range(8):
                    X, xk = xs(tt)
                    for f in range(NF):
                        b = f % 2
                        for k in range(8):
                            tr.op('pe', lambda e, k=k, f=f, b=b: e.matmul(
                                out=pb[b][:], lhsT=WG[:, k, f * 128:(f + 1) * 128], rhs=H[:, k, :],
                                start=(k == 0), stop=(k == 7)), r=['WG', hkeys[k]], w=['pg%d' % b])
                        for k in range(8):
                            tr.op('pe', lambda e, k=k, f=f, b=b: e.matmul(
                                out=pb[2 + b][:], lhsT=WU[:, k, f * 128:(f + 1) * 128], rhs=H[:, k, :],
                                start=(k == 0), stop=(k == 7)), r=['WU', hkeys[k]], w=['pu%d' % b])
                        tr.op('act', lambda e, b=b: e.activation(out=SG[b][:], in_=pb[b][:], func=AF.Silu),
                              r=['pg%d' % b], w=['sg%d' % b])
                        tr.op('dve', lambda e, b=b, f=f: e.tensor_tensor(out=AT[:, f, :], in0=SG[b][:],
                                                                         in1=pb[2 + b][:], op=ALU.mult),
                              r=['sg%d' % b, 'pu%d' % b], w=['at%d' % f])
                        if f == 2 and tt > 0:
                            post_bc(tt - 1)
                    if tt + 1 < 8:
                        load(tt + 1)
                        Xn, xkn = xs(tt + 1)
                        norm_a(Xn, xkn, H[:], hkeys)
                    for dk in range(8):
                        b = dk % 2
                        for f in range(NF):
                            tr.op('pe', lambda e, f=f, dk=dk, b=b: e.matmul(
                                out=pb[4 + b][:], lhsT=WD[:, f, dk * 128:(dk + 1) * 128], rhs=AT[:, f, :],
                                start=(f == 0), stop=(f == NF - 1)), r=['WD', 'at%d' % f], w=['pd%d' % b])
                        tr.op('dve', lambda e, dk=dk, b=b, X=X: e.scalar_tensor_tensor(
                            out=X[:, dk, :], in0=pb[4 + b][:], scalar=0.5, in1=X[:, dk, :],
                            op0=ALU.mult, op1=ALU.add), r=['pd%d' % b, xk], w=[xk])
                        if dk == 2 and tt + 1 < 8:
                            norm_b(Hk, hkeys, pb[6], 'pn')
                            norm_c(Xn, xkn, Hk, hkeys, pb[6], 'pn', RS[0], 'rs0', g0)
                    post_a(tt)
                post_bc(7)
            tr.barrier()

        def attention_phase():
            with contextlib.ExitStack() as a_:
                def asb(name, shape, dt):
                    return a_.enter_context(nc.sbuf_tensor(name, list(shape), dt))
                CBt = asb("CBt", [128, 512], BF16)
                WBt = asb("WBt", [128, 512], BF16)
                CMt = asb("CMt", [128, 512], BF16)
                BDt = asb("BDt", [128, 384], BF16)
                U0t = asb("U0t", [128, 128], F32)
                L0t = asb("L0t", [128, 128], F32)
                GAT = asb("GAT", [128, 512], F32)
                QN = [asb("QN%d" % i, [128, 512], BF16) for i in range(2)]
                ET = [asb("ET%d" % i, [128, 512], BF16) for i in range(3)]
                SCO = asb("SCO", [128, 64], F32)
                M8 = asb("M8", [128, 16], F32)
                WK = asb("WK", [128, 64], F32)
                SELW = asb("SELW", [128, 128], BF16)
                RZ = asb("RZ", [128, 4], F32)
                DEN = asb("DEN", [128, 12], F32)
                ATT = asb("ATT", [128, 512], F32)
                ATN = asb("ATN", [128, 512], BF16)
                JUNK = asb("JUNK", [128, 512], F32)
                SSQ = asb("SSQ", [128, 1], F32)
                MST = asb("MST", [128, 4, 128], BF16)
                tr.dma('pool', 'aconst', CBt[:], c_CB[:, :], w=['ac'])
                tr.dma('pool', 'aconst', WBt[:], c_WB[:, :], w=['ac'])
                tr.dma('pool', 'aconst', CMt[:], c_CM[:, :], w=['ac'])
                tr.dma('pool', 'aconst', BDt[:], c_BD[:, :], w=['ac'])
                tr.dma('sp', 'aconst', U0t[:], c_U0[:, :], w=['ac'])
                tr.dma('sp', 'aconst', L0t[:], c_L0[:, :], w=['ac'])
                tr.dma('sp', 'aconst', GAT[:], grow_attn.partition_broadcast(128), w=['ac'])
                tr.op('dve', lambda e: e.memset(SELW[:], 0.0), w=['selw'])
                OS = pb[3][:, 0:260].rearrange("p (r x) -> p r x", x=65)
                OW = pb[4][:, 0:260].rearrange("p (r x) -> p r x", x=65)
                OC = pb[5][:, 0:260].rearrange("p (r x) -> p r x", x=65)
                IM = pb[6][:, 0:256].rearrange("p (r x) -> p r x", x=64)
                PT = pb[5][:, 320:384].bitcast(BF16)
                PT4 = pb[6][:, 256:512].bitcast(BF16)
                PA = pb[7]
                ETA = [asb("ETA%d" % i, [128, 512], BF16) for i in range(2)]
                dq = []

                def pump(k):
                    for _ in range(min(k, len(dq))):
                        dq.pop(0)()

                def deferred(f, *a):
                    tr.defer = dq
                    f(*a)
                    tr.defer = None
                sring = [0]
                ering = [0]

                def nexts():
                    b = sring[0] % 3
                    sring[0] += 1
                    return pb[b], 'ps%d' % b

                def nexte():
                    b = ering[0] % 3
                    ering[0] += 1
                    return ET[b], 'et%d' % b

                qview = qTd.rearrange("p (c x) -> p c x", c=32)
                QN3 = [QN[0], QN[1], asb("QN2", [128, 512], BF16)]
                OCS = [asb("OCS%d" % i, [128, 4, 65], F32) for i in range(2)]
                OSS = [asb("OSS%d" % i, [128, 4, 65], F32) for i in range(2)]
                OWS = [asb("OWS%d" % i, [128, 4, 65], F32) for i in range(2)]
                ATN2 = [ATN, asb("ATN1", [128, 512], BF16)]
                NIT = 64
                mixv = mixT.rearrange("(k p) t -> p k t", p=128)
                pending = []

                def qload(it):
                    c, g = divmod(it, 2)
                    sl = it % 3
                    tr.dma('sp', 'qin%d' % sl, QN3[sl][0:64, :], qview[:, c, g * 512:(g + 1) * 512], r=['qTd'],
                           w=['qnq%d' % sl])

                def stageA(it):
                    c, g = divmod(it, 2)
                    sl = it % 3
                    Q = QN3[sl]
                    qq, qs = 'qnq%d' % sl, 'qns%d' % sl
                    nts = 1 if c < 16 else 2
                    tl = []
                    for nt in range(nts):
                        Mn = min(128, 8 * c + 7 - 128 * nt)
                        m = 8 * c - 128 * nt
                        need = m <= 192
                        PS, psk = PA, 'pa'
                        tr.op('pe', lambda e, PS=PS, Mn=Mn, nt=nt, need=need: e.matmul(
                            out=PS[0:Mn, :], lhsT=KC[g][:, 128 * nt:128 * nt + Mn], rhs=Q[0:64, :],
                            start=True, stop=not need), r=['KC%d' % g, qq], w=[psk])
                        if need:
                            tr.op('pe', lambda e, PS=PS, Mn=Mn, m=m: e.matmul(
                                out=PS[0:Mn, :], lhsT=BDt[:, 192 - m:192 - m + Mn], rhs=CMt[:],
                                start=False, stop=True), r=['ac'], w=[psk])
                        Et, ek = ETA[nt], 'eta%d' % nt
                        tr.op('act', lambda e, PS=PS, Et=Et, Mn=Mn: e.activation(
                            out=Et[0:Mn, :], in_=PS[0:Mn, :], func=AF.Exp, scale=0.125), r=[psk], w=[ek])
                        tl.append((nt, Mn, Et, ek))
                    first = True
                    for (nt, Mn, Et, ek) in tl:
                        for r in range(4):
                            tr.op('pe', lambda e, Et=Et, Mn=Mn, nt=nt, r=r, first=first: e.matmul(
                                out=OC[:, r, :], lhsT=Et[0:Mn, r * 128:(r + 1) * 128], rhs=VCX[g][0:Mn, nt, 0:65],
                                start=first, stop=(nt == nts - 1), skip_group_check=True),
                                r=[ek, 'VCX%d' % g], w=['oc'])
                            tr.op('pe', lambda e, Et=Et, Mn=Mn, nt=nt, r=r, first=first: e.matmul(
                                out=IM[:, r, :], lhsT=Et[0:Mn, r * 128:(r + 1) * 128], rhs=VCX[g][0:Mn, nt, 65:129],
                                start=first, stop=(nt == nts - 1), skip_group_check=True),
                                r=[ek, 'VCX%d' % g], w=['im'])
                            first = False
                    OCb, ock = OCS[it % 2], 'ocs%d' % (it % 2)
                    tr.op('act', lambda e, OCb=OCb: e.activation(out=OCb[:], in_=OC, func=AF.Copy), r=['oc'], w=[ock])
                    tr.op('dve', lambda e, OCb=OCb: e.tensor_scalar(out=RZ[:], in0=OCb[:, :, 64], scalar1=1e-30,
                                                                   scalar2=None, op0=ALU.max), r=[ock], w=['rz'])
                    tr.op('dve', lambda e: e.reciprocal(out=RZ[:], in_=RZ[:]), r=['rz'], w=['rz'])
                    tr.op('dve', lambda e: e.tensor_scalar(out=SCO[:], in0=IM[:, 0, :], scalar1=RZ[:, 0:1],
                                                          scalar2=None, op0=ALU.mult), r=['im', 'rz'], w=['sco'])
                    for r in range(1, 4):
                        tr.op('dve', lambda e, r=r: e.scalar_tensor_tensor(
                            out=SCO[:], in0=IM[:, r, :], scalar=RZ[:, r:r + 1], in1=SCO[:],
                            op0=ALU.mult, op1=ALU.add), r=['im', 'rz', 'sco'], w=['sco'])
                    lo = 62 - 2 * c
                    tr.op('dve', lambda e, lo=lo: e.tensor_tensor(out=SCO[:], in0=SCO[:], in1=L0t[:, lo:lo + 64],
                                                                 op=ALU.max), r=['sco', 'ac'], w=['sco'])
                    tr.op('dve', lambda e, lo=lo: e.tensor_tensor(out=SCO[:], in0=SCO[:], in1=U0t[:, lo:lo + 64],
                                                                 op=ALU.min), r=['sco', 'ac'], w=['sco'])
                    tr.op('dve', lambda e: e.memset(SCO[:, 0:1], 3e30), r=['sco'], w=['sco'])
                    tr.op('dve', lambda e: e.max(out=M8[:, 0:8], in_=SCO[:]), r=['sco'], w=['m8'])
                    tr.op('dve', lambda e: e.match_replace(out=WK[:], in_to_replace=M8[:, 0:8], in_values=SCO[:],
                                                          imm_value=-3e38), r=['sco', 'm8'], w=['wk'])
                    tr.op('dve', lambda e: e.max(out=M8[:, 8:16], in_=WK[:]), r=['wk'], w=['m8'])
                    tr.op('dve', lambda e: e.tensor_scalar(out=SELW[:, 64:128], in0=SCO[:], scalar1=M8[:, 15:16],
                                                          scalar2=None, op0=ALU.is_ge), r=['sco', 'm8'], w=['selw'])
                    tr.op('pe', lambda e: e.transpose(out=PT, in_=SELW[:], identity=IDN[:]), r=['selw', 'c'], w=['pt'])
                    tr.op('dve', lambda e, Q=Q: e.tensor_scalar(
                        out=Q[64:128, :].rearrange("p (r q) -> p r q", r=4),
                        in0=PT[64:128, :].unsqueeze(1).to_broadcast([64, 4, 128]),
                        scalar1=-1.0, scalar2=BIGM, op0=ALU.add, op1=ALU.mult), r=['pt'], w=[qs])

                def finish_pe(c):
                    AN = ATN2[c % 2]
                    for j in range(4):
                        tr.op('pe', lambda e, j=j, AN=AN: e.transpose(out=PT4[:, j * 128:(j + 1) * 128],
                                                                     in_=AN[:, j * 128:(j + 1) * 128], identity=IDN[:]),
                              r=['atn%d' % (c % 2), 'c'], w=['pt4'])
                    tr.op('dve', lambda e: e.tensor_copy(out=MST[:].rearrange("p a b -> p (a b)"), in_=PT4),
                          r=['pt4'], w=['mst'])
                    tr.dma('sp', 'mout', mixv[:, 0:4, c * 128:(c + 1) * 128], MST[:], r=['mst'], w=['mixT'])

                def stageB(it):
                    c, g = divmod(it, 2)
                    sl = it % 3
                    Q = QN3[sl]
                    qq, qs = 'qnq%d' % sl, 'qns%d' % sl
                    k0 = max(0, c - 4)
                    tiles = [('w', kt) for kt in range(k0, c + 1)] + [('s', kt) for kt in range(c + 1)]
                    n = len(tiles)
                    info = {}

                    def qk(i):
                        br, kt = tiles[i]
                        PS, psk = nexts()
                        extra = []
                        if kt == c:
                            extra.append(CBt)
                        if br == 'w' and kt == c - 4:
                            extra.append(WBt)
                        if br == 'w':
                            tr.op('pe', lambda e, PS=PS, kt=kt, ne=len(extra): e.matmul(
                                out=PS[:], lhsT=KW[g][:, kt * 128:(kt + 1) * 128], rhs=Q[0:64, :],
                                start=True, stop=(ne == 0)), r=['KW%d' % g, qq], w=[psk])
                        else:
                            tr.op('pe', lambda e, PS=PS, kt=kt, ne=len(extra): e.matmul(
                                out=PS[:], lhsT=KE[g][:, kt * 128:(kt + 1) * 128], rhs=Q[:, :],
                                start=True, stop=(ne == 0)), r=['KE%d' % g, qq, qs], w=[psk])
                        for j, bt in enumerate(extra):
                            tr.op('pe', lambda e, PS=PS, bt=bt, last=(j == len(extra) - 1): e.matmul(
                                out=PS[:], lhsT=IDN[:], rhs=bt[:], start=False, stop=last), r=['ac', 'c'], w=[psk])
                        Et, ek = nexte()
                        tr.op('act', lambda e, PS=PS, Et=Et: e.activation(
                            out=Et[:], in_=PS[:], func=AF.Exp, scale=0.125), r=[psk], w=[ek])
                        info[i] = (Et, ek)

                    started = {'w': False, 's': False}
                    lastw = max(i for i in range(n) if tiles[i][0] == 'w')

                    def pv(i):
                        br, kt = tiles[i]
                        Et, ek = info[i]
                        O, ok, V, vk = (OW, 'ow', VW1[g], 'VW1%d' % g) if br == 'w' else (OS, 'os', VS1[g], 'VS1%d' % g)
                        last = (i == lastw) if br == 'w' else (i == n - 1)
                        for r in range(4):
                            st = not started[br]
                            started[br] = True
                            tr.op('pe', lambda e, Et=Et, kt=kt, r=r, O=O, V=V, st=st, last=last: e.matmul(
                                out=O[:, r, :], lhsT=Et[:, r * 128:(r + 1) * 128], rhs=V[:, kt, :],
                                start=st, stop=last, skip_group_check=True), r=[ek, vk], w=[ok])

                    for i in range(min(2, n)):
                        qk(i)
                    for i in range(n):
                        if i + 2 < n:
                            qk(i + 2)
                        pv(i)
                        left = max(1, n - 1 - i)
                        pump((len(dq) + left - 1) // left)
                    pump(len(dq))
                    b2 = it % 2
                    tr.op('act', lambda e: e.activation(out=OWS[b2][:], in_=OW, func=AF.Copy), r=['ow'], w=['ows%d' % b2])
                    tr.op('act', lambda e: e.activation(out=OSS[b2][:], in_=OS, func=AF.Copy), r=['os'], w=['oss%d' % b2])

                def tailB(it):
                    c, g = divmod(it, 2)
                    b2 = it % 2
                    srcs = ((OCS[b2], 'ocs%d' % b2), (OSS[b2], 'oss%d' % b2), (OWS[b2], 'ows%d' % b2))
                    for bi, (O, ok) in enumerate(srcs):
                        tr.op('dve', lambda e, O=O, bi=bi: e.tensor_scalar(
                            out=DEN[:, bi * 4:bi * 4 + 4], in0=O[:, :, 64], scalar1=1e-30, scalar2=None,
                            op0=ALU.max), r=[ok], w=['den'])
                    tr.op('dve', lambda e: e.reciprocal(out=DEN[:], in_=DEN[:]), r=['den'], w=['den'])
                    gv = GT[:, c, 12 * g:12 * g + 12].rearrange("p (r b) -> p b r", b=3)
                    tr.op('dve', lambda e, gv=gv: e.tensor_tensor(
                        out=DEN[:].rearrange("p (b r) -> p b r", b=3), in0=DEN[:].rearrange("p (b r) -> p b r", b=3),
                        in1=gv, op=ALU.mult), r=['den', 'GT'], w=['den'])
                    for r in range(4):
                        dst = ATT[:, (4 * g + r) * 64:(4 * g + r + 1) * 64]
                        tr.op('dve', lambda e, r=r, dst=dst: e.tensor_scalar(
                            out=dst, in0=OCS[b2][:, r, 0:64], scalar1=DEN[:, r:r + 1], scalar2=None, op0=ALU.mult),
                            r=['ocs%d' % b2, 'den'], w=['att'])
                        tr.op('dve', lambda e, r=r, dst=dst: e.scalar_tensor_tensor(
                            out=dst, in0=OSS[b2][:, r, 0:64], scalar=DEN[:, 4 + r:5 + r], in1=dst,
                            op0=ALU.mult, op1=ALU.add), r=['oss%d' % b2, 'den', 'att'], w=['att'])
                        tr.op('dve', lambda e, r=r, dst=dst: e.scalar_tensor_tensor(
                            out=dst, in0=OWS[b2][:, r, 0:64], scalar=DEN[:, 8 + r:9 + r], in1=dst,
                            op0=ALU.mult, op1=ALU.add), r=['ows%d' % b2, 'den', 'att'], w=['att'])
                    if g == 1:
                        AN, ank = ATN2[c % 2], 'atn%d' % (c % 2)
                        tr.op('act', lambda e: e.activation(out=JUNK[:], in_=ATT[:], func=AF.Square, accum_out=SSQ[:]),
                              r=['att'], w=['junk', 'ssq'])
                        tr.op('dve', lambda e: e.tensor_scalar(out=SSQ[:], in0=SSQ[:], scalar1=1.0 / 512, scalar2=EPS,
                                                              op0=ALU.mult, op1=ALU.add), r=['ssq'], w=['ssq'])
                        tr.op('act', lambda e: e.activation(out=SSQ[:], in_=SSQ[:], func=AF.Ln), r=['ssq'], w=['ssq'])
                        tr.op('act', lambda e: e.activation(out=SSQ[:], in_=SSQ[:], func=AF.Exp, scale=-0.5),
                              r=['ssq'], w=['ssq'])
                        tr.op('dve', lambda e, AN=AN: e.scalar_tensor_tensor(out=AN[:], in0=ATT[:], scalar=SSQ[:, 0:1],
                                                                            in1=GAT[:], op0=ALU.mult, op1=ALU.mult),
                              r=['att', 'ssq', 'ac'], w=[ank])

                qload(0)
                qload(1)
                stageA(0)
                for it in range(NIT):
                    if it >= 1:
                        deferred(tailB, it - 1)
                    if it + 1 < NIT:
                        deferred(stageA, it + 1)
                    if it >= 1 and (it - 1) % 2 == 1:
                        deferred(finish_pe, (it - 1) // 2)
                    if it + 2 < NIT:
                        qload(it + 2)
                    stageB(it)
                tailB(NIT - 1)
                finish_pe((NIT - 1) // 2)
            tr.barrier()

        def s5_phase():
            PI = float(np.pi)
            with contextlib.ExitStack() as s_:
                def ssb(name, shape, dt):
                    return s_.enter_context(nc.sbuf_tensor(name, list(shape), dt))
                ROW = [ssb("ROW%d" % i, [128, 2048], F32) for i in range(3)]
                PRE_RE = ssb("PRE_RE", [128, 2048], F32)
                PRE_IM = ssb("PRE_IM", [128, 2048], F32)
                TH = ssb("TH", [128, 2048], F32)
                TMPI = ssb("TMPI", [128, 2048], mybir.dt.int32)
                JC = ssb("JC", [128, 1], F32)
                NJC = ssb("NJC", [128, 1], F32)
                NPI = ssb("NPI", [128, 1], F32)
                COL = ssb("COL", [128, 48], F32)
                IOT = ssb("IOT", [128, 128], F32)
                POST_RE = ssb("POST_RE", [128, 16, 128], F32)
                POST_IM = ssb("POST_IM", [128, 16, 128], F32)
                A128 = ssb("A128", [128, 2, 16], F32)
                SM = [ssb("SM%d" % i, [128, 16], F32) for i in range(3)]
                REP = ssb("REP", [128, 4, 3, 64], F32)
                BT = ssb("BT", [128, 4, 2, 64], F32)
                BB = ssb("BB", [128, 4, 2, 64], F32)
                RT = [ssb("RT%d" % i, [128, 4, 64], F32) for i in range(6)]
                BBD = ssb("BBD", [128, 4, 1024], BF16)
                MASKB = ssb("MASKB", [128, 8], F32)
                CC = ssb("CC", [128, 16, 2, 16], F32)
                MASKC = ssb("MASKC", [128, 4, 8], F32)
                CMAT = ssb("CMAT", [128, 16, 2, 128], BF16)
                DCOL = ssb("DCOL", [128, 4], F32)
                BGL = ssb("BGL", [128, 4], F32)
                WGL = ssb("WGL", [128, 4, 512], BF16)
                TRI = ssb("TRI", [128, 128], BF16)
                UT = [ssb("UT%d" % i, [128, 4, 512], BF16) for i in range(2)]
                W = ssb("W", [128, 16, 2, 128], BF16)
                T1 = [ssb("T1_%d" % i, [128, 512], F32) for i in range(2)]
                T2 = [ssb("T2_%d" % i, [128, 512], F32) for i in range(2)]
                ZCF = ssb("ZCF", [128, 16, 2, 128], F32)
                CRE = ssb("CRE", [128, 16], F32)
                CIM = ssb("CIM", [128, 16], F32)
                TQ = [ssb("TQ%d" % i, [128, 16], F32) for i in range(4)]
                U1 = [ssb("U1_%d" % i, [128, 512], F32) for i in range(2)]
                U2 = [ssb("U2_%d" % i, [128, 512], F32) for i in range(2)]
                X = ssb("X", [128, 16, 2, 128], BF16)
                CARRY = ssb("CARRY", [128, 16, 2], F32)
                CT = [ssb("CT%d" % i, [128, 2, 2], F32) for i in range(2)]
                Y = ROW[0][:].rearrange("p (a b) -> p a b", a=4)
                YB = ROW[1][:].rearrange("p (a b) -> p a b", a=4)
                YG = ROW[2][:].rearrange("p (a b) -> p a b", a=4)
                YGB = ssb("YGB", [128, 4, 512], BF16)
                SGL = [ssb("SGL%d" % i, [128, 512], F32) for i in range(2)]
                S = TH[:].rearrange("p (a b) -> p a b", a=4)
                SQ = ssb("SQ", [128, 4, 512], BF16)
                RS5 = ssb("RS5", [128, 512], F32)
                OUT = ssb("OUT", [128, 4, 512], BF16)
                ck = ['s5c']
                for i in range(3):
                    tr.dma('sp', 's5const', ROW[i][:], s5_rows[i:i + 1, :].partition_broadcast(128), w=ck)
                tr.dma('sp', 's5const', JC[:], c_jcol[:, :], w=ck)
                tr.dma('sp', 's5const', COL[:], s5_cols[:, :], w=ck)
                tr.dma('sp', 's5const', IOT[:], c_iota[:, :], w=ck)
                tr.dma('sp', 's5const', REP[:].rearrange("p a b c -> p (a b c)"), s5_rep[:, :], w=ck)
                tr.dma('sp', 's5const', BT[:].rearrange("p a b c -> p (a b c)"), s5_bT[:, :], w=ck)
                tr.dma('sp', 's5const', MASKB[:], c_maskB[:, :], w=ck)
                tr.dma('sp', 's5const', CC[:].rearrange("p a b c -> p (a b c)"), s5_c[:, :], w=ck)
                tr.dma('sp', 's5const', MASKC[:].rearrange("p a b -> p (a b)"), c_maskC[:, :], w=ck)
                tr.dma('sp', 's5const', DCOL[:], s5_dcol[:, :], w=ck)
                tr.dma('sp', 's5const', BGL[:], bglu_col[:, :], w=ck)
                tr.dma('pool', 's5const', TRI[:], c_tri[:, :], w=ck)
                for k in range(4):
                    tr.dma('pool', 's5const', WGL[:, k, :], w_glu[k * 128:(k + 1) * 128, :], w=ck)
                V = lambda fn, r, w: tr.op('dve', fn, r=r, w=w)
                A = lambda fn, r, w: tr.op('act', fn, r=r, w=w)
                V(lambda e: e.memset(NPI[:], -PI), [], ck)
                V(lambda e: e.tensor_scalar(out=NJC[:], in0=JC[:], scalar1=-1.0, scalar2=None, op0=ALU.mult), ck, ck)
                V(lambda e: e.memset(CRE[:], 0.0), [], ['carry'])
                V(lambda e: e.memset(CIM[:], 0.0), [], ['carry'])

                def sincos(out_sin, out_cos, theta, tmpf, tmpi):
                    for out, shift in ((out_sin, 0.0), (out_cos, 0.5 * PI)):
                        V(lambda e, shift=shift: e.tensor_scalar(out=tmpf, in0=theta, scalar1=shift, scalar2=1.0 / (2 * PI),
                                                                 op0=ALU.add, op1=ALU.mult), ck, ck)
                        V(lambda e: e.tensor_copy(out=tmpi, in_=tmpf), ck, ck)
                        V(lambda e: e.tensor_copy(out=tmpf, in_=tmpi), ck, ck)
                        V(lambda e, out=out: e.scalar_tensor_tensor(out=out, in0=tmpf, scalar=-2 * PI, in1=theta,
                                                                    op0=ALU.mult, op1=ALU.add), ck, ck)
                        if shift:
                            V(lambda e, out=out, shift=shift: e.tensor_scalar(out=out, in0=out, scalar1=shift, scalar2=None,
                                                                              op0=ALU.add), ck, ck)
                        V(lambda e, out=out: e.tensor_scalar(out=tmpf, in0=out, scalar1=PI, scalar2=-2 * PI,
                                                             op0=ALU.is_gt, op1=ALU.mult), ck, ck)
                        V(lambda e, out=out: e.tensor_tensor(out=out, in0=out, in1=tmpf, op=ALU.add), ck, ck)
                        A(lambda e, out=out: e.activation(out=out, in_=out, func=AF.Sin), ck, ck)

                A(lambda e: e.activation(out=ROW[2][:], in_=ROW[2][:], func=AF.Exp), ck, ck)
                V(lambda e: e.tensor_tensor(out=ROW[0][:], in0=ROW[0][:], in1=ROW[2][:], op=ALU.mult), ck, ck)
                V(lambda e: e.tensor_tensor(out=ROW[1][:], in0=ROW[1][:], in1=ROW[2][:], op=ALU.mult), ck, ck)
                A(lambda e: e.activation(out=ROW[2][:], in_=ROW[0][:], func=AF.Exp, scale=NJC[:, 0:1]), ck, ck)
                V(lambda e: e.tensor_scalar(out=TH[:], in0=ROW[1][:], scalar1=JC[:, 0:1], scalar2=None, op0=ALU.mult),
                  ck, ck)
                sincos(PRE_IM[:], PRE_RE[:], TH[:], ROW[0][:], TMPI[:])
                V(lambda e: e.tensor_tensor(out=PRE_RE[:], in0=PRE_RE[:], in1=ROW[2][:], op=ALU.mult), ck, ck)
                V(lambda e: e.scalar_tensor_tensor(out=PRE_IM[:], in0=PRE_IM[:], scalar=-1.0, in1=ROW[2][:],
                                                   op0=ALU.mult, op1=ALU.mult), ck, ck)
                LRc, LIc, DTc = COL[:, 0:16], COL[:, 16:32], COL[:, 32:48]
                A(lambda e: e.activation(out=DTc, in_=DTc, func=AF.Exp), ck, ck)
                V(lambda e: e.tensor_tensor(out=LRc, in0=LRc, in1=DTc, op=ALU.mult), ck, ck)
                V(lambda e: e.tensor_tensor(out=LIc, in0=LIc, in1=DTc, op=ALU.mult), ck, ck)
                iob = IOT[:].unsqueeze(1).to_broadcast([128, 16, 128])
                THv = TH[:].rearrange("p (a b) -> p a b", a=16)
                R0v = ROW[0][:].rearrange("p (a b) -> p a b", a=16)
                R2v = ROW[2][:].rearrange("p (a b) -> p a b", a=16)
                V(lambda e: e.tensor_tensor(out=R2v, in0=iob, in1=LRc.unsqueeze(2).to_broadcast([128, 16, 128]),
                                            op=ALU.mult), ck, ck)
                A(lambda e: e.activation(out=ROW[2][:], in_=ROW[2][:], func=AF.Exp), ck, ck)
                V(lambda e: e.tensor_tensor(out=THv, in0=iob, in1=LIc.unsqueeze(2).to_broadcast([128, 16, 128]),
                                            op=ALU.mult), ck, ck)
                sincos(POST_IM[:].rearrange("p a b -> p (a b)"), POST_RE[:].rearrange("p a b -> p (a b)"), TH[:],
                       ROW[0][:], TMPI[:])
                V(lambda e: e.tensor_tensor(out=POST_RE[:], in0=POST_RE[:], in1=R2v, op=ALU.mult), ck, ck)
                V(lambda e: e.tensor_tensor(out=POST_IM[:], in0=POST_IM[:], in1=R2v, op=ALU.mult), ck, ck)
                V(lambda e: e.tensor_scalar(out=SM[0][:], in0=LRc, scalar1=128.0, scalar2=None, op0=ALU.mult), ck, ck)
                A(lambda e: e.activation(out=SM[0][:], in_=SM[0][:], func=AF.Exp), ck, ck)
                V(lambda e: e.tensor_scalar(out=SM[1][:], in0=LIc, scalar1=128.0, scalar2=None, op0=ALU.mult), ck, ck)
                sincos(A128[:, 1, :], A128[:, 0, :], SM[1][:], SM[2][:], TMPI[:, 0:16])
                V(lambda e: e.tensor_tensor(out=A128[:, 0, :], in0=A128[:, 0, :], in1=SM[0][:], op=ALU.mult), ck, ck)
                V(lambda e: e.tensor_tensor(out=A128[:, 1, :], in0=A128[:, 1, :], in1=SM[0][:], op=ALU.mult), ck, ck)
                lr, li, ldt = REP[:, :, 0, :], REP[:, :, 1, :], REP[:, :, 2, :]
                dtv, ldr, ldi, mag, sn, cs = [t[:] for t in RT]
                A(lambda e: e.activation(out=dtv, in_=ldt, func=AF.Exp), ck, ck)
                V(lambda e: e.tensor_tensor(out=ldr, in0=lr, in1=dtv, op=ALU.mult), ck, ck)
                V(lambda e: e.tensor_tensor(out=ldi, in0=li, in1=dtv, op=ALU.mult), ck, ck)
                A(lambda e: e.activation(out=mag, in_=ldr, func=AF.Exp), ck, ck)
                sincos(sn, cs, ldi, dtv, TMPI[:, 0:256].rearrange("p (a b) -> p a b", a=4))
                V(lambda e: e.tensor_tensor(out=cs, in0=cs, in1=mag, op=ALU.mult), ck, ck)
                V(lambda e: e.tensor_tensor(out=sn, in0=sn, in1=mag, op=ALU.mult), ck, ck)
                V(lambda e: e.tensor_scalar(out=cs, in0=cs, scalar1=-1.0, scalar2=None, op0=ALU.add), ck, ck)
                V(lambda e: e.tensor_tensor(out=dtv, in0=lr, in1=lr, op=ALU.mult), ck, ck)
                V(lambda e: e.tensor_tensor(out=mag, in0=li, in1=li, op=ALU.mult), ck, ck)
                V(lambda e: e.tensor_tensor(out=dtv, in0=dtv, in1=mag, op=ALU.add), ck, ck)
                V(lambda e: e.reciprocal(out=dtv, in_=dtv), ck, ck)
                V(lambda e: e.tensor_tensor(out=ldr, in0=cs, in1=lr, op=ALU.mult), ck, ck)
                V(lambda e: e.tensor_tensor(out=mag, in0=sn, in1=li, op=ALU.mult), ck, ck)
                V(lambda e: e.tensor_tensor(out=ldr, in0=ldr, in1=mag, op=ALU.add), ck, ck)
                V(lambda e: e.tensor_tensor(out=ldr, in0=ldr, in1=dtv, op=ALU.mult), ck, ck)
                V(lambda e: e.tensor_tensor(out=ldi, in0=sn, in1=lr, op=ALU.mult), ck, ck)
                V(lambda e: e.tensor_tensor(out=mag, in0=cs, in1=li, op=ALU.mult), ck, ck)
                V(lambda e: e.tensor_tensor(out=ldi, in0=ldi, in1=mag, op=ALU.subtract), ck, ck)
                V(lambda e: e.tensor_tensor(out=ldi, in0=ldi, in1=dtv, op=ALU.mult), ck, ck)
                br, bi_ = BT[:, :, 0, :], BT[:, :, 1, :]
                V(lambda e: e.tensor_tensor(out=BB[:, :, 0, :], in0=ldr, in1=br, op=ALU.mult), ck, ck)
                V(lambda e: e.tensor_tensor(out=mag, in0=ldi, in1=bi_, op=ALU.mult), ck, ck)
                V(lambda e: e.tensor_tensor(out=BB[:, :, 0, :], in0=BB[:, :, 0, :], in1=mag, op=ALU.subtract), ck, ck)
                V(lambda e: e.tensor_tensor(out=BB[:, :, 1, :], in0=ldr, in1=bi_, op=ALU.mult), ck, ck)
                V(lambda e: e.tensor_tensor(out=mag, in0=ldi, in1=br, op=ALU.mult), ck, ck)
                V(lambda e: e.tensor_tensor(out=BB[:, :, 1, :], in0=BB[:, :, 1, :], in1=mag, op=ALU.add), ck, ck)
                BBDv = BBD[:].rearrange("p k (a r g x) -> p k a r g x", a=4, r=2, g=2)
                for kt in range(4):
                    for a in range(4):
                        for ri in range(2):
                            V(lambda e, kt=kt, a=a, ri=ri: e.tensor_tensor(
                                out=BBDv[:, kt, a, ri, :, :],
                                in0=BB[:, kt, ri, :].unsqueeze(1).to_broadcast([128, 2, 64]),
                                in1=MASKB[:, 2 * a:2 * a + 2].unsqueeze(2).to_broadcast([128, 2, 64]),
                                op=ALU.mult), ck, ck)
                CMv = CMAT[:].rearrange("p a r (g h) -> p a r g h", g=8)
                for pr in range(16):
                    for ri in range(2):
                        in0 = CC[:, pr, ri, :].unsqueeze(1).to_broadcast([128, 8, 16])
                        in1 = MASKC[:, pr % 4, :].unsqueeze(2).to_broadcast([128, 8, 16])
                        if ri == 0:
                            V(lambda e, pr=pr, in0=in0, in1=in1: e.tensor_tensor(out=CMv[:, pr, 0, :, :], in0=in0, in1=in1,
                                                                               op=ALU.mult), ck, ck)
                        else:
                            V(lambda e, pr=pr, in0=in0, in1=in1: e.scalar_tensor_tensor(
                                out=CMv[:, pr, 1, :, :], in0=in0, scalar=-1.0, in1=in1, op0=ALU.mult, op1=ALU.mult),
                                ck, ck)
                uview = uTd.rearrange("p (k t) -> p k t", k=4)
                mview = mixT.rearrange("(k p) t -> p k t", p=128)
                bring = [0]
                WB2 = [W, TMPI[:].bitcast(BF16).rearrange("p (a r x) -> p a r x", a=16, r=2)]
                def uload(tt):
                    us = tt % 2
                    tr.dma('sp', 'uin%d' % us, UT[us][:], uview[:, :, tt * 512:tt * 512 + 512], r=['uTd'], w=['ut%d' % us])
                def pre(cc, jj):
                    tt, sub = divmod(cc, 4)
                    us = tt % 2; uk = 'ut%d' % us; U = UT[us]; Wc = WB2[cc % 2]; wp = 'w%d_' % (cc % 2)
                    tsl = slice(sub * 128, (sub + 1) * 128)
                    for kt in [jj // 2]:
                        for half in [jj % 2]:
                            i = bring[0] % 2
                            bring[0] += 1
                            PB = pb[i]
                            pk = 'pbu%d' % i
                            pr0 = 4 * kt + 2 * half
                            tr.op('pe', lambda e, PB=PB, kt=kt, half=half, tsl=tsl: e.matmul(
                                out=PB[:], lhsT=U[:, kt, tsl], rhs=BBD[:, kt, half * 512:(half + 1) * 512],
                                start=True, stop=True), r=[uk, 's5c'], w=[pk])
                            PBv = PB[:].rearrange("p (a r x) -> p a r x", a=2, r=2)
                            pre_r = PRE_RE[:, pr0 * 128:pr0 * 128 + 256].rearrange("p (a x) -> p a x", a=2) \
                                .unsqueeze(2).to_broadcast([128, 2, 2, 128])
                            pre_i = PRE_IM[:, pr0 * 128:pr0 * 128 + 256].rearrange("p (a x) -> p a x", a=2) \
                                .unsqueeze(2).to_broadcast([128, 2, 2, 128])
                            t1 = T1[i][:].rearrange("p (a r x) -> p a r x", a=2, r=2)
                            t2 = T2[i][:].rearrange("p (a r x) -> p a r x", a=2, r=2)
                            tr.op('dve', lambda e, PBv=PBv, pre_r=pre_r, t1=t1: e.tensor_tensor(
                                out=t1, in0=PBv, in1=pre_r, op=ALU.mult), r=[pk, 's5c'], w=['t1_%d' % i])
                            tr.op('dve', lambda e, PBv=PBv, pre_i=pre_i, t2=t2: e.tensor_tensor(
                                out=t2, in0=PBv, in1=pre_i, op=ALU.mult), r=[pk, 's5c'], w=['t2_%d' % i])
                            tr.op('dve', lambda e, t1=t1, t2=t2, pr0=pr0: e.tensor_tensor(
                                out=Wc[:, pr0:pr0 + 2, 0, :], in0=t1[:, :, 0, :], in1=t2[:, :, 1, :], op=ALU.subtract),
                                r=['t1_%d' % i, 't2_%d' % i], w=[wp + str(pr0 // 2)])
                            tr.op('pool', lambda e, t1=t1, t2=t2, pr0=pr0: e.tensor_tensor(
                                out=Wc[:, pr0:pr0 + 2, 1, :], in0=t1[:, :, 1, :], in1=t2[:, :, 0, :], op=ALU.add),
                                r=['t1_%d' % i, 't2_%d' % i], w=[wp + str(pr0 // 2)])
                def post(cc, jj):
                    tt, sub = divmod(cc, 4)
                    us = tt % 2; uk = 'ut%d' % us; U = UT[us]; Wc = WB2[cc % 2]; wp = 'w%d_' % (cc % 2)
                    tsl = slice(sub * 128, (sub + 1) * 128)
                    for pg in [jj]:
                        i = pg % 2
                        PZ = pb[2 + i]
                        zk = 'pz%d' % i
                        pr0 = 2 * pg
                        PZv = PZ[:].rearrange("p (a r x) -> p a r x", a=2, r=2)
                        for a in range(2):
                            for ri in range(2):
                                tr.op('pe', lambda e, PZv=PZv, a=a, ri=ri, pr0=pr0: e.matmul(
                                    out=PZv[:, a, ri, :], lhsT=Wc[:, pr0 + a, ri, :], rhs=TRI[:],
                                    start=True, stop=True, skip_group_check=True), r=[wp + str(pg), 's5c'], w=[zk])
                        Z = ZCF[:, pr0:pr0 + 2, :, :]
                        zck = 'zc%d' % pg
                        for a in range(2):
                            for ri in range(2):
                                CB_ = CRE if ri == 0 else CIM
                                tr.op('act', lambda e, PZv=PZv, pr0=pr0, a=a, ri=ri, CB_=CB_: e.activation(
                                    out=ZCF[:, pr0 + a, ri, :], in_=PZv[:, a, ri, :], func=AF.Identity,
                                    bias=CB_[:, pr0 + a:pr0 + a + 1]), r=[zk, 'carry'], w=[zck])
                        po_r = POST_RE[:, pr0:pr0 + 2, :].unsqueeze(2).to_broadcast([128, 2, 2, 128])
                        po_i = POST_IM[:, pr0:pr0 + 2, :].unsqueeze(2).to_broadcast([128, 2, 2, 128])
                        u1 = U1[i][:].rearrange("p (a r x) -> p a r x", a=2, r=2)
                        u2 = U2[i][:].rearrange("p (a r x) -> p a r x", a=2, r=2)
                        tr.op('dve', lambda e, Z=Z, po_r=po_r, u1=u1: e.tensor_tensor(
                            out=u1, in0=Z, in1=po_r, op=ALU.mult), r=[zck, 's5c'], w=['u1_%d' % i])
                        tr.op('pool', lambda e, Z=Z, po_i=po_i, u2=u2: e.tensor_tensor(
                            out=u2, in0=Z, in1=po_i, op=ALU.mult), r=[zck, 's5c'], w=['u2_%d' % i])
                        tr.op('dve', lambda e, u1=u1, u2=u2, pr0=pr0: e.tensor_tensor(
                            out=X[:, pr0:pr0 + 2, 0, :], in0=u1[:, :, 0, :], in1=u2[:, :, 1, :], op=ALU.subtract),
                            r=['u1_%d' % i, 'u2_%d' % i], w=['x%d' % pg])
                        tr.op('pool', lambda e, u1=u1, u2=u2, pr0=pr0: e.tensor_tensor(
                            out=X[:, pr0:pr0 + 2, 1, :], in0=u1[:, :, 1, :], in1=u2[:, :, 0, :], op=ALU.add),
                            r=['u1_%d' % i, 'u2_%d' % i], w=['x%d' % pg])
                def post_end(cc):
                    tt, sub = divmod(cc, 4)
                    us = tt % 2; uk = 'ut%d' % us; U = UT[us]
                    tsl = slice(sub * 128, (sub + 1) * 128)
                    zall = ['zc%d' % pg for pg in range(8)]
                    zr = ZCF[:, :, 0, 127]
                    zi = ZCF[:, :, 1, 127]
                    tr.op('dve', lambda e: e.tensor_tensor(out=TQ[0][:], in0=zr, in1=A128[:, 0, :], op=ALU.mult),
                          r=zall + ['s5c'], w=['tq'])
                    tr.op('dve', lambda e: e.tensor_tensor(out=TQ[1][:], in0=zi, in1=A128[:, 1, :], op=ALU.mult),
                          r=zall + ['s5c'], w=['tq'])
                    tr.op('dve', lambda e: e.tensor_tensor(out=TQ[2][:], in0=zr, in1=A128[:, 1, :], op=ALU.mult),
                          r=zall + ['s5c'], w=['tq'])
                    tr.op('dve', lambda e: e.tensor_tensor(out=TQ[3][:], in0=zi, in1=A128[:, 0, :], op=ALU.mult),
                          r=zall + ['s5c'], w=['tq'])
                    tr.op('dve', lambda e: e.tensor_tensor(out=CRE[:], in0=TQ[0][:], in1=TQ[1][:], op=ALU.subtract),
                          r=['tq'], w=['carry'])
                    tr.op('dve', lambda e: e.tensor_tensor(out=CIM[:], in0=TQ[2][:], in1=TQ[3][:], op=ALU.add),
                          r=['tq'], w=['carry'])
                    for kt in range(4):
                        i = kt % 2
                        PY = pb[4 + i]
                        yk = 'py%d' % i
                        n = 0
                        for a in range(4):
                            pr = 4 * kt + a
                            for ri in range(2):
                                tr.op('pe', lambda e, PY=PY, pr=pr, ri=ri, n=n: e.matmul(
                                    out=PY[:, 0:128], lhsT=CMAT[:, pr, ri, :], rhs=X[:, pr, ri, :],
                                    start=(n == 0), stop=(n == 7)), r=['s5c', 'x%d' % (pr // 2)], w=[yk])
                                n += 1
                        tr.op('dve', lambda e, PY=PY, kt=kt, tsl=tsl: e.scalar_tensor_tensor(
                            out=Y[:, kt, tsl], in0=U[:, kt, tsl], scalar=DCOL[:, kt:kt + 1], in1=PY[:, 0:128],
                            op0=ALU.mult, op1=ALU.add), r=[uk, yk, 's5c'], w=['y'])
                def tail(tt):
                    t0 = tt * 512
                    Yf = ROW[0][:]
                    YBf = ROW[1][:]
                    YGf = ROW[2][:]
                    tr.op('pool', lambda e: e.tensor_tensor(out=YBf, in0=Yf, in1=Yf, op=ALU.mult), r=['y'], w=['yb'])
                    tr.op('pool', lambda e: e.tensor_scalar(out=YBf, in0=YBf, scalar1=0.044715, scalar2=1.0,
                                                           op0=ALU.mult, op1=ALU.add), r=['yb'], w=['yb'])
                    tr.op('pool', lambda e: e.tensor_tensor(out=YBf, in0=YBf, in1=Yf, op=ALU.mult), r=['yb', 'y'], w=['yb'])
                    tr.op('act', lambda e: e.activation(out=YBf, in_=YBf, func=AF.Sigmoid, scale=GELU_C), r=['yb'], w=['yb'])
                    tr.op('dve', lambda e: e.tensor_tensor(out=YGf, in0=Yf, in1=YBf, op=ALU.mult), r=['y', 'yb'], w=['yg'])
                    tr.op('pool', lambda e: e.tensor_copy(out=YGB[:].rearrange("p a b -> p (a b)"), in_=YGf),
                          r=['yg'], w=['ygb'])
                    for mt in range(4):
                        i = mt % 2
                        PG = pb[6 + i]
                        gk = 'pgl%d' % i
                        for kt in range(4):
                            tr.op('pe', lambda e, PG=PG, kt=kt, mt=mt: e.matmul(
                                out=PG[:], lhsT=WGL[:, kt, mt * 128:(mt + 1) * 128], rhs=YGB[:, kt, :],
                                start=(kt == 0), stop=(kt == 3)), r=['s5c', 'ygb'], w=[gk])
                        tr.op('act', lambda e, PG=PG, mt=mt, i=i: e.activation(
                            out=SGL[i][:], in_=PG[:], func=AF.Sigmoid, bias=BGL[:, mt:mt + 1]), r=[gk, 's5c'],
                            w=['sgl%d' % i])
                        tr.op('dve', lambda e, mt=mt, i=i: e.tensor_tensor(out=S[:, mt, :], in0=YG[:, mt, :],
                                                                          in1=SGL[i][:], op=ALU.mult),
                              r=['yg', 'sgl%d' % i], w=['s'])
                    tr.op('act', lambda e: e.activation(out=SQ[:].rearrange("p a b -> p (a b)"),
                                                       in_=TH[:], func=AF.Square),
                          r=['s'], w=['sq'])
                    PN = pb[6]
                    for mt in range(4):
                        tr.op('pe', lambda e, mt=mt: e.matmul(out=PN[:], lhsT=ONES[:], rhs=SQ[:, mt, :],
                                                             start=(mt == 0), stop=(mt == 3)), r=['sq', 'c'], w=['pgl0'])
                    tr.op('dve', lambda e: e.tensor_scalar(out=RS5[:], in0=PN[:], scalar1=1.0 / 512, scalar2=EPS,
                                                          op0=ALU.mult, op1=ALU.add), r=['pgl0'], w=['rs5'])
                    tr.op('act', lambda e: e.activation(out=RS5[:], in_=RS5[:], func=AF.Sqrt), r=['rs5'], w=['rs5'])
                    tr.op('dve', lambda e: e.reciprocal(out=RS5[:], in_=RS5[:]), r=['rs5'], w=['rs5'])
                    for mt in range(4):
                        tr.op('dve', lambda e, mt=mt: e.scalar_tensor_tensor(
                            out=OUT[:, mt, :], in0=S[:, mt, :], scalar=GC[:, 32 + mt:33 + mt], in1=RS5[:],
                            op0=ALU.mult, op1=ALU.mult), r=['s', 'rs5', 'c'], w=['out5'])
                    tr.dma('sp', 'sout', mview[:, 4:8, t0:t0 + 512], OUT[:], r=['out5'], w=['mixT'])
                uload(0)
                uload(1)
                for jj in range(8):
                    pre(0, jj)
                for cc in range(32):
                    for jj in range(8):
                        if cc + 1 < 32:
                            pre(cc + 1, jj)
                        post(cc, jj)
                    post_end(cc)
                    if cc % 4 == 3:
                        tail(cc // 4)
                        if cc // 4 + 2 < 8:
                            uload(cc // 4 + 2)
            tr.barrier()

        def wout_phase():
            with contextlib.ExitStack() as w_:
                def wsb(name, shape, dt):
                    return w_.enter_context(nc.sbuf_tensor(name, list(shape), dt))
                WO = wsb("WO", [128, 8, D], BF16)
                XW = [wsb("XW%d" % i, [128, 8, 512], F32) for i in range(2)]
                MX = [wsb("MX%d" % i, [128, 8, 512], BF16) for i in range(2)]
                for k in range(8):
                    tr.dma('pool', 'wo', WO[:, k, :], w_out[k * 128:(k + 1) * 128, :], w=['WO'])
                for tt in range(8):
                    s = tt % 2
                    t0 = tt * 512
                    xk, mk = 'xw%d' % s, 'mx%d' % s
                    tr.dma('sp', 'xwin%d' % s, XW[s][:], xview(x1T, t0, 512), r=['x1T_%d' % tt], w=[xk])
                    tr.dma('act', 'mxin%d' % s, MX[s][:], xview(mixT, t0, 512), r=['mixT'], w=[mk])
                    for dk in range(8):
                        b = dk % 4
                        for k in range(8):
                            tr.op('pe', lambda e, k=k, dk=dk, b=b, s=s: e.matmul(
                                out=pb[b][:], lhsT=WO[:, k, dk * 128:(dk + 1) * 128], rhs=MX[s][:, k, :],
                                start=(k == 0), stop=(k == 7)), r=['WO', mk], w=['pw%d' % b])
                        tr.op('dve', lambda e, dk=dk, b=b, s=s: e.tensor_tensor(
                            out=XW[s][:, dk, :], in0=pb[b][:], in1=XW[s][:, dk, :], op=ALU.add),
                            r=['pw%d' % b, xk], w=[xk])
                    tr.dma('act', 'xwout%d' % s, xview(x1T, t0, 512), XW[s][:], r=[xk], w=['x1T_%d' % tt])
            tr.barrier()

        ffn_phase(1)

        with contextlib.ExitStack() as ms:
            if stop < 1.2:
                return nc
            def msb(name, shape, dt):
                return ms.enter_context(nc.sbuf_tensor(name, list(shape), dt))
            KE = [msb("KE%d" % g, [128, T], BF16) for g in range(2)]
            KW = [msb("KW%d" % g, [64, T], BF16) for g in range(2)]
            VS1 = [msb("VS1%d" % g, [128, 32, 65], BF16) for g in range(2)]
            VW1 = [msb("VW1%d" % g, [128, 32, 65], BF16) for g in range(2)]
            GT = msb("GT", [128, 32, 24], F32)
            KC = [msb("KC%d" % g, [64, 256], BF16) for g in range(2)]
            VCX = [msb("VCX%d" % g, [128, 2, 129], BF16) for g in range(2)]
            for g in range(2):
                tr.dma('pool', 'const', KE[g][64:128, :], c_E0[:, :], w=['KE%d' % g])
                tr.op('dve', lambda e, g=g: e.memset(VS1[g][:, :, 64:65], 1.0), w=['VS1%d' % g])
                tr.op('dve', lambda e, g=g: e.memset(VW1[g][:, :, 64:65], 1.0), w=['VW1%d' % g])
                tr.op('dve', lambda e, g=g: e.memset(VCX[g][:, :, 64:65], 1.0), w=['VCX%d' % g])
                tr.dma('pool', 'const', VCX[g][:, :, 65:129], c_ovl.rearrange("p (a b) -> p a b", a=2),
                       w=['VCX%d' % g])

            with contextlib.ExitStack() as ps_:
                def psb(name, shape, dt):
                    return ps_.enter_context(nc.sbuf_tensor(name, list(shape), dt))
                WIN = psb("WIN", [128, 8, 1816], BF16)
                H2 = [psb("H2_%d" % i, [128, 8, 512], BF16) for i in range(2)]
                QST = psb("QST", [64, 4, 8, 128], BF16)
                UST = psb("UST", [128, 4, 512], BF16)
                KCin = [psb("KCin%d" % g, [64, T], BF16) for g in range(2)]
                VCin = [psb("VCin%d" % g, [64, T], BF16) for g in range(2)]
                GB = psb("GB", [128, 24], F32)
                W1 = [psb("W1_%d" % i, [64, 32, 256], BF16) for i in range(2)]
                W2 = [psb("W2_%d" % i, [128, 2, 64], BF16) for i in range(2)]
                CB1 = psb("CB1", [128, 4], F32)
                POST = psb("POST", [64, 64], BF16)
                HID = psb("HID", [128, 2, 256], BF16)
                GTMP = [psb("GTMP%d" % i, [128, 256], F32) for i in range(3)]
                PBIAS = psb("PBIAS", [128, 1], F32)
                for k in range(8):
                    tr.dma('pool', 'win', WIN[:, k, :], w_in[k * 128:(k + 1) * 128, :], w=['WIN'])
                tr.dma('sp', 'const', GB[:], gate_bias.partition_broadcast(128), w=['GB'])
                tr.dma('sp', 'const', CB1[:], cb1[:, :], w=['cmpw'])
                tr.dma('pool', 'const', POST[:], posT[:, :], w=['cmpw'])
                for i in range(2):
                    w1v = cw1[i].rearrange("(l d) h -> d l h", d=64)
                    for lc in range(8):
                        tr.dma('pool', 'const', W1[i][:, 4 * lc:4 * lc + 4, :], w1v[:, 4 * lc:4 * lc + 4, :], w=['cmpw'])
                    tr.dma('pool', 'const', W2[i][:], cw2[i].rearrange("(t p) d -> p t d", p=128), w=['cmpw'])
                if stop < 1.6:
                    tr.barrier()
                    return nc
                ring = [0]

                def nextbank():
                    b = ring[0] % 6
                    ring[0] += 1
                    return pb[b], 'pp%d' % b

                evq = [0]

                def evac(out, in_, rk, wk, force=None):
                    evq[0] += 1
                    if (evq[0] % 2 == 0 and force is None) or force == 'act':
                        tr.op('act', lambda e: e.activation(out=out, in_=in_, func=AF.Copy), r=[rk], w=[wk])
                    else:
                        tr.op('dve', lambda e: e.tensor_copy(out=out, in_=in_), r=[rk], w=[wk])

                for tt in range(8):
                    s = tt % 2
                    t0 = tt * 512
                    hk = 'h2_%d' % s
                    HH = H2[s]
                    tr.dma('sp', 'h2in%d' % s, HH[:], xview(h2T, t0, 512), r=['h2T'], w=[hk])
                    for h in range(8):
                        P, pk = nextbank()
                        for k in range(8):
                            tr.op('pe', lambda e, k=k, h=h, P=P: e.matmul(
                                out=P[0:64, :], lhsT=WIN[:, k, 64 * h:64 * h + 64], rhs=HH[:, k, :],
                                start=(k == 0), stop=(k == 7)), r=['WIN', hk], w=[pk])
                        evac(QST[:, :, h, :], P[0:64, :].rearrange("p (c q) -> p c q", q=128), pk, 'qst')
                    tr.dma('sp', 'qout', qTd.rearrange("p (c x) -> p c x", c=32)[:, 4 * tt:4 * tt + 4, :],
                           QST[:].rearrange("p c h q -> p c (h q)"), r=['qst'], w=['qTd'])
                    for (c0, dest, dn) in ((512, KCin, 'KCin'), (640, VCin, 'VCin'), (768, KE, 'KE'), (1024, KW, 'KW')):
                        for g in range(2):
                            P, pk = nextbank()
                            for k in range(8):
                                tr.op('pe', lambda e, k=k, P=P, cc=c0 + 64 * g: e.matmul(
                                    out=P[0:64, :], lhsT=WIN[:, k, cc:cc + 64], rhs=HH[:, k, :],
                                    start=(k == 0), stop=(k == 7)), r=['WIN', hk], w=[pk])
                            evac(dest[g][0:64, t0:t0 + 512], P[0:64, :], pk, '%s%d' % (dn, g))
                    for kt in range(4):
                        P, pk = nextbank()
                        for k in range(8):
                            tr.op('pe', lambda e, k=k, P=P, cc=1304 + 128 * kt: e.matmul(
                                out=P[:, :], lhsT=WIN[:, k, cc:cc + 128], rhs=HH[:, k, :],
                                start=(k == 0), stop=(k == 7)), r=['WIN', hk], w=[pk])
                        evac(UST[:, kt, :], P[:, :], pk, 'ust')
                    tr.dma('sp', 'uout', uTd.rearrange("p (k t) -> p k t", k=4)[:, :, t0:t0 + 512], UST[:],
                           r=['ust'], w=['uTd'])
                    for sbk in range(4):
                        blk = tt * 4 + sbk
                        P, pk = nextbank()
                        for k in range(8):
                            tr.op('pe', lambda e, k=k, P=P, sbk=sbk: e.matmul(
                                out=P[:, 0:408], lhsT=HH[:, k, sbk * 128:(sbk + 1) * 128], rhs=WIN[:, k, 896:1304],
                                start=(k == 0), stop=(k == 7)), r=['WIN', hk], w=[pk])
                        for g in range(2):
                            evac(VS1[g][:, blk, 0:64], P[:, 64 * g:64 * g + 64], pk, 'VS1%d' % g, force='dve')
                            evac(VW1[g][:, blk, 0:64], P[:, 256 + 64 * g:256 + 64 * g + 64], pk, 'VW1%d' % g, force='dve')
                        tr.op('dve', lambda e, P=P, blk=blk: e.tensor_tensor(out=GT[:, blk, :], in0=P[:, 384:408],
                                                                            in1=GB[:], op=ALU.add),
                              r=[pk, 'GB'], w=['GT'])
                        tr.op('act', lambda e, blk=blk: e.activation(out=GT[:, blk, :], in_=GT[:, blk, :],
                                                                    func=AF.Sigmoid), r=['GT'], w=['GT'])

                if stop < 1.8:
                    tr.barrier()
                    return nc
                HIDS = [psb("HIDS%d" % g, [128, 2, 256], BF16) for g in range(2)]
                for g in range(2):
                    tr.op('dve', lambda e, g=g: e.memset(HIDS[g][:], 0.0), w=['hid%d_0' % g, 'hid%d_1' % g])
                A, B_, C_ = GTMP
                for kind in range(2):
                    srcs = KCin if kind == 0 else VCin
                    sname = 'KCin' if kind == 0 else 'VCin'
                    for hh in range(2):
                        P, pk = nextbank()
                        for l in range(32):
                            tr.op('pe', lambda e, l=l, P=P, hh=hh: e.matmul(
                                out=P[:, 0:1], lhsT=W1[kind][:, l, hh * 128:(hh + 1) * 128],
                                rhs=POST[:, kind * 32 + l:kind * 32 + l + 1], start=(l == 0), stop=(l == 31)),
                                r=['cmpw'], w=[pk])
                        tr.op('dve', lambda e, P=P, hh=hh: e.tensor_tensor(
                            out=PBIAS[:], in0=P[:, 0:1], in1=CB1[:, kind * 2 + hh:kind * 2 + hh + 1], op=ALU.add),
                            r=[pk, 'cmpw'], w=['pbias'])
                        for g in range(2):
                            P2, pk2 = nextbank()
                            for l in range(32):
                                rhs = bass.AP(srcs[g].tensor if hasattr(srcs[g], 'tensor') else srcs[g], l,
                                              [[T, 64], [16, 255]])
                                tr.op('pe', lambda e, l=l, P2=P2, rhs=rhs, hh=hh: e.matmul(
                                    out=P2[:, 0:255], lhsT=W1[kind][:, l, hh * 128:(hh + 1) * 128], rhs=rhs,
                                    start=(l == 0), stop=(l == 31)), r=['cmpw', '%s%d' % (sname, g)], w=[pk2])
                            tr.op('dve', lambda e, P2=P2: e.tensor_scalar(out=A[:, 0:255], in0=P2[:, 0:255],
                                                                         scalar1=PBIAS[:, 0:1], scalar2=None,
                                                                         op0=ALU.add), r=[pk2, 'pbias'], w=['ga'])
                            tr.op('dve', lambda e: e.tensor_tensor(out=B_[:, 0:255], in0=A[:, 0:255], in1=A[:, 0:255],
                                                                  op=ALU.mult), r=['ga'], w=['gb'])
                            tr.op('dve', lambda e: e.tensor_scalar(out=B_[:, 0:255], in0=B_[:, 0:255], scalar1=0.044715,
                                                                  scalar2=1.0, op0=ALU.mult, op1=ALU.add),
                                  r=['gb'], w=['gb'])
                            tr.op('dve', lambda e: e.tensor_tensor(out=B_[:, 0:255], in0=B_[:, 0:255], in1=A[:, 0:255],
                                                                  op=ALU.mult), r=['gb', 'ga'], w=['gb'])
                            tr.op('act', lambda e: e.activation(out=C_[:, 0:255], in_=B_[:, 0:255], func=AF.Sigmoid,
                                                               scale=GELU_C), r=['gb'], w=['gc'])
                            tr.op('dve', lambda e, g=g, hh=hh: e.tensor_tensor(
                                out=HIDS[g][:, hh, 0:255], in0=A[:, 0:255], in1=C_[:, 0:255], op=ALU.mult),
                                r=['ga', 'gc'], w=['hid%d_%d' % (g, hh)])
                    for g in range(2):
                        hk2 = ['hid%d_0' % g, 'hid%d_1' % g]
                        if kind == 0:
                            P, pk = nextbank()
                            for hh in range(2):
                                tr.op('pe', lambda e, hh=hh, P=P, g=g: e.matmul(
                                    out=P[0:64, 0:256], lhsT=W2[0][:, hh, :], rhs=HIDS[g][:, hh, :],
                                    start=(hh == 0), stop=(hh == 1)), r=['cmpw'] + hk2, w=[pk])
                            evac(KC[g][:, :], P[0:64, 0:256], pk, 'KC%d' % g)
                        else:
                            for nt in range(2):
                                P, pk = nextbank()
                                for hh in range(2):
                                    tr.op('pe', lambda e, hh=hh, P=P, g=g, nt=nt: e.matmul(
                                        out=P[:, 0:64], lhsT=HIDS[g][:, hh, nt * 128:(nt + 1) * 128],
                                        rhs=W2[1][:, hh, :], start=(hh == 0), stop=(hh == 1)),
                                        r=['cmpw'] + hk2, w=[pk])
                                evac(VCX[g][:, nt, 0:64], P[:, 0:64], pk, 'VCX%d' % g)
            tr.barrier()
            if stop >= 3:
                attention_phase()
        if stop >= 4:
            s5_phase()
        with contextlib.ExitStack() as w2s:
            wts2 = ffn_weights(2, w2s) if stop >= 6 else None
            if stop >= 5:
                wout_phase()
            if stop >= 6:
                ffn_phase(2, wts2)
    return nc


def _consts():
    f = np.float32
    c = {}
    c["c_ident"] = np.eye(128, dtype=f)
    j = np.arange(128)
    c["c_tri"] = (j[:, None] <= j[None, :]).astype(f)
    c["c_E0"] = (np.arange(T)[None, :] // 64 == np.arange(64)[:, None]).astype(f)
    cb = np.where(j[:, None] > j[None, :], -BIGM, 0.0).astype(f)
    wb = np.where(j[:, None] <= j[None, :], -BIGM, 0.0).astype(f)
    cm = np.where(16 * (j[:, None] - 64) > j[None, :] - 31, -BIGM, 0.0).astype(f)
    c["c_CB"] = np.tile(cb, (1, 4))
    c["c_WB"] = np.tile(wb, (1, 4))
    c["c_CM"] = np.tile(cm, (1, 4))
    c["c_BD"] = (np.arange(384)[None, :] == j[:, None] + 128).astype(f)
    hh = (j >= 64).astype(np.int64)[:, None]
    m = np.arange(128)[None, :]
    c["c_U0"] = np.where(m > 62 + hh, -1e30, 3e38).astype(f)
    l0 = np.full((128, 128), -3e38, dtype=f)
    l0[m == 62 + hh] = 2e30
    l0[m == 61 + hh] = 1e30
    c["c_L0"] = l0
    n = np.arange(256)[:, None]
    jj = np.arange(64)[None, :]
    ov = ((16 * n < 64 * jj + 64) & (16 * n + 32 > 64 * jj)).astype(f)
    ov[255] = 0
    c["c_ovl"] = np.ascontiguousarray(ov.reshape(2, 128, 64).transpose(1, 0, 2).reshape(128, 128))
    c["c_maskB"] = (np.arange(8)[None, :] == (j // 16)[:, None]).astype(f)
    gi = (j // 64)[:, None, None]
    p4 = np.arange(4)[None, :, None]
    g8 = np.arange(8)[None, None, :]
    c["c_maskC"] = (g8 == 2 * p4 + gi).astype(f).reshape(128, 32)
    c["c_iota"] = np.tile(np.arange(128, dtype=f)[None, :], (128, 1))
    c["c_jcol"] = np.arange(128, dtype=f)[:, None].copy()
    return c


def _prep_shared(inp):
    f = np.float32
    A = lambda a: np.ascontiguousarray(np.asarray(a, dtype=f))
    d = {}
    d["wg1"], d["wu1"], d["wd1"] = A(inp["ffn1_w_gate"][0]), A(inp["ffn1_w_up"][0]), A(inp["ffn1_w_down"][0])
    d["wg2"], d["wu2"], d["wd2"] = A(inp["ffn2_w_gate"][0]), A(inp["ffn2_w_up"][0]), A(inp["ffn2_w_down"][0])
    d["w_in"], d["w_out"], d["w_glu"] = A(inp["w_in"][0]), A(inp["w_out"][0]), A(inp["s5_w_glu"][0])
    col8 = lambda v: np.asarray(v, dtype=f).reshape(-1, 128).T
    d["gcols"] = A(np.concatenate([col8(inp["ffn1_norm"][0]), col8(inp["mix_norm"][0]), col8(inp["ffn2_norm"][0]),
                                   col8(inp["final_norm"]), col8(inp["ssm_out_norm"][0])], axis=1))
    d["grow_attn"] = A(np.asarray(inp["attn_out_norm"][0]).reshape(1, 512))
    d["gate_bias"] = A(np.asarray(inp["gate_bias"][0]).reshape(1, 24))
    d["cw1k"], d["cw1v"] = A(inp["cmp_k_w1"][0]), A(inp["cmp_v_w1"][0])
    d["cw2k"], d["cw2v"] = A(inp["cmp_k_w2"][0]), A(inp["cmp_v_w2"][0])
    d["cb1"] = A(np.concatenate([col8(inp["cmp_k_b1"][0]), col8(inp["cmp_v_b1"][0])], axis=1))
    d["posT"] = A(np.concatenate([np.asarray(inp["cmp_pos_k"][0]).T, np.asarray(inp["cmp_pos_v"][0]).T], axis=1))
    lr = np.asarray(inp["s5_lambda_re"][0], dtype=f)
    li = np.asarray(inp["s5_lambda_im"][0], dtype=f)
    ldt = np.repeat(np.asarray(inp["s5_log_dt"][0], dtype=f)[:, None], 64, axis=1)
    d["s5_rows"] = A(np.stack([lr.reshape(2048), li.reshape(2048), ldt.reshape(2048)]))
    colify = lambda a: a.reshape(16, 128).T
    d["s5_cols"] = A(np.concatenate([colify(lr), colify(li), colify(ldt)], axis=1))

    def rows_gh(a):
        return np.repeat(a.reshape(4, 8, 1, 64), 16, axis=2).transpose(1, 2, 0, 3).reshape(128, 4, 64)
    d["s5_rep"] = A(np.stack([rows_gh(lr), rows_gh(li), rows_gh(ldt)], axis=2).reshape(128, 4 * 3 * 64))

    def bt(a):
        return np.asarray(a, dtype=f).reshape(4, 8, 64, 16).transpose(1, 3, 0, 2).reshape(128, 4, 64)
    d["s5_bT"] = A(np.stack([bt(inp["s5_b_re"][0]), bt(inp["s5_b_im"][0])], axis=2).reshape(128, 4 * 2 * 64))

    def ct(a):
        return np.asarray(a, dtype=f).reshape(16, 2, 16, 64).transpose(1, 3, 0, 2).reshape(128, 16, 16)
    d["s5_c"] = A(np.stack([ct(inp["s5_c_re"][0]), ct(inp["s5_c_im"][0])], axis=2).reshape(128, 16 * 2 * 16))
    d["s5_dcol"] = A(np.asarray(inp["s5_d"][0], dtype=f).reshape(4, 128).T)
    d["bglu_col"] = A(np.asarray(inp["s5_b_glu"][0], dtype=f).reshape(4, 128).T)
    d.update(_consts())
    return d


def kernel(**inputs):
    x = np.asarray(inputs["x"], dtype=np.float32)
    shared = _prep_shared(inputs)
    nc = build(dbg=False)
    in_maps = []
    for b in range(NCORES):
        m = dict(shared)
        m["xT"] = np.ascontiguousarray(x[b].T)
        in_maps.append(m)
    res = run_bass_kernel_spmd(nc, in_maps, core_ids=list(range(NCORES)))
    out = np.empty((NCORES, T, D), dtype=np.float32)
    for b in range(NCORES):
        out[b] = np.asarray(res.results[b]["yT"], dtype=np.float32).T
    return out
```

```python
import contextlib
import numpy as np
import concourse.bass as bass
import concourse.mybir as mybir
from concourse.bass_utils import run_bass_kernel_spmd

F32 = mybir.dt.float32
BF16 = mybir.dt.bfloat16
ALU = mybir.AluOpType
AF = mybir.ActivationFunctionType

T = 4096
D = 1024
FF = 2816
NF = FF // 128
EPS = 1e-6
BIGM = 30000.0
NCORES = 8
GELU_C = 1.5957691216057308


class TR:
    def __init__(self, nc, es):
        self.nc, self.es = nc, es
        self.eng = dict(pe=nc.tensor, dve=nc.vector, act=nc.scalar, pool=nc.gpsimd, sp=nc.sync)
        self.sem, self.cnt = {}, {}
        self.seen = {e: {} for e in self.eng}
        for e in self.eng:
            self.sem[e] = es.enter_context(nc.semaphore("s_" + e))
            self.cnt[e] = 0
        self.bs = {}
        self.defer = None

    def _st(self, k):
        s = self.bs.get(k)
        if s is None:
            s = self.bs[k] = ({}, {})
        return s

    def _deps(self, r, w):
        d = {}
        for k in r:
            for sk, v in self._st(k)[0].items():
                if d.get(sk, 0) < v:
                    d[sk] = v
        for k in w:
            s = self._st(k)
            for dd in s:
                for sk, v in dd.items():
                    if d.get(sk, 0) < v:
                        d[sk] = v
        return d

    def _wait(self, e, d):
        for k, v in d.items():
            if k == e and e == 'pe':
                continue
            if self.seen[e].get(k, 0) >= v:
                continue
            self.eng[e].wait_ge(self.sem[k], v)
            self.seen[e][k] = v

    def _mark(self, src, v, r, w):
        for k in r:
            self._st(k)[1][src] = v
        for k in w:
            self._st(k)[0][src] = v

    def op(self, e, fn, r=(), w=()):
        if self.defer is not None:
            r, w = list(r), list(w)
            self.defer.append(lambda: self.op(e, fn, r, w))
            return
        self._wait(e, self._deps(r, w))
        ins = fn(self.eng[e])
        self.cnt[e] += 1
        ins.then_inc(self.sem[e], 1)
        self._mark(e, self.cnt[e], r, w)

    def dma(self, q, ch, out, in_, r=(), w=()):
        if self.defer is not None:
            r, w = list(r), list(w)
            self.defer.append(lambda: self.dma(q, ch, out, in_, r, w))
            return
        if ch not in self.sem:
            self.sem[ch] = self.es.enter_context(self.nc.semaphore("c_" + ch))
            self.cnt[ch] = 0
        self._wait(q, self._deps(r, w))
        ins = self.eng[q].dma_start(out=out, in_=in_)
        self.cnt[ch] += 16
        ins.then_inc(self.sem[ch], 16)
        self._mark(ch, self.cnt[ch], r, w)

    def barrier(self):
        allk = {k: v for k, v in self.cnt.items() if v > 0}
        for e in self.eng:
            self._wait(e, allk)


def build(dbg=False, stop=9):
    nc = bass.Bass("TRN2", target_bir_lowering=False)
    es = contextlib.ExitStack()
    with es:
        tr = TR(nc, es)

        def din(name, shape):
            return nc.dram_tensor(name, list(shape), F32, kind="ExternalInput").ap()

        def dscr(name, shape, dt):
            kind = "ExternalOutput" if dbg else "Internal"
            return nc.dram_tensor(name, list(shape), dt, kind=kind).ap()

        def sb(name, shape, dt):
            return es.enter_context(nc.sbuf_tensor(name, list(shape), dt))

        xT = din("xT", [D, T])
        wgs = [din("wg1", [D, FF]), din("wg2", [D, FF])]
        wus = [din("wu1", [D, FF]), din("wu2", [D, FF])]
        wds = [din("wd1", [FF, D]), din("wd2", [FF, D])]
        w_in = din("w_in", [D, 1816])
        w_out = din("w_out", [D, D])
        w_glu = din("w_glu", [512, 512])
        gcols = din("gcols", [128, 36])
        grow_attn = din("grow_attn", [1, 512])
        gate_bias = din("gate_bias", [1, 24])
        cw1 = [din("cw1k", [2048, 256]), din("cw1v", [2048, 256])]
        cw2 = [din("cw2k", [256, 64]), din("cw2v", [256, 64])]
        cb1 = din("cb1", [128, 4])
        posT = din("posT", [64, 64])
        s5_rows = din("s5_rows", [3, 2048])
        s5_cols = din("s5_cols", [128, 48])
        s5_rep = din("s5_rep", [128, 4 * 3 * 64])
        s5_bT = din("s5_bT", [128, 4 * 2 * 64])
        s5_c = din("s5_c", [128, 16 * 2 * 16])
        s5_dcol = din("s5_dcol", [128, 4])
        bglu_col = din("bglu_col", [128, 4])
        c_ident = din("c_ident", [128, 128])
        c_tri = din("c_tri", [128, 128])
        c_E0 = din("c_E0", [64, T])
        c_CB = din("c_CB", [128, 512])
        c_WB = din("c_WB", [128, 512])
        c_CM = din("c_CM", [128, 512])
        c_BD = din("c_BD", [128, 384])
        c_U0 = din("c_U0", [128, 128])
        c_L0 = din("c_L0", [128, 128])
        c_ovl = din("c_ovl", [128, 128])
        c_maskB = din("c_maskB", [128, 8])
        c_maskC = din("c_maskC", [128, 32])
        c_iota = din("c_iota", [128, 128])
        c_jcol = din("c_jcol", [128, 1])

        yT = nc.dram_tensor("yT", [D, T], F32, kind="ExternalOutput").ap()
        x1T = dscr("x1T", [D, T], F32)
        h2T = dscr("h2T", [D, T], BF16)
        qTd = dscr("qTd", [64, 32 * 8 * 128], BF16)
        uTd = dscr("uTd", [128, 4 * T], BF16)
        mixT = dscr("mixT", [D, T], BF16)

        GC = sb("GC", [128, 36], F32)
        ONES = sb("ONES", [128, 128], BF16)
        IDN = sb("IDN", [128, 128], BF16)
        EPSC = sb("EPSC", [128, 1], F32)
        tr.dma('sp', 'const', GC[:], gcols[:, :], w=['c'])
        tr.dma('pool', 'const', IDN[:], c_ident[:, :], w=['c'])
        tr.op('dve', lambda e: e.memset(ONES[:], 1.0), w=['c'])
        tr.op('dve', lambda e: e.memset(EPSC[:], 0.0), w=['c'])

        pb = [es.enter_context(nc.psum_tensor("pb%d" % i, [128, 512], F32)) for i in range(8)]

        def xview(dram, t0, n):
            return dram.rearrange("(k p) t -> p k t", p=128)[:, :, t0:t0 + n]

        def ffn_weights(ph, stk, load=True):
            WG = stk.enter_context(nc.sbuf_tensor("WG_%d" % ph, [128, 8, FF], BF16))
            WU = stk.enter_context(nc.sbuf_tensor("WU_%d" % ph, [128, 8, FF], BF16))
            WD = stk.enter_context(nc.sbuf_tensor("WD_%d" % ph, [128, NF, D], BF16))
            if load:
                ffn_wload(ph, (WG, WU, WD))
            return WG, WU, WD

        def ffn_wload(ph, wts):
            WG, WU, WD = wts
            wgd, wud, wdd = wgs[ph - 1], wus[ph - 1], wds[ph - 1]
            for k in range(8):
                tr.dma('pool', 'wg', WG[:, k, :], wgd[k * 128:(k + 1) * 128, :], w=['WG'])
                tr.dma('pool', 'wu', WU[:, k, :], wud[k * 128:(k + 1) * 128, :], w=['WU'])
            for f in range(NF):
                tr.dma('pool', 'wd', WD[:, f, :], wdd[f * 128:(f + 1) * 128, :], w=['WD'])

        def ffn_phase(ph, wts=None):
            with contextlib.ExitStack() as fs:
                def fsb(name, shape, dt):
                    return fs.enter_context(nc.sbuf_tensor("%s_%d" % (name, ph), list(shape), dt))
                WG, WU, WD = wts if wts is not None else ffn_weights(ph, fs)
                XT = [fsb("XT%d" % i, [128, 8, 512], F32) for i in range(2)]
                H = fsb("H", [128, 8, 512], BF16)
                AT = fsb("AT", [128, NF, 512], BF16)
                SG = [fsb("SG%d" % i, [128, 512], BF16) for i in range(2)]
                RS = [fsb("RS%d" % i, [128, 512], F32) for i in range(2)]
                src = xT if ph == 1 else x1T
                g0 = 0 if ph == 1 else 16
                hkeys = ['h%d' % k for k in range(8)]
                atk2 = ['at%d' % f for f in range(14, 22)]

                def xs(tt):
                    return XT[tt % 2], 'xt%d' % (tt % 2)

                def load(tt):
                    X, xk = xs(tt)
                    tr.dma('sp', 'xin%d' % (tt % 2), X[:], xview(src, tt * 512, 512), w=[xk])

                def norm_a(X, xk, HB, hk):
                    tr.op('act', lambda e: e.activation(out=HB, in_=X[:], func=AF.Square), r=[xk], w=hk)

                def norm_b(HBk, hk, PN, pnk):
                    for k in range(8):
                        tr.op('pe', lambda e, k=k: e.matmul(out=PN[:], lhsT=ONES[:], rhs=HBk(k),
                                                           start=(k == 0), stop=(k == 7)), r=[hk[k], 'c'], w=[pnk])

                def norm_c(X, xk, HBk, hk, PN, pnk, R, rk, goff, inplace=False):
                    tr.op('dve', lambda e: e.tensor_scalar(out=R[:], in0=PN[:], scalar1=1.0 / D, scalar2=EPS,
                                                          op0=ALU.mult, op1=ALU.add), r=[pnk], w=[rk])
                    tr.op('act', lambda e: e.activation(out=R[:], in_=R[:], func=AF.Sqrt), r=[rk], w=[rk])
                    tr.op('dve', lambda e: e.reciprocal(out=R[:], in_=R[:]), r=[rk], w=[rk])
                    for k in range(8):
                        dst = X[:, k, :] if inplace else HBk(k)
                        tr.op('dve', lambda e, k=k, dst=dst: e.scalar_tensor_tensor(
                            out=dst, in0=X[:, k, :], scalar=GC[:, goff + k:goff + k + 1], in1=R[:],
                            op0=ALU.mult, op1=ALU.mult), r=[xk, rk, 'c'], w=[xk if inplace else hk[k]])

                Hk = lambda k: H[:, k, :]
                A2k = lambda k: AT[:, 14 + k, :]

                def post_a(tt):
                    X, xk = xs(tt)
                    if ph == 1:
                        tr.dma('sp', 'xout%d' % (tt % 2), xview(x1T, tt * 512, 512), X[:], r=[xk], w=['x1T'])
                    norm_a(X, xk, AT[:, 14:22, :], atk2)

                def post_bc(tt):
                    X, xk = xs(tt)
                    norm_b(A2k, atk2, pb[7], 'pn2')
                    if ph == 1:
                        norm_c(X, xk, A2k, atk2, pb[7], 'pn2', RS[1], 'rs1', 8)
                        tr.dma('sp', 'hout', xview(h2T, tt * 512, 512), AT[:, 14:22, :], r=atk2, w=['h2T'])
                    else:
                        norm_c(X, xk, A2k, atk2, pb[7], 'pn2', RS[1], 'rs1', 24, inplace=True)
                        tr.dma('sp', 'yout%d' % (tt % 2), xview(yT, tt * 512, 512), X[:], r=[xk], w=['yT'])

                load(0)
                X0, xk0 = xs(0)
                norm_a(X0, xk0, H[:], hkeys)
                norm_b(Hk, hkeys, pb[6], 'pn')
                norm_c(X0, xk0, Hk, hkeys, pb[6], 'pn', RS[0], 'rs0', g0)
                for tt in range(8):
                    X, xk = xs(tt)
                    for f in range(NF):
                        b = f % 2
                        for k in range(8):
                            tr.op('pe', lambda e, k=k, f=f, b=b: e.matmul(
                                out=pb[b][:], lhsT=WG[:, k, f * 128:(f + 1) * 128], rhs=H[:, k, :],
                                start=(k == 0), stop=(k == 7)), r=['WG', hkeys[k]], w=['pg%d' % b])
                        for k in range(8):
                            tr.op('pe', lambda e, k=k, f=f, b=b: e.matmul(
                                out=pb[2 + b][:], lhsT=WU[:, k, f * 128:(f + 1) * 128], rhs=H[:, k, :],
                                start=(k == 0), stop=(k == 7)), r=['WU', hkeys[k]], w=['pu%d' % b])
                        tr.op('act', lambda e, b=b: e.activation(out=SG[b][:], in_=pb[b][:], func=AF.Silu),
                              r=['pg%d' % b], w=['sg%d' % b])
                        tr.op('dve', lambda e, b=b, f=f: e.tensor_tensor(out=AT[:, f, :], in0=SG[b][:],
                                                                         in1=pb[2 + b][:], op=ALU.mult),
                              r=['sg%d' % b, 'pu%d' % b], w=['at%d' % f])
                        if f == 2 and tt > 0:
                            post_bc(tt - 1)
                    if tt + 1 < 8:
                        load(tt + 1)
                        Xn, xkn = xs(tt + 1)
                        norm_a(Xn, xkn, H[:], hkeys)
                    for dk in range(8):
                        b = dk % 2
                        for f in range(NF):
                            tr.op('pe', lambda e, f=f, dk=dk, b=b: e.matmul(
                                out=pb[4 + b][:], lhsT=WD[:, f, dk * 128:(dk + 1) * 128], rhs=AT[:, f, :],
                                start=(f == 0), stop=(f == NF - 1)), r=['WD', 'at%d' % f], w=['pd%d' % b])
                        tr.op('dve', lambda e, dk=dk, b=b, X=X: e.scalar_tensor_tensor(
                            out=X[:, dk, :], in0=pb[4 + b][:], scalar=0.5, in1=X[:, dk, :],
                            op0=ALU.mult, op1=ALU.add), r=['pd%d' % b, xk], w=[xk])
                        if dk == 2 and tt + 1 < 8:
                            norm_b(Hk, hkeys, pb[6], 'pn')
                            norm_c(Xn, xkn, Hk, hkeys, pb[6], 'pn', RS[0], 'rs0', g0)
                    post_a(tt)
                post_bc(7)
            tr.barrier()

        def attention_phase():
            with contextlib.ExitStack() as a_:
                def asb(name, shape, dt):
                    return a_.enter_context(nc.sbuf_tensor(name, list(shape), dt))
                CBt = asb("CBt", [128, 512], BF16)
                WBt = asb("WBt", [128, 512], BF16)
                CMt = asb("CMt", [128, 512], BF16)
                BDt = asb("BDt", [128, 384], BF16)
                U0t = asb("U0t", [128, 128], F32)
                L0t = asb("L0t", [128, 128], F32)
                GAT = asb("GAT", [128, 512], F32)
                QN = [asb("QN%d" % i, [128, 512], BF16) for i in range(2)]
                ET = [asb("ET%d" % i, [128, 512], BF16) for i in range(3)]
                SCO = asb("SCO", [128, 64], F32)
                M8 = asb("M8", [128, 16], F32)
                WK = asb("WK", [128, 64], F32)
                SELW = asb("SELW", [128, 128], BF16)
                RZ = asb("RZ", [128, 4], F32)
                DEN = asb("DEN", [128, 12], F32)
                ATT = asb("ATT", [128, 512], F32)
                ATN = asb("ATN", [128, 512], BF16)
                JUNK = asb("JUNK", [128, 512], F32)
                SSQ = asb("SSQ", [128, 1], F32)
                MST = asb("MST", [128, 4, 128], BF16)
                tr.dma('pool', 'aconst', CBt[:], c_CB[:, :], w=['ac'])
                tr.dma('pool', 'aconst', WBt[:], c_WB[:, :], w=['ac'])
                tr.dma('pool', 'aconst', CMt[:], c_CM[:, :], w=['ac'])
                tr.dma('pool', 'aconst', BDt[:], c_BD[:, :], w=['ac'])
                tr.dma('sp', 'aconst', U0t[:], c_U0[:, :], w=['ac'])
                tr.dma('sp', 'aconst', L0t[:], c_L0[:, :], w=['ac'])
                tr.dma('sp', 'aconst', GAT[:], grow_attn.partition_broadcast(128), w=['ac'])
                tr.op('dve', lambda e: e.memset(SELW[:], 0.0), w=['selw'])
                OS = pb[3][:, 0:260].rearrange("p (r x) -> p r x", x=65)
                OW = pb[4][:, 0:260].rearrange("p (r x) -> p r x", x=65)
                OC = pb[5][:, 0:260].rearrange("p (r x) -> p r x", x=65)
                IM = pb[6][:, 0:256].rearrange("p (r x) -> p r x", x=64)
                PT = pb[5][:, 320:384].bitcast(BF16)
                PT4 = pb[6][:, 256:512].bitcast(BF16)
                PA = pb[7]
                ETA = [asb("ETA%d" % i, [128, 512], BF16) for i in range(2)]
                dq = []

                def pump(k):
                    for _ in range(min(k, len(dq))):
                        dq.pop(0)()

                def deferred(f, *a):
                    tr.defer = dq
                    f(*a)
                    tr.defer = None
                sring = [0]
                ering = [0]

                def nexts():
                    b = sring[0] % 3
                    sring[0] += 1
                    return pb[b], 'ps%d' % b

                def nexte():
                    b = ering[0] % 3
                    ering[0] += 1
                    return ET[b], 'et%d' % b

                qview = qTd.rearrange("p (c x) -> p c x", c=32)
                QN3 = [QN[0], QN[1], asb("QN2", [128, 512], BF16)]
                OCS = [asb("OCS%d" % i, [128, 4, 65], F32) for i in range(2)]
                OSS = [asb("OSS%d" % i, [128, 4, 65], F32) for i in range(2)]
                OWS = [asb("OWS%d" % i, [128, 4, 65], F32) for i in range(2)]
                ATN2 = [ATN, asb("ATN1", [128, 512], BF16)]
                NIT = 64
                mixv = mixT.rearrange("(k p) t -> p k t", p=128)
                pending = []

                def qload(it):
                    c, g = divmod(it, 2)
                    sl = it % 3
                    tr.dma('sp', 'qin%d' % sl, QN3[sl][0:64, :], qview[:, c, g * 512:(g + 1) * 512], r=['qTd'],
                           w=['qnq%d' % sl])

                def stageA(it):
                    c, g = divmod(it, 2)
                    sl = it % 3
                    Q = QN3[sl]
                    qq, qs = 'qnq%d' % sl, 'qns%d' % sl
                    nts = 1 if c < 16 else 2
                    tl = []
                    for nt in range(nts):
                        Mn = min(128, 8 * c + 7 - 128 * nt)
                        m = 8 * c - 128 * nt
                        need = m <= 192
                        PS, psk = PA, 'pa'
                        tr.op('pe', lambda e, PS=PS, Mn=Mn, nt=nt, need=need: e.matmul(
                            out=PS[0:Mn, :], lhsT=KC[g][:, 128 * nt:128 * nt + Mn], rhs=Q[0:64, :],
                            start=True, stop=not need), r=['KC%d' % g, qq], w=[psk])
                        if need:
                            tr.op('pe', lambda e, PS=PS, Mn=Mn, m=m: e.matmul(
                                out=PS[0:Mn, :], lhsT=BDt[:, 192 - m:192 - m + Mn], rhs=CMt[:],
                                start=False, stop=True), r=['ac'], w=[psk])
                        Et, ek = ETA[nt], 'eta%d' % nt
                        tr.op('act', lambda e, PS=PS, Et=Et, Mn=Mn: e.activation(
                            out=Et[0:Mn, :], in_=PS[0:Mn, :], func=AF.Exp, scale=0.125), r=[psk], w=[ek])
                        tl.append((nt, Mn, Et, ek))
                    first = True
                    for (nt, Mn, Et, ek) in tl:
                        for r in range(4):
                            tr.op('pe', lambda e, Et=Et, Mn=Mn, nt=nt, r=r, first=first: e.matmul(
                                out=OC[:, r, :], lhsT=Et[0:Mn, r * 128:(r + 1) * 128], rhs=VCX[g][0:Mn, nt, 0:65],
                                start=first, stop=(nt == nts - 1), skip_group_check=True),
                                r=[ek, 'VCX%d' % g], w=['oc'])
                            tr.op('pe', lambda e, Et=Et, Mn=Mn, nt=nt, r=r, first=first: e.matmul(
                                out=IM[:, r, :], lhsT=Et[0:Mn, r * 128:(r + 1) * 128], rhs=VCX[g][0:Mn, nt, 65:129],
                                start=first, stop=(nt == nts - 1), skip_group_check=True),
                                r=[ek, 'VCX%d' % g], w=['im'])
                            first = False
                    OCb, ock = OCS[it % 2], 'ocs%d' % (it % 2)
                    tr.op('act', lambda e, OCb=OCb: e.activation(out=OCb[:], in_=OC, func=AF.Copy), r=['oc'], w=[ock])
                    tr.op('dve', lambda e, OCb=OCb: e.tensor_scalar(out=RZ[:], in0=OCb[:, :, 64], scalar1=1e-30,
                                                                   scalar2=None, op0=ALU.max), r=[ock], w=['rz'])
                    tr.op('dve', lambda e: e.reciprocal(out=RZ[:], in_=RZ[:]), r=['rz'], w=['rz'])
                    tr.op('dve', lambda e: e.tensor_scalar(out=SCO[:], in0=IM[:, 0, :], scalar1=RZ[:, 0:1],
                                                          scalar2=None, op0=ALU.mult), r=['im', 'rz'], w=['sco'])
                    for r in range(1, 4):
                        tr.op('dve', lambda e, r=r: e.scalar_tensor_tensor(
                            out=SCO[:], in0=IM[:, r, :], scalar=RZ[:, r:r + 1], in1=SCO[:],
                            op0=ALU.mult, op1=ALU.add), r=['im', 'rz', 'sco'], w=['sco'])
                    lo = 62 - 2 * c
                    tr.op('dve', lambda e, lo=lo: e.tensor_tensor(out=SCO[:], in0=SCO[:], in1=L0t[:, lo:lo + 64],
                                                                 op=ALU.max), r=['sco', 'ac'], w=['sco'])
                    tr.op('dve', lambda e, lo=lo: e.tensor_tensor(out=SCO[:], in0=SCO[:], in1=U0t[:, lo:lo + 64],
                                                                 op=ALU.min), r=['sco', 'ac'], w=['sco'])
                    tr.op('dve', lambda e: e.memset(SCO[:, 0:1], 3e30), r=['sco'], w=['sco'])
                    tr.op('dve', lambda e: e.max(out=M8[:, 0:8], in_=SCO[:]), r=['sco'], w=['m8'])
                    tr.op('dve', lambda e: e.match_replace(out=WK[:], in_to_replace=M8[:, 0:8], in_values=SCO[:],
                                                          imm_value=-3e38), r=['sco', 'm8'], w=['wk'])
                    tr.op('dve', lambda e: e.max(out=M8[:, 8:16], in_=WK[:]), r=['wk'], w=['m8'])
                    tr.op('dve', lambda e: e.tensor_scalar(out=SELW[:, 64:128], in0=SCO[:], scalar1=M8[:, 15:16],
                                                          scalar2=None, op0=ALU.is_ge), r=['sco', 'm8'], w=['selw'])
                    tr.op('pe', lambda e: e.transpose(out=PT, in_=SELW[:], identity=IDN[:]), r=['selw', 'c'], w=['pt'])
                    tr.op('dve', lambda e, Q=Q: e.tensor_scalar(
                        out=Q[64:128, :].rearrange("p (r q) -> p r q", r=4),
                        in0=PT[64:128, :].unsqueeze(1).to_broadcast([64, 4, 128]),
                        scalar1=-1.0, scalar2=BIGM, op0=ALU.add, op1=ALU.mult), r=['pt'], w=[qs])

                def finish_pe(c):
                    AN = ATN2[c % 2]
                    for j in range(4):
                        tr.op('pe', lambda e, j=j, AN=AN: e.transpose(out=PT4[:, j * 128:(j + 1) * 128],
                                                                     in_=AN[:, j * 128:(j + 1) * 128], identity=IDN[:]),
                              r=['atn%d' % (c % 2), 'c'], w=['pt4'])
                    tr.op('dve', lambda e: e.tensor_copy(out=MST[:].rearrange("p a b -> p (a b)"), in_=PT4),
                          r=['pt4'], w=['mst'])
                    tr.dma('sp', 'mout', mixv[:, 0:4, c * 128:(c + 1) * 128], MST[:], r=['mst'], w=['mixT'])

                def stageB(it):
                    c, g = divmod(it, 2)
                    sl = it % 3
                    Q = QN3[sl]
                    qq, qs = 'qnq%d' % sl, 'qns%d' % sl
                    k0 = max(0, c - 4)
                    tiles = [('w', kt) for kt in range(k0, c + 1)] + [('s', kt) for kt in range(c + 1)]
                    n = len(tiles)
                    info = {}

                    def qk(i):
                        br, kt = tiles[i]
                        PS, psk = nexts()
                        extra = []
                        if kt == c:
                            extra.append(CBt)
                        if br == 'w' and kt == c - 4:
                            extra.append(WBt)
                        if br == 'w':
                            tr.op('pe', lambda e, PS=PS, kt=kt, ne=len(extra): e.matmul(
                                out=PS[:], lhsT=KW[g][:, kt * 128:(kt + 1) * 128], rhs=Q[0:64, :],
                                start=True, stop=(ne == 0)), r=['KW%d' % g, qq], w=[psk])
                        else:
                            tr.op('pe', lambda e, PS=PS, kt=kt, ne=len(extra): e.matmul(
                                out=PS[:], lhsT=KE[g][:, kt * 128:(kt + 1) * 128], rhs=Q[:, :],
                                start=True, stop=(ne == 0)), r=['KE%d' % g, qq, qs], w=[psk])
                        for j, bt in enumerate(extra):
                            tr.op('pe', lambda e, PS=PS, bt=bt, last=(j == len(extra) - 1): e.matmul(
                                out=PS[:], lhsT=IDN[:], rhs=bt[:], start=False, stop=last), r=['ac', 'c'], w=[psk])
                        Et, ek = nexte()
                        tr.op('act', lambda e, PS=PS, Et=Et: e.activation(
                            out=Et[:], in_=PS[:], func=AF.Exp, scale=0.125), r=[psk], w=[ek])
                        info[i] = (Et, ek)

                    started = {'w': False, 's': False}
                    lastw = max(i for i in range(n) if tiles[i][0] == 'w')

                    def pv(i):
                        br, kt = tiles[i]
                        Et, ek = info[i]
                        O, ok, V, vk = (OW, 'ow', VW1[g], 'VW1%d' % g) if br == 'w' else (OS, 'os', VS1[g], 'VS1%d' % g)
                        last = (i == lastw) if br == 'w' else (i == n - 1)
                        for r in range(4):
                            st = not started[br]
                            started[br] = True
                            tr.op('pe', lambda e, Et=Et, kt=kt, r=r, O=O, V=V, st=st, last=last: e.matmul(
                                out=O[:, r, :], lhsT=Et[:, r * 128:(r + 1) * 128], rhs=V[:, kt, :],
                                start=st, stop=last, skip_group_check=True), r=[ek, vk], w=[ok])

                    for i in range(min(2, n)):
                        qk(i)
                    for i in range(n):
                        if i + 2 < n:
                            qk(i + 2)
                        pv(i)
                        left = max(1, n - 1 - i)
                        pump((len(dq) + left - 1) // left)
                    pump(len(dq))
                    b2 = it % 2
                    tr.op('act', lambda e: e.activation(out=OWS[b2][:], in_=OW, func=AF.Copy), r=['ow'], w=['ows%d' % b2])
                    tr.op('act', lambda e: e.activation(out=OSS[b2][:], in_=OS, func=AF.Copy), r=['os'], w=['oss%d' % b2])

                def tailB(it):
                    c, g = divmod(it, 2)
                    b2 = it % 2
                    srcs = ((OCS[b2], 'ocs%d' % b2), (OSS[b2], 'oss%d' % b2), (OWS[b2], 'ows%d' % b2))
                    for bi, (O, ok) in enumerate(srcs):
                        tr.op('dve', lambda e, O=O, bi=bi: e.tensor_scalar(
                            out=DEN[:, bi * 4:bi * 4 + 4], in0=O[:, :, 64], scalar1=1e-30, scalar2=None,
                            op0=ALU.max), r=[ok], w=['den'])
                    tr.op('dve', lambda e: e.reciprocal(out=DEN[:], in_=DEN[:]), r=['den'], w=['den'])
                    gv = GT[:, c, 12 * g:12 * g + 12].rearrange("p (r b) -> p b r", b=3)
                    tr.op('dve', lambda e, gv=gv: e.tensor_tensor(
                        out=DEN[:].rearrange("p (b r) -> p b r", b=3), in0=DEN[:].rearrange("p (b r) -> p b r", b=3),
                        in1=gv, op=ALU.mult), r=['den', 'GT'], w=['den'])
                    for r in range(4):
                        dst = ATT[:, (4 * g + r) * 64:(4 * g + r + 1) * 64]
                        tr.op('dve', lambda e, r=r, dst=dst: e.tensor_scalar(
                            out=dst, in0=OCS[b2][:, r, 0:64], scalar1=DEN[:, r:r + 1], scalar2=None, op0=ALU.mult),
                            r=['ocs%d' % b2, 'den'], w=['att'])
                        tr.op('dve', lambda e, r=r, dst=dst: e.scalar_tensor_tensor(
                            out=dst, in0=OSS[b2][:, r, 0:64], scalar=DEN[:, 4 + r:5 + r], in1=dst,
                            op0=ALU.mult, op1=ALU.add), r=['oss%d' % b2, 'den', 'att'], w=['att'])
                        tr.op('dve', lambda e, r=r, dst=dst: e.scalar_tensor_tensor(
                            out=dst, in0=OWS[b2][:, r, 0:64], scalar=DEN[:, 8 + r:9 + r], in1=dst,
                            op0=ALU.mult, op1=ALU.add), r=['ows%d' % b2, 'den', 'att'], w=['att'])
                    if g == 1:
                        AN, ank = ATN2[c % 2], 'atn%d' % (c % 2)
                        tr.op('act', lambda e: e.activation(out=JUNK[:], in_=ATT[:], func=AF.Square, accum_out=SSQ[:]),
                              r=['att'], w=['junk', 'ssq'])
                        tr.op('dve', lambda e: e.tensor_scalar(out=SSQ[:], in0=SSQ[:], scalar1=1.0 / 512, scalar2=EPS,
                                                              op0=ALU.mult, op1=ALU.add), r=['ssq'], w=['ssq'])
                        tr.op('act', lambda e: e.activation(out=SSQ[:], in_=SSQ[:], func=AF.Ln), r=['ssq'], w=['ssq'])
                        tr.op('act', lambda e: e.activation(out=SSQ[:], in_=SSQ[:], func=AF.Exp, scale=-0.5),
                              r=['ssq'], w=['ssq'])
                        tr.op('dve', lambda e, AN=AN: e.scalar_tensor_tensor(out=AN[:], in0=ATT[:], scalar=SSQ[:, 0:1],
                                                                            in1=GAT[:], op0=ALU.mult, op1=ALU.mult),
                              r=['att', 'ssq', 'ac'], w=[ank])

                qload(0)
                qload(1)
                stageA(0)
                for it in range(NIT):
                    if it >= 1:
                        deferred(tailB, it - 1)
                    if it + 1 < NIT:
                        deferred(stageA, it + 1)
                    if it >= 1 and (it - 1) % 2 == 1:
                        deferred(finish_pe, (it - 1) // 2)
                    if it + 2 < NIT:
                        qload(it + 2)
                    stageB(it)
                tailB(NIT - 1)
                finish_pe((NIT - 1) // 2)
            tr.barrier()

        def s5_phase():
            PI = float(np.pi)
            with contextlib.ExitStack() as s_:
                def ssb(name, shape, dt):
                    return s_.enter_context(nc.sbuf_tensor(name, list(shape), dt))
                ROW = [ssb("ROW%d" % i, [128, 2048], F32) for i in range(3)]
                PRE_RE = ssb("PRE_RE", [128, 2048], F32)
                PRE_IM = ssb("PRE_IM", [128, 2048], F32)
                TH = ssb("TH", [128, 2048], F32)
                TMPI = ssb("TMPI", [128, 2048], mybir.dt.int32)
                JC = ssb("JC", [128, 1], F32)
                NJC = ssb("NJC", [128, 1], F32)
                NPI = ssb("NPI", [128, 1], F32)
                COL = ssb("COL", [128, 48], F32)
                IOT = ssb("IOT", [128, 128], F32)
                POST_RE = ssb("POST_RE", [128, 16, 128], F32)
                POST_IM = ssb("POST_IM", [128, 16, 128], F32)
                A128 = ssb("A128", [128, 2, 16], F32)
                SM = [ssb("SM%d" % i, [128, 16], F32) for i in range(3)]
                REP = ssb("REP", [128, 4, 3, 64], F32)
                BT = ssb("BT", [128, 4, 2, 64], F32)
                BB = ssb("BB", [128, 4, 2, 64], F32)
                RT = [ssb("RT%d" % i, [128, 4, 64], F32) for i in range(6)]
                BBD = ssb("BBD", [128, 4, 1024], BF16)
                MASKB = ssb("MASKB", [128, 8], F32)
                CC = ssb("CC", [128, 16, 2, 16], F32)
                MASKC = ssb("MASKC", [128, 4, 8], F32)
                CMAT = ssb("CMAT", [128, 16, 2, 128], BF16)
                DCOL = ssb("DCOL", [128, 4], F32)
                BGL = ssb("BGL", [128, 4], F32)
                WGL = ssb("WGL", [128, 4, 512], BF16)
                TRI = ssb("TRI", [128, 128], BF16)
                UT = [ssb("UT%d" % i, [128, 4, 512], BF16) for i in range(2)]
                T1B = ssb("T1B", [128, 8, 512], BF16)
                T2B = ssb("T2B", [128, 8, 512], BF16)
                NTRI = ssb("NTRI", [128, 128], BF16)
                NCM = ssb("NCM", [128, 16, 128], BF16)
                ZCF = ssb("ZCF", [128, 16, 2, 128], F32)
                CRE = ssb("CRE", [128, 16], F32)
                CIM = ssb("CIM", [128, 16], F32)
                TQ = [ssb("TQ%d" % i, [128, 16], F32) for i in range(4)]
                XU1 = ssb("XU1", [128, 16, 2, 128], BF16)
                XU2 = ssb("XU2", [128, 16, 2, 128], BF16)
                CARRY = ssb("CARRY", [128, 16, 2], F32)
                CT = [ssb("CT%d" % i, [128, 2, 2], F32) for i in range(2)]
                Y = ROW[0][:].rearrange("p (a b) -> p a b", a=4)
                YB = ROW[1][:].rearrange("p (a b) -> p a b", a=4)
                YG = ROW[2][:].rearrange("p (a b) -> p a b", a=4)
                YGB = ssb("YGB", [128, 4, 512], BF16)
                SGL = [ssb("SGL%d" % i, [128, 512], F32) for i in range(2)]
                S = TH[:].rearrange("p (a b) -> p a b", a=4)
                SQ = ssb("SQ", [128, 4, 512], BF16)
                RS5 = ssb("RS5", [128, 512], F32)
                OUT = ssb("OUT", [128, 4, 512], BF16)
                ck = ['s5c']
                for i in range(3):
                    tr.dma('sp', 's5const', ROW[i][:], s5_rows[i:i + 1, :].partition_broadcast(128), w=ck)
                tr.dma('sp', 's5const', JC[:], c_jcol[:, :], w=ck)
                tr.dma('sp', 's5const', COL[:], s5_cols[:, :], w=ck)
                tr.dma('sp', 's5const', IOT[:], c_iota[:, :], w=ck)
                tr.dma('sp', 's5const', REP[:].rearrange("p a b c -> p (a b c)"), s5_rep[:, :], w=ck)
                tr.dma('sp', 's5const', BT[:].rearrange("p a b c -> p (a b c)"), s5_bT[:, :], w=ck)
                tr.dma('sp', 's5const', MASKB[:], c_maskB[:, :], w=ck)
                tr.dma('sp', 's5const', CC[:].rearrange("p a b c -> p (a b c)"), s5_c[:, :], w=ck)
                tr.dma('sp', 's5const', MASKC[:].rearrange("p a b -> p (a b)"), c_maskC[:, :], w=ck)
                tr.dma('sp', 's5const', DCOL[:], s5_dcol[:, :], w=ck)
                tr.dma('sp', 's5const', BGL[:], bglu_col[:, :], w=ck)
                tr.dma('pool', 's5const', TRI[:], c_tri[:, :], w=ck)
                for k in range(4):
                    tr.dma('pool', 's5const', WGL[:, k, :], w_glu[k * 128:(k + 1) * 128, :], w=ck)
                V = lambda fn, r, w: tr.op('dve', fn, r=r, w=w)
                A = lambda fn, r, w: tr.op('act', fn, r=r, w=w)
                V(lambda e: e.memset(NPI[:], -PI), [], ck)
                V(lambda e: e.tensor_scalar(out=NJC[:], in0=JC[:], scalar1=-1.0, scalar2=None, op0=ALU.mult), ck, ck)
                V(lambda e: e.memset(CRE[:], 0.0), [], ['carry'])
                V(lambda e: e.memset(CIM[:], 0.0), [], ['carry'])

                def sincos(out_sin, out_cos, theta, tmpf, tmpi):
                    for out, shift in ((out_sin, 0.0), (out_cos, 0.5 * PI)):
                        V(lambda e, shift=shift: e.tensor_scalar(out=tmpf, in0=theta, scalar1=shift, scalar2=1.0 / (2 * PI),
                                                                 op0=ALU.add, op1=ALU.mult), ck, ck)
                        V(lambda e: e.tensor_copy(out=tmpi, in_=tmpf), ck, ck)
                        V(lambda e: e.tensor_copy(out=tmpf, in_=tmpi), ck, ck)
                        V(lambda e, out=out: e.scalar_tensor_tensor(out=out, in0=tmpf, scalar=-2 * PI, in1=theta,
                                                                    op0=ALU.mult, op1=ALU.add), ck, ck)
                        if shift:
                            V(lambda e, out=out, shift=shift: e.tensor_scalar(out=out, in0=out, scalar1=shift, scalar2=None,
                                                                              op0=ALU.add), ck, ck)
                        V(lambda e, out=out: e.tensor_scalar(out=tmpf, in0=out, scalar1=PI, scalar2=-2 * PI,
                                                             op0=ALU.is_gt, op1=ALU.mult), ck, ck)
                        V(lambda e, out=out: e.tensor_tensor(out=out, in0=out, in1=tmpf, op=ALU.add), ck, ck)
                        A(lambda e, out=out: e.activation(out=out, in_=out, func=AF.Sin), ck, ck)

                A(lambda e: e.activation(out=ROW[2][:], in_=ROW[2][:], func=AF.Exp), ck, ck)
                V(lambda e: e.tensor_tensor(out=ROW[0][:], in0=ROW[0][:], in1=ROW[2][:], op=ALU.mult), ck, ck)
                V(lambda e: e.tensor_tensor(out=ROW[1][:], in0=ROW[1][:], in1=ROW[2][:], op=ALU.mult), ck, ck)
                A(lambda e: e.activation(out=ROW[2][:], in_=ROW[0][:], func=AF.Exp, scale=NJC[:, 0:1]), ck, ck)
                V(lambda e: e.tensor_scalar(out=TH[:], in0=ROW[1][:], scalar1=JC[:, 0:1], scalar2=None, op0=ALU.mult),
                  ck, ck)
                sincos(PRE_IM[:], PRE_RE[:], TH[:], ROW[0][:], TMPI[:])
                V(lambda e: e.tensor_tensor(out=PRE_RE[:], in0=PRE_RE[:], in1=ROW[2][:], op=ALU.mult), ck, ck)
                V(lambda e: e.scalar_tensor_tensor(out=PRE_IM[:], in0=PRE_IM[:], scalar=-1.0, in1=ROW[2][:],
                                                   op0=ALU.mult, op1=ALU.mult), ck, ck)
                LRc, LIc, DTc = COL[:, 0:16], COL[:, 16:32], COL[:, 32:48]
                A(lambda e: e.activation(out=DTc, in_=DTc, func=AF.Exp), ck, ck)
                V(lambda e: e.tensor_tensor(out=LRc, in0=LRc, in1=DTc, op=ALU.mult), ck, ck)
                V(lambda e: e.tensor_tensor(out=LIc, in0=LIc, in1=DTc, op=ALU.mult), ck, ck)
                iob = IOT[:].unsqueeze(1).to_broadcast([128, 16, 128])
                THv = TH[:].rearrange("p (a b) -> p a b", a=16)
                R0v = ROW[0][:].rearrange("p (a b) -> p a b", a=16)
                R2v = ROW[2][:].rearrange("p (a b) -> p a b", a=16)
                V(lambda e: e.tensor_tensor(out=R2v, in0=iob, in1=LRc.unsqueeze(2).to_broadcast([128, 16, 128]),
                                            op=ALU.mult), ck, ck)
                A(lambda e: e.activation(out=ROW[2][:], in_=ROW[2][:], func=AF.Exp), ck, ck)
                V(lambda e: e.tensor_tensor(out=THv, in0=iob, in1=LIc.unsqueeze(2).to_broadcast([128, 16, 128]),
                                            op=ALU.mult), ck, ck)
                sincos(POST_IM[:].rearrange("p a b -> p (a b)"), POST_RE[:].rearrange("p a b -> p (a b)"), TH[:],
                       ROW[0][:], TMPI[:])
                V(lambda e: e.tensor_tensor(out=POST_RE[:], in0=POST_RE[:], in1=R2v, op=ALU.mult), ck, ck)
                V(lambda e: e.tensor_tensor(out=POST_IM[:], in0=POST_IM[:], in1=R2v, op=ALU.mult), ck, ck)
                V(lambda e: e.tensor_scalar(out=SM[0][:], in0=LRc, scalar1=128.0, scalar2=None, op0=ALU.mult), ck, ck)
                A(lambda e: e.activation(out=SM[0][:], in_=SM[0][:], func=AF.Exp), ck, ck)
                V(lambda e: e.tensor_scalar(out=SM[1][:], in0=LIc, scalar1=128.0, scalar2=None, op0=ALU.mult), ck, ck)
                sincos(A128[:, 1, :], A128[:, 0, :], SM[1][:], SM[2][:], TMPI[:, 0:16])
                V(lambda e: e.tensor_tensor(out=A128[:, 0, :], in0=A128[:, 0, :], in1=SM[0][:], op=ALU.mult), ck, ck)
                V(lambda e: e.tensor_tensor(out=A128[:, 1, :], in0=A128[:, 1, :], in1=SM[0][:], op=ALU.mult), ck, ck)
                lr, li, ldt = REP[:, :, 0, :], REP[:, :, 1, :], REP[:, :, 2, :]
                dtv, ldr, ldi, mag, sn, cs = [t[:] for t in RT]
                A(lambda e: e.activation(out=dtv, in_=ldt, func=AF.Exp), ck, ck)
                V(lambda e: e.tensor_tensor(out=ldr, in0=lr, in1=dtv, op=ALU.mult), ck, ck)
                V(lambda e: e.tensor_tensor(out=ldi, in0=li, in1=dtv, op=ALU.mult), ck, ck)
                A(lambda e: e.activation(out=mag, in_=ldr, func=AF.Exp), ck, ck)
                sincos(sn, cs, ldi, dtv, TMPI[:, 0:256].rearrange("p (a b) -> p a b", a=4))
                V(lambda e: e.tensor_tensor(out=cs, in0=cs, in1=mag, op=ALU.mult), ck, ck)
                V(lambda e: e.tensor_tensor(out=sn, in0=sn, in1=mag, op=ALU.mult), ck, ck)
                V(lambda e: e.tensor_scalar(out=cs, in0=cs, scalar1=-1.0, scalar2=None, op0=ALU.add), ck, ck)
                V(lambda e: e.tensor_tensor(out=dtv, in0=lr, in1=lr, op=ALU.mult), ck, ck)
                V(lambda e: e.tensor_tensor(out=mag, in0=li, in1=li, op=ALU.mult), ck, ck)
                V(lambda e: e.tensor_tensor(out=dtv, in0=dtv, in1=mag, op=ALU.add), ck, ck)
                V(lambda e: e.reciprocal(out=dtv, in_=dtv), ck, ck)
                V(lambda e: e.tensor_tensor(out=ldr, in0=cs, in1=lr, op=ALU.mult), ck, ck)
                V(lambda e: e.tensor_tensor(out=mag, in0=sn, in1=li, op=ALU.mult), ck, ck)
                V(lambda e: e.tensor_tensor(out=ldr, in0=ldr, in1=mag, op=ALU.add), ck, ck)
                V(lambda e: e.tensor_tensor(out=ldr, in0=ldr, in1=dtv, op=ALU.mult), ck, ck)
                V(lambda e: e.tensor_tensor(out=ldi, in0=sn, in1=lr, op=ALU.mult), ck, ck)
                V(lambda e: e.tensor_tensor(out=mag, in0=cs, in1=li, op=ALU.mult), ck, ck)
                V(lambda e: e.tensor_tensor(out=ldi, in0=ldi, in1=mag, op=ALU.subtract), ck, ck)
                V(lambda e: e.tensor_tensor(out=ldi, in0=ldi, in1=dtv, op=ALU.mult), ck, ck)
                br, bi_ = BT[:, :, 0, :], BT[:, :, 1, :]
                V(lambda e: e.tensor_tensor(out=BB[:, :, 0, :], in0=ldr, in1=br, op=ALU.mult), ck, ck)
                V(lambda e: e.tensor_tensor(out=mag, in0=ldi, in1=bi_, op=ALU.mult), ck, ck)
                V(lambda e: e.tensor_tensor(out=BB[:, :, 0, :], in0=BB[:, :, 0, :], in1=mag, op=ALU.subtract), ck, ck)
                V(lambda e: e.tensor_tensor(out=BB[:, :, 1, :], in0=ldr, in1=bi_, op=ALU.mult), ck, ck)
                V(lambda e: e.tensor_tensor(out=mag, in0=ldi, in1=br, op=ALU.mult), ck, ck)
                V(lambda e: e.tensor_tensor(out=BB[:, :, 1, :], in0=BB[:, :, 1, :], in1=mag, op=ALU.add), ck, ck)
                BBDv = BBD[:].rearrange("p k (a r g x) -> p k a r g x", a=4, r=2, g=2)
                for kt in range(4):
                    for a in range(4):
                        for ri in range(2):
                            V(lambda e, kt=kt, a=a, ri=ri: e.tensor_tensor(
                                out=BBDv[:, kt, a, ri, :, :],
                                in0=BB[:, kt, ri, :].unsqueeze(1).to_broadcast([128, 2, 64]),
                                in1=MASKB[:, 2 * a:2 * a + 2].unsqueeze(2).to_broadcast([128, 2, 64]),
                                op=ALU.mult), ck, ck)
                V(lambda e: e.tensor_scalar(out=NTRI[:], in0=TRI[:], scalar1=-1.0, scalar2=None, op0=ALU.mult), ck, ck)
                CMv = CMAT[:].rearrange("p a r (g h) -> p a r g h", g=8)
                NCMv = NCM[:].rearrange("p a (g h) -> p a g h", g=8)
                for pr in range(16):
                    in0 = CC[:, pr, 0, :].unsqueeze(1).to_broadcast([128, 8, 16])
                    in1 = MASKC[:, pr % 4, :].unsqueeze(2).to_broadcast([128, 8, 16])
                    V(lambda e, pr=pr, in0=in0, in1=in1: e.scalar_tensor_tensor(
                        out=NCMv[:, pr, :, :], in0=in0, scalar=-1.0, in1=in1, op0=ALU.mult, op1=ALU.mult), ck, ck)
                for pr in range(16):
                    for ri in range(2):
                        in0 = CC[:, pr, ri, :].unsqueeze(1).to_broadcast([128, 8, 16])
                        in1 = MASKC[:, pr % 4, :].unsqueeze(2).to_broadcast([128, 8, 16])
                        if ri == 0:
                            V(lambda e, pr=pr, in0=in0, in1=in1: e.tensor_tensor(out=CMv[:, pr, 0, :, :], in0=in0, in1=in1,
                                                                               op=ALU.mult), ck, ck)
                        else:
                            V(lambda e, pr=pr, in0=in0, in1=in1: e.scalar_tensor_tensor(
                                out=CMv[:, pr, 1, :, :], in0=in0, scalar=-1.0, in1=in1, op0=ALU.mult, op1=ALU.mult),
                                ck, ck)
                uview = uTd.rearrange("p (k t) -> p k t", k=4)
                mview = mixT.rearrange("(k p) t -> p k t", p=128)
                bring = [0]
                def uload(tt):
                    us = tt % 2
                    tr.dma('sp', 'uin%d' % us, UT[us][:], uview[:, :, tt * 512:tt * 512 + 512], r=['uTd'], w=['ut%d' % us])
                def pre(cc, jj):
                    tt, sub = divmod(cc, 4)
                    us = tt % 2; uk = 'ut%d' % us; U = UT[us]
                    tsl = slice(sub * 128, (sub + 1) * 128)
                    kt, half = divmod(jj, 2)
                    i = bring[0] % 2
                    bring[0] += 1
                    PB = pb[i]
                    pk = 'pbu%d' % i
                    pr0 = 4 * kt + 2 * half
                    tr.op('pe', lambda e: e.matmul(
                        out=PB[:], lhsT=U[:, kt, tsl], rhs=BBD[:, kt, half * 512:(half + 1) * 512],
                        start=True, stop=True), r=[uk, 's5c'], w=[pk])
                    PBv = PB[:].rearrange("p (a r x) -> p a r x", a=2, r=2)
                    pre_r = PRE_RE[:, pr0 * 128:pr0 * 128 + 256].rearrange("p (a x) -> p a x", a=2) \
                        .unsqueeze(2).to_broadcast([128, 2, 2, 128])
                    pre_i = PRE_IM[:, pr0 * 128:pr0 * 128 + 256].rearrange("p (a x) -> p a x", a=2) \
                        .unsqueeze(2).to_broadcast([128, 2, 2, 128])
                    t1 = T1B[:, jj, :].rearrange("p (a r x) -> p a r x", a=2, r=2)
                    t2 = T2B[:, jj, :].rearrange("p (a r x) -> p a r x", a=2, r=2)
                    tr.op('dve', lambda e: e.tensor_tensor(out=t1, in0=PBv, in1=pre_r, op=ALU.mult),
                          r=[pk, 's5c'], w=['t1_%d' % jj])
                    tr.op('dve', lambda e: e.tensor_tensor(out=t2, in0=PBv, in1=pre_i, op=ALU.mult),
                          r=[pk, 's5c'], w=['t2_%d' % jj])
                def post_a(cc, jj):
                    pg = jj
                    i = pg % 2
                    PZ = pb[2 + i]
                    zk = 'pz%d' % i
                    pr0 = 2 * pg
                    PZv = PZ[:].rearrange("p (a r x) -> p a r x", a=2, r=2)
                    t1 = T1B[:, jj, :].rearrange("p (a r x) -> p a r x", a=2, r=2)
                    t2 = T2B[:, jj, :].rearrange("p (a r x) -> p a r x", a=2, r=2)
                    rk = ['t1_%d' % jj, 't2_%d' % jj, 's5c']
                    for a in range(2):
                        tr.op('pe', lambda e, a=a: e.matmul(out=PZv[:, a, 0, :], lhsT=t1[:, a, 0, :], rhs=TRI[:],
                                                            start=True, stop=False, skip_group_check=True), r=rk, w=[zk])
                        tr.op('pe', lambda e, a=a: e.matmul(out=PZv[:, a, 0, :], lhsT=t2[:, a, 1, :], rhs=NTRI[:],
                                                            start=False, stop=True, skip_group_check=True), r=rk, w=[zk])
                        tr.op('pe', lambda e, a=a: e.matmul(out=PZv[:, a, 1, :], lhsT=t1[:, a, 1, :], rhs=TRI[:],
                                                            start=True, stop=False, skip_group_check=True), r=rk, w=[zk])
                        tr.op('pe', lambda e, a=a: e.matmul(out=PZv[:, a, 1, :], lhsT=t2[:, a, 0, :], rhs=TRI[:],
                                                            start=False, stop=True, skip_group_check=True), r=rk, w=[zk])
                    zck = 'zc%d' % pg
                    for a in range(2):
                        for ri in range(2):
                            CB_ = CRE if ri == 0 else CIM
                            tr.op('act', lambda e, a=a, ri=ri, CB_=CB_: e.activation(
                                out=ZCF[:, pr0 + a, ri, :], in_=PZv[:, a, ri, :], func=AF.Identity,
                                bias=CB_[:, pr0 + a:pr0 + a + 1]), r=[zk, 'carry'], w=[zck])
                def post_b(cc, jj):
                    pg = jj
                    pr0 = 2 * pg
                    zck = 'zc%d' % pg
                    Z = ZCF[:, pr0:pr0 + 2, :, :]
                    po_r = POST_RE[:, pr0:pr0 + 2, :].unsqueeze(2).to_broadcast([128, 2, 2, 128])
                    po_i = POST_IM[:, pr0:pr0 + 2, :].unsqueeze(2).to_broadcast([128, 2, 2, 128])
                    tr.op('dve', lambda e: e.tensor_tensor(out=XU1[:, pr0:pr0 + 2, :, :], in0=Z, in1=po_r, op=ALU.mult),
                          r=[zck, 's5c'], w=['x%d' % pg])
                    tr.op('dve', lambda e: e.tensor_tensor(out=XU2[:, pr0:pr0 + 2, :, :], in0=Z, in1=po_i, op=ALU.mult),
                          r=[zck, 's5c'], w=['x%d' % pg])
                def post_end(cc):
                    tt, sub = divmod(cc, 4)
                    us = tt % 2; uk = 'ut%d' % us; U = UT[us]
                    tsl = slice(sub * 128, (sub + 1) * 128)
                    zall = ['zc%d' % pg for pg in range(8)]
                    zr = ZCF[:, :, 0, 127]
                    zi = ZCF[:, :, 1, 127]
                    tr.op('dve', lambda e: e.tensor_tensor(out=TQ[0][:], in0=zr, in1=A128[:, 0, :], op=ALU.mult),
                          r=zall + ['s5c'], w=['tq'])
                    tr.op('dve', lambda e: e.tensor_tensor(out=TQ[1][:], in0=zi, in1=A128[:, 1, :], op=ALU.mult),
                          r=zall + ['s5c'], w=['tq'])
                    tr.op('dve', lambda e: e.tensor_tensor(out=TQ[2][:], in0=zr, in1=A128[:, 1, :], op=ALU.mult),
                          r=zall + ['s5c'], w=['tq'])
                    tr.op('dve', lambda e: e.tensor_tensor(out=TQ[3][:], in0=zi, in1=A128[:, 0, :], op=ALU.mult),
                          r=zall + ['s5c'], w=['tq'])
                    tr.op('dve', lambda e: e.tensor_tensor(out=CRE[:], in0=TQ[0][:], in1=TQ[1][:], op=ALU.subtract),
                          r=['tq'], w=['carry'])
                    tr.op('dve', lambda e: e.tensor_tensor(out=CIM[:], in0=TQ[2][:], in1=TQ[3][:], op=ALU.add),
                          r=['tq'], w=['carry'])
                    for kt in range(4):
                        i = kt % 2
                        PY = pb[4 + i]
                        yk = 'py%d' % i
                        n = 0
                        for a in range(4):
                            pr = 4 * kt + a
                            terms = ((CMAT[:, pr, 0, :], XU1[:, pr, 0, :]), (NCM[:, pr, :], XU2[:, pr, 1, :]),
                                     (CMAT[:, pr, 1, :], XU1[:, pr, 1, :]), (CMAT[:, pr, 1, :], XU2[:, pr, 0, :]))
                            for (cm, xx) in terms:
                                tr.op('pe', lambda e, PY=PY, cm=cm, xx=xx, n=n: e.matmul(
                                    out=PY[:, 0:128], lhsT=cm, rhs=xx,
                                    start=(n == 0), stop=(n == 15)), r=['s5c', 'x%d' % (pr // 2)], w=[yk])
                                n += 1
                        tr.op('dve', lambda e, PY=PY, kt=kt, tsl=tsl: e.scalar_tensor_tensor(
                            out=Y[:, kt, tsl], in0=U[:, kt, tsl], scalar=DCOL[:, kt:kt + 1], in1=PY[:, 0:128],
                            op0=ALU.mult, op1=ALU.add), r=[uk, yk, 's5c'], w=['y'])
                def tail(tt):
                    t0 = tt * 512
                    Yf = ROW[0][:]
                    YBf = ROW[1][:]
                    YGf = ROW[2][:]
                    tr.op('act', lambda e: e.activation(out=YBf, in_=Yf, func=AF.Square), r=['y'], w=['yb'])
                    tr.op('dve', lambda e: e.tensor_scalar(out=YBf, in0=YBf, scalar1=0.044715, scalar2=1.0,
                                                          op0=ALU.mult, op1=ALU.add), r=['yb'], w=['yb'])
                    tr.op('dve', lambda e: e.tensor_tensor(out=YBf, in0=YBf, in1=Yf, op=ALU.mult), r=['yb', 'y'], w=['yb'])
                    tr.op('act', lambda e: e.activation(out=YBf, in_=YBf, func=AF.Sigmoid, scale=GELU_C), r=['yb'], w=['yb'])
                    tr.op('dve', lambda e: e.tensor_tensor(out=YGf, in0=Yf, in1=YBf, op=ALU.mult), r=['y', 'yb'], w=['yg'])
                    tr.op('act', lambda e: e.activation(out=YGB[:].rearrange("p a b -> p (a b)"), in_=YGf, func=AF.Copy),
                          r=['yg'], w=['ygb'])
                    for mt in range(4):
                        i = mt % 2
                        PG = pb[6 + i]
                        gk = 'pgl%d' % i
                        for kt in range(4):
                            tr.op('pe', lambda e, PG=PG, kt=kt, mt=mt: e.matmul(
                                out=PG[:], lhsT=WGL[:, kt, mt * 128:(mt + 1) * 128], rhs=YGB[:, kt, :],
                                start=(kt == 0), stop=(kt == 3)), r=['s5c', 'ygb'], w=[gk])
                        tr.op('act', lambda e, PG=PG, mt=mt, i=i: e.activation(
                            out=SGL[i][:], in_=PG[:], func=AF.Sigmoid, bias=BGL[:, mt:mt + 1]), r=[gk, 's5c'],
                            w=['sgl%d' % i])
                        tr.op('dve', lambda e, mt=mt, i=i: e.tensor_tensor(out=S[:, mt, :], in0=YG[:, mt, :],
                                                                          in1=SGL[i][:], op=ALU.mult),
                              r=['yg', 'sgl%d' % i], w=['s'])
                    tr.op('act', lambda e: e.activation(out=SQ[:].rearrange("p a b -> p (a b)"),
                                                       in_=TH[:], func=AF.Square),
                          r=['s'], w=['sq'])
                    PN = pb[6]
                    for mt in range(4):
                        tr.op('pe', lambda e, mt=mt: e.matmul(out=PN[:], lhsT=ONES[:], rhs=SQ[:, mt, :],
                                                             start=(mt == 0), stop=(mt == 3)), r=['sq', 'c'], w=['pgl0'])
                    tr.op('dve', lambda e: e.tensor_scalar(out=RS5[:], in0=PN[:], scalar1=1.0 / 512, scalar2=EPS,
                                                          op0=ALU.mult, op1=ALU.add), r=['pgl0'], w=['rs5'])
                    tr.op('act', lambda e: e.activation(out=RS5[:], in_=RS5[:], func=AF.Sqrt), r=['rs5'], w=['rs5'])
                    tr.op('dve', lambda e: e.reciprocal(out=RS5[:], in_=RS5[:]), r=['rs5'], w=['rs5'])
                    for mt in range(4):
                        tr.op('dve', lambda e, mt=mt: e.scalar_tensor_tensor(
                            out=OUT[:, mt, :], in0=S[:, mt, :], scalar=GC[:, 32 + mt:33 + mt], in1=RS5[:],
                            op0=ALU.mult, op1=ALU.mult), r=['s', 'rs5', 'c'], w=['out5'])
                    tr.dma('sp', 'sout', mview[:, 4:8, t0:t0 + 512], OUT[:], r=['out5'], w=['mixT'])
                uload(0)
                uload(1)
                for jj in range(8):
                    pre(0, jj)
                for cc in range(32):
                    post_a(cc, 0)
                    for jj in range(8):
                        if jj + 1 < 8:
                            post_a(cc, jj + 1)
                        if cc + 1 < 32:
                            pre(cc + 1, jj)
                        post_b(cc, jj)
                    post_end(cc)
                    if cc % 4 == 3:
                        tail(cc // 4)
                        if cc // 4 + 2 < 8:
                            uload(cc // 4 + 2)
            tr.barrier()

        def wout_phase(after_wo=None):
            with contextlib.ExitStack() as w_:
                def wsb(name, shape, dt):
                    return w_.enter_context(nc.sbuf_tensor(name, list(shape), dt))
                WO = wsb("WO", [128, 8, D], BF16)
                XW = [wsb("XW%d" % i, [128, 8, 512], F32) for i in range(2)]
                MX = [wsb("MX%d" % i, [128, 8, 512], BF16) for i in range(2)]
                for k in range(8):
                    tr.dma('pool', 'wo', WO[:, k, :], w_out[k * 128:(k + 1) * 128, :], w=['WO'])
                if after_wo is not None:
                    after_wo()
                for tt in range(8):
                    s = tt % 2
                    t0 = tt * 512
                    xk, mk = 'xw%d' % s, 'mx%d' % s
                    tr.dma('sp', 'xwin%d' % s, XW[s][:], xview(x1T, t0, 512), r=['x1T_%d' % tt], w=[xk])
                    tr.dma('act', 'mxin%d' % s, MX[s][:], xview(mixT, t0, 512), r=['mixT'], w=[mk])
                    for dk in range(8):
                        b = dk % 4
                        for k in range(8):
                            tr.op('pe', lambda e, k=k, dk=dk, b=b, s=s: e.matmul(
                                out=pb[b][:], lhsT=WO[:, k, dk * 128:(dk + 1) * 128], rhs=MX[s][:, k, :],
                                start=(k == 0), stop=(k == 7)), r=['WO', mk], w=['pw%d' % b])
                        tr.op('dve', lambda e, dk=dk, b=b, s=s: e.tensor_tensor(
                            out=XW[s][:, dk, :], in0=pb[b][:], in1=XW[s][:, dk, :], op=ALU.add),
                            r=['pw%d' % b, xk], w=[xk])
                    tr.dma('act', 'xwout%d' % s, xview(x1T, t0, 512), XW[s][:], r=[xk], w=['x1T_%d' % tt])
            tr.barrier()

        ffn_phase(1)

        with contextlib.ExitStack() as ms:
            if stop < 1.2:
                return nc
            def msb(name, shape, dt):
                return ms.enter_context(nc.sbuf_tensor(name, list(shape), dt))
            KE = [msb("KE%d" % g, [128, T], BF16) for g in range(2)]
            KW = [msb("KW%d" % g, [64, T], BF16) for g in range(2)]
            VS1 = [msb("VS1%d" % g, [128, 32, 65], BF16) for g in range(2)]
            VW1 = [msb("VW1%d" % g, [128, 32, 65], BF16) for g in range(2)]
            GT = msb("GT", [128, 32, 24], F32)
            KC = [msb("KC%d" % g, [64, 256], BF16) for g in range(2)]
            VCX = [msb("VCX%d" % g, [128, 2, 129], BF16) for g in range(2)]
            for g in range(2):
                tr.dma('pool', 'const', KE[g][64:128, :], c_E0[:, :], w=['KE%d' % g])
                tr.op('dve', lambda e, g=g: e.memset(VS1[g][:, :, 64:65], 1.0), w=['VS1%d' % g])
                tr.op('dve', lambda e, g=g: e.memset(VW1[g][:, :, 64:65], 1.0), w=['VW1%d' % g])
                tr.op('dve', lambda e, g=g: e.memset(VCX[g][:, :, 64:65], 1.0), w=['VCX%d' % g])
                tr.dma('pool', 'const', VCX[g][:, :, 65:129], c_ovl.rearrange("p (a b) -> p a b", a=2),
                       w=['VCX%d' % g])

            with contextlib.ExitStack() as ps_:
                def psb(name, shape, dt):
                    return ps_.enter_context(nc.sbuf_tensor(name, list(shape), dt))
                WIN = psb("WIN", [128, 8, 1816], BF16)
                H2 = [psb("H2_%d" % i, [128, 8, 512], BF16) for i in range(2)]
                QST = psb("QST", [64, 4, 8, 128], BF16)
                UST = psb("UST", [128, 4, 512], BF16)
                KCin = [psb("KCin%d" % g, [64, T], BF16) for g in range(2)]
                VCin = [psb("VCin%d" % g, [64, T], BF16) for g in range(2)]
                GB = psb("GB", [128, 24], F32)
                W1 = [psb("W1_%d" % i, [64, 32, 256], BF16) for i in range(2)]
                W2 = [psb("W2_%d" % i, [128, 2, 64], BF16) for i in range(2)]
                CB1 = psb("CB1", [128, 4], F32)
                POST = psb("POST", [64, 64], BF16)
                HID = psb("HID", [128, 2, 256], BF16)
                GTMP = [psb("GTMP%d" % i, [128, 256], F32) for i in range(3)]
                PBIAS = psb("PBIAS", [128, 1], F32)
                for k in range(8):
                    tr.dma('pool', 'win', WIN[:, k, :], w_in[k * 128:(k + 1) * 128, :], w=['WIN'])
                tr.dma('sp', 'const', GB[:], gate_bias.partition_broadcast(128), w=['GB'])
                tr.dma('sp', 'const', CB1[:], cb1[:, :], w=['cmpw'])
                tr.dma('pool', 'const', POST[:], posT[:, :], w=['cmpw'])
                for i in range(2):
                    w1v = cw1[i].rearrange("(l d) h -> d l h", d=64)
                    for lc in range(8):
                        tr.dma('pool', 'const', W1[i][:, 4 * lc:4 * lc + 4, :], w1v[:, 4 * lc:4 * lc + 4, :], w=['cmpw'])
                    tr.dma('pool', 'const', W2[i][:], cw2[i].rearrange("(t p) d -> p t d", p=128), w=['cmpw'])
                if stop < 1.6:
                    tr.barrier()
                    return nc
                ring = [0]

                def nextbank():
                    b = ring[0] % 6
                    ring[0] += 1
                    return pb[b], 'pp%d' % b

                evq = [0]

                def evac(out, in_, rk, wk, force=None):
                    evq[0] += 1
                    if (evq[0] % 2 == 0 and force is None) or force == 'act':
                        tr.op('act', lambda e: e.activation(out=out, in_=in_, func=AF.Copy), r=[rk], w=[wk])
                    else:
                        tr.op('dve', lambda e: e.tensor_copy(out=out, in_=in_), r=[rk], w=[wk])

                for tt in range(8):
                    s = tt % 2
                    t0 = tt * 512
                    hk = 'h2_%d' % s
                    HH = H2[s]
                    tr.dma('sp', 'h2in%d' % s, HH[:], xview(h2T, t0, 512), r=['h2T'], w=[hk])
                    for h in range(8):
                        P, pk = nextbank()
                        for k in range(8):
                            tr.op('pe', lambda e, k=k, h=h, P=P: e.matmul(
                                out=P[0:64, :], lhsT=WIN[:, k, 64 * h:64 * h + 64], rhs=HH[:, k, :],
                                start=(k == 0), stop=(k == 7)), r=['WIN', hk], w=[pk])
                        evac(QST[:, :, h, :], P[0:64, :].rearrange("p (c q) -> p c q", q=128), pk, 'qst')
                    tr.dma('sp', 'qout', qTd.rearrange("p (c x) -> p c x", c=32)[:, 4 * tt:4 * tt + 4, :],
                           QST[:].rearrange("p c h q -> p c (h q)"), r=['qst'], w=['qTd'])
                    for (c0, dest, dn) in ((512, KCin, 'KCin'), (640, VCin, 'VCin'), (768, KE, 'KE'), (1024, KW, 'KW')):
                        for g in range(2):
                            P, pk = nextbank()
                            for k in range(8):
                                tr.op('pe', lambda e, k=k, P=P, cc=c0 + 64 * g: e.matmul(
                                    out=P[0:64, :], lhsT=WIN[:, k, cc:cc + 64], rhs=HH[:, k, :],
                                    start=(k == 0), stop=(k == 7)), r=['WIN', hk], w=[pk])
                            evac(dest[g][0:64, t0:t0 + 512], P[0:64, :], pk, '%s%d' % (dn, g))
                    for kt in range(4):
                        P, pk = nextbank()
                        for k in range(8):
                            tr.op('pe', lambda e, k=k, P=P, cc=1304 + 128 * kt: e.matmul(
                                out=P[:, :], lhsT=WIN[:, k, cc:cc + 128], rhs=HH[:, k, :],
                                start=(k == 0), stop=(k == 7)), r=['WIN', hk], w=[pk])
                        evac(UST[:, kt, :], P[:, :], pk, 'ust')
                    tr.dma('sp', 'uout', uTd.rearrange("p (k t) -> p k t", k=4)[:, :, t0:t0 + 512], UST[:],
                           r=['ust'], w=['uTd'])
                    for sbk in range(4):
                        blk = tt * 4 + sbk
                        P, pk = nextbank()
                        for k in range(8):
                            tr.op('pe', lambda e, k=k, P=P, sbk=sbk: e.matmul(
                                out=P[:, 0:408], lhsT=HH[:, k, sbk * 128:(sbk + 1) * 128], rhs=WIN[:, k, 896:1304],
                                start=(k == 0), stop=(k == 7)), r=['WIN', hk], w=[pk])
                        for g in range(2):
                            evac(VS1[g][:, blk, 0:64], P[:, 64 * g:64 * g + 64], pk, 'VS1%d' % g, force='dve')
                            evac(VW1[g][:, blk, 0:64], P[:, 256 + 64 * g:256 + 64 * g + 64], pk, 'VW1%d' % g, force='dve')
                        tr.op('dve', lambda e, P=P, blk=blk: e.tensor_tensor(out=GT[:, blk, :], in0=P[:, 384:408],
                                                                            in1=GB[:], op=ALU.add),
                              r=[pk, 'GB'], w=['GT'])
                        tr.op('act', lambda e, blk=blk: e.activation(out=GT[:, blk, :], in_=GT[:, blk, :],
                                                                    func=AF.Sigmoid), r=['GT'], w=['GT'])

                if stop < 1.8:
                    tr.barrier()
                    return nc
                HIDS = [psb("HIDS%d" % g, [128, 2, 256], BF16) for g in range(2)]
                for g in range(2):
                    tr.op('dve', lambda e, g=g: e.memset(HIDS[g][:], 0.0), w=['hid%d_0' % g, 'hid%d_1' % g])
                A, B_, C_ = GTMP
                for kind in range(2):
                    srcs = KCin if kind == 0 else VCin
                    sname = 'KCin' if kind == 0 else 'VCin'
                    for hh in range(2):
                        P, pk = nextbank()
                        for l in range(32):
                            tr.op('pe', lambda e, l=l, P=P, hh=hh: e.matmul(
                                out=P[:, 0:1], lhsT=W1[kind][:, l, hh * 128:(hh + 1) * 128],
                                rhs=POST[:, kind * 32 + l:kind * 32 + l + 1], start=(l == 0), stop=(l == 31)),
                                r=['cmpw'], w=[pk])
                        tr.op('dve', lambda e, P=P, hh=hh: e.tensor_tensor(
                            out=PBIAS[:], in0=P[:, 0:1], in1=CB1[:, kind * 2 + hh:kind * 2 + hh + 1], op=ALU.add),
                            r=[pk, 'cmpw'], w=['pbias'])
                        for g in range(2):
                            P2, pk2 = nextbank()
                            for l in range(32):
                                rhs = bass.AP(srcs[g].tensor if hasattr(srcs[g], 'tensor') else srcs[g], l,
                                              [[T, 64], [16, 255]])
                                tr.op('pe', lambda e, l=l, P2=P2, rhs=rhs, hh=hh: e.matmul(
                                    out=P2[:, 0:255], lhsT=W1[kind][:, l, hh * 128:(hh + 1) * 128], rhs=rhs,
                                    start=(l == 0), stop=(l == 31)), r=['cmpw', '%s%d' % (sname, g)], w=[pk2])
                            tr.op('dve', lambda e, P2=P2: e.tensor_scalar(out=A[:, 0:255], in0=P2[:, 0:255],
                                                                         scalar1=PBIAS[:, 0:1], scalar2=None,
                                                                         op0=ALU.add), r=[pk2, 'pbias'], w=['ga'])
                            tr.op('dve', lambda e: e.tensor_tensor(out=B_[:, 0:255], in0=A[:, 0:255], in1=A[:, 0:255],
                                                                  op=ALU.mult), r=['ga'], w=['gb'])
                            tr.op('dve', lambda e: e.tensor_scalar(out=B_[:, 0:255], in0=B_[:, 0:255], scalar1=0.044715,
                                                                  scalar2=1.0, op0=ALU.mult, op1=ALU.add),
                                  r=['gb'], w=['gb'])
                            tr.op('dve', lambda e: e.tensor_tensor(out=B_[:, 0:255], in0=B_[:, 0:255], in1=A[:, 0:255],
                                                                  op=ALU.mult), r=['gb', 'ga'], w=['gb'])
                            tr.op('act', lambda e: e.activation(out=C_[:, 0:255], in_=B_[:, 0:255], func=AF.Sigmoid,
                                                               scale=GELU_C), r=['gb'], w=['gc'])
                            tr.op('dve', lambda e, g=g, hh=hh: e.tensor_tensor(
                                out=HIDS[g][:, hh, 0:255], in0=A[:, 0:255], in1=C_[:, 0:255], op=ALU.mult),
                                r=['ga', 'gc'], w=['hid%d_%d' % (g, hh)])
                    for g in range(2):
                        hk2 = ['hid%d_0' % g, 'hid%d_1' % g]
                        if kind == 0:
                            P, pk = nextbank()
                            for hh in range(2):
                                tr.op('pe', lambda e, hh=hh, P=P, g=g: e.matmul(
                                    out=P[0:64, 0:256], lhsT=W2[0][:, hh, :], rhs=HIDS[g][:, hh, :],
                                    start=(hh == 0), stop=(hh == 1)), r=['cmpw'] + hk2, w=[pk])
                            evac(KC[g][:, :], P[0:64, 0:256], pk, 'KC%d' % g)
                        else:
                            for nt in range(2):
                                P, pk = nextbank()
                                for hh in range(2):
                                    tr.op('pe', lambda e, hh=hh, P=P, g=g, nt=nt: e.matmul(
                                        out=P[:, 0:64], lhsT=HIDS[g][:, hh, nt * 128:(nt + 1) * 128],
                                        rhs=W2[1][:, hh, :], start=(hh == 0), stop=(hh == 1)),
                                        r=['cmpw'] + hk2, w=[pk])
                                evac(VCX[g][:, nt, 0:64], P[:, 0:64], pk, 'VCX%d' % g)
            tr.barrier()
            if stop >= 3:
                attention_phase()
        if stop >= 4:
            s5_phase()
        with contextlib.ExitStack() as w2s:
            wts2 = ffn_weights(2, w2s, load=False) if stop >= 6 else None
            if stop >= 5:
                wout_phase((lambda: ffn_wload(2, wts2)) if stop >= 6 else None)
            if stop >= 6:
                ffn_phase(2, wts2)
    return nc


def _consts():
    f = np.float32
    c = {}
    c["c_ident"] = np.eye(128, dtype=f)
    j = np.arange(128)
    c["c_tri"] = (j[:, None] <= j[None, :]).astype(f)
    c["c_E0"] = (np.arange(T)[None, :] // 64 == np.arange(64)[:, None]).astype(f)
    cb = np.where(j[:, None] > j[None, :], -BIGM, 0.0).astype(f)
    wb = np.where(j[:, None] <= j[None, :], -BIGM, 0.0).astype(f)
    cm = np.where(16 * (j[:, None] - 64) > j[None, :] - 31, -BIGM, 0.0).astype(f)
    c["c_CB"] = np.tile(cb, (1, 4))
    c["c_WB"] = np.tile(wb, (1, 4))
    c["c_CM"] = np.tile(cm, (1, 4))
    c["c_BD"] = (np.arange(384)[None, :] == j[:, None] + 128).astype(f)
    hh = (j >= 64).astype(np.int64)[:, None]
    m = np.arange(128)[None, :]
    c["c_U0"] = np.where(m > 62 + hh, -1e30, 3e38).astype(f)
    l0 = np.full((128, 128), -3e38, dtype=f)
    l0[m == 62 + hh] = 2e30
    l0[m == 61 + hh] = 1e30
    c["c_L0"] = l0
    n = np.arange(256)[:, None]
    jj = np.arange(64)[None, :]
    ov = ((16 * n < 64 * jj + 64) & (16 * n + 32 > 64 * jj)).astype(f)
    ov[255] = 0
    c["c_ovl"] = np.ascontiguousarray(ov.reshape(2, 128, 64).transpose(1, 0, 2).reshape(128, 128))
    c["c_maskB"] = (np.arange(8)[None, :] == (j // 16)[:, None]).astype(f)
    gi = (j // 64)[:, None, None]
    p4 = np.arange(4)[None, :, None]
    g8 = np.arange(8)[None, None, :]
    c["c_maskC"] = (g8 == 2 * p4 + gi).astype(f).reshape(128, 32)
    c["c_iota"] = np.tile(np.arange(128, dtype=f)[None, :], (128, 1))
    c["c_jcol"] = np.arange(128, dtype=f)[:, None].copy()
    return c


def _prep_shared(inp):
    f = np.float32
    A = lambda a: np.ascontiguousarray(np.asarray(a, dtype=f))
    d = {}
    d["wg1"], d["wu1"], d["wd1"] = A(inp["ffn1_w_gate"][0]), A(inp["ffn1_w_up"][0]), A(inp["ffn1_w_down"][0])
    d["wg2"], d["wu2"], d["wd2"] = A(inp["ffn2_w_gate"][0]), A(inp["ffn2_w_up"][0]), A(inp["ffn2_w_down"][0])
    d["w_in"], d["w_out"], d["w_glu"] = A(inp["w_in"][0]), A(inp["w_out"][0]), A(inp["s5_w_glu"][0])
    col8 = lambda v: np.asarray(v, dtype=f).reshape(-1, 128).T
    d["gcols"] = A(np.concatenate([col8(inp["ffn1_norm"][0]), col8(inp["mix_norm"][0]), col8(inp["ffn2_norm"][0]),
                                   col8(inp["final_norm"]), col8(inp["ssm_out_norm"][0])], axis=1))
    d["grow_attn"] = A(np.asarray(inp["attn_out_norm"][0]).reshape(1, 512))
    d["gate_bias"] = A(np.asarray(inp["gate_bias"][0]).reshape(1, 24))
    d["cw1k"], d["cw1v"] = A(inp["cmp_k_w1"][0]), A(inp["cmp_v_w1"][0])
    d["cw2k"], d["cw2v"] = A(inp["cmp_k_w2"][0]), A(inp["cmp_v_w2"][0])
    d["cb1"] = A(np.concatenate([col8(inp["cmp_k_b1"][0]), col8(inp["cmp_v_b1"][0])], axis=1))
    d["posT"] = A(np.concatenate([np.asarray(inp["cmp_pos_k"][0]).T, np.asarray(inp["cmp_pos_v"][0]).T], axis=1))
    lr = np.asarray(inp["s5_lambda_re"][0], dtype=f)
    li = np.asarray(inp["s5_lambda_im"][0], dtype=f)
    ldt = np.repeat(np.asarray(inp["s5_log_dt"][0], dtype=f)[:, None], 64, axis=1)
    d["s5_rows"] = A(np.stack([lr.reshape(2048), li.reshape(2048), ldt.reshape(2048)]))
    colify = lambda a: a.reshape(16, 128).T
    d["s5_cols"] = A(np.concatenate([colify(lr), colify(li), colify(ldt)], axis=1))

    def rows_gh(a):
        return np.repeat(a.reshape(4, 8, 1, 64), 16, axis=2).transpose(1, 2, 0, 3).reshape(128, 4, 64)
    d["s5_rep"] = A(np.stack([rows_gh(lr), rows_gh(li), rows_gh(ldt)], axis=2).reshape(128, 4 * 3 * 64))

    def bt(a):
        return np.asarray(a, dtype=f).reshape(4, 8, 64, 16).transpose(1, 3, 0, 2).reshape(128, 4, 64)
    d["s5_bT"] = A(np.stack([bt(inp["s5_b_re"][0]), bt(inp["s5_b_im"][0])], axis=2).reshape(128, 4 * 2 * 64))

    def ct(a):
        return np.asarray(a, dtype=f).reshape(16, 2, 16, 64).transpose(1, 3, 0, 2).reshape(128, 16, 16)
    d["s5_c"] = A(np.stack([ct(inp["s5_c_re"][0]), ct(inp["s5_c_im"][0])], axis=2).reshape(128, 16 * 2 * 16))
    d["s5_dcol"] = A(np.asarray(inp["s5_d"][0], dtype=f).reshape(4, 128).T)
    d["bglu_col"] = A(np.asarray(inp["s5_b_glu"][0], dtype=f).reshape(4, 128).T)
    d.update(_consts())
    return d


def kernel(**inputs):
    x = np.asarray(inputs["x"], dtype=np.float32)
    shared = _prep_shared(inputs)
    nc = build(dbg=False)
    in_maps = []
    for b in range(NCORES):
        m = dict(shared)
        m["xT"] = np.ascontiguousarray(x[b].T)
        in_maps.append(m)
    res = run_bass_kernel_spmd(nc, in_maps, core_ids=list(range(NCORES)))
    out = np.empty((NCORES, T, D), dtype=np.float32)
    for b in range(NCORES):
        out[b] = np.asarray(res.results[b]["yT"], dtype=np.float32).T
    return out
```

```python
import contextlib
import numpy as np
import concourse.bass as bass
import concourse.mybir as mybir
from concourse.bass_utils import run_bass_kernel_spmd

F32 = mybir.dt.float32
BF16 = mybir.dt.bfloat16
ALU = mybir.AluOpType
AF = mybir.ActivationFunctionType

T = 4096
D = 1024
FF = 2816
NF = FF // 128
EPS = 1e-6
BIGM = 30000.0
NCORES = 8
GELU_C = 1.5957691216057308


class TR:
    def __init__(self, nc, es):
        self.nc, self.es = nc, es
        self.eng = dict(pe=nc.tensor, dve=nc.vector, act=nc.scalar, pool=nc.gpsimd, sp=nc.sync)
        self.sem, self.cnt = {}, {}
        self.seen = {e: {} for e in self.eng}
        for e in self.eng:
            self.sem[e] = es.enter_context(nc.semaphore("s_" + e))
            self.cnt[e] = 0
        self.bs = {}
        self.defer = None

    def _st(self, k):
        s = self.bs.get(k)
        if s is None:
            s = self.bs[k] = ({}, {})
        return s

    def _deps(self, r, w):
        d = {}
        for k in r:
            for sk, v in self._st(k)[0].items():
                if d.get(sk, 0) < v:
                    d[sk] = v
        for k in w:
            s = self._st(k)
            for dd in s:
                for sk, v in dd.items():
                    if d.get(sk, 0) < v:
                        d[sk] = v
        return d

    def _wait(self, e, d):
        for k, v in d.items():
            if k == e and e == 'pe':
                continue
            if self.seen[e].get(k, 0) >= v:
                continue
            self.eng[e].wait_ge(self.sem[k], v)
            self.seen[e][k] = v

    def _mark(self, src, v, r, w):
        for k in r:
            self._st(k)[1][src] = v
        for k in w:
            self._st(k)[0][src] = v

    def op(self, e, fn, r=(), w=()):
        if self.defer is not None:
            r, w = list(r), list(w)
            self.defer.append(lambda: self.op(e, fn, r, w))
            return
        self._wait(e, self._deps(r, w))
        ins = fn(self.eng[e])
        self.cnt[e] += 1
        ins.then_inc(self.sem[e], 1)
        self._mark(e, self.cnt[e], r, w)

    def dma(self, q, ch, out, in_, r=(), w=()):
        if self.defer is not None:
            r, w = list(r), list(w)
            self.defer.append(lambda: self.dma(q, ch, out, in_, r, w))
            return
        if ch not in self.sem:
            self.sem[ch] = self.es.enter_context(self.nc.semaphore("c_" + ch))
            self.cnt[ch] = 0
        self._wait(q, self._deps(r, w))
        ins = self.eng[q].dma_start(out=out, in_=in_)
        self.cnt[ch] += 16
        ins.then_inc(self.sem[ch], 16)
        self._mark(ch, self.cnt[ch], r, w)

    def barrier(self):
        allk = {k: v for k, v in self.cnt.items() if v > 0}
        for e in self.eng:
            self._wait(e, allk)


def build(dbg=False, stop=9):
    nc = bass.Bass("TRN2", target_bir_lowering=False)
    es = contextlib.ExitStack()
    with es:
        tr = TR(nc, es)

        def din(name, shape):
            return nc.dram_tensor(name, list(shape), F32, kind="ExternalInput").ap()

        def dscr(name, shape, dt):
            kind = "ExternalOutput" if dbg else "Internal"
            return nc.dram_tensor(name, list(shape), dt, kind=kind).ap()

        def sb(name, shape, dt):
            return es.enter_context(nc.sbuf_tensor(name, list(shape), dt))

        xT = din("xT", [D, T])
        wgs = [din("wg1", [D, FF]), din("wg2", [D, FF])]
        wus = [din("wu1", [D, FF]), din("wu2", [D, FF])]
        wds = [din("wd1", [FF, D]), din("wd2", [FF, D])]
        w_in = din("w_in", [D, 1816])
        w_out = din("w_out", [D, D])
        w_glu = din("w_glu", [512, 512])
        gcols = din("gcols", [128, 36])
        grow_attn = din("grow_attn", [1, 512])
        gate_bias = din("gate_bias", [1, 24])
        cw1 = [din("cw1k", [2048, 256]), din("cw1v", [2048, 256])]
        cw2 = [din("cw2k", [256, 64]), din("cw2v", [256, 64])]
        cb1 = din("cb1", [128, 4])
        posT = din("posT", [64, 64])
        s5_rows = din("s5_rows", [3, 2048])
        s5_cols = din("s5_cols", [128, 48])
        s5_rep = din("s5_rep", [128, 4 * 3 * 64])
        s5_bT = din("s5_bT", [128, 4 * 2 * 64])
        s5_c = din("s5_c", [128, 16 * 2 * 16])
        s5_dcol = din("s5_dcol", [128, 4])
        bglu_col = din("bglu_col", [128, 4])
        c_ident = din("c_ident", [128, 128])
        c_tri = din("c_tri", [128, 128])
        c_E0 = din("c_E0", [64, T])
        c_CB = din("c_CB", [128, 512])
        c_WB = din("c_WB", [128, 512])
        c_CM = din("c_CM", [128, 512])
        c_BD = din("c_BD", [128, 384])
        c_U0 = din("c_U0", [128, 128])
        c_L0 = din("c_L0", [128, 128])
        c_ovl = din("c_ovl", [128, 128])
        c_maskB = din("c_maskB", [128, 8])
        c_maskC = din("c_maskC", [128, 32])
        c_iota = din("c_iota", [128, 128])
        c_jcol = din("c_jcol", [128, 1])

        yT = nc.dram_tensor("yT", [D, T], F32, kind="ExternalOutput").ap()
        x1T = dscr("x1T", [D, T], F32)
        h2T = dscr("h2T", [D, T], BF16)
        qTd = dscr("qTd", [64, 32 * 8 * 128], BF16)
        uTd = dscr("uTd", [128, 4 * T], BF16)
        mixT = dscr("mixT", [D, T], BF16)

        GC = sb("GC", [128, 36], F32)
        ONES = sb("ONES", [128, 128], BF16)
        IDN = sb("IDN", [128, 128], BF16)
        EPSC = sb("EPSC", [128, 1], F32)
        tr.dma('sp', 'const', GC[:], gcols[:, :], w=['c'])
        tr.dma('pool', 'const', IDN[:], c_ident[:, :], w=['c'])
        tr.op('dve', lambda e: e.memset(ONES[:], 1.0), w=['c'])
        tr.op('dve', lambda e: e.memset(EPSC[:], 0.0), w=['c'])

        pb = [es.enter_context(nc.psum_tensor("pb%d" % i, [128, 512], F32)) for i in range(8)]

        def xview(dram, t0, n):
            return dram.rearrange("(k p) t -> p k t", p=128)[:, :, t0:t0 + n]

        def ffn_weights(ph, stk, load=True):
            WG = stk.enter_context(nc.sbuf_tensor("WG_%d" % ph, [128, 8, FF], BF16))
            WU = stk.enter_context(nc.sbuf_tensor("WU_%d" % ph, [128, 8, FF], BF16))
            WD = stk.enter_context(nc.sbuf_tensor("WD_%d" % ph, [128, NF, D], BF16))
            if load:
                ffn_wload(ph, (WG, WU, WD))
            return WG, WU, WD

        def ffn_wload(ph, wts):
            WG, WU, WD = wts
            wgd, wud, wdd = wgs[ph - 1], wus[ph - 1], wds[ph - 1]
            HF = FF // 2
            for hf, sfx in ((0, 'a'), (1, 'b')):
                for k in range(8):
                    tr.dma('pool', 'wg' + sfx, WG[:, k, hf * HF:(hf + 1) * HF],
                           wgd[k * 128:(k + 1) * 128, hf * HF:(hf + 1) * HF], w=['WG' + sfx])
                    tr.dma('pool', 'wu' + sfx, WU[:, k, hf * HF:(hf + 1) * HF],
                           wud[k * 128:(k + 1) * 128, hf * HF:(hf + 1) * HF], w=['WU' + sfx])
            for f in range(NF):
                tr.dma('pool', 'wd', WD[:, f, :], wdd[f * 128:(f + 1) * 128, :], w=['WD'])

        def ffn_phase(ph, wts=None):
            with contextlib.ExitStack() as fs:
                def fsb(name, shape, dt):
                    return fs.enter_context(nc.sbuf_tensor("%s_%d" % (name, ph), list(shape), dt))
                WG, WU, WD = wts if wts is not None else ffn_weights(ph, fs)
                XT = [fsb("XT%d" % i, [128, 8, 512], F32) for i in range(2)]
                H = fsb("H", [128, 8, 512], BF16)
                AT = fsb("AT", [128, NF, 512], BF16)
                SG = [fsb("SG%d" % i, [128, 512], BF16) for i in range(2)]
                RS = [fsb("RS%d" % i, [128, 512], F32) for i in range(2)]
                src = xT if ph == 1 else x1T
                g0 = 0 if ph == 1 else 16
                hkeys = ['h%d' % k for k in range(8)]
                atk2 = ['at%d' % f for f in range(14, 22)]

                def xs(tt):
                    return XT[tt % 2], 'xt%d' % (tt % 2)

                def load(tt):
                    X, xk = xs(tt)
                    tr.dma('sp', 'xin%d' % (tt % 2), X[:], xview(src, tt * 512, 512), w=[xk])

                def norm_a(X, xk, HB, hk):
                    tr.op('act', lambda e: e.activation(out=HB, in_=X[:], func=AF.Square), r=[xk], w=hk)

                def norm_b(HBk, hk, PN, pnk):
                    for k in range(8):
                        tr.op('pe', lambda e, k=k: e.matmul(out=PN[:], lhsT=ONES[:], rhs=HBk(k),
                                                           start=(k == 0), stop=(k == 7)), r=[hk[k], 'c'], w=[pnk])

                def norm_c(X, xk, HBk, hk, PN, pnk, R, rk, goff, inplace=False):
                    tr.op('dve', lambda e: e.tensor_scalar(out=R[:], in0=PN[:], scalar1=1.0 / D, scalar2=EPS,
                                                          op0=ALU.mult, op1=ALU.add), r=[pnk], w=[rk])
                    tr.op('act', lambda e: e.activation(out=R[:], in_=R[:], func=AF.Sqrt), r=[rk], w=[rk])
                    tr.op('dve', lambda e: e.reciprocal(out=R[:], in_=R[:]), r=[rk], w=[rk])
                    for k in range(8):
                        dst = X[:, k, :] if inplace else HBk(k)
                        tr.op('dve', lambda e, k=k, dst=dst: e.scalar_tensor_tensor(
                            out=dst, in0=X[:, k, :], scalar=GC[:, goff + k:goff + k + 1], in1=R[:],
                            op0=ALU.mult, op1=ALU.mult), r=[xk, rk, 'c'], w=[xk if inplace else hk[k]])

                Hk = lambda k: H[:, k, :]
                A2k = lambda k: AT[:, 14 + k, :]

                def post_a(tt):
                    X, xk = xs(tt)
                    if ph == 1:
                        tr.dma('sp', 'xout%d' % (tt % 2), xview(x1T, tt * 512, 512), X[:], r=[xk], w=['x1T'])
                    norm_a(X, xk, AT[:, 14:22, :], atk2)

                def post_bc(tt):
                    X, xk = xs(tt)
                    norm_b(A2k, atk2, pb[7], 'pn2')
                    if ph == 1:
                        norm_c(X, xk, A2k, atk2, pb[7], 'pn2', RS[1], 'rs1', 8)
                        tr.dma('sp', 'hout', xview(h2T, tt * 512, 512), AT[:, 14:22, :], r=atk2, w=['h2T'])
                    else:
                        norm_c(X, xk, A2k, atk2, pb[7], 'pn2', RS[1], 'rs1', 24, inplace=True)
                        tr.dma('sp', 'yout%d' % (tt % 2), xview(yT, tt * 512, 512), X[:], r=[xk], w=['yT'])

                load(0)
                X0, xk0 = xs(0)
                norm_a(X0, xk0, H[:], hkeys)
                norm_b(Hk, hkeys, pb[6], 'pn')
                norm_c(X0, xk0, Hk, hkeys, pb[6], 'pn', RS[0], 'rs0', g0)
                for tt in range(8):
                    X, xk = xs(tt)
                    for f in range(NF):
                        b = f % 2
                        for k in range(8):
                            tr.op('pe', lambda e, k=k, f=f, b=b: e.matmul(
                                out=pb[b][:], lhsT=WG[:, k, f * 128:(f + 1) * 128], rhs=H[:, k, :],
                                start=(k == 0), stop=(k == 7)), r=['WG' + ('a' if f < NF // 2 else 'b'), hkeys[k]],
                                w=['pg%d' % b])
                        for k in range(8):
                            tr.op('pe', lambda e, k=k, f=f, b=b: e.matmul(
                                out=pb[2 + b][:], lhsT=WU[:, k, f * 128:(f + 1) * 128], rhs=H[:, k, :],
                                start=(k == 0), stop=(k == 7)), r=['WU' + ('a' if f < NF // 2 else 'b'), hkeys[k]],
                                w=['pu%d' % b])
                        tr.op('act', lambda e, b=b: e.activation(out=SG[b][:], in_=pb[b][:], func=AF.Silu),
                              r=['pg%d' % b], w=['sg%d' % b])
                        tr.op('dve', lambda e, b=b, f=f: e.tensor_tensor(out=AT[:, f, :], in0=SG[b][:],
                                                                         in1=pb[2 + b][:], op=ALU.mult),
                              r=['sg%d' % b, 'pu%d' % b], w=['at%d' % f])
                        if f == 2 and tt > 0:
                            post_bc(tt - 1)
                    if tt + 1 < 8:
                        load(tt + 1)
                        Xn, xkn = xs(tt + 1)
                        norm_a(Xn, xkn, H[:], hkeys)
                    for dk in range(8):
                        b = dk % 2
                        for f in range(NF):
                            tr.op('pe', lambda e, f=f, dk=dk, b=b: e.matmul(
                                out=pb[4 + b][:], lhsT=WD[:, f, dk * 128:(dk + 1) * 128], rhs=AT[:, f, :],
                                start=(f == 0), stop=(f == NF - 1)), r=['WD', 'at%d' % f], w=['pd%d' % b])
                        tr.op('dve', lambda e, dk=dk, b=b, X=X: e.scalar_tensor_tensor(
                            out=X[:, dk, :], in0=pb[4 + b][:], scalar=0.5, in1=X[:, dk, :],
                            op0=ALU.mult, op1=ALU.add), r=['pd%d' % b, xk], w=[xk])
                        if dk == 2 and tt + 1 < 8:
                            norm_b(Hk, hkeys, pb[6], 'pn')
                            norm_c(Xn, xkn, Hk, hkeys, pb[6], 'pn', RS[0], 'rs0', g0)
                    post_a(tt)
                post_bc(7)
            tr.barrier()

        def attention_phase():
            with contextlib.ExitStack() as a_:
                def asb(name, shape, dt):
                    return a_.enter_context(nc.sbuf_tensor(name, list(shape), dt))
                CBt = asb("CBt", [128, 512], BF16)
                WBt = asb("WBt", [128, 512], BF16)
                CMt = asb("CMt", [128, 512], BF16)
                BDt = asb("BDt", [128, 384], BF16)
                U0t = asb("U0t", [128, 128], F32)
                L0t = asb("L0t", [128, 128], F32)
                GAT = asb("GAT", [128, 512], F32)
                QN = [asb("QN%d" % i, [128, 512], BF16) for i in range(2)]
                ET = [asb("ET%d" % i, [128, 512], BF16) for i in range(3)]
                SCO = asb("SCO", [128, 64], F32)
                M8 = asb("M8", [128, 16], F32)
                WK = asb("WK", [128, 64], F32)
                SELW = asb("SELW", [128, 128], BF16)
                RZ = asb("RZ", [128, 4], F32)
                DEN = asb("DEN", [128, 12], F32)
                ATT = asb("ATT", [128, 512], F32)
                ATN = asb("ATN", [128, 512], BF16)
                JUNK = asb("JUNK", [128, 512], F32)
                SSQ = asb("SSQ", [128, 1], F32)
                MST = asb("MST", [128, 4, 128], BF16)
                tr.dma('pool', 'aconst', CBt[:], c_CB[:, :], w=['ac'])
                tr.dma('pool', 'aconst', WBt[:], c_WB[:, :], w=['ac'])
                tr.dma('pool', 'aconst', CMt[:], c_CM[:, :], w=['ac'])
                tr.dma('pool', 'aconst', BDt[:], c_BD[:, :], w=['ac'])
                tr.dma('sp', 'aconst', U0t[:], c_U0[:, :], w=['ac'])
                tr.dma('sp', 'aconst', L0t[:], c_L0[:, :], w=['ac'])
                tr.dma('sp', 'aconst', GAT[:], grow_attn.partition_broadcast(128), w=['ac'])
                tr.op('dve', lambda e: e.memset(SELW[:], 0.0), w=['selw'])
                OS = pb[3][:, 0:260].rearrange("p (r x) -> p r x", x=65)
                OW = pb[4][:, 0:260].rearrange("p (r x) -> p r x", x=65)
                OC = pb[5][:, 0:260].rearrange("p (r x) -> p r x", x=65)
                IM = pb[6][:, 0:256].rearrange("p (r x) -> p r x", x=64)
                PT = pb[5][:, 320:384].bitcast(BF16)
                PT4 = pb[6][:, 256:512].bitcast(BF16)
                PA = pb[7]
                ETA = [asb("ETA%d" % i, [128, 512], BF16) for i in range(2)]
                dq = []

                def pump(k):
                    for _ in range(min(k, len(dq))):
                        dq.pop(0)()

                def deferred(f, *a):
                    tr.defer = dq
                    f(*a)
                    tr.defer = None
                sring = [0]
                ering = [0]

                def nexts():
                    b = sring[0] % 3
                    sring[0] += 1
                    return pb[b], 'ps%d' % b

                def nexte():
                    b = ering[0] % 3
                    ering[0] += 1
                    return ET[b], 'et%d' % b

                qview = qTd.rearrange("p (c x) -> p c x", c=32)
                QN3 = [QN[0], QN[1], asb("QN2", [128, 512], BF16)]
                OCS = [asb("OCS%d" % i, [128, 4, 65], F32) for i in range(2)]
                OSS = [asb("OSS%d" % i, [128, 4, 65], F32) for i in range(2)]
                OWS = [asb("OWS%d" % i, [128, 4, 65], F32) for i in range(2)]
                ATN2 = [ATN, asb("ATN1", [128, 512], BF16)]
                NIT = 64
                mixv = mixT.rearrange("(k p) t -> p k t", p=128)
                pending = []

                def qload(it):
                    c, g = divmod(it, 2)
                    sl = it % 3
                    tr.dma('sp', 'qin%d' % sl, QN3[sl][0:64, :], qview[:, c, g * 512:(g + 1) * 512], r=['qTd'],
                           w=['qnq%d' % sl])

                def stageA(it):
                    c, g = divmod(it, 2)
                    sl = it % 3
                    Q = QN3[sl]
                    qq, qs = 'qnq%d' % sl, 'qns%d' % sl
                    nts = 1 if c < 16 else 2
                    tl = []
                    for nt in range(nts):
                        Mn = min(128, 8 * c + 7 - 128 * nt)
                        m = 8 * c - 128 * nt
                        need = m <= 192
                        PS, psk = PA, 'pa'
                        tr.op('pe', lambda e, PS=PS, Mn=Mn, nt=nt, need=need: e.matmul(
                            out=PS[0:Mn, :], lhsT=KC[g][:, 128 * nt:128 * nt + Mn], rhs=Q[0:64, :],
                            start=True, stop=not need), r=['KC%d' % g, qq], w=[psk])
                        if need:
                            tr.op('pe', lambda e, PS=PS, Mn=Mn, m=m: e.matmul(
                                out=PS[0:Mn, :], lhsT=BDt[:, 192 - m:192 - m + Mn], rhs=CMt[:],
                                start=False, stop=True), r=['ac'], w=[psk])
                        Et, ek = ETA[nt], 'eta%d' % nt
                        tr.op('act', lambda e, PS=PS, Et=Et, Mn=Mn: e.activation(
                            out=Et[0:Mn, :], in_=PS[0:Mn, :], func=AF.Exp, scale=0.125), r=[psk], w=[ek])
                        tl.append((nt, Mn, Et, ek))
                    first = True
                    for (nt, Mn, Et, ek) in tl:
                        for r in range(4):
                            tr.op('pe', lambda e, Et=Et, Mn=Mn, nt=nt, r=r, first=first: e.matmul(
                                out=OC[:, r, :], lhsT=Et[0:Mn, r * 128:(r + 1) * 128], rhs=VCX[g][0:Mn, nt, 0:65],
                                start=first, stop=(nt == nts - 1), skip_group_check=True),
                                r=[ek, 'VCX%d' % g], w=['oc'])
                            tr.op('pe', lambda e, Et=Et, Mn=Mn, nt=nt, r=r, first=first: e.matmul(
                                out=IM[:, r, :], lhsT=Et[0:Mn, r * 128:(r + 1) * 128], rhs=VCX[g][0:Mn, nt, 65:129],
                                start=first, stop=(nt == nts - 1), skip_group_check=True),
                                r=[ek, 'VCX%d' % g], w=['im'])
                            first = False
                    OCb, ock = OCS[it % 2], 'ocs%d' % (it % 2)
                    tr.op('dve', lambda e, OCb=OCb: e.tensor_copy(out=OCb[:], in_=OC), r=['oc'], w=[ock])
                    tr.op('dve', lambda e, OCb=OCb: e.tensor_scalar(out=RZ[:], in0=OCb[:, :, 64], scalar1=1e-30,
                                                                   scalar2=None, op0=ALU.max), r=[ock], w=['rz'])
                    tr.op('dve', lambda e: e.reciprocal(out=RZ[:], in_=RZ[:]), r=['rz'], w=['rz'])
                    tr.op('dve', lambda e: e.tensor_scalar(out=SCO[:], in0=IM[:, 0, :], scalar1=RZ[:, 0:1],
                                                          scalar2=None, op0=ALU.mult), r=['im', 'rz'], w=['sco'])
                    for r in range(1, 4):
                        tr.op('dve', lambda e, r=r: e.scalar_tensor_tensor(
                            out=SCO[:], in0=IM[:, r, :], scalar=RZ[:, r:r + 1], in1=SCO[:],
                            op0=ALU.mult, op1=ALU.add), r=['im', 'rz', 'sco'], w=['sco'])
                    lo = 62 - 2 * c
                    tr.op('dve', lambda e, lo=lo: e.tensor_tensor(out=SCO[:], in0=SCO[:], in1=L0t[:, lo:lo + 64],
                                                                 op=ALU.max), r=['sco', 'ac'], w=['sco'])
                    tr.op('dve', lambda e, lo=lo: e.tensor_tensor(out=SCO[:], in0=SCO[:], in1=U0t[:, lo:lo + 64],
                                                                 op=ALU.min), r=['sco', 'ac'], w=['sco'])
                    tr.op('dve', lambda e: e.memset(SCO[:, 0:1], 3e30), r=['sco'], w=['sco'])
                    tr.op('dve', lambda e: e.max(out=M8[:, 0:8], in_=SCO[:]), r=['sco'], w=['m8'])
                    tr.op('dve', lambda e: e.match_replace(out=WK[:], in_to_replace=M8[:, 0:8], in_values=SCO[:],
                                                          imm_value=-3e38), r=['sco', 'm8'], w=['wk'])
                    tr.op('dve', lambda e: e.max(out=M8[:, 8:16], in_=WK[:]), r=['wk'], w=['m8'])
                    tr.op('dve', lambda e: e.tensor_scalar(out=SELW[:, 64:128], in0=SCO[:], scalar1=M8[:, 15:16],
                                                          scalar2=None, op0=ALU.is_ge), r=['sco', 'm8'], w=['selw'])
                    tr.op('pe', lambda e: e.transpose(out=PT, in_=SELW[:], identity=IDN[:]), r=['selw', 'c'], w=['pt'])
                    tr.op('dve', lambda e, Q=Q: e.tensor_scalar(
                        out=Q[64:128, :].rearrange("p (r q) -> p r q", r=4),
                        in0=PT[64:128, :].unsqueeze(1).to_broadcast([64, 4, 128]),
                        scalar1=-1.0, scalar2=BIGM, op0=ALU.add, op1=ALU.mult), r=['pt'], w=[qs])

                def finish_pe(c):
                    AN = ATN2[c % 2]
                    for j in range(4):
                        tr.op('pe', lambda e, j=j, AN=AN: e.transpose(out=PT4[:, j * 128:(j + 1) * 128],
                                                                     in_=AN[:, j * 128:(j + 1) * 128], identity=IDN[:]),
                              r=['atn%d' % (c % 2), 'c'], w=['pt4'])
                    tr.op('dve', lambda e: e.tensor_copy(out=MST[:].rearrange("p a b -> p (a b)"), in_=PT4),
                          r=['pt4'], w=['mst'])
                    tr.dma('sp', 'mout', mixv[:, 0:4, c * 128:(c + 1) * 128], MST[:], r=['mst'], w=['mixT'])

                def stageB(it):
                    c, g = divmod(it, 2)
                    sl = it % 3
                    Q = QN3[sl]
                    qq, qs = 'qnq%d' % sl, 'qns%d' % sl
                    k0 = max(0, c - 4)
                    tiles = [('w', kt) for kt in range(k0, c + 1)] + [('s', kt) for kt in range(c + 1)]
                    n = len(tiles)
                    info = {}

                    def qk(i):
                        br, kt = tiles[i]
                        PS, psk = nexts()
                        extra = []
                        if kt == c:
                            extra.append(CBt)
                        if br == 'w' and kt == c - 4:
                            extra.append(WBt)
                        if br == 'w':
                            tr.op('pe', lambda e, PS=PS, kt=kt, ne=len(extra): e.matmul(
                                out=PS[:], lhsT=KW[g][:, kt * 128:(kt + 1) * 128], rhs=Q[0:64, :],
                                start=True, stop=(ne == 0)), r=['KW%d' % g, qq], w=[psk])
                        else:
                            tr.op('pe', lambda e, PS=PS, kt=kt, ne=len(extra): e.matmul(
                                out=PS[:], lhsT=KE[g][:, kt * 128:(kt + 1) * 128], rhs=Q[:, :],
                                start=True, stop=(ne == 0)), r=['KE%d' % g, qq, qs], w=[psk])
                        for j, bt in enumerate(extra):
                            tr.op('pe', lambda e, PS=PS, bt=bt, last=(j == len(extra) - 1): e.matmul(
                                out=PS[:], lhsT=IDN[:], rhs=bt[:], start=False, stop=last), r=['ac', 'c'], w=[psk])
                        Et, ek = nexte()
                        tr.op('act', lambda e, PS=PS, Et=Et: e.activation(
                            out=Et[:], in_=PS[:], func=AF.Exp, scale=0.125), r=[psk], w=[ek])
                        info[i] = (Et, ek)

                    started = {'w': False, 's': False}
                    lastw = max(i for i in range(n) if tiles[i][0] == 'w')

                    def pv(i):
                        br, kt = tiles[i]
                        Et, ek = info[i]
                        O, ok, V, vk = (OW, 'ow', VW1[g], 'VW1%d' % g) if br == 'w' else (OS, 'os', VS1[g], 'VS1%d' % g)
                        last = (i == lastw) if br == 'w' else (i == n - 1)
                        for r in range(4):
                            st = not started[br]
                            started[br] = True
                            tr.op('pe', lambda e, Et=Et, kt=kt, r=r, O=O, V=V, st=st, last=last: e.matmul(
                                out=O[:, r, :], lhsT=Et[:, r * 128:(r + 1) * 128], rhs=V[:, kt, :],
                                start=st, stop=last, skip_group_check=True), r=[ek, vk], w=[ok])

                    for i in range(min(2, n)):
                        qk(i)
                    for i in range(n):
                        if i + 2 < n:
                            qk(i + 2)
                        pv(i)
                        left = max(1, n - 1 - i)
                        pump((len(dq) + left - 1) // left)
                    pump(len(dq))
                    b2 = it % 2
                    tr.op('dve', lambda e: e.tensor_copy(out=OWS[b2][:], in_=OW), r=['ow'], w=['ows%d' % b2])
                    tr.op('dve', lambda e: e.tensor_copy(out=OSS[b2][:], in_=OS), r=['os'], w=['oss%d' % b2])

                def tailB(it):
                    c, g = divmod(it, 2)
                    b2 = it % 2
                    srcs = ((OCS[b2], 'ocs%d' % b2), (OSS[b2], 'oss%d' % b2), (OWS[b2], 'ows%d' % b2))
                    for bi, (O, ok) in enumerate(srcs):
                        tr.op('dve', lambda e, O=O, bi=bi: e.tensor_scalar(
                            out=DEN[:, bi * 4:bi * 4 + 4], in0=O[:, :, 64], scalar1=1e-30, scalar2=None,
                            op0=ALU.max), r=[ok], w=['den'])
                    tr.op('dve', lambda e: e.reciprocal(out=DEN[:], in_=DEN[:]), r=['den'], w=['den'])
                    gv = GT[:, c, 12 * g:12 * g + 12].rearrange("p (r b) -> p b r", b=3)
                    tr.op('dve', lambda e, gv=gv: e.tensor_tensor(
                        out=DEN[:].rearrange("p (b r) -> p b r", b=3), in0=DEN[:].rearrange("p (b r) -> p b r", b=3),
                        in1=gv, op=ALU.mult), r=['den', 'GT'], w=['den'])
                    for r in range(4):
                        dst = ATT[:, (4 * g + r) * 64:(4 * g + r + 1) * 64]
                        tr.op('dve', lambda e, r=r, dst=dst: e.tensor_scalar(
                            out=dst, in0=OCS[b2][:, r, 0:64], scalar1=DEN[:, r:r + 1], scalar2=None, op0=ALU.mult),
                            r=['ocs%d' % b2, 'den'], w=['att'])
                        tr.op('dve', lambda e, r=r, dst=dst: e.scalar_tensor_tensor(
                            out=dst, in0=OSS[b2][:, r, 0:64], scalar=DEN[:, 4 + r:5 + r], in1=dst,
                            op0=ALU.mult, op1=ALU.add), r=['oss%d' % b2, 'den', 'att'], w=['att'])
                        tr.op('dve', lambda e, r=r, dst=dst: e.scalar_tensor_tensor(
                            out=dst, in0=OWS[b2][:, r, 0:64], scalar=DEN[:, 8 + r:9 + r], in1=dst,
                            op0=ALU.mult, op1=ALU.add), r=['ows%d' % b2, 'den', 'att'], w=['att'])
                    if g == 1:
                        AN, ank = ATN2[c % 2], 'atn%d' % (c % 2)
                        tr.op('act', lambda e: e.activation(out=JUNK[:], in_=ATT[:], func=AF.Square, accum_out=SSQ[:]),
                              r=['att'], w=['junk', 'ssq'])
                        tr.op('dve', lambda e: e.tensor_scalar(out=SSQ[:], in0=SSQ[:], scalar1=1.0 / 512, scalar2=EPS,
                                                              op0=ALU.mult, op1=ALU.add), r=['ssq'], w=['ssq'])
                        tr.op('act', lambda e: e.activation(out=SSQ[:], in_=SSQ[:], func=AF.Ln), r=['ssq'], w=['ssq'])
                        tr.op('act', lambda e: e.activation(out=SSQ[:], in_=SSQ[:], func=AF.Exp, scale=-0.5),
                              r=['ssq'], w=['ssq'])
                        tr.op('dve', lambda e, AN=AN: e.scalar_tensor_tensor(out=AN[:], in0=ATT[:], scalar=SSQ[:, 0:1],
                                                                            in1=GAT[:], op0=ALU.mult, op1=ALU.mult),
                              r=['att', 'ssq', 'ac'], w=[ank])

                qload(0)
                qload(1)
                stageA(0)
                for it in range(NIT):
                    if it >= 1:
                        deferred(tailB, it - 1)
                    if it + 1 < NIT:
                        deferred(stageA, it + 1)
                    if it >= 1 and (it - 1) % 2 == 1:
                        deferred(finish_pe, (it - 1) // 2)
                    if it + 2 < NIT:
                        qload(it + 2)
                    stageB(it)
                tailB(NIT - 1)
                finish_pe((NIT - 1) // 2)
            tr.barrier()

        def s5_phase():
            PI = float(np.pi)
            with contextlib.ExitStack() as s_:
                def ssb(name, shape, dt):
                    return s_.enter_context(nc.sbuf_tensor(name, list(shape), dt))
                ROW = [ssb("ROW%d" % i, [128, 2048], F32) for i in range(3)]
                PRE_RE = ssb("PRE_RE", [128, 2048], F32)
                PRE_IM = ssb("PRE_IM", [128, 2048], F32)
                TH = ssb("TH", [128, 2048], F32)
                TMPI = ssb("TMPI", [128, 2048], mybir.dt.int32)
                JC = ssb("JC", [128, 1], F32)
                NJC = ssb("NJC", [128, 1], F32)
                NPI = ssb("NPI", [128, 1], F32)
                COL = ssb("COL", [128, 48], F32)
                IOT = ssb("IOT", [128, 128], F32)
                POST_RE = ssb("POST_RE", [128, 16, 128], F32)
                POST_IM = ssb("POST_IM", [128, 16, 128], F32)
                A128 = ssb("A128", [128, 2, 16], F32)
                SM = [ssb("SM%d" % i, [128, 16], F32) for i in range(3)]
                REP = ssb("REP", [128, 4, 3, 64], F32)
                BT = ssb("BT", [128, 4, 2, 64], F32)
                BB = ssb("BB", [128, 4, 2, 64], F32)
                RT = [ssb("RT%d" % i, [128, 4, 64], F32) for i in range(6)]
                BBD = ssb("BBD", [128, 4, 1024], BF16)
                MASKB = ssb("MASKB", [128, 8], F32)
                CC = ssb("CC", [128, 16, 2, 16], F32)
                MASKC = ssb("MASKC", [128, 4, 8], F32)
                CMAT = ssb("CMAT", [128, 16, 2, 128], BF16)
                DCOL = ssb("DCOL", [128, 4], F32)
                BGL = ssb("BGL", [128, 4], F32)
                WGL = ssb("WGL", [128, 4, 512], BF16)
                TRI = ssb("TRI", [128, 128], BF16)
                UT = [ssb("UT%d" % i, [128, 4, 512], BF16) for i in range(2)]
                T1B = ssb("T1B", [128, 8, 512], BF16)
                T2B = ssb("T2B", [128, 8, 512], BF16)
                NTRI = ssb("NTRI", [128, 128], BF16)
                NCM = ssb("NCM", [128, 16, 128], BF16)
                ZCF = ssb("ZCF", [128, 16, 2, 128], F32)
                CRE = ssb("CRE", [128, 16], F32)
                CIM = ssb("CIM", [128, 16], F32)
                TQ = [ssb("TQ%d" % i, [128, 16], F32) for i in range(4)]
                XU1 = ssb("XU1", [128, 16, 2, 128], BF16)
                XU2 = ssb("XU2", [128, 16, 2, 128], BF16)
                CARRY = ssb("CARRY", [128, 16, 2], F32)
                CT = [ssb("CT%d" % i, [128, 2, 2], F32) for i in range(2)]
                Y = ROW[0][:].rearrange("p (a b) -> p a b", a=4)
                YB = ROW[1][:].rearrange("p (a b) -> p a b", a=4)
                YG = ROW[2][:].rearrange("p (a b) -> p a b", a=4)
                YGB = ssb("YGB", [128, 4, 512], BF16)
                SGL = [ssb("SGL%d" % i, [128, 512], F32) for i in range(2)]
                S = TH[:].rearrange("p (a b) -> p a b", a=4)
                SQ = ssb("SQ", [128, 4, 512], BF16)
                RS5 = ssb("RS5", [128, 512], F32)
                OUT = ssb("OUT", [128, 4, 512], BF16)
                ck = ['s5c']
                for i in range(3):
                    tr.dma('sp', 's5const', ROW[i][:], s5_rows[i:i + 1, :].partition_broadcast(128), w=ck)
                tr.dma('sp', 's5const', JC[:], c_jcol[:, :], w=ck)
                tr.dma('sp', 's5const', COL[:], s5_cols[:, :], w=ck)
                tr.dma('sp', 's5const', IOT[:], c_iota[:, :], w=ck)
                tr.dma('sp', 's5const', REP[:].rearrange("p a b c -> p (a b c)"), s5_rep[:, :], w=ck)
                tr.dma('sp', 's5const', BT[:].rearrange("p a b c -> p (a b c)"), s5_bT[:, :], w=ck)
                tr.dma('sp', 's5const', MASKB[:], c_maskB[:, :], w=ck)
                tr.dma('sp', 's5const', CC[:].rearrange("p a b c -> p (a b c)"), s5_c[:, :], w=ck)
                tr.dma('sp', 's5const', MASKC[:].rearrange("p a b -> p (a b)"), c_maskC[:, :], w=ck)
                tr.dma('sp', 's5const', DCOL[:], s5_dcol[:, :], w=ck)
                tr.dma('sp', 's5const', BGL[:], bglu_col[:, :], w=ck)
                tr.dma('pool', 's5const', TRI[:], c_tri[:, :], w=ck)
                for k in range(4):
                    tr.dma('pool', 's5const', WGL[:, k, :], w_glu[k * 128:(k + 1) * 128, :], w=ck)
                V = lambda fn, r, w: tr.op('dve', fn, r=r, w=w)
                A = lambda fn, r, w: tr.op('act', fn, r=r, w=w)
                V(lambda e: e.memset(NPI[:], -PI), [], ck)
                V(lambda e: e.tensor_scalar(out=NJC[:], in0=JC[:], scalar1=-1.0, scalar2=None, op0=ALU.mult), ck, ck)
                V(lambda e: e.memset(CRE[:], 0.0), [], ['carry'])
                V(lambda e: e.memset(CIM[:], 0.0), [], ['carry'])

                def sincos(out_sin, out_cos, theta, tmpf, tmpi):
                    for out, shift in ((out_sin, 0.0), (out_cos, 0.5 * PI)):
                        V(lambda e, shift=shift: e.tensor_scalar(out=tmpf, in0=theta, scalar1=shift, scalar2=1.0 / (2 * PI),
                                                                 op0=ALU.add, op1=ALU.mult), ck, ck)
                        V(lambda e: e.tensor_copy(out=tmpi, in_=tmpf), ck, ck)
                        V(lambda e: e.tensor_copy(out=tmpf, in_=tmpi), ck, ck)
                        V(lambda e, out=out: e.scalar_tensor_tensor(out=out, in0=tmpf, scalar=-2 * PI, in1=theta,
                                                                    op0=ALU.mult, op1=ALU.add), ck, ck)
                        if shift:
                            V(lambda e, out=out, shift=shift: e.tensor_scalar(out=out, in0=out, scalar1=shift, scalar2=None,
                                                                              op0=ALU.add), ck, ck)
                        V(lambda e, out=out: e.tensor_scalar(out=tmpf, in0=out, scalar1=PI, scalar2=-2 * PI,
                                                             op0=ALU.is_gt, op1=ALU.mult), ck, ck)
                        V(lambda e, out=out: e.tensor_tensor(out=out, in0=out, in1=tmpf, op=ALU.add), ck, ck)
                        A(lambda e, out=out: e.activation(out=out, in_=out, func=AF.Sin), ck, ck)

                A(lambda e: e.activation(out=ROW[2][:], in_=ROW[2][:], func=AF.Exp), ck, ck)
                V(lambda e: e.tensor_tensor(out=ROW[0][:], in0=ROW[0][:], in1=ROW[2][:], op=ALU.mult), ck, ck)
                V(lambda e: e.tensor_tensor(out=ROW[1][:], in0=ROW[1][:], in1=ROW[2][:], op=ALU.mult), ck, ck)
                A(lambda e: e.activation(out=ROW[2][:], in_=ROW[0][:], func=AF.Exp, scale=NJC[:, 0:1]), ck, ck)
                V(lambda e: e.tensor_scalar(out=TH[:], in0=ROW[1][:], scalar1=JC[:, 0:1], scalar2=None, op0=ALU.mult),
                  ck, ck)
                sincos(PRE_IM[:], PRE_RE[:], TH[:], ROW[0][:], TMPI[:])
                V(lambda e: e.tensor_tensor(out=PRE_RE[:], in0=PRE_RE[:], in1=ROW[2][:], op=ALU.mult), ck, ck)
                V(lambda e: e.scalar_tensor_tensor(out=PRE_IM[:], in0=PRE_IM[:], scalar=-1.0, in1=ROW[2][:],
                                                   op0=ALU.mult, op1=ALU.mult), ck, ck)
                LRc, LIc, DTc = COL[:, 0:16], COL[:, 16:32], COL[:, 32:48]
                A(lambda e: e.activation(out=DTc, in_=DTc, func=AF.Exp), ck, ck)
                V(lambda e: e.tensor_tensor(out=LRc, in0=LRc, in1=DTc, op=ALU.mult), ck, ck)
                V(lambda e: e.tensor_tensor(out=LIc, in0=LIc, in1=DTc, op=ALU.mult), ck, ck)
                iob = IOT[:].unsqueeze(1).to_broadcast([128, 16, 128])
                THv = TH[:].rearrange("p (a b) -> p a b", a=16)
                R0v = ROW[0][:].rearrange("p (a b) -> p a b", a=16)
                R2v = ROW[2][:].rearrange("p (a b) -> p a b", a=16)
                V(lambda e: e.tensor_tensor(out=R2v, in0=iob, in1=LRc.unsqueeze(2).to_broadcast([128, 16, 128]),
                                            op=ALU.mult), ck, ck)
                A(lambda e: e.activation(out=ROW[2][:], in_=ROW[2][:], func=AF.Exp), ck, ck)
                V(lambda e: e.tensor_tensor(out=THv, in0=iob, in1=LIc.unsqueeze(2).to_broadcast([128, 16, 128]),
                                            op=ALU.mult), ck, ck)
                sincos(POST_IM[:].rearrange("p a b -> p (a b)"), POST_RE[:].rearrange("p a b -> p (a b)"), TH[:],
                       ROW[0][:], TMPI[:])
                V(lambda e: e.tensor_tensor(out=POST_RE[:], in0=POST_RE[:], in1=R2v, op=ALU.mult), ck, ck)
                V(lambda e: e.tensor_tensor(out=POST_IM[:], in0=POST_IM[:], in1=R2v, op=ALU.mult), ck, ck)
                V(lambda e: e.tensor_scalar(out=SM[0][:], in0=LRc, scalar1=128.0, scalar2=None, op0=ALU.mult), ck, ck)
                A(lambda e: e.activation(out=SM[0][:], in_=SM[0][:], func=AF.Exp), ck, ck)
                V(lambda e: e.tensor_scalar(out=SM[1][:], in0=LIc, scalar1=128.0, scalar2=None, op0=ALU.mult), ck, ck)
                sincos(A128[:, 1, :], A128[:, 0, :], SM[1][:], SM[2][:], TMPI[:, 0:16])
                V(lambda e: e.tensor_tensor(out=A128[:, 0, :], in0=A128[:, 0, :], in1=SM[0][:], op=ALU.mult), ck, ck)
                V(lambda e: e.tensor_tensor(out=A128[:, 1, :], in0=A128[:, 1, :], in1=SM[0][:], op=ALU.mult), ck, ck)
                lr, li, ldt = REP[:, :, 0, :], REP[:, :, 1, :], REP[:, :, 2, :]
                dtv, ldr, ldi, mag, sn, cs = [t[:] for t in RT]
                A(lambda e: e.activation(out=dtv, in_=ldt, func=AF.Exp), ck, ck)
                V(lambda e: e.tensor_tensor(out=ldr, in0=lr, in1=dtv, op=ALU.mult), ck, ck)
                V(lambda e: e.tensor_tensor(out=ldi, in0=li, in1=dtv, op=ALU.mult), ck, ck)
                A(lambda e: e.activation(out=mag, in_=ldr, func=AF.Exp), ck, ck)
                sincos(sn, cs, ldi, dtv, TMPI[:, 0:256].rearrange("p (a b) -> p a b", a=4))
                V(lambda e: e.tensor_tensor(out=cs, in0=cs, in1=mag, op=ALU.mult), ck, ck)
                V(lambda e: e.tensor_tensor(out=sn, in0=sn, in1=mag, op=ALU.mult), ck, ck)
                V(lambda e: e.tensor_scalar(out=cs, in0=cs, scalar1=-1.0, scalar2=None, op0=ALU.add), ck, ck)
                V(lambda e: e.tensor_tensor(out=dtv, in0=lr, in1=lr, op=ALU.mult), ck, ck)
                V(lambda e: e.tensor_tensor(out=mag, in0=li, in1=li, op=ALU.mult), ck, ck)
                V(lambda e: e.tensor_tensor(out=dtv, in0=dtv, in1=mag, op=ALU.add), ck, ck)
                V(lambda e: e.reciprocal(out=dtv, in_=dtv), ck, ck)
                V(lambda e: e.tensor_tensor(out=ldr, in0=cs, in1=lr, op=ALU.mult), ck, ck)
                V(lambda e: e.tensor_tensor(out=mag, in0=sn, in1=li, op=ALU.mult), ck, ck)
                V(lambda e: e.tensor_tensor(out=ldr, in0=ldr, in1=mag, op=ALU.add), ck, ck)
                V(lambda e: e.tensor_tensor(out=ldr, in0=ldr, in1=dtv, op=ALU.mult), ck, ck)
                V(lambda e: e.tensor_tensor(out=ldi, in0=sn, in1=lr, op=ALU.mult), ck, ck)
                V(lambda e: e.tensor_tensor(out=mag, in0=cs, in1=li, op=ALU.mult), ck, ck)
                V(lambda e: e.tensor_tensor(out=ldi, in0=ldi, in1=mag, op=ALU.subtract), ck, ck)
                V(lambda e: e.tensor_tensor(out=ldi, in0=ldi, in1=dtv, op=ALU.mult), ck, ck)
                br, bi_ = BT[:, :, 0, :], BT[:, :, 1, :]
                V(lambda e: e.tensor_tensor(out=BB[:, :, 0, :], in0=ldr, in1=br, op=ALU.mult), ck, ck)
                V(lambda e: e.tensor_tensor(out=mag, in0=ldi, in1=bi_, op=ALU.mult), ck, ck)
                V(lambda e: e.tensor_tensor(out=BB[:, :, 0, :], in0=BB[:, :, 0, :], in1=mag, op=ALU.subtract), ck, ck)
                V(lambda e: e.tensor_tensor(out=BB[:, :, 1, :], in0=ldr, in1=bi_, op=ALU.mult), ck, ck)
                V(lambda e: e.tensor_tensor(out=mag, in0=ldi, in1=br, op=ALU.mult), ck, ck)
                V(lambda e: e.tensor_tensor(out=BB[:, :, 1, :], in0=BB[:, :, 1, :], in1=mag, op=ALU.add), ck, ck)
                BBDv = BBD[:].rearrange("p k (a r g x) -> p k a r g x", a=4, r=2, g=2)
                for kt in range(4):
                    for a in range(4):
                        for ri in range(2):
                            V(lambda e, kt=kt, a=a, ri=ri: e.tensor_tensor(
                                out=BBDv[:, kt, a, ri, :, :],
                                in0=BB[:, kt, ri, :].unsqueeze(1).to_broadcast([128, 2, 64]),
                                in1=MASKB[:, 2 * a:2 * a + 2].unsqueeze(2).to_broadcast([128, 2, 64]),
                                op=ALU.mult), ck, ck)
                V(lambda e: e.tensor_scalar(out=NTRI[:], in0=TRI[:], scalar1=-1.0, scalar2=None, op0=ALU.mult), ck, ck)
                CMv = CMAT[:].rearrange("p a r (g h) -> p a r g h", g=8)
                NCMv = NCM[:].rearrange("p a (g h) -> p a g h", g=8)
                for pr in range(16):
                    in0 = CC[:, pr, 0, :].unsqueeze(1).to_broadcast([128, 8, 16])
                    in1 = MASKC[:, pr % 4, :].unsqueeze(2).to_broadcast([128, 8, 16])
                    V(lambda e, pr=pr, in0=in0, in1=in1: e.scalar_tensor_tensor(
                        out=NCMv[:, pr, :, :], in0=in0, scalar=-1.0, in1=in1, op0=ALU.mult, op1=ALU.mult), ck, ck)
                for pr in range(16):
                    for ri in range(2):
                        in0 = CC[:, pr, ri, :].unsqueeze(1).to_broadcast([128, 8, 16])
                        in1 = MASKC[:, pr % 4, :].unsqueeze(2).to_broadcast([128, 8, 16])
                        if ri == 0:
                            V(lambda e, pr=pr, in0=in0, in1=in1: e.tensor_tensor(out=CMv[:, pr, 0, :, :], in0=in0, in1=in1,
                                                                               op=ALU.mult), ck, ck)
                        else:
                            V(lambda e, pr=pr, in0=in0, in1=in1: e.scalar_tensor_tensor(
                                out=CMv[:, pr, 1, :, :], in0=in0, scalar=-1.0, in1=in1, op0=ALU.mult, op1=ALU.mult),
                                ck, ck)
                uview = uTd.rearrange("p (k t) -> p k t", k=4)
                mview = mixT.rearrange("(k p) t -> p k t", p=128)
                bring = [0]
                def uload(tt):
                    us = tt % 2
                    tr.dma('sp', 'uin%d' % us, UT[us][:], uview[:, :, tt * 512:tt * 512 + 512], r=['uTd'], w=['ut%d' % us])
                def pre(cc, jj):
                    tt, sub = divmod(cc, 4)
                    us = tt % 2; uk = 'ut%d' % us; U = UT[us]
                    tsl = slice(sub * 128, (sub + 1) * 128)
                    kt, half = divmod(jj, 2)
                    i = bring[0] % 2
                    bring[0] += 1
                    PB = pb[i]
                    pk = 'pbu%d' % i
                    pr0 = 4 * kt + 2 * half
                    tr.op('pe', lambda e: e.matmul(
                        out=PB[:], lhsT=U[:, kt, tsl], rhs=BBD[:, kt, half * 512:(half + 1) * 512],
                        start=True, stop=True), r=[uk, 's5c'], w=[pk])
                    PBv = PB[:].rearrange("p (a r x) -> p a r x", a=2, r=2)
                    pre_r = PRE_RE[:, pr0 * 128:pr0 * 128 + 256].rearrange("p (a x) -> p a x", a=2) \
                        .unsqueeze(2).to_broadcast([128, 2, 2, 128])
                    pre_i = PRE_IM[:, pr0 * 128:pr0 * 128 + 256].rearrange("p (a x) -> p a x", a=2) \
                        .unsqueeze(2).to_broadcast([128, 2, 2, 128])
                    t1 = T1B[:, jj, :].rearrange("p (a r x) -> p a r x", a=2, r=2)
                    t2 = T2B[:, jj, :].rearrange("p (a r x) -> p a r x", a=2, r=2)
                    tr.op('dve', lambda e: e.tensor_tensor(out=t1, in0=PBv, in1=pre_r, op=ALU.mult),
                          r=[pk, 's5c'], w=['t1_%d' % jj])
                    tr.op('dve', lambda e: e.tensor_tensor(out=t2, in0=PBv, in1=pre_i, op=ALU.mult),
                          r=[pk, 's5c'], w=['t2_%d' % jj])
                def post_a(cc, jj):
                    pg = jj
                    i = pg % 2
                    PZ = pb[2 + i]
                    zk = 'pz%d' % i
                    pr0 = 2 * pg
                    PZv = PZ[:].rearrange("p (a r x) -> p a r x", a=2, r=2)
                    t1 = T1B[:, jj, :].rearrange("p (a r x) -> p a r x", a=2, r=2)
                    t2 = T2B[:, jj, :].rearrange("p (a r x) -> p a r x", a=2, r=2)
                    rk = ['t1_%d' % jj, 't2_%d' % jj, 's5c']
                    for a in range(2):
                        tr.op('pe', lambda e, a=a: e.matmul(out=PZv[:, a, 0, :], lhsT=t1[:, a, 0, :], rhs=TRI[:],
                                                            start=True, stop=False, skip_group_check=True), r=rk, w=[zk])
                        tr.op('pe', lambda e, a=a: e.matmul(out=PZv[:, a, 0, :], lhsT=t2[:, a, 1, :], rhs=NTRI[:],
                                                            start=False, stop=True, skip_group_check=True), r=rk, w=[zk])
                        tr.op('pe', lambda e, a=a: e.matmul(out=PZv[:, a, 1, :], lhsT=t1[:, a, 1, :], rhs=TRI[:],
                                                            start=True, stop=False, skip_group_check=True), r=rk, w=[zk])
                        tr.op('pe', lambda e, a=a: e.matmul(out=PZv[:, a, 1, :], lhsT=t2[:, a, 0, :], rhs=TRI[:],
                                                            start=False, stop=True, skip_group_check=True), r=rk, w=[zk])
                    zck = 'zc%d' % pg
                    for a in range(2):
                        for ri in range(2):
                            CB_ = CRE if ri == 0 else CIM
                            tr.op('act', lambda e, a=a, ri=ri, CB_=CB_: e.activation(
                                out=ZCF[:, pr0 + a, ri, :], in_=PZv[:, a, ri, :], func=AF.Identity,
                                bias=CB_[:, pr0 + a:pr0 + a + 1]), r=[zk, 'carry'], w=[zck])
                def post_b(cc, jj):
                    pg = jj
                    pr0 = 2 * pg
                    zck = 'zc%d' % pg
                    Z = ZCF[:, pr0:pr0 + 2, :, :]
                    po_r = POST_RE[:, pr0:pr0 + 2, :].unsqueeze(2).to_broadcast([128, 2, 2, 128])
                    po_i = POST_IM[:, pr0:pr0 + 2, :].unsqueeze(2).to_broadcast([128, 2, 2, 128])
                    tr.op('dve', lambda e: e.tensor_tensor(out=XU1[:, pr0:pr0 + 2, :, :], in0=Z, in1=po_r, op=ALU.mult),
                          r=[zck, 's5c'], w=['x%d' % pg])
                    tr.op('dve', lambda e: e.tensor_tensor(out=XU2[:, pr0:pr0 + 2, :, :], in0=Z, in1=po_i, op=ALU.mult),
                          r=[zck, 's5c'], w=['x%d' % pg])
                def post_end(cc):
                    tt, sub = divmod(cc, 4)
                    us = tt % 2; uk = 'ut%d' % us; U = UT[us]
                    tsl = slice(sub * 128, (sub + 1) * 128)
                    zall = ['zc%d' % pg for pg in range(8)]
                    zr = ZCF[:, :, 0, 127]
                    zi = ZCF[:, :, 1, 127]
                    tr.op('dve', lambda e: e.tensor_tensor(out=TQ[0][:], in0=zr, in1=A128[:, 0, :], op=ALU.mult),
                          r=zall + ['s5c'], w=['tq'])
                    tr.op('dve', lambda e: e.tensor_tensor(out=TQ[1][:], in0=zi, in1=A128[:, 1, :], op=ALU.mult),
                          r=zall + ['s5c'], w=['tq'])
                    tr.op('dve', lambda e: e.tensor_tensor(out=TQ[2][:], in0=zr, in1=A128[:, 1, :], op=ALU.mult),
                          r=zall + ['s5c'], w=['tq'])
                    tr.op('dve', lambda e: e.tensor_tensor(out=TQ[3][:], in0=zi, in1=A128[:, 0, :], op=ALU.mult),
                          r=zall + ['s5c'], w=['tq'])
                    tr.op('dve', lambda e: e.tensor_tensor(out=CRE[:], in0=TQ[0][:], in1=TQ[1][:], op=ALU.subtract),
                          r=['tq'], w=['carry'])
                    tr.op('dve', lambda e: e.tensor_tensor(out=CIM[:], in0=TQ[2][:], in1=TQ[3][:], op=ALU.add),
                          r=['tq'], w=['carry'])
                    for kt in range(4):
                        i = kt % 2
                        PY = pb[4 + i]
                        yk = 'py%d' % i
                        n = 0
                        for a in range(4):
                            pr = 4 * kt + a
                            terms = ((CMAT[:, pr, 0, :], XU1[:, pr, 0, :]), (NCM[:, pr, :], XU2[:, pr, 1, :]),
                                     (CMAT[:, pr, 1, :], XU1[:, pr, 1, :]), (CMAT[:, pr, 1, :], XU2[:, pr, 0, :]))
                            for (cm, xx) in terms:
                                tr.op('pe', lambda e, PY=PY, cm=cm, xx=xx, n=n: e.matmul(
                                    out=PY[:, 0:128], lhsT=cm, rhs=xx,
                                    start=(n == 0), stop=(n == 15)), r=['s5c', 'x%d' % (pr // 2)], w=[yk])
                                n += 1
                        tr.op('dve', lambda e, PY=PY, kt=kt, tsl=tsl: e.scalar_tensor_tensor(
                            out=Y[:, kt, tsl], in0=U[:, kt, tsl], scalar=DCOL[:, kt:kt + 1], in1=PY[:, 0:128],
                            op0=ALU.mult, op1=ALU.add), r=[uk, yk, 's5c'], w=['y'])
                def tail(tt):
                    t0 = tt * 512
                    Yf = ROW[0][:]
                    YBf = ROW[1][:]
                    YGf = ROW[2][:]
                    tr.op('act', lambda e: e.activation(out=YBf, in_=Yf, func=AF.Square), r=['y'], w=['yb'])
                    tr.op('dve', lambda e: e.tensor_scalar(out=YBf, in0=YBf, scalar1=0.044715, scalar2=1.0,
                                                          op0=ALU.mult, op1=ALU.add), r=['yb'], w=['yb'])
                    tr.op('dve', lambda e: e.tensor_tensor(out=YBf, in0=YBf, in1=Yf, op=ALU.mult), r=['yb', 'y'], w=['yb'])
                    tr.op('act', lambda e: e.activation(out=YBf, in_=YBf, func=AF.Sigmoid, scale=GELU_C), r=['yb'], w=['yb'])
                    tr.op('dve', lambda e: e.tensor_tensor(out=YGf, in0=Yf, in1=YBf, op=ALU.mult), r=['y', 'yb'], w=['yg'])
                    tr.op('act', lambda e: e.activation(out=YGB[:].rearrange("p a b -> p (a b)"), in_=YGf, func=AF.Copy),
                          r=['yg'], w=['ygb'])
                    for mt in range(4):
                        i = mt % 2
                        PG = pb[6 + i]
                        gk = 'pgl%d' % i
                        for kt in range(4):
                            tr.op('pe', lambda e, PG=PG, kt=kt, mt=mt: e.matmul(
                                out=PG[:], lhsT=WGL[:, kt, mt * 128:(mt + 1) * 128], rhs=YGB[:, kt, :],
                                start=(kt == 0), stop=(kt == 3)), r=['s5c', 'ygb'], w=[gk])
                        tr.op('act', lambda e, PG=PG, mt=mt, i=i: e.activation(
                            out=SGL[i][:], in_=PG[:], func=AF.Sigmoid, bias=BGL[:, mt:mt + 1]), r=[gk, 's5c'],
                            w=['sgl%d' % i])
                        tr.op('dve', lambda e, mt=mt, i=i: e.tensor_tensor(out=S[:, mt, :], in0=YG[:, mt, :],
                                                                          in1=SGL[i][:], op=ALU.mult),
                              r=['yg', 'sgl%d' % i], w=['s'])
                    tr.op('act', lambda e: e.activation(out=SQ[:].rearrange("p a b -> p (a b)"),
                                                       in_=TH[:], func=AF.Square),
                          r=['s'], w=['sq'])
                    PN = pb[6]
                    for mt in range(4):
                        tr.op('pe', lambda e, mt=mt: e.matmul(out=PN[:], lhsT=ONES[:], rhs=SQ[:, mt, :],
                                                             start=(mt == 0), stop=(mt == 3)), r=['sq', 'c'], w=['pgl0'])
                    tr.op('dve', lambda e: e.tensor_scalar(out=RS5[:], in0=PN[:], scalar1=1.0 / 512, scalar2=EPS,
                                                          op0=ALU.mult, op1=ALU.add), r=['pgl0'], w=['rs5'])
                    tr.op('act', lambda e: e.activation(out=RS5[:], in_=RS5[:], func=AF.Sqrt), r=['rs5'], w=['rs5'])
                    tr.op('dve', lambda e: e.reciprocal(out=RS5[:], in_=RS5[:]), r=['rs5'], w=['rs5'])
                    for mt in range(4):
                        tr.op('dve', lambda e, mt=mt: e.scalar_tensor_tensor(
                            out=OUT[:, mt, :], in0=S[:, mt, :], scalar=GC[:, 32 + mt:33 + mt], in1=RS5[:],
                            op0=ALU.mult, op1=ALU.mult), r=['s', 'rs5', 'c'], w=['out5'])
                    tr.dma('sp', 'sout', mview[:, 4:8, t0:t0 + 512], OUT[:], r=['out5'], w=['mixT'])
                uload(0)
                uload(1)
                for jj in range(8):
                    pre(0, jj)
                for cc in range(32):
                    post_a(cc, 0)
                    for jj in range(8):
                        if jj + 1 < 8:
                            post_a(cc, jj + 1)
                        if cc + 1 < 32:
                            pre(cc + 1, jj)
                        post_b(cc, jj)
                    post_end(cc)
                    if cc % 4 == 3:
                        tail(cc // 4)
                        if cc // 4 + 2 < 8:
                            uload(cc // 4 + 2)
            tr.barrier()

        def wout_phase(after_wo=None):
            with contextlib.ExitStack() as w_:
                def wsb(name, shape, dt):
                    return w_.enter_context(nc.sbuf_tensor(name, list(shape), dt))
                WO = wsb("WO", [128, 8, D], BF16)
                XW = [wsb("XW%d" % i, [128, 8, 512], F32) for i in range(2)]
                MX = [wsb("MX%d" % i, [128, 8, 512], BF16) for i in range(2)]
                for k in range(8):
                    tr.dma('pool', 'wo', WO[:, k, :], w_out[k * 128:(k + 1) * 128, :], w=['WO'])
                if after_wo is not None:
                    after_wo()
                for tt in range(8):
                    s = tt % 2
                    t0 = tt * 512
                    xk, mk = 'xw%d' % s, 'mx%d' % s
                    tr.dma('sp', 'xwin%d' % s, XW[s][:], xview(x1T, t0, 512), r=['x1T_%d' % tt], w=[xk])
                    tr.dma('act', 'mxin%d' % s, MX[s][:], xview(mixT, t0, 512), r=['mixT'], w=[mk])
                    for dk in range(8):
                        b = dk % 4
                        for k in range(8):
                            tr.op('pe', lambda e, k=k, dk=dk, b=b, s=s: e.matmul(
                                out=pb[b][:], lhsT=WO[:, k, dk * 128:(dk + 1) * 128], rhs=MX[s][:, k, :],
                                start=(k == 0), stop=(k == 7)), r=['WO', mk], w=['pw%d' % b])
                        tr.op('dve', lambda e, dk=dk, b=b, s=s: e.tensor_tensor(
                            out=XW[s][:, dk, :], in0=pb[b][:], in1=XW[s][:, dk, :], op=ALU.add),
                            r=['pw%d' % b, xk], w=[xk])
                    tr.dma('act', 'xwout%d' % s, xview(x1T, t0, 512), XW[s][:], r=[xk], w=['x1T_%d' % tt])
            tr.barrier()

        ffn_phase(1)

        with contextlib.ExitStack() as ms:
            if stop < 1.2:
                return nc
            def msb(name, shape, dt):
                return ms.enter_context(nc.sbuf_tensor(name, list(shape), dt))
            KE = [msb("KE%d" % g, [128, T], BF16) for g in range(2)]
            KW = [msb("KW%d" % g, [64, T], BF16) for g in range(2)]
            VS1 = [msb("VS1%d" % g, [128, 32, 65], BF16) for g in range(2)]
            VW1 = [msb("VW1%d" % g, [128, 32, 65], BF16) for g in range(2)]
            GT = msb("GT", [128, 32, 24], F32)
            KC = [msb("KC%d" % g, [64, 256], BF16) for g in range(2)]
            VCX = [msb("VCX%d" % g, [128, 2, 129], BF16) for g in range(2)]
            for g in range(2):
                tr.dma('pool', 'const', KE[g][64:128, :], c_E0[:, :], w=['KE%d' % g])
                tr.op('dve', lambda e, g=g: e.memset(VS1[g][:, :, 64:65], 1.0), w=['VS1%d' % g])
                tr.op('dve', lambda e, g=g: e.memset(VW1[g][:, :, 64:65], 1.0), w=['VW1%d' % g])
                tr.op('dve', lambda e, g=g: e.memset(VCX[g][:, :, 64:65], 1.0), w=['VCX%d' % g])
                tr.dma('pool', 'const', VCX[g][:, :, 65:129], c_ovl.rearrange("p (a b) -> p a b", a=2),
                       w=['VCX%d' % g])

            with contextlib.ExitStack() as ps_:
                def psb(name, shape, dt):
                    return ps_.enter_context(nc.sbuf_tensor(name, list(shape), dt))
                WIN = psb("WIN", [128, 8, 1816], BF16)
                H2 = [psb("H2_%d" % i, [128, 8, 512], BF16) for i in range(2)]
                QST = psb("QST", [64, 4, 8, 128], BF16)
                UST = psb("UST", [128, 4, 512], BF16)
                KCin = [psb("KCin%d" % g, [64, 16, 256], BF16) for g in range(2)]
                VCin = [psb("VCin%d" % g, [64, 16, 256], BF16) for g in range(2)]
                GB = psb("GB", [128, 24], F32)
                W1 = [psb("W1_%d" % i, [64, 32, 256], BF16) for i in range(2)]
                W2 = [psb("W2_%d" % i, [128, 2, 64], BF16) for i in range(2)]
                CB1 = psb("CB1", [128, 4], F32)
                POST = psb("POST", [64, 64], BF16)
                HID = psb("HID", [128, 2, 256], BF16)
                GTMP = [psb("GTMP%d" % i, [128, 256], F32) for i in range(3)]
                PBIAS = psb("PBIAS", [128, 1], F32)
                for k in range(8):
                    tr.dma('pool', 'win', WIN[:, k, :], w_in[k * 128:(k + 1) * 128, :], w=['WIN'])
                tr.dma('sp', 'const', GB[:], gate_bias.partition_broadcast(128), w=['GB'])
                tr.dma('sp', 'const', CB1[:], cb1[:, :], w=['cmpw'])
                tr.dma('pool', 'const', POST[:], posT[:, :], w=['cmpw'])
                for i in range(2):
                    w1v = cw1[i].rearrange("(l d) h -> d l h", d=64)
                    for lc in range(8):
                        tr.dma('pool', 'const', W1[i][:, 4 * lc:4 * lc + 4, :], w1v[:, 4 * lc:4 * lc + 4, :], w=['cmpw'])
                    tr.dma('pool', 'const', W2[i][:], cw2[i].rearrange("(t p) d -> p t d", p=128), w=['cmpw'])
                if stop < 1.6:
                    tr.barrier()
                    return nc
                ring = [0]

                def nextbank():
                    b = ring[0] % 6
                    ring[0] += 1
                    return pb[b], 'pp%d' % b

                evq = [0]

                def evac(out, in_, rk, wk, force=None):
                    evq[0] += 1
                    if (evq[0] % 2 == 0 and force is None) or force == 'act':
                        tr.op('act', lambda e: e.activation(out=out, in_=in_, func=AF.Copy), r=[rk], w=[wk])
                    else:
                        tr.op('dve', lambda e: e.tensor_copy(out=out, in_=in_), r=[rk], w=[wk])

                for tt in range(8):
                    s = tt % 2
                    t0 = tt * 512
                    hk = 'h2_%d' % s
                    HH = H2[s]
                    tr.dma('sp', 'h2in%d' % s, HH[:], xview(h2T, t0, 512), r=['h2T'], w=[hk])
                    for h in range(8):
                        P, pk = nextbank()
                        for k in range(8):
                            tr.op('pe', lambda e, k=k, h=h, P=P: e.matmul(
                                out=P[0:64, :], lhsT=WIN[:, k, 64 * h:64 * h + 64], rhs=HH[:, k, :],
                                start=(k == 0), stop=(k == 7)), r=['WIN', hk], w=[pk])
                        evac(QST[:, :, h, :], P[0:64, :].rearrange("p (c q) -> p c q", q=128), pk, 'qst')
                    tr.dma('sp', 'qout', qTd.rearrange("p (c x) -> p c x", c=32)[:, 4 * tt:4 * tt + 4, :],
                           QST[:].rearrange("p c h q -> p c (h q)"), r=['qst'], w=['qTd'])
                    for (c0, dest, dn) in ((512, KCin, 'KCin'), (640, VCin, 'VCin'), (768, KE, 'KE'), (1024, KW, 'KW')):
                        for g in range(2):
                            P, pk = nextbank()
                            for k in range(8):
                                tr.op('pe', lambda e, k=k, P=P, cc=c0 + 64 * g: e.matmul(
                                    out=P[0:64, :], lhsT=WIN[:, k, cc:cc + 64], rhs=HH[:, k, :],
                                    start=(k == 0), stop=(k == 7)), r=['WIN', hk], w=[pk])
                            if dn in ('KCin', 'VCin'):
                                evac(dest[g][:, :, tt * 32:(tt + 1) * 32],
                                     P[0:64, :].rearrange("p (m r) -> p r m", r=16), pk, '%s%d' % (dn, g))
                            else:
                                evac(dest[g][0:64, t0:t0 + 512], P[0:64, :], pk, '%s%d' % (dn, g))
                    for kt in range(4):
                        P, pk = nextbank()
                        for k in range(8):
                            tr.op('pe', lambda e, k=k, P=P, cc=1304 + 128 * kt: e.matmul(
                                out=P[:, :], lhsT=WIN[:, k, cc:cc + 128], rhs=HH[:, k, :],
                                start=(k == 0), stop=(k == 7)), r=['WIN', hk], w=[pk])
                        evac(UST[:, kt, :], P[:, :], pk, 'ust')
                    tr.dma('sp', 'uout', uTd.rearrange("p (k t) -> p k t", k=4)[:, :, t0:t0 + 512], UST[:],
                           r=['ust'], w=['uTd'])
                    for sbk in range(4):
                        blk = tt * 4 + sbk
                        P, pk = nextbank()
                        for k in range(8):
                            tr.op('pe', lambda e, k=k, P=P, sbk=sbk: e.matmul(
                                out=P[:, 0:408], lhsT=HH[:, k, sbk * 128:(sbk + 1) * 128], rhs=WIN[:, k, 896:1304],
                                start=(k == 0), stop=(k == 7)), r=['WIN', hk], w=[pk])
                        for g in range(2):
                            evac(VS1[g][:, blk, 0:64], P[:, 64 * g:64 * g + 64], pk, 'VS1%d' % g, force='dve')
                            evac(VW1[g][:, blk, 0:64], P[:, 256 + 64 * g:256 + 64 * g + 64], pk, 'VW1%d' % g, force='dve')
                        tr.op('dve', lambda e, P=P, blk=blk: e.tensor_tensor(out=GT[:, blk, :], in0=P[:, 384:408],
                                                                            in1=GB[:], op=ALU.add),
                              r=[pk, 'GB'], w=['GT'])
                        tr.op('act', lambda e, blk=blk: e.activation(out=GT[:, blk, :], in_=GT[:, blk, :],
                                                                    func=AF.Sigmoid), r=['GT'], w=['GT'])

                if stop < 1.8:
                    tr.barrier()
                    return nc
                HIDS = [psb("HIDS%d" % g, [128, 2, 256], BF16) for g in range(2)]
                for g in range(2):
                    tr.op('dve', lambda e, g=g: e.memset(HIDS[g][:], 0.0), w=['hid%d_0' % g, 'hid%d_1' % g])
                A, B_, C_ = GTMP
                for kind in range(2):
                    srcs = KCin if kind == 0 else VCin
                    sname = 'KCin' if kind == 0 else 'VCin'
                    for hh in range(2):
                        P, pk = nextbank()
                        for l in range(32):
                            tr.op('pe', lambda e, l=l, P=P, hh=hh: e.matmul(
                                out=P[:, 0:1], lhsT=W1[kind][:, l, hh * 128:(hh + 1) * 128],
                                rhs=POST[:, kind * 32 + l:kind * 32 + l + 1], start=(l == 0), stop=(l == 31)),
                                r=['cmpw'], w=[pk])
                        tr.op('dve', lambda e, P=P, hh=hh: e.tensor_tensor(
                            out=PBIAS[:], in0=P[:, 0:1], in1=CB1[:, kind * 2 + hh:kind * 2 + hh + 1], op=ALU.add),
                            r=[pk, 'cmpw'], w=['pbias'])
                        for g in range(2):
                            P2, pk2 = nextbank()
                            for l in range(32):
                                rhs = srcs[g][:, l % 16, (l // 16):(l // 16) + 255]
                                tr.op('pe', lambda e, l=l, P2=P2, rhs=rhs, hh=hh: e.matmul(
                                    out=P2[:, 0:255], lhsT=W1[kind][:, l, hh * 128:(hh + 1) * 128], rhs=rhs,
                                    start=(l == 0), stop=(l == 31)), r=['cmpw', '%s%d' % (sname, g)], w=[pk2])
                            tr.op('dve', lambda e, P2=P2: e.tensor_scalar(out=A[:, 0:255], in0=P2[:, 0:255],
                                                                         scalar1=PBIAS[:, 0:1], scalar2=None,
                                                                         op0=ALU.add), r=[pk2, 'pbias'], w=['ga'])
                            tr.op('dve', lambda e: e.tensor_tensor(out=B_[:, 0:255], in0=A[:, 0:255], in1=A[:, 0:255],
                                                                  op=ALU.mult), r=['ga'], w=['gb'])
                            tr.op('dve', lambda e: e.tensor_scalar(out=B_[:, 0:255], in0=B_[:, 0:255], scalar1=0.044715,
                                                                  scalar2=1.0, op0=ALU.mult, op1=ALU.add),
                                  r=['gb'], w=['gb'])
                            tr.op('dve', lambda e: e.tensor_tensor(out=B_[:, 0:255], in0=B_[:, 0:255], in1=A[:, 0:255],
                                                                  op=ALU.mult), r=['gb', 'ga'], w=['gb'])
                            tr.op('act', lambda e: e.activation(out=C_[:, 0:255], in_=B_[:, 0:255], func=AF.Sigmoid,
                                                               scale=GELU_C), r=['gb'], w=['gc'])
                            tr.op('dve', lambda e, g=g, hh=hh: e.tensor_tensor(
                                out=HIDS[g][:, hh, 0:255], in0=A[:, 0:255], in1=C_[:, 0:255], op=ALU.mult),
                                r=['ga', 'gc'], w=['hid%d_%d' % (g, hh)])
                    for g in range(2):
                        hk2 = ['hid%d_0' % g, 'hid%d_1' % g]
                        if kind == 0:
                            P, pk = nextbank()
                            for hh in range(2):
                                tr.op('pe', lambda e, hh=hh, P=P, g=g: e.matmul(
                                    out=P[0:64, 0:256], lhsT=W2[0][:, hh, :], rhs=HIDS[g][:, hh, :],
                                    start=(hh == 0), stop=(hh == 1)), r=['cmpw'] + hk2, w=[pk])
                            evac(KC[g][:, :], P[0:64, 0:256], pk, 'KC%d' % g)
                        else:
                            for nt in range(2):
                                P, pk = nextbank()
                                for hh in range(2):
                                    tr.op('pe', lambda e, hh=hh, P=P, g=g, nt=nt: e.matmul(
                                        out=P[:, 0:64], lhsT=HIDS[g][:, hh, nt * 128:(nt + 1) * 128],
                                        rhs=W2[1][:, hh, :], start=(hh == 0), stop=(hh == 1)),
                                        r=['cmpw'] + hk2, w=[pk])
                                evac(VCX[g][:, nt, 0:64], P[:, 0:64], pk, 'VCX%d' % g)
            tr.barrier()
            if stop >= 3:
                attention_phase()
        if stop >= 4:
            s5_phase()
        with contextlib.ExitStack() as w2s:
            wts2 = ffn_weights(2, w2s, load=False) if stop >= 6 else None
            if stop >= 5:
                wout_phase((lambda: ffn_wload(2, wts2)) if stop >= 6 else None)
            if stop >= 6:
                ffn_phase(2, wts2)
    return nc


def _consts():
    f = np.float32
    c = {}
    c["c_ident"] = np.eye(128, dtype=f)
    j = np.arange(128)
    c["c_tri"] = (j[:, None] <= j[None, :]).astype(f)
    c["c_E0"] = (np.arange(T)[None, :] // 64 == np.arange(64)[:, None]).astype(f)
    cb = np.where(j[:, None] > j[None, :], -BIGM, 0.0).astype(f)
    wb = np.where(j[:, None] <= j[None, :], -BIGM, 0.0).astype(f)
    cm = np.where(16 * (j[:, None] - 64) > j[None, :] - 31, -BIGM, 0.0).astype(f)
    c["c_CB"] = np.tile(cb, (1, 4))
    c["c_WB"] = np.tile(wb, (1, 4))
    c["c_CM"] = np.tile(cm, (1, 4))
    c["c_BD"] = (np.arange(384)[None, :] == j[:, None] + 128).astype(f)
    hh = (j >= 64).astype(np.int64)[:, None]
    m = np.arange(128)[None, :]
    c["c_U0"] = np.where(m > 62 + hh, -1e30, 3e38).astype(f)
    l0 = np.full((128, 128), -3e38, dtype=f)
    l0[m == 62 + hh] = 2e30
    l0[m == 61 + hh] = 1e30
    c["c_L0"] = l0
    n = np.arange(256)[:, None]
    jj = np.arange(64)[None, :]
    ov = ((16 * n < 64 * jj + 64) & (16 * n + 32 > 64 * jj)).astype(f)
    ov[255] = 0
    c["c_ovl"] = np.ascontiguousarray(ov.reshape(2, 128, 64).transpose(1, 0, 2).reshape(128, 128))
    c["c_maskB"] = (np.arange(8)[None, :] == (j // 16)[:, None]).astype(f)
    gi = (j // 64)[:, None, None]
    p4 = np.arange(4)[None, :, None]
    g8 = np.arange(8)[None, None, :]
    c["c_maskC"] = (g8 == 2 * p4 + gi).astype(f).reshape(128, 32)
    c["c_iota"] = np.tile(np.arange(128, dtype=f)[None, :], (128, 1))
    c["c_jcol"] = np.arange(128, dtype=f)[:, None].copy()
    return c


def _prep_shared(inp):
    f = np.float32
    A = lambda a: np.ascontiguousarray(np.asarray(a, dtype=f))
    d = {}
    d["wg1"], d["wu1"], d["wd1"] = A(inp["ffn1_w_gate"][0]), A(inp["ffn1_w_up"][0]), A(inp["ffn1_w_down"][0])
    d["wg2"], d["wu2"], d["wd2"] = A(inp["ffn2_w_gate"][0]), A(inp["ffn2_w_up"][0]), A(inp["ffn2_w_down"][0])
    d["w_in"], d["w_out"], d["w_glu"] = A(inp["w_in"][0]), A(inp["w_out"][0]), A(inp["s5_w_glu"][0])
    col8 = lambda v: np.asarray(v, dtype=f).reshape(-1, 128).T
    d["gcols"] = A(np.concatenate([col8(inp["ffn1_norm"][0]), col8(inp["mix_norm"][0]), col8(inp["ffn2_norm"][0]),
                                   col8(inp["final_norm"]), col8(inp["ssm_out_norm"][0])], axis=1))
    d["grow_attn"] = A(np.asarray(inp["attn_out_norm"][0]).reshape(1, 512))
    d["gate_bias"] = A(np.asarray(inp["gate_bias"][0]).reshape(1, 24))
    d["cw1k"], d["cw1v"] = A(inp["cmp_k_w1"][0]), A(inp["cmp_v_w1"][0])
    d["cw2k"], d["cw2v"] = A(inp["cmp_k_w2"][0]), A(inp["cmp_v_w2"][0])
    d["cb1"] = A(np.concatenate([col8(inp["cmp_k_b1"][0]), col8(inp["cmp_v_b1"][0])], axis=1))
    d["posT"] = A(np.concatenate([np.asarray(inp["cmp_pos_k"][0]).T, np.asarray(inp["cmp_pos_v"][0]).T], axis=1))
    lr = np.asarray(inp["s5_lambda_re"][0], dtype=f)
    li = np.asarray(inp["s5_lambda_im"][0], dtype=f)
    ldt = np.repeat(np.asarray(inp["s5_log_dt"][0], dtype=f)[:, None], 64, axis=1)
    d["s5_rows"] = A(np.stack([lr.reshape(2048), li.reshape(2048), ldt.reshape(2048)]))
    colify = lambda a: a.reshape(16, 128).T
    d["s5_cols"] = A(np.concatenate([colify(lr), colify(li), colify(ldt)], axis=1))

    def rows_gh(a):
        return np.repeat(a.reshape(4, 8, 1, 64), 16, axis=2).transpose(1, 2, 0, 3).reshape(128, 4, 64)
    d["s5_rep"] = A(np.stack([rows_gh(lr), rows_gh(li), rows_gh(ldt)], axis=2).reshape(128, 4 * 3 * 64))

    def bt(a):
        return np.asarray(a, dtype=f).reshape(4, 8, 64, 16).transpose(1, 3, 0, 2).reshape(128, 4, 64)
    d["s5_bT"] = A(np.stack([bt(inp["s5_b_re"][0]), bt(inp["s5_b_im"][0])], axis=2).reshape(128, 4 * 2 * 64))

    def ct(a):
        return np.asarray(a, dtype=f).reshape(16, 2, 16, 64).transpose(1, 3, 0, 2).reshape(128, 16, 16)
    d["s5_c"] = A(np.stack([ct(inp["s5_c_re"][0]), ct(inp["s5_c_im"][0])], axis=2).reshape(128, 16 * 2 * 16))
    d["s5_dcol"] = A(np.asarray(inp["s5_d"][0], dtype=f).reshape(4, 128).T)
    d["bglu_col"] = A(np.asarray(inp["s5_b_glu"][0], dtype=f).reshape(4, 128).T)
    d.update(_consts())
    return d


def kernel(**inputs):
    x = np.asarray(inputs["x"], dtype=np.float32)
    shared = _prep_shared(inputs)
    nc = build(dbg=False)
    in_maps = []
    for b in range(NCORES):
        m = dict(shared)
        m["xT"] = np.ascontiguousarray(x[b].T)
        in_maps.append(m)
    res = run_bass_kernel_spmd(nc, in_maps, core_ids=list(range(NCORES)))
    out = np.empty((NCORES, T, D), dtype=np.float32)
    for b in range(NCORES):
        out[b] = np.asarray(res.results[b]["yT"], dtype=np.float32).T
    return out
```

```python
import contextlib
import numpy as np
import concourse.bass as bass
import concourse.mybir as mybir
from concourse.bass_utils import run_bass_kernel_spmd

F32 = mybir.dt.float32
BF16 = mybir.dt.bfloat16
ALU = mybir.AluOpType
AF = mybir.ActivationFunctionType

T = 4096
D = 1024
FF = 2816
NF = FF // 128
EPS = 1e-6
BIGM = 30000.0
NCORES = 8
GELU_C = 1.5957691216057308


class TR:
    def __init__(self, nc, es):
        self.nc, self.es = nc, es
        self.eng = dict(pe=nc.tensor, dve=nc.vector, act=nc.scalar, pool=nc.gpsimd, sp=nc.sync)
        self.sem, self.cnt = {}, {}
        self.seen = {e: {} for e in self.eng}
        for e in self.eng:
            self.sem[e] = es.enter_context(nc.semaphore("s_" + e))
            self.cnt[e] = 0
        self.bs = {}
        self.defer = None

    def _st(self, k):
        s = self.bs.get(k)
        if s is None:
            s = self.bs[k] = ({}, {})
        return s

    def _deps(self, r, w):
        d = {}
        for k in r:
            for sk, v in self._st(k)[0].items():
                if d.get(sk, 0) < v:
                    d[sk] = v
        for k in w:
            s = self._st(k)
            for dd in s:
                for sk, v in dd.items():
                    if d.get(sk, 0) < v:
                        d[sk] = v
        return d

    def _wait(self, e, d):
        for k, v in d.items():
            if k == e and e == 'pe':
                continue
            if self.seen[e].get(k, 0) >= v:
                continue
            self.eng[e].wait_ge(self.sem[k], v)
            self.seen[e][k] = v

    def _mark(self, src, v, r, w):
        for k in r:
            self._st(k)[1][src] = v
        for k in w:
            self._st(k)[0][src] = v

    def op(self, e, fn, r=(), w=()):
        if self.defer is not None:
            r, w = list(r), list(w)
            self.defer.append(lambda: self.op(e, fn, r, w))
            return
        self._wait(e, self._deps(r, w))
        ins = fn(self.eng[e])
        self.cnt[e] += 1
        ins.then_inc(self.sem[e], 1)
        self._mark(e, self.cnt[e], r, w)

    def dma(self, q, ch, out, in_, r=(), w=()):
        if self.defer is not None:
            r, w = list(r), list(w)
            self.defer.append(lambda: self.dma(q, ch, out, in_, r, w))
            return
        if ch not in self.sem:
            self.sem[ch] = self.es.enter_context(self.nc.semaphore("c_" + ch))
            self.cnt[ch] = 0
        self._wait(q, self._deps(r, w))
        ins = self.eng[q].dma_start(out=out, in_=in_)
        self.cnt[ch] += 16
        ins.then_inc(self.sem[ch], 16)
        self._mark(ch, self.cnt[ch], r, w)

    def barrier(self):
        allk = {k: v for k, v in self.cnt.items() if v > 0}
        for e in self.eng:
            self._wait(e, allk)


def build(dbg=False, stop=9):
    nc = bass.Bass("TRN2", target_bir_lowering=False)
    es = contextlib.ExitStack()
    with es:
        tr = TR(nc, es)

        def din(name, shape):
            return nc.dram_tensor(name, list(shape), F32, kind="ExternalInput").ap()

        def dscr(name, shape, dt):
            kind = "ExternalOutput" if dbg else "Internal"
            return nc.dram_tensor(name, list(shape), dt, kind=kind).ap()

        def sb(name, shape, dt):
            return es.enter_context(nc.sbuf_tensor(name, list(shape), dt))

        xT = din("xT", [D, T])
        wgs = [din("wg1", [D, FF]), din("wg2", [D, FF])]
        wus = [din("wu1", [D, FF]), din("wu2", [D, FF])]
        wds = [din("wd1", [FF, D]), din("wd2", [FF, D])]
        w_in = din("w_in", [D, 1816])
        w_out = din("w_out", [D, D])
        w_glu = din("w_glu", [512, 512])
        gcols = din("gcols", [128, 36])
        grow_attn = din("grow_attn", [1, 512])
        gate_bias = din("gate_bias", [1, 24])
        cw1 = [din("cw1k", [2048, 256]), din("cw1v", [2048, 256])]
        cw2 = [din("cw2k", [256, 64]), din("cw2v", [256, 64])]
        cb1 = din("cb1", [128, 4])
        posT = din("posT", [64, 64])
        s5_rows = din("s5_rows", [3, 2048])
        s5_cols = din("s5_cols", [128, 48])
        s5_rep = din("s5_rep", [128, 4 * 3 * 64])
        s5_bT = din("s5_bT", [128, 4 * 2 * 64])
        s5_c = din("s5_c", [128, 16 * 2 * 16])
        s5_dcol = din("s5_dcol", [128, 4])
        bglu_col = din("bglu_col", [128, 4])
        c_ident = din("c_ident", [128, 128])
        c_tri = din("c_tri", [128, 128])
        c_E0 = din("c_E0", [64, T])
        c_CB = din("c_CB", [128, 512])
        c_WB = din("c_WB", [128, 512])
        c_CM = din("c_CM", [128, 512])
        c_BD = din("c_BD", [128, 384])
        c_U0 = din("c_U0", [128, 128])
        c_L0 = din("c_L0", [128, 128])
        c_ovl = din("c_ovl", [128, 128])
        c_maskB = din("c_maskB", [128, 8])
        c_maskC = din("c_maskC", [128, 32])
        c_iota = din("c_iota", [128, 128])
        c_jcol = din("c_jcol", [128, 1])

        yT = nc.dram_tensor("yT", [D, T], F32, kind="ExternalOutput").ap()
        x1T = dscr("x1T", [D, T], F32)
        h2T = dscr("h2T", [D, T], BF16)
        qTd = dscr("qTd", [64, 32 * 8 * 128], BF16)
        uTd = dscr("uTd", [128, 4 * T], BF16)
        mixT = dscr("mixT", [D, T], BF16)

        GC = sb("GC", [128, 36], F32)
        ONES = sb("ONES", [128, 128], BF16)
        IDN = sb("IDN", [128, 128], BF16)
        EPSC = sb("EPSC", [128, 1], F32)
        tr.dma('sp', 'const', GC[:], gcols[:, :], w=['c'])
        tr.dma('pool', 'const', IDN[:], c_ident[:, :], w=['c'])
        tr.op('dve', lambda e: e.memset(ONES[:], 1.0), w=['c'])
        tr.op('dve', lambda e: e.memset(EPSC[:], 0.0), w=['c'])

        pb = [es.enter_context(nc.psum_tensor("pb%d" % i, [128, 512], F32)) for i in range(8)]

        def xview(dram, t0, n):
            return dram.rearrange("(k p) t -> p k t", p=128)[:, :, t0:t0 + n]

        def ffn_weights(ph, stk, load=True):
            WG = stk.enter_context(nc.sbuf_tensor("WG_%d" % ph, [128, 8, FF], BF16))
            WU = stk.enter_context(nc.sbuf_tensor("WU_%d" % ph, [128, 8, FF], BF16))
            WD = stk.enter_context(nc.sbuf_tensor("WD_%d" % ph, [128, NF, D], BF16))
            if load:
                ffn_wload(ph, (WG, WU, WD))
            return WG, WU, WD

        def ffn_wload(ph, wts, gu=True, wd=True):
            WG, WU, WD = wts
            wgd, wud, wdd = wgs[ph - 1], wus[ph - 1], wds[ph - 1]
            HF = FF // 2
            for hf, sfx in ((0, 'a'), (1, 'b')) if gu else ():
                for k in range(8):
                    tr.dma('pool', 'wg' + sfx, WG[:, k, hf * HF:(hf + 1) * HF],
                           wgd[k * 128:(k + 1) * 128, hf * HF:(hf + 1) * HF], w=['WG' + sfx])
                    tr.dma('pool', 'wu' + sfx, WU[:, k, hf * HF:(hf + 1) * HF],
                           wud[k * 128:(k + 1) * 128, hf * HF:(hf + 1) * HF], w=['WU' + sfx])
            for f in range(NF) if wd else ():
                tr.dma('pool', 'wd', WD[:, f, :], wdd[f * 128:(f + 1) * 128, :], w=['WD'])

        def ffn_phase(ph, wts=None):
            with contextlib.ExitStack() as fs:
                def fsb(name, shape, dt):
                    return fs.enter_context(nc.sbuf_tensor("%s_%d" % (name, ph), list(shape), dt))
                WG, WU, WD = wts if wts is not None else ffn_weights(ph, fs)
                if wts is not None:
                    ffn_wload(ph, wts, gu=False, wd=True)
                XT = [fsb("XT%d" % i, [128, 8, 512], F32) for i in range(2)]
                H = fsb("H", [128, 8, 512], BF16)
                AT = fsb("AT", [128, NF, 512], BF16)
                SG = [fsb("SG%d" % i, [128, 512], BF16) for i in range(2)]
                RS = [fsb("RS%d" % i, [128, 512], F32) for i in range(2)]
                src = xT if ph == 1 else x1T
                g0 = 0 if ph == 1 else 16
                hkeys = ['h%d' % k for k in range(8)]
                atk2 = ['at%d' % f for f in range(14, 22)]

                def xs(tt):
                    return XT[tt % 2], 'xt%d' % (tt % 2)

                def load(tt):
                    X, xk = xs(tt)
                    tr.dma('sp', 'xin%d' % (tt % 2), X[:], xview(src, tt * 512, 512), w=[xk])

                def norm_a(X, xk, HB, hk):
                    tr.op('act', lambda e: e.activation(out=HB, in_=X[:], func=AF.Square), r=[xk], w=hk)

                def norm_b(HBk, hk, PN, pnk):
                    for k in range(8):
                        tr.op('pe', lambda e, k=k: e.matmul(out=PN[:], lhsT=ONES[:], rhs=HBk(k),
                                                           start=(k == 0), stop=(k == 7)), r=[hk[k], 'c'], w=[pnk])

                def norm_c(X, xk, HBk, hk, PN, pnk, R, rk, goff, inplace=False):
                    tr.op('dve', lambda e: e.tensor_scalar(out=R[:], in0=PN[:], scalar1=1.0 / D, scalar2=EPS,
                                                          op0=ALU.mult, op1=ALU.add), r=[pnk], w=[rk])
                    tr.op('act', lambda e: e.activation(out=R[:], in_=R[:], func=AF.Sqrt), r=[rk], w=[rk])
                    tr.op('dve', lambda e: e.reciprocal(out=R[:], in_=R[:]), r=[rk], w=[rk])
                    for k in range(8):
                        dst = X[:, k, :] if inplace else HBk(k)
                        tr.op('dve', lambda e, k=k, dst=dst: e.scalar_tensor_tensor(
                            out=dst, in0=X[:, k, :], scalar=GC[:, goff + k:goff + k + 1], in1=R[:],
                            op0=ALU.mult, op1=ALU.mult), r=[xk, rk, 'c'], w=[xk if inplace else hk[k]])

                Hk = lambda k: H[:, k, :]
                A2k = lambda k: AT[:, 14 + k, :]

                def post_a(tt):
                    X, xk = xs(tt)
                    if ph == 1:
                        tr.dma('sp', 'xout%d' % (tt % 2), xview(x1T, tt * 512, 512), X[:], r=[xk], w=['x1T'])
                    norm_a(X, xk, AT[:, 14:22, :], atk2)

                def post_bc(tt):
                    X, xk = xs(tt)
                    norm_b(A2k, atk2, pb[7], 'pn2')
                    if ph == 1:
                        norm_c(X, xk, A2k, atk2, pb[7], 'pn2', RS[1], 'rs1', 8)
                        tr.dma('sp', 'hout', xview(h2T, tt * 512, 512), AT[:, 14:22, :], r=atk2, w=['h2T'])
                    else:
                        norm_c(X, xk, A2k, atk2, pb[7], 'pn2', RS[1], 'rs1', 24, inplace=True)
                        tr.dma('sp', 'yout%d' % (tt % 2), xview(yT, tt * 512, 512), X[:], r=[xk], w=['yT'])

                load(0)
                X0, xk0 = xs(0)
                norm_a(X0, xk0, H[:], hkeys)
                norm_b(Hk, hkeys, pb[6], 'pn')
                norm_c(X0, xk0, Hk, hkeys, pb[6], 'pn', RS[0], 'rs0', g0)
                for tt in range(8):
                    X, xk = xs(tt)
                    for f in range(NF):
                        b = f % 2
                        for k in range(8):
                            tr.op('pe', lambda e, k=k, f=f, b=b: e.matmul(
                                out=pb[b][:], lhsT=WG[:, k, f * 128:(f + 1) * 128], rhs=H[:, k, :],
                                start=(k == 0), stop=(k == 7)), r=['WG' + ('a' if f < NF // 2 else 'b'), hkeys[k]],
                                w=['pg%d' % b])
                        for k in range(8):
                            tr.op('pe', lambda e, k=k, f=f, b=b: e.matmul(
                                out=pb[2 + b][:], lhsT=WU[:, k, f * 128:(f + 1) * 128], rhs=H[:, k, :],
                                start=(k == 0), stop=(k == 7)), r=['WU' + ('a' if f < NF // 2 else 'b'), hkeys[k]],
                                w=['pu%d' % b])
                        tr.op('act', lambda e, b=b: e.activation(out=SG[b][:], in_=pb[b][:], func=AF.Silu),
                              r=['pg%d' % b], w=['sg%d' % b])
                        tr.op('dve', lambda e, b=b, f=f: e.tensor_tensor(out=AT[:, f, :], in0=SG[b][:],
                                                                         in1=pb[2 + b][:], op=ALU.mult),
                              r=['sg%d' % b, 'pu%d' % b], w=['at%d' % f])
                        if f == 2 and tt > 0:
                            post_bc(tt - 1)
                    if tt + 1 < 8:
                        load(tt + 1)
                        Xn, xkn = xs(tt + 1)
                        norm_a(Xn, xkn, H[:], hkeys)
                    for dk in range(8):
                        b = dk % 2
                        for f in range(NF):
                            tr.op('pe', lambda e, f=f, dk=dk, b=b: e.matmul(
                                out=pb[4 + b][:], lhsT=WD[:, f, dk * 128:(dk + 1) * 128], rhs=AT[:, f, :],
                                start=(f == 0), stop=(f == NF - 1)), r=['WD', 'at%d' % f], w=['pd%d' % b])
                        tr.op('dve', lambda e, dk=dk, b=b, X=X: e.scalar_tensor_tensor(
                            out=X[:, dk, :], in0=pb[4 + b][:], scalar=0.5, in1=X[:, dk, :],
                            op0=ALU.mult, op1=ALU.add), r=['pd%d' % b, xk], w=[xk])
                        if dk == 2 and tt + 1 < 8:
                            norm_b(Hk, hkeys, pb[6], 'pn')
                            norm_c(Xn, xkn, Hk, hkeys, pb[6], 'pn', RS[0], 'rs0', g0)
                    post_a(tt)
                post_bc(7)
            tr.barrier()

        def attention_phase():
            with contextlib.ExitStack() as a_:
                def asb(name, shape, dt):
                    return a_.enter_context(nc.sbuf_tensor(name, list(shape), dt))
                CBt = asb("CBt", [128, 512], BF16)
                WBt = asb("WBt", [128, 512], BF16)
                CMt = asb("CMt", [128, 512], BF16)
                BDt = asb("BDt", [128, 384], BF16)
                U0t = asb("U0t", [128, 128], F32)
                L0t = asb("L0t", [128, 128], F32)
                GAT = asb("GAT", [128, 512], F32)
                QN = [asb("QN%d" % i, [128, 512], BF16) for i in range(2)]
                ET = [asb("ET%d" % i, [128, 512], BF16) for i in range(3)]
                SCO = asb("SCO", [128, 64], F32)
                M8 = asb("M8", [128, 16], F32)
                WK = asb("WK", [128, 64], F32)
                SELW = asb("SELW", [128, 128], BF16)
                RZ = asb("RZ", [128, 4], F32)
                DEN = asb("DEN", [128, 12], F32)
                ATT = asb("ATT", [128, 512], F32)
                ATN = asb("ATN", [128, 512], BF16)
                JUNK = asb("JUNK", [128, 512], F32)
                SSQ = asb("SSQ", [128, 1], F32)
                MST = asb("MST", [128, 4, 128], BF16)
                tr.dma('pool', 'aconst', CBt[:], c_CB[:, :], w=['ac'])
                tr.dma('pool', 'aconst', WBt[:], c_WB[:, :], w=['ac'])
                tr.dma('pool', 'aconst', CMt[:], c_CM[:, :], w=['ac'])
                tr.dma('pool', 'aconst', BDt[:], c_BD[:, :], w=['ac'])
                tr.dma('sp', 'aconst', U0t[:], c_U0[:, :], w=['ac'])
                tr.dma('sp', 'aconst', L0t[:], c_L0[:, :], w=['ac'])
                tr.dma('sp', 'aconst', GAT[:], grow_attn.partition_broadcast(128), w=['ac'])
                tr.op('dve', lambda e: e.memset(SELW[:], 0.0), w=['selw'])
                OS = pb[3][:, 0:260].rearrange("p (r x) -> p r x", x=65)
                OW = pb[4][:, 0:260].rearrange("p (r x) -> p r x", x=65)
                OC = pb[5][:, 0:260].rearrange("p (r x) -> p r x", x=65)
                IM = pb[6][:, 0:256].rearrange("p (r x) -> p r x", x=64)
                PT = pb[5][:, 320:384].bitcast(BF16)
                PT4 = pb[6][:, 256:512].bitcast(BF16)
                PA = pb[7]
                ETA = [asb("ETA%d" % i, [128, 512], BF16) for i in range(2)]
                dq = []

                def pump(k):
                    for _ in range(min(k, len(dq))):
                        dq.pop(0)()

                def deferred(f, *a):
                    tr.defer = dq
                    f(*a)
                    tr.defer = None
                sring = [0]
                ering = [0]

                def nexts():
                    b = sring[0] % 3
                    sring[0] += 1
                    return pb[b], 'ps%d' % b

                def nexte():
                    b = ering[0] % 3
                    ering[0] += 1
                    return ET[b], 'et%d' % b

                qview = qTd.rearrange("p (c x) -> p c x", c=32)
                QN3 = [QN[0], QN[1], asb("QN2", [128, 512], BF16)]
                OCS = [asb("OCS%d" % i, [128, 4, 65], F32) for i in range(4)]
                TMPA = asb("TMPA", [128, 4, 64], F32)
                TMPB = asb("TMPB", [128, 4, 64], F32)
                OSS = [asb("OSS%d" % i, [128, 4, 65], F32) for i in range(2)]
                OWS = [asb("OWS%d" % i, [128, 4, 65], F32) for i in range(2)]
                ATN2 = [ATN, asb("ATN1", [128, 512], BF16)]
                NIT = 64
                mixv = mixT.rearrange("(k p) t -> p k t", p=128)
                pending = []

                def qload(it):
                    c, g = divmod(it, 2)
                    sl = it % 3
                    tr.dma('sp', 'qin%d' % sl, QN3[sl][0:64, :], qview[:, c, g * 512:(g + 1) * 512], r=['qTd'],
                           w=['qnq%d' % sl])

                def stageA(it):
                    c, g = divmod(it, 2)
                    sl = it % 3
                    Q = QN3[sl]
                    qq, qs = 'qnq%d' % sl, 'qns%d' % sl
                    nts = 1 if c < 16 else 2
                    tl = []
                    for nt in range(nts):
                        Mn = min(128, 8 * c + 7 - 128 * nt)
                        m = 8 * c - 128 * nt
                        need = m <= 192
                        PS, psk = PA, 'pa'
                        tr.op('pe', lambda e, PS=PS, Mn=Mn, nt=nt, need=need: e.matmul(
                            out=PS[0:Mn, :], lhsT=KC[g][:, 128 * nt:128 * nt + Mn], rhs=Q[0:64, :],
                            start=True, stop=not need), r=['KC%d' % g, qq], w=[psk])
                        if need:
                            tr.op('pe', lambda e, PS=PS, Mn=Mn, m=m: e.matmul(
                                out=PS[0:Mn, :], lhsT=BDt[:, 192 - m:192 - m + Mn], rhs=CMt[:],
                                start=False, stop=True), r=['ac'], w=[psk])
                        Et, ek = ETA[nt], 'eta%d' % nt
                        tr.op('act', lambda e, PS=PS, Et=Et, Mn=Mn: e.activation(
                            out=Et[0:Mn, :], in_=PS[0:Mn, :], func=AF.Exp, scale=0.125), r=[psk], w=[ek])
                        tl.append((nt, Mn, Et, ek))
                    first = True
                    for (nt, Mn, Et, ek) in tl:
                        for r in range(4):
                            tr.op('pe', lambda e, Et=Et, Mn=Mn, nt=nt, r=r, first=first: e.matmul(
                                out=OC[:, r, :], lhsT=Et[0:Mn, r * 128:(r + 1) * 128], rhs=VCX[g][0:Mn, nt, 0:65],
                                start=first, stop=(nt == nts - 1), skip_group_check=True),
                                r=[ek, 'VCX%d' % g], w=['oc'])
                            tr.op('pe', lambda e, Et=Et, Mn=Mn, nt=nt, r=r, first=first: e.matmul(
                                out=IM[:, r, :], lhsT=Et[0:Mn, r * 128:(r + 1) * 128], rhs=VCX[g][0:Mn, nt, 65:129],
                                start=first, stop=(nt == nts - 1), skip_group_check=True),
                                r=[ek, 'VCX%d' % g], w=['im'])
                            first = False
                    OCb, ock = OCS[it % 4], 'ocs%d' % (it % 4)
                    tr.op('dve', lambda e, OCb=OCb: e.tensor_copy(out=OCb[:], in_=OC), r=['oc'], w=[ock])
                    tr.op('dve', lambda e, OCb=OCb: e.tensor_scalar(out=RZ[:], in0=OCb[:, :, 64], scalar1=1e-30,
                                                                   scalar2=None, op0=ALU.max), r=[ock], w=['rz'])
                    tr.op('dve', lambda e: e.reciprocal(out=RZ[:], in_=RZ[:]), r=['rz'], w=['rz'])
                    tr.op('dve', lambda e: e.tensor_scalar(out=SCO[:], in0=IM[:, 0, :], scalar1=RZ[:, 0:1],
                                                          scalar2=None, op0=ALU.mult), r=['im', 'rz'], w=['sco'])
                    for r in range(1, 4):
                        tr.op('dve', lambda e, r=r: e.scalar_tensor_tensor(
                            out=SCO[:], in0=IM[:, r, :], scalar=RZ[:, r:r + 1], in1=SCO[:],
                            op0=ALU.mult, op1=ALU.add), r=['im', 'rz', 'sco'], w=['sco'])
                    lo = 62 - 2 * c
                    tr.op('dve', lambda e, lo=lo: e.tensor_tensor(out=SCO[:], in0=SCO[:], in1=L0t[:, lo:lo + 64],
                                                                 op=ALU.max), r=['sco', 'ac'], w=['sco'])
                    tr.op('dve', lambda e, lo=lo: e.tensor_tensor(out=SCO[:], in0=SCO[:], in1=U0t[:, lo:lo + 64],
                                                                 op=ALU.min), r=['sco', 'ac'], w=['sco'])
                    tr.op('dve', lambda e: e.memset(SCO[:, 0:1], 3e30), r=['sco'], w=['sco'])
                    tr.op('dve', lambda e: e.max(out=M8[:, 0:8], in_=SCO[:]), r=['sco'], w=['m8'])
                    tr.op('dve', lambda e: e.match_replace(out=WK[:], in_to_replace=M8[:, 0:8], in_values=SCO[:],
                                                          imm_value=-3e38), r=['sco', 'm8'], w=['wk'])
                    tr.op('dve', lambda e: e.max(out=M8[:, 8:16], in_=WK[:]), r=['wk'], w=['m8'])
                    tr.op('dve', lambda e: e.tensor_scalar(out=SELW[:, 64:128], in0=SCO[:], scalar1=M8[:, 15:16],
                                                          scalar2=None, op0=ALU.is_ge), r=['sco', 'm8'], w=['selw'])
                    tr.op('pe', lambda e: e.transpose(out=PT, in_=SELW[:], identity=IDN[:]), r=['selw', 'c'], w=['pt'])
                    tr.op('dve', lambda e, Q=Q: e.tensor_scalar(
                        out=Q[64:128, :].rearrange("p (r q) -> p r q", r=4),
                        in0=PT[64:128, :].unsqueeze(1).to_broadcast([64, 4, 128]),
                        scalar1=-1.0, scalar2=BIGM, op0=ALU.add, op1=ALU.mult), r=['pt'], w=[qs])

                def finish_pe(c):
                    AN = ATN2[c % 2]
                    for j in range(4):
                        tr.op('pe', lambda e, j=j, AN=AN: e.transpose(out=PT4[:, j * 128:(j + 1) * 128],
                                                                     in_=AN[:, j * 128:(j + 1) * 128], identity=IDN[:]),
                              r=['atn%d' % (c % 2), 'c'], w=['pt4'])
                    tr.op('dve', lambda e: e.tensor_copy(out=MST[:].rearrange("p a b -> p (a b)"), in_=PT4),
                          r=['pt4'], w=['mst'])
                    tr.dma('sp', 'mout', mixv[:, 0:4, c * 128:(c + 1) * 128], MST[:], r=['mst'], w=['mixT'])

                def stageB(it):
                    c, g = divmod(it, 2)
                    sl = it % 3
                    Q = QN3[sl]
                    qq, qs = 'qnq%d' % sl, 'qns%d' % sl
                    k0 = max(0, c - 4)
                    tiles = [('w', kt) for kt in range(k0, c + 1)] + [('s', kt) for kt in range(c + 1)]
                    n = len(tiles)
                    info = {}

                    def qk(i):
                        br, kt = tiles[i]
                        PS, psk = nexts()
                        extra = []
                        if kt == c:
                            extra.append(CBt)
                        if br == 'w' and kt == c - 4:
                            extra.append(WBt)
                        if br == 'w':
                            tr.op('pe', lambda e, PS=PS, kt=kt, ne=len(extra): e.matmul(
                                out=PS[:], lhsT=KW[g][:, kt * 128:(kt + 1) * 128], rhs=Q[0:64, :],
                                start=True, stop=(ne == 0)), r=['KW%d' % g, qq], w=[psk])
                        else:
                            tr.op('pe', lambda e, PS=PS, kt=kt, ne=len(extra): e.matmul(
                                out=PS[:], lhsT=KE[g][:, kt * 128:(kt + 1) * 128], rhs=Q[:, :],
                                start=True, stop=(ne == 0)), r=['KE%d' % g, qq, qs], w=[psk])
                        for j, bt in enumerate(extra):
                            tr.op('pe', lambda e, PS=PS, bt=bt, last=(j == len(extra) - 1): e.matmul(
                                out=PS[:], lhsT=IDN[:], rhs=bt[:], start=False, stop=last), r=['ac', 'c'], w=[psk])
                        Et, ek = nexte()
                        tr.op('act', lambda e, PS=PS, Et=Et: e.activation(
                            out=Et[:], in_=PS[:], func=AF.Exp, scale=0.125), r=[psk], w=[ek])
                        info[i] = (Et, ek)

                    started = {'w': False, 's': False}
                    lastw = max(i for i in range(n) if tiles[i][0] == 'w')

                    def pv(i):
                        br, kt = tiles[i]
                        Et, ek = info[i]
                        O, ok, V, vk = (OW, 'ow', VW1[g], 'VW1%d' % g) if br == 'w' else (OS, 'os', VS1[g], 'VS1%d' % g)
                        last = (i == lastw) if br == 'w' else (i == n - 1)
                        for r in range(4):
                            st = not started[br]
                            started[br] = True
                            tr.op('pe', lambda e, Et=Et, kt=kt, r=r, O=O, V=V, st=st, last=last: e.matmul(
                                out=O[:, r, :], lhsT=Et[:, r * 128:(r + 1) * 128], rhs=V[:, kt, :],
                                start=st, stop=last, skip_group_check=True), r=[ek, vk], w=[ok])

                    for i in range(min(2, n)):
                        qk(i)
                    for i in range(n):
                        if i + 2 < n:
                            qk(i + 2)
                        pv(i)
                        left = max(1, n - 1 - i)
                        pump((len(dq) + left - 1) // left)
                    pump(len(dq))
                    b2 = it % 2
                    tr.op('dve', lambda e: e.tensor_copy(out=OWS[b2][:], in_=OW), r=['ow'], w=['ows%d' % b2])
                    tr.op('dve', lambda e: e.tensor_copy(out=OSS[b2][:], in_=OS), r=['os'], w=['oss%d' % b2])

                def tailB(it):
                    c, g = divmod(it, 2)
                    b2 = it % 2
                    b4 = it % 4
                    srcs = ((OCS[b4], 'ocs%d' % b4), (OSS[b2], 'oss%d' % b2), (OWS[b2], 'ows%d' % b2))
                    for bi, (O, ok) in enumerate(srcs):
                        tr.op('dve', lambda e, O=O, bi=bi: e.tensor_scalar(
                            out=DEN[:, bi * 4:bi * 4 + 4], in0=O[:, :, 64], scalar1=1e-30, scalar2=None,
                            op0=ALU.max), r=[ok], w=['den'])
                    tr.op('dve', lambda e: e.reciprocal(out=DEN[:], in_=DEN[:]), r=['den'], w=['den'])
                    gv = GT[:, c, 12 * g:12 * g + 12].rearrange("p (r b) -> p b r", b=3)
                    tr.op('dve', lambda e, gv=gv: e.tensor_tensor(
                        out=DEN[:].rearrange("p (b r) -> p b r", b=3), in0=DEN[:].rearrange("p (b r) -> p b r", b=3),
                        in1=gv, op=ALU.mult), r=['den', 'GT'], w=['den'])
                    ATTg = ATT[:, g * 256:(g + 1) * 256].rearrange("p (r x) -> p r x", r=4)
                    denb = lambda bi: DEN[:, bi * 4:bi * 4 + 4].unsqueeze(2).to_broadcast([128, 4, 64])
                    tr.op('dve', lambda e: e.tensor_tensor(out=ATTg, in0=OCS[b4][:, :, 0:64], in1=denb(0), op=ALU.mult),
                          r=['ocs%d' % b4, 'den'], w=['att'])
                    tr.op('dve', lambda e: e.tensor_tensor(out=TMPA[:], in0=OSS[b2][:, :, 0:64], in1=denb(1), op=ALU.mult),
                          r=['oss%d' % b2, 'den'], w=['tmpa'])
                    tr.op('dve', lambda e: e.tensor_tensor(out=TMPB[:], in0=OWS[b2][:, :, 0:64], in1=denb(2), op=ALU.mult),
                          r=['ows%d' % b2, 'den'], w=['tmpb'])
                    tr.op('dve', lambda e: e.tensor_tensor(out=ATTg, in0=ATTg, in1=TMPA[:], op=ALU.add),
                          r=['att', 'tmpa'], w=['att'])
                    tr.op('dve', lambda e: e.tensor_tensor(out=ATTg, in0=ATTg, in1=TMPB[:], op=ALU.add),
                          r=['att', 'tmpb'], w=['att'])
                    if g == 1:
                        AN, ank = ATN2[c % 2], 'atn%d' % (c % 2)
                        tr.op('act', lambda e: e.activation(out=JUNK[:], in_=ATT[:], func=AF.Square, accum_out=SSQ[:]),
                              r=['att'], w=['junk', 'ssq'])
                        tr.op('dve', lambda e: e.tensor_scalar(out=SSQ[:], in0=SSQ[:], scalar1=1.0 / 512, scalar2=EPS,
                                                              op0=ALU.mult, op1=ALU.add), r=['ssq'], w=['ssq'])
                        tr.op('act', lambda e: e.activation(out=SSQ[:], in_=SSQ[:], func=AF.Ln), r=['ssq'], w=['ssq'])
                        tr.op('act', lambda e: e.activation(out=SSQ[:], in_=SSQ[:], func=AF.Exp, scale=-0.5),
                              r=['ssq'], w=['ssq'])
                        tr.op('dve', lambda e, AN=AN: e.scalar_tensor_tensor(out=AN[:], in0=ATT[:], scalar=SSQ[:, 0:1],
                                                                            in1=GAT[:], op0=ALU.mult, op1=ALU.mult),
                              r=['att', 'ssq', 'ac'], w=[ank])

                qload(0)
                qload(1)
                stageA(0)
                for it in range(NIT):
                    qa, qb = [], []
                    if it >= 1:
                        tr.defer = qa
                        tailB(it - 1)
                        tr.defer = None
                    if it + 1 < NIT:
                        tr.defer = qb
                        stageA(it + 1)
                        tr.defer = None
                    while qa or qb:
                        if qb:
                            dq.append(qb.pop(0))
                        if qa:
                            dq.append(qa.pop(0))
                    if it >= 1 and (it - 1) % 2 == 1:
                        deferred(finish_pe, (it - 1) // 2)
                    if it + 2 < NIT:
                        qload(it + 2)
                    stageB(it)
                tailB(NIT - 1)
                finish_pe((NIT - 1) // 2)
            tr.barrier()

        def s5_phase():
            PI = float(np.pi)
            with contextlib.ExitStack() as s_:
                def ssb(name, shape, dt):
                    return s_.enter_context(nc.sbuf_tensor(name, list(shape), dt))
                ROW = [ssb("ROW%d" % i, [128, 2048], F32) for i in range(3)]
                PRE_RE = ssb("PRE_RE", [128, 2048], F32)
                PRE_IM = ssb("PRE_IM", [128, 2048], F32)
                TH = ssb("TH", [128, 2048], F32)
                TMPI = ssb("TMPI", [128, 2048], mybir.dt.int32)
                JC = ssb("JC", [128, 1], F32)
                NJC = ssb("NJC", [128, 1], F32)
                NPI = ssb("NPI", [128, 1], F32)
                COL = ssb("COL", [128, 48], F32)
                IOT = ssb("IOT", [128, 128], F32)
                POST_RE = ssb("POST_RE", [128, 16, 128], F32)
                POST_IM = ssb("POST_IM", [128, 16, 128], F32)
                A128 = ssb("A128", [128, 2, 16], F32)
                SM = [ssb("SM%d" % i, [128, 16], F32) for i in range(3)]
                REP = ssb("REP", [128, 4, 3, 64], F32)
                BT = ssb("BT", [128, 4, 2, 64], F32)
                BB = ssb("BB", [128, 4, 2, 64], F32)
                RT = [ssb("RT%d" % i, [128, 4, 64], F32) for i in range(6)]
                BBD = ssb("BBD", [128, 4, 1024], BF16)
                MASKB = ssb("MASKB", [128, 8], F32)
                CC = ssb("CC", [128, 16, 2, 16], F32)
                MASKC = ssb("MASKC", [128, 4, 8], F32)
                CMAT = ssb("CMAT", [128, 16, 2, 128], BF16)
                DCOL = ssb("DCOL", [128, 4], F32)
                BGL = ssb("BGL", [128, 4], F32)
                WGL = ssb("WGL", [128, 4, 512], BF16)
                TRI = ssb("TRI", [128, 128], BF16)
                UT = [ssb("UT%d" % i, [128, 4, 512], BF16) for i in range(2)]
                T1B = ssb("T1B", [128, 8, 512], BF16)
                T2B = ssb("T2B", [128, 8, 512], BF16)
                NTRI = ssb("NTRI", [128, 128], BF16)
                NCM = ssb("NCM", [128, 16, 128], BF16)
                ZCF = ssb("ZCF", [128, 16, 2, 128], F32)
                CRE = ssb("CRE", [128, 16], F32)
                CIM = ssb("CIM", [128, 16], F32)
                TQ = [ssb("TQ%d" % i, [128, 16], F32) for i in range(4)]
                XU1 = ssb("XU1", [128, 16, 2, 128], BF16)
                XU2 = ssb("XU2", [128, 16, 2, 128], BF16)
                CARRY = ssb("CARRY", [128, 16, 2], F32)
                CT = [ssb("CT%d" % i, [128, 2, 2], F32) for i in range(2)]
                Y = ROW[0][:].rearrange("p (a b) -> p a b", a=4)
                YB = ROW[1][:].rearrange("p (a b) -> p a b", a=4)
                YG = ROW[2][:].rearrange("p (a b) -> p a b", a=4)
                YGB = ssb("YGB", [128, 4, 512], BF16)
                SGL = [ssb("SGL%d" % i, [128, 512], F32) for i in range(2)]
                S = TH[:].rearrange("p (a b) -> p a b", a=4)
                SQ = ssb("SQ", [128, 4, 512], BF16)
                RS5 = ssb("RS5", [128, 512], F32)
                OUT = ssb("OUT", [128, 4, 512], BF16)
                ck = ['s5c']
                for i in range(3):
                    tr.dma('sp', 's5const', ROW[i][:], s5_rows[i:i + 1, :].partition_broadcast(128), w=ck)
                tr.dma('sp', 's5const', JC[:], c_jcol[:, :], w=ck)
                tr.dma('sp', 's5const', COL[:], s5_cols[:, :], w=ck)
                tr.dma('sp', 's5const', IOT[:], c_iota[:, :], w=ck)
                tr.dma('sp', 's5const', REP[:].rearrange("p a b c -> p (a b c)"), s5_rep[:, :], w=ck)
                tr.dma('sp', 's5const', BT[:].rearrange("p a b c -> p (a b c)"), s5_bT[:, :], w=ck)
                tr.dma('sp', 's5const', MASKB[:], c_maskB[:, :], w=ck)
                tr.dma('sp', 's5const', CC[:].rearrange("p a b c -> p (a b c)"), s5_c[:, :], w=ck)
                tr.dma('sp', 's5const', MASKC[:].rearrange("p a b -> p (a b)"), c_maskC[:, :], w=ck)
                tr.dma('sp', 's5const', DCOL[:], s5_dcol[:, :], w=ck)
                tr.dma('sp', 's5const', BGL[:], bglu_col[:, :], w=ck)
                tr.dma('pool', 's5const', TRI[:], c_tri[:, :], w=ck)
                for k in range(4):
                    tr.dma('pool', 's5const', WGL[:, k, :], w_glu[k * 128:(k + 1) * 128, :], w=ck)
                V = lambda fn, r, w: tr.op('dve', fn, r=r, w=w)
                A = lambda fn, r, w: tr.op('act', fn, r=r, w=w)
                V(lambda e: e.memset(NPI[:], -PI), [], ck)
                V(lambda e: e.tensor_scalar(out=NJC[:], in0=JC[:], scalar1=-1.0, scalar2=None, op0=ALU.mult), ck, ck)
                V(lambda e: e.memset(CRE[:], 0.0), [], ['carry'])
                V(lambda e: e.memset(CIM[:], 0.0), [], ['carry'])

                def sincos(out_sin, out_cos, theta, tmpf, tmpi):
                    for out, shift in ((out_sin, 0.0), (out_cos, 0.5 * PI)):
                        V(lambda e, shift=shift: e.tensor_scalar(out=tmpf, in0=theta, scalar1=shift, scalar2=1.0 / (2 * PI),
                                                                 op0=ALU.add, op1=ALU.mult), ck, ck)
                        V(lambda e: e.tensor_copy(out=tmpi, in_=tmpf), ck, ck)
                        V(lambda e: e.tensor_copy(out=tmpf, in_=tmpi), ck, ck)
                        V(lambda e, out=out: e.scalar_tensor_tensor(out=out, in0=tmpf, scalar=-2 * PI, in1=theta,
                                                                    op0=ALU.mult, op1=ALU.add), ck, ck)
                        if shift:
                            V(lambda e, out=out, shift=shift: e.tensor_scalar(out=out, in0=out, scalar1=shift, scalar2=None,
                                                                              op0=ALU.add), ck, ck)
                        V(lambda e, out=out: e.tensor_scalar(out=tmpf, in0=out, scalar1=PI, scalar2=-2 * PI,
                                                             op0=ALU.is_gt, op1=ALU.mult), ck, ck)
                        V(lambda e, out=out: e.tensor_tensor(out=out, in0=out, in1=tmpf, op=ALU.add), ck, ck)
                        A(lambda e, out=out: e.activation(out=out, in_=out, func=AF.Sin), ck, ck)

                A(lambda e: e.activation(out=ROW[2][:], in_=ROW[2][:], func=AF.Exp), ck, ck)
                V(lambda e: e.tensor_tensor(out=ROW[0][:], in0=ROW[0][:], in1=ROW[2][:], op=ALU.mult), ck, ck)
                V(lambda e: e.tensor_tensor(out=ROW[1][:], in0=ROW[1][:], in1=ROW[2][:], op=ALU.mult), ck, ck)
                A(lambda e: e.activation(out=ROW[2][:], in_=ROW[0][:], func=AF.Exp, scale=NJC[:, 0:1]), ck, ck)
                V(lambda e: e.tensor_scalar(out=TH[:], in0=ROW[1][:], scalar1=JC[:, 0:1], scalar2=None, op0=ALU.mult),
                  ck, ck)
                sincos(PRE_IM[:], PRE_RE[:], TH[:], ROW[0][:], TMPI[:])
                V(lambda e: e.tensor_tensor(out=PRE_RE[:], in0=PRE_RE[:], in1=ROW[2][:], op=ALU.mult), ck, ck)
                V(lambda e: e.scalar_tensor_tensor(out=PRE_IM[:], in0=PRE_IM[:], scalar=-1.0, in1=ROW[2][:],
                                                   op0=ALU.mult, op1=ALU.mult), ck, ck)
                LRc, LIc, DTc = COL[:, 0:16], COL[:, 16:32], COL[:, 32:48]
                A(lambda e: e.activation(out=DTc, in_=DTc, func=AF.Exp), ck, ck)
                V(lambda e: e.tensor_tensor(out=LRc, in0=LRc, in1=DTc, op=ALU.mult), ck, ck)
                V(lambda e: e.tensor_tensor(out=LIc, in0=LIc, in1=DTc, op=ALU.mult), ck, ck)
                iob = IOT[:].unsqueeze(1).to_broadcast([128, 16, 128])
                THv = TH[:].rearrange("p (a b) -> p a b", a=16)
                R0v = ROW[0][:].rearrange("p (a b) -> p a b", a=16)
                R2v = ROW[2][:].rearrange("p (a b) -> p a b", a=16)
                V(lambda e: e.tensor_tensor(out=R2v, in0=iob, in1=LRc.unsqueeze(2).to_broadcast([128, 16, 128]),
                                            op=ALU.mult), ck, ck)
                A(lambda e: e.activation(out=ROW[2][:], in_=ROW[2][:], func=AF.Exp), ck, ck)
                V(lambda e: e.tensor_tensor(out=THv, in0=iob, in1=LIc.unsqueeze(2).to_broadcast([128, 16, 128]),
                                            op=ALU.mult), ck, ck)
                sincos(POST_IM[:].rearrange("p a b -> p (a b)"), POST_RE[:].rearrange("p a b -> p (a b)"), TH[:],
                       ROW[0][:], TMPI[:])
                V(lambda e: e.tensor_tensor(out=POST_RE[:], in0=POST_RE[:], in1=R2v, op=ALU.mult), ck, ck)
                V(lambda e: e.tensor_tensor(out=POST_IM[:], in0=POST_IM[:], in1=R2v, op=ALU.mult), ck, ck)
                V(lambda e: e.tensor_scalar(out=SM[0][:], in0=LRc, scalar1=128.0, scalar2=None, op0=ALU.mult), ck, ck)
                A(lambda e: e.activation(out=SM[0][:], in_=SM[0][:], func=AF.Exp), ck, ck)
                V(lambda e: e.tensor_scalar(out=SM[1][:], in0=LIc, scalar1=128.0, scalar2=None, op0=ALU.mult), ck, ck)
                sincos(A128[:, 1, :], A128[:, 0, :], SM[1][:], SM[2][:], TMPI[:, 0:16])
                V(lambda e: e.tensor_tensor(out=A128[:, 0, :], in0=A128[:, 0, :], in1=SM[0][:], op=ALU.mult), ck, ck)
                V(lambda e: e.tensor_tensor(out=A128[:, 1, :], in0=A128[:, 1, :], in1=SM[0][:], op=ALU.mult), ck, ck)
                lr, li, ldt = REP[:, :, 0, :], REP[:, :, 1, :], REP[:, :, 2, :]
                dtv, ldr, ldi, mag, sn, cs = [t[:] for t in RT]
                A(lambda e: e.activation(out=dtv, in_=ldt, func=AF.Exp), ck, ck)
                V(lambda e: e.tensor_tensor(out=ldr, in0=lr, in1=dtv, op=ALU.mult), ck, ck)
                V(lambda e: e.tensor_tensor(out=ldi, in0=li, in1=dtv, op=ALU.mult), ck, ck)
                A(lambda e: e.activation(out=mag, in_=ldr, func=AF.Exp), ck, ck)
                sincos(sn, cs, ldi, dtv, TMPI[:, 0:256].rearrange("p (a b) -> p a b", a=4))
                V(lambda e: e.tensor_tensor(out=cs, in0=cs, in1=mag, op=ALU.mult), ck, ck)
                V(lambda e: e.tensor_tensor(out=sn, in0=sn, in1=mag, op=ALU.mult), ck, ck)
                V(lambda e: e.tensor_scalar(out=cs, in0=cs, scalar1=-1.0, scalar2=None, op0=ALU.add), ck, ck)
                V(lambda e: e.tensor_tensor(out=dtv, in0=lr, in1=lr, op=ALU.mult), ck, ck)
                V(lambda e: e.tensor_tensor(out=mag, in0=li, in1=li, op=ALU.mult), ck, ck)
                V(lambda e: e.tensor_tensor(out=dtv, in0=dtv, in1=mag, op=ALU.add), ck, ck)
                V(lambda e: e.reciprocal(out=dtv, in_=dtv), ck, ck)
                V(lambda e: e.tensor_tensor(out=ldr, in0=cs, in1=lr, op=ALU.mult), ck, ck)
                V(lambda e: e.tensor_tensor(out=mag, in0=sn, in1=li, op=ALU.mult), ck, ck)
                V(lambda e: e.tensor_tensor(out=ldr, in0=ldr, in1=mag, op=ALU.add), ck, ck)
                V(lambda e: e.tensor_tensor(out=ldr, in0=ldr, in1=dtv, op=ALU.mult), ck, ck)
                V(lambda e: e.tensor_tensor(out=ldi, in0=sn, in1=lr, op=ALU.mult), ck, ck)
                V(lambda e: e.tensor_tensor(out=mag, in0=cs, in1=li, op=ALU.mult), ck, ck)
                V(lambda e: e.tensor_tensor(out=ldi, in0=ldi, in1=mag, op=ALU.subtract), ck, ck)
                V(lambda e: e.tensor_tensor(out=ldi, in0=ldi, in1=dtv, op=ALU.mult), ck, ck)
                br, bi_ = BT[:, :, 0, :], BT[:, :, 1, :]
                V(lambda e: e.tensor_tensor(out=BB[:, :, 0, :], in0=ldr, in1=br, op=ALU.mult), ck, ck)
                V(lambda e: e.tensor_tensor(out=mag, in0=ldi, in1=bi_, op=ALU.mult), ck, ck)
                V(lambda e: e.tensor_tensor(out=BB[:, :, 0, :], in0=BB[:, :, 0, :], in1=mag, op=ALU.subtract), ck, ck)
                V(lambda e: e.tensor_tensor(out=BB[:, :, 1, :], in0=ldr, in1=bi_, op=ALU.mult), ck, ck)
                V(lambda e: e.tensor_tensor(out=mag, in0=ldi, in1=br, op=ALU.mult), ck, ck)
                V(lambda e: e.tensor_tensor(out=BB[:, :, 1, :], in0=BB[:, :, 1, :], in1=mag, op=ALU.add), ck, ck)
                BBDv = BBD[:].rearrange("p k (a r g x) -> p k a r g x", a=4, r=2, g=2)
                for kt in range(4):
                    for a in range(4):
                        for ri in range(2):
                            V(lambda e, kt=kt, a=a, ri=ri: e.tensor_tensor(
                                out=BBDv[:, kt, a, ri, :, :],
                                in0=BB[:, kt, ri, :].unsqueeze(1).to_broadcast([128, 2, 64]),
                                in1=MASKB[:, 2 * a:2 * a + 2].unsqueeze(2).to_broadcast([128, 2, 64]),
                                op=ALU.mult), ck, ck)
                V(lambda e: e.tensor_scalar(out=NTRI[:], in0=TRI[:], scalar1=-1.0, scalar2=None, op0=ALU.mult), ck, ck)
                CMv = CMAT[:].rearrange("p a r (g h) -> p a r g h", g=8)
                NCMv = NCM[:].rearrange("p a (g h) -> p a g h", g=8)
                for pr in range(16):
                    in0 = CC[:, pr, 0, :].unsqueeze(1).to_broadcast([128, 8, 16])
                    in1 = MASKC[:, pr % 4, :].unsqueeze(2).to_broadcast([128, 8, 16])
                    V(lambda e, pr=pr, in0=in0, in1=in1: e.scalar_tensor_tensor(
                        out=NCMv[:, pr, :, :], in0=in0, scalar=-1.0, in1=in1, op0=ALU.mult, op1=ALU.mult), ck, ck)
                for pr in range(16):
                    for ri in range(2):
                        in0 = CC[:, pr, ri, :].unsqueeze(1).to_broadcast([128, 8, 16])
                        in1 = MASKC[:, pr % 4, :].unsqueeze(2).to_broadcast([128, 8, 16])
                        if ri == 0:
                            V(lambda e, pr=pr, in0=in0, in1=in1: e.tensor_tensor(out=CMv[:, pr, 0, :, :], in0=in0, in1=in1,
                                                                               op=ALU.mult), ck, ck)
                        else:
                            V(lambda e, pr=pr, in0=in0, in1=in1: e.scalar_tensor_tensor(
                                out=CMv[:, pr, 1, :, :], in0=in0, scalar=-1.0, in1=in1, op0=ALU.mult, op1=ALU.mult),
                                ck, ck)
                uview = uTd.rearrange("p (k t) -> p k t", k=4)
                mview = mixT.rearrange("(k p) t -> p k t", p=128)
                bring = [0]
                def uload(tt):
                    us = tt % 2
                    tr.dma('sp', 'uin%d' % us, UT[us][:], uview[:, :, tt * 512:tt * 512 + 512], r=['uTd'], w=['ut%d' % us])
                def pre(cc, jj):
                    tt, sub = divmod(cc, 4)
                    us = tt % 2; uk = 'ut%d' % us; U = UT[us]
                    tsl = slice(sub * 128, (sub + 1) * 128)
                    kt, half = divmod(jj, 2)
                    i = bring[0] % 2
                    bring[0] += 1
                    PB = pb[i]
                    pk = 'pbu%d' % i
                    pr0 = 4 * kt + 2 * half
                    tr.op('pe', lambda e: e.matmul(
                        out=PB[:], lhsT=U[:, kt, tsl], rhs=BBD[:, kt, half * 512:(half + 1) * 512],
                        start=True, stop=True), r=[uk, 's5c'], w=[pk])
                    PBv = PB[:].rearrange("p (a r x) -> p a r x", a=2, r=2)
                    pre_r = PRE_RE[:, pr0 * 128:pr0 * 128 + 256].rearrange("p (a x) -> p a x", a=2) \
                        .unsqueeze(2).to_broadcast([128, 2, 2, 128])
                    pre_i = PRE_IM[:, pr0 * 128:pr0 * 128 + 256].rearrange("p (a x) -> p a x", a=2) \
                        .unsqueeze(2).to_broadcast([128, 2, 2, 128])
                    t1 = T1B[:, jj, :].rearrange("p (a r x) -> p a r x", a=2, r=2)
                    t2 = T2B[:, jj, :].rearrange("p (a r x) -> p a r x", a=2, r=2)
                    tr.op('dve', lambda e: e.tensor_tensor(out=t1, in0=PBv, in1=pre_r, op=ALU.mult),
                          r=[pk, 's5c'], w=['t1_%d' % jj])
                    tr.op('dve', lambda e: e.tensor_tensor(out=t2, in0=PBv, in1=pre_i, op=ALU.mult),
                          r=[pk, 's5c'], w=['t2_%d' % jj])
                def post_a(cc, jj):
                    pg = jj
                    i = pg % 2
                    PZ = pb[2 + i]
                    zk = 'pz%d' % i
                    pr0 = 2 * pg
                    PZv = PZ[:].rearrange("p (a r x) -> p a r x", a=2, r=2)
                    t1 = T1B[:, jj, :].rearrange("p (a r x) -> p a r x", a=2, r=2)
                    t2 = T2B[:, jj, :].rearrange("p (a r x) -> p a r x", a=2, r=2)
                    rk = ['t1_%d' % jj, 't2_%d' % jj, 's5c']
                    for a in range(2):
                        tr.op('pe', lambda e, a=a: e.matmul(out=PZv[:, a, 0, :], lhsT=t1[:, a, 0, :], rhs=TRI[:],
                                                            start=True, stop=False, skip_group_check=True), r=rk, w=[zk])
                        tr.op('pe', lambda e, a=a: e.matmul(out=PZv[:, a, 0, :], lhsT=t2[:, a, 1, :], rhs=NTRI[:],
                                                            start=False, stop=True, skip_group_check=True), r=rk, w=[zk])
                        tr.op('pe', lambda e, a=a: e.matmul(out=PZv[:, a, 1, :], lhsT=t1[:, a, 1, :], rhs=TRI[:],
                                                            start=True, stop=False, skip_group_check=True), r=rk, w=[zk])
                        tr.op('pe', lambda e, a=a: e.matmul(out=PZv[:, a, 1, :], lhsT=t2[:, a, 0, :], rhs=TRI[:],
                                                            start=False, stop=True, skip_group_check=True), r=rk, w=[zk])
                    zck = 'zc%d' % pg
                    for a in range(2):
                        for ri in range(2):
                            CB_ = CRE if ri == 0 else CIM
                            tr.op('act', lambda e, a=a, ri=ri, CB_=CB_: e.activation(
                                out=ZCF[:, pr0 + a, ri, :], in_=PZv[:, a, ri, :], func=AF.Identity,
                                bias=CB_[:, pr0 + a:pr0 + a + 1]), r=[zk, 'carry'], w=[zck])
                def post_b(cc, jj):
                    pg = jj
                    pr0 = 2 * pg
                    zck = 'zc%d' % pg
                    Z = ZCF[:, pr0:pr0 + 2, :, :]
                    po_r = POST_RE[:, pr0:pr0 + 2, :].unsqueeze(2).to_broadcast([128, 2, 2, 128])
                    po_i = POST_IM[:, pr0:pr0 + 2, :].unsqueeze(2).to_broadcast([128, 2, 2, 128])
                    tr.op('dve', lambda e: e.tensor_tensor(out=XU1[:, pr0:pr0 + 2, :, :], in0=Z, in1=po_r, op=ALU.mult),
                          r=[zck, 's5c'], w=['x%d' % pg])
                    tr.op('dve', lambda e: e.tensor_tensor(out=XU2[:, pr0:pr0 + 2, :, :], in0=Z, in1=po_i, op=ALU.mult),
                          r=[zck, 's5c'], w=['x%d' % pg])
                def post_end(cc):
                    tt, sub = divmod(cc, 4)
                    us = tt % 2; uk = 'ut%d' % us; U = UT[us]
                    tsl = slice(sub * 128, (sub + 1) * 128)
                    zall = ['zc%d' % pg for pg in range(8)]
                    zr = ZCF[:, :, 0, 127]
                    zi = ZCF[:, :, 1, 127]
                    tr.op('dve', lambda e: e.tensor_tensor(out=TQ[0][:], in0=zr, in1=A128[:, 0, :], op=ALU.mult),
                          r=zall + ['s5c'], w=['tq'])
                    tr.op('dve', lambda e: e.tensor_tensor(out=TQ[1][:], in0=zi, in1=A128[:, 1, :], op=ALU.mult),
                          r=zall + ['s5c'], w=['tq'])
                    tr.op('dve', lambda e: e.tensor_tensor(out=TQ[2][:], in0=zr, in1=A128[:, 1, :], op=ALU.mult),
                          r=zall + ['s5c'], w=['tq'])
                    tr.op('dve', lambda e: e.tensor_tensor(out=TQ[3][:], in0=zi, in1=A128[:, 0, :], op=ALU.mult),
                          r=zall + ['s5c'], w=['tq'])
                    tr.op('dve', lambda e: e.tensor_tensor(out=CRE[:], in0=TQ[0][:], in1=TQ[1][:], op=ALU.subtract),
                          r=['tq'], w=['carry'])
                    tr.op('dve', lambda e: e.tensor_tensor(out=CIM[:], in0=TQ[2][:], in1=TQ[3][:], op=ALU.add),
                          r=['tq'], w=['carry'])
                    for kt in range(4):
                        i = kt % 2
                        PY = pb[4 + i]
                        yk = 'py%d' % i
                        n = 0
                        for a in range(4):
                            pr = 4 * kt + a
                            terms = ((CMAT[:, pr, 0, :], XU1[:, pr, 0, :]), (NCM[:, pr, :], XU2[:, pr, 1, :]),
                                     (CMAT[:, pr, 1, :], XU1[:, pr, 1, :]), (CMAT[:, pr, 1, :], XU2[:, pr, 0, :]))
                            for (cm, xx) in terms:
                                tr.op('pe', lambda e, PY=PY, cm=cm, xx=xx, n=n: e.matmul(
                                    out=PY[:, 0:128], lhsT=cm, rhs=xx,
                                    start=(n == 0), stop=(n == 15)), r=['s5c', 'x%d' % (pr // 2)], w=[yk])
                                n += 1
                        tr.op('dve', lambda e, PY=PY, kt=kt, tsl=tsl: e.scalar_tensor_tensor(
                            out=Y[:, kt, tsl], in0=U[:, kt, tsl], scalar=DCOL[:, kt:kt + 1], in1=PY[:, 0:128],
                            op0=ALU.mult, op1=ALU.add), r=[uk, yk, 's5c'], w=['y'])
                def tail(tt):
                    t0 = tt * 512
                    Yf = ROW[0][:]
                    YBf = ROW[1][:]
                    YGf = ROW[2][:]
                    tr.op('act', lambda e: e.activation(out=YBf, in_=Yf, func=AF.Square), r=['y'], w=['yb'])
                    tr.op('dve', lambda e: e.tensor_scalar(out=YBf, in0=YBf, scalar1=0.044715, scalar2=1.0,
                                                          op0=ALU.mult, op1=ALU.add), r=['yb'], w=['yb'])
                    tr.op('dve', lambda e: e.tensor_tensor(out=YBf, in0=YBf, in1=Yf, op=ALU.mult), r=['yb', 'y'], w=['yb'])
                    tr.op('act', lambda e: e.activation(out=YBf, in_=YBf, func=AF.Sigmoid, scale=GELU_C), r=['yb'], w=['yb'])
                    tr.op('dve', lambda e: e.tensor_tensor(out=YGf, in0=Yf, in1=YBf, op=ALU.mult), r=['y', 'yb'], w=['yg'])
                    tr.op('act', lambda e: e.activation(out=YGB[:].rearrange("p a b -> p (a b)"), in_=YGf, func=AF.Copy),
                          r=['yg'], w=['ygb'])
                    for mt in range(4):
                        i = mt % 2
                        PG = pb[6 + i]
                        gk = 'pgl%d' % i
                        for kt in range(4):
                            tr.op('pe', lambda e, PG=PG, kt=kt, mt=mt: e.matmul(
                                out=PG[:], lhsT=WGL[:, kt, mt * 128:(mt + 1) * 128], rhs=YGB[:, kt, :],
                                start=(kt == 0), stop=(kt == 3)), r=['s5c', 'ygb'], w=[gk])
                        tr.op('act', lambda e, PG=PG, mt=mt, i=i: e.activation(
                            out=SGL[i][:], in_=PG[:], func=AF.Sigmoid, bias=BGL[:, mt:mt + 1]), r=[gk, 's5c'],
                            w=['sgl%d' % i])
                        tr.op('dve', lambda e, mt=mt, i=i: e.tensor_tensor(out=S[:, mt, :], in0=YG[:, mt, :],
                                                                          in1=SGL[i][:], op=ALU.mult),
                              r=['yg', 'sgl%d' % i], w=['s'])
                    tr.op('act', lambda e: e.activation(out=SQ[:].rearrange("p a b -> p (a b)"),
                                                       in_=TH[:], func=AF.Square),
                          r=['s'], w=['sq'])
                    PN = pb[6]
                    for mt in range(4):
                        tr.op('pe', lambda e, mt=mt: e.matmul(out=PN[:], lhsT=ONES[:], rhs=SQ[:, mt, :],
                                                             start=(mt == 0), stop=(mt == 3)), r=['sq', 'c'], w=['pgl0'])
                    tr.op('dve', lambda e: e.tensor_scalar(out=RS5[:], in0=PN[:], scalar1=1.0 / 512, scalar2=EPS,
                                                          op0=ALU.mult, op1=ALU.add), r=['pgl0'], w=['rs5'])
                    tr.op('act', lambda e: e.activation(out=RS5[:], in_=RS5[:], func=AF.Sqrt), r=['rs5'], w=['rs5'])
                    tr.op('dve', lambda e: e.reciprocal(out=RS5[:], in_=RS5[:]), r=['rs5'], w=['rs5'])
                    for mt in range(4):
                        tr.op('dve', lambda e, mt=mt: e.scalar_tensor_tensor(
                            out=OUT[:, mt, :], in0=S[:, mt, :], scalar=GC[:, 32 + mt:33 + mt], in1=RS5[:],
                            op0=ALU.mult, op1=ALU.mult), r=['s', 'rs5', 'c'], w=['out5'])
                    tr.dma('sp', 'sout', mview[:, 4:8, t0:t0 + 512], OUT[:], r=['out5'], w=['mixT'])
                uload(0)
                uload(1)
                for jj in range(8):
                    pre(0, jj)
                for cc in range(32):
                    post_a(cc, 0)
                    for jj in range(8):
                        if jj + 1 < 8:
                            post_a(cc, jj + 1)
                        if cc + 1 < 32:
                            pre(cc + 1, jj)
                        post_b(cc, jj)
                    post_end(cc)
                    if cc % 4 == 3:
                        tail(cc // 4)
                        if cc // 4 + 2 < 8:
                            uload(cc // 4 + 2)
            tr.barrier()

        def wout_phase(after_wo=None):
            with contextlib.ExitStack() as w_:
                def wsb(name, shape, dt):
                    return w_.enter_context(nc.sbuf_tensor(name, list(shape), dt))
                WO = wsb("WO", [128, 8, D], BF16)
                XW = [wsb("XW%d" % i, [128, 8, 512], F32) for i in range(2)]
                MX = [wsb("MX%d" % i, [128, 8, 512], BF16) for i in range(2)]
                for k in range(8):
                    tr.dma('pool', 'wo', WO[:, k, :], w_out[k * 128:(k + 1) * 128, :], w=['WO'])
                if after_wo is not None:
                    after_wo()
                for tt in range(8):
                    s = tt % 2
                    t0 = tt * 512
                    xk, mk = 'xw%d' % s, 'mx%d' % s
                    tr.dma('sp', 'xwin%d' % s, XW[s][:], xview(x1T, t0, 512), r=['x1T_%d' % tt], w=[xk])
                    tr.dma('act', 'mxin%d' % s, MX[s][:], xview(mixT, t0, 512), r=['mixT'], w=[mk])
                    for dk in range(8):
                        b = dk % 4
                        for k in range(8):
                            tr.op('pe', lambda e, k=k, dk=dk, b=b, s=s: e.matmul(
                                out=pb[b][:], lhsT=WO[:, k, dk * 128:(dk + 1) * 128], rhs=MX[s][:, k, :],
                                start=(k == 0), stop=(k == 7)), r=['WO', mk], w=['pw%d' % b])
                        tr.op('dve', lambda e, dk=dk, b=b, s=s: e.tensor_tensor(
                            out=XW[s][:, dk, :], in0=pb[b][:], in1=XW[s][:, dk, :], op=ALU.add),
                            r=['pw%d' % b, xk], w=[xk])
                    tr.dma('act', 'xwout%d' % s, xview(x1T, t0, 512), XW[s][:], r=[xk], w=['x1T_%d' % tt])
            tr.barrier()

        ffn_phase(1)

        with contextlib.ExitStack() as ms:
            if stop < 1.2:
                return nc
            def msb(name, shape, dt):
                return ms.enter_context(nc.sbuf_tensor(name, list(shape), dt))
            KE = [msb("KE%d" % g, [128, T], BF16) for g in range(2)]
            KW = [msb("KW%d" % g, [64, T], BF16) for g in range(2)]
            VS1 = [msb("VS1%d" % g, [128, 32, 65], BF16) for g in range(2)]
            VW1 = [msb("VW1%d" % g, [128, 32, 65], BF16) for g in range(2)]
            GT = msb("GT", [128, 32, 24], F32)
            KC = [msb("KC%d" % g, [64, 256], BF16) for g in range(2)]
            VCX = [msb("VCX%d" % g, [128, 2, 129], BF16) for g in range(2)]
            for g in range(2):
                tr.dma('pool', 'const', KE[g][64:128, :], c_E0[:, :], w=['KE%d' % g])
                tr.op('dve', lambda e, g=g: e.memset(VS1[g][:, :, 64:65], 1.0), w=['VS1%d' % g])
                tr.op('dve', lambda e, g=g: e.memset(VW1[g][:, :, 64:65], 1.0), w=['VW1%d' % g])
                tr.op('dve', lambda e, g=g: e.memset(VCX[g][:, :, 64:65], 1.0), w=['VCX%d' % g])
                tr.dma('pool', 'const', VCX[g][:, :, 65:129], c_ovl.rearrange("p (a b) -> p a b", a=2),
                       w=['VCX%d' % g])

            with contextlib.ExitStack() as ps_:
                def psb(name, shape, dt):
                    return ps_.enter_context(nc.sbuf_tensor(name, list(shape), dt))
                WIN = psb("WIN", [128, 8, 1816], BF16)
                H2 = [psb("H2_%d" % i, [128, 8, 512], BF16) for i in range(2)]
                QST = psb("QST", [64, 4, 8, 128], BF16)
                UST = psb("UST", [128, 4, 512], BF16)
                KCin = [psb("KCin%d" % g, [64, 16, 256], BF16) for g in range(2)]
                VCin = [psb("VCin%d" % g, [64, 16, 256], BF16) for g in range(2)]
                GB = psb("GB", [128, 24], F32)
                W1 = [psb("W1_%d" % i, [64, 32, 256], BF16) for i in range(2)]
                W2 = [psb("W2_%d" % i, [128, 2, 64], BF16) for i in range(2)]
                CB1 = psb("CB1", [128, 4], F32)
                POST = psb("POST", [64, 64], BF16)
                HID = psb("HID", [128, 2, 256], BF16)
                GTMP = [psb("GTMP%d" % i, [128, 256], F32) for i in range(3)]
                PBIAS = psb("PBIAS", [128, 1], F32)
                for k in range(8):
                    tr.dma('pool', 'win', WIN[:, k, :], w_in[k * 128:(k + 1) * 128, :], w=['WIN'])
                tr.dma('sp', 'const', GB[:], gate_bias.partition_broadcast(128), w=['GB'])
                tr.dma('sp', 'const', CB1[:], cb1[:, :], w=['cmpw'])
                tr.dma('pool', 'const', POST[:], posT[:, :], w=['cmpw'])
                for i in range(2):
                    w1v = cw1[i].rearrange("(l d) h -> d l h", d=64)
                    for lc in range(8):
                        tr.dma('pool', 'const', W1[i][:, 4 * lc:4 * lc + 4, :], w1v[:, 4 * lc:4 * lc + 4, :], w=['cmpw'])
                    tr.dma('pool', 'const', W2[i][:], cw2[i].rearrange("(t p) d -> p t d", p=128), w=['cmpw'])
                if stop < 1.6:
                    tr.barrier()
                    return nc
                ring = [0]

                def nextbank():
                    b = ring[0] % 6
                    ring[0] += 1
                    return pb[b], 'pp%d' % b

                evq = [0]

                def evac(out, in_, rk, wk, force=None):
                    evq[0] += 1
                    if (evq[0] % 2 == 0 and force is None) or force == 'act':
                        tr.op('act', lambda e: e.activation(out=out, in_=in_, func=AF.Copy), r=[rk], w=[wk])
                    else:
                        tr.op('dve', lambda e: e.tensor_copy(out=out, in_=in_), r=[rk], w=[wk])

                for tt in range(8):
                    s = tt % 2
                    t0 = tt * 512
                    hk = 'h2_%d' % s
                    HH = H2[s]
                    tr.dma('sp', 'h2in%d' % s, HH[:], xview(h2T, t0, 512), r=['h2T'], w=[hk])
                    for h in range(8):
                        P, pk = nextbank()
                        for k in range(8):
                            tr.op('pe', lambda e, k=k, h=h, P=P: e.matmul(
                                out=P[0:64, :], lhsT=WIN[:, k, 64 * h:64 * h + 64], rhs=HH[:, k, :],
                                start=(k == 0), stop=(k == 7)), r=['WIN', hk], w=[pk])
                        evac(QST[:, :, h, :], P[0:64, :].rearrange("p (c q) -> p c q", q=128), pk, 'qst')
                    tr.dma('sp', 'qout', qTd.rearrange("p (c x) -> p c x", c=32)[:, 4 * tt:4 * tt + 4, :],
                           QST[:].rearrange("p c h q -> p c (h q)"), r=['qst'], w=['qTd'])
                    for (c0, dest, dn) in ((512, KCin, 'KCin'), (640, VCin, 'VCin'), (768, KE, 'KE'), (1024, KW, 'KW')):
                        for g in range(2):
                            P, pk = nextbank()
                            for k in range(8):
                                tr.op('pe', lambda e, k=k, P=P, cc=c0 + 64 * g: e.matmul(
                                    out=P[0:64, :], lhsT=WIN[:, k, cc:cc + 64], rhs=HH[:, k, :],
                                    start=(k == 0), stop=(k == 7)), r=['WIN', hk], w=[pk])
                            if dn in ('KCin', 'VCin'):
                                evac(dest[g][:, :, tt * 32:(tt + 1) * 32],
                                     P[0:64, :].rearrange("p (m r) -> p r m", r=16), pk, '%s%d' % (dn, g))
                            else:
                                evac(dest[g][0:64, t0:t0 + 512], P[0:64, :], pk, '%s%d' % (dn, g))
                    for kt in range(4):
                        P, pk = nextbank()
                        for k in range(8):
                            tr.op('pe', lambda e, k=k, P=P, cc=1304 + 128 * kt: e.matmul(
                                out=P[:, :], lhsT=WIN[:, k, cc:cc + 128], rhs=HH[:, k, :],
                                start=(k == 0), stop=(k == 7)), r=['WIN', hk], w=[pk])
                        evac(UST[:, kt, :], P[:, :], pk, 'ust')
                    tr.dma('sp', 'uout', uTd.rearrange("p (k t) -> p k t", k=4)[:, :, t0:t0 + 512], UST[:],
                           r=['ust'], w=['uTd'])
                    for sbk in range(4):
                        blk = tt * 4 + sbk
                        P, pk = nextbank()
                        for k in range(8):
                            tr.op('pe', lambda e, k=k, P=P, sbk=sbk: e.matmul(
                                out=P[:, 0:408], lhsT=HH[:, k, sbk * 128:(sbk + 1) * 128], rhs=WIN[:, k, 896:1304],
                                start=(k == 0), stop=(k == 7)), r=['WIN', hk], w=[pk])
                        for g in range(2):
                            evac(VS1[g][:, blk, 0:64], P[:, 64 * g:64 * g + 64], pk, 'VS1%d' % g, force='dve')
                            evac(VW1[g][:, blk, 0:64], P[:, 256 + 64 * g:256 + 64 * g + 64], pk, 'VW1%d' % g, force='dve')
                        tr.op('dve', lambda e, P=P, blk=blk: e.tensor_tensor(out=GT[:, blk, :], in0=P[:, 384:408],
                                                                            in1=GB[:], op=ALU.add),
                              r=[pk, 'GB'], w=['GT'])
                        tr.op('act', lambda e, blk=blk: e.activation(out=GT[:, blk, :], in_=GT[:, blk, :],
                                                                    func=AF.Sigmoid), r=['GT'], w=['GT'])

                if stop < 1.8:
                    tr.barrier()
                    return nc
                HIDS = [psb("HIDS%d" % g, [128, 2, 256], BF16) for g in range(2)]
                for g in range(2):
                    tr.op('dve', lambda e, g=g: e.memset(HIDS[g][:], 0.0), w=['hid%d_0' % g, 'hid%d_1' % g])
                A, B_, C_ = GTMP
                for kind in range(2):
                    srcs = KCin if kind == 0 else VCin
                    sname = 'KCin' if kind == 0 else 'VCin'
                    for hh in range(2):
                        P, pk = nextbank()
                        for l in range(32):
                            tr.op('pe', lambda e, l=l, P=P, hh=hh: e.matmul(
                                out=P[:, 0:1], lhsT=W1[kind][:, l, hh * 128:(hh + 1) * 128],
                                rhs=POST[:, kind * 32 + l:kind * 32 + l + 1], start=(l == 0), stop=(l == 31)),
                                r=['cmpw'], w=[pk])
                        tr.op('dve', lambda e, P=P, hh=hh: e.tensor_tensor(
                            out=PBIAS[:], in0=P[:, 0:1], in1=CB1[:, kind * 2 + hh:kind * 2 + hh + 1], op=ALU.add),
                            r=[pk, 'cmpw'], w=['pbias'])
                        for g in range(2):
                            P2, pk2 = nextbank()
                            for l in range(32):
                                rhs = srcs[g][:, l % 16, (l // 16):(l // 16) + 255]
                                tr.op('pe', lambda e, l=l, P2=P2, rhs=rhs, hh=hh: e.matmul(
                                    out=P2[:, 0:255], lhsT=W1[kind][:, l, hh * 128:(hh + 1) * 128], rhs=rhs,
                                    start=(l == 0), stop=(l == 31)), r=['cmpw', '%s%d' % (sname, g)], w=[pk2])
                            tr.op('dve', lambda e, P2=P2: e.tensor_scalar(out=A[:, 0:255], in0=P2[:, 0:255],
                                                                         scalar1=PBIAS[:, 0:1], scalar2=None,
                                                                         op0=ALU.add), r=[pk2, 'pbias'], w=['ga'])
                            tr.op('dve', lambda e: e.tensor_tensor(out=B_[:, 0:255], in0=A[:, 0:255], in1=A[:, 0:255],
                                                                  op=ALU.mult), r=['ga'], w=['gb'])
                            tr.op('dve', lambda e: e.tensor_scalar(out=B_[:, 0:255], in0=B_[:, 0:255], scalar1=0.044715,
                                                                  scalar2=1.0, op0=ALU.mult, op1=ALU.add),
                                  r=['gb'], w=['gb'])
                            tr.op('dve', lambda e: e.tensor_tensor(out=B_[:, 0:255], in0=B_[:, 0:255], in1=A[:, 0:255],
                                                                  op=ALU.mult), r=['gb', 'ga'], w=['gb'])
                            tr.op('act', lambda e: e.activation(out=C_[:, 0:255], in_=B_[:, 0:255], func=AF.Sigmoid,
                                                               scale=GELU_C), r=['gb'], w=['gc'])
                            tr.op('dve', lambda e, g=g, hh=hh: e.tensor_tensor(
                                out=HIDS[g][:, hh, 0:255], in0=A[:, 0:255], in1=C_[:, 0:255], op=ALU.mult),
                                r=['ga', 'gc'], w=['hid%d_%d' % (g, hh)])
                    for g in range(2):
                        hk2 = ['hid%d_0' % g, 'hid%d_1' % g]
                        if kind == 0:
                            P, pk = nextbank()
                            for hh in range(2):
                                tr.op('pe', lambda e, hh=hh, P=P, g=g: e.matmul(
                                    out=P[0:64, 0:256], lhsT=W2[0][:, hh, :], rhs=HIDS[g][:, hh, :],
                                    start=(hh == 0), stop=(hh == 1)), r=['cmpw'] + hk2, w=[pk])
                            evac(KC[g][:, :], P[0:64, 0:256], pk, 'KC%d' % g)
                        else:
                            for nt in range(2):
                                P, pk = nextbank()
                                for hh in range(2):
                                    tr.op('pe', lambda e, hh=hh, P=P, g=g, nt=nt: e.matmul(
                                        out=P[:, 0:64], lhsT=HIDS[g][:, hh, nt * 128:(nt + 1) * 128],
                                        rhs=W2[1][:, hh, :], start=(hh == 0), stop=(hh == 1)),
                                        r=['cmpw'] + hk2, w=[pk])
                                evac(VCX[g][:, nt, 0:64], P[:, 0:64], pk, 'VCX%d' % g)
            tr.barrier()
            if stop >= 3:
                attention_phase()
        if stop >= 4:
            s5_phase()
        with contextlib.ExitStack() as w2s:
            wts2 = ffn_weights(2, w2s, load=False) if stop >= 6 else None
            if stop >= 5:
                wout_phase((lambda: ffn_wload(2, wts2, gu=True, wd=False)) if stop >= 6 else None)
            if stop >= 6:
                ffn_phase(2, wts2)
    return nc


def _consts():
    f = np.float32
    c = {}
    c["c_ident"] = np.eye(128, dtype=f)
    j = np.arange(128)
    c["c_tri"] = (j[:, None] <= j[None, :]).astype(f)
    c["c_E0"] = (np.arange(T)[None, :] // 64 == np.arange(64)[:, None]).astype(f)
    cb = np.where(j[:, None] > j[None, :], -BIGM, 0.0).astype(f)
    wb = np.where(j[:, None] <= j[None, :], -BIGM, 0.0).astype(f)
    cm = np.where(16 * (j[:, None] - 64) > j[None, :] - 31, -BIGM, 0.0).astype(f)
    c["c_CB"] = np.tile(cb, (1, 4))
    c["c_WB"] = np.tile(wb, (1, 4))
    c["c_CM"] = np.tile(cm, (1, 4))
    c["c_BD"] = (np.arange(384)[None, :] == j[:, None] + 128).astype(f)
    hh = (j >= 64).astype(np.int64)[:, None]
    m = np.arange(128)[None, :]
    c["c_U0"] = np.where(m > 62 + hh, -1e30, 3e38).astype(f)
    l0 = np.full((128, 128), -3e38, dtype=f)
    l0[m == 62 + hh] = 2e30
    l0[m == 61 + hh] = 1e30
    c["c_L0"] = l0
    n = np.arange(256)[:, None]
    jj = np.arange(64)[None, :]
    ov = ((16 * n < 64 * jj + 64) & (16 * n + 32 > 64 * jj)).astype(f)
    ov[255] = 0
    c["c_ovl"] = np.ascontiguousarray(ov.reshape(2, 128, 64).transpose(1, 0, 2).reshape(128, 128))
    c["c_maskB"] = (np.arange(8)[None, :] == (j // 16)[:, None]).astype(f)
    gi = (j // 64)[:, None, None]
    p4 = np.arange(4)[None, :, None]
    g8 = np.arange(8)[None, None, :]
    c["c_maskC"] = (g8 == 2 * p4 + gi).astype(f).reshape(128, 32)
    c["c_iota"] = np.tile(np.arange(128, dtype=f)[None, :], (128, 1))
    c["c_jcol"] = np.arange(128, dtype=f)[:, None].copy()
    return c


def _prep_shared(inp):
    f = np.float32
    A = lambda a: np.ascontiguousarray(np.asarray(a, dtype=f))
    d = {}
    d["wg1"], d["wu1"], d["wd1"] = A(inp["ffn1_w_gate"][0]), A(inp["ffn1_w_up"][0]), A(inp["ffn1_w_down"][0])
    d["wg2"], d["wu2"], d["wd2"] = A(inp["ffn2_w_gate"][0]), A(inp["ffn2_w_up"][0]), A(inp["ffn2_w_down"][0])
    d["w_in"], d["w_out"], d["w_glu"] = A(inp["w_in"][0]), A(inp["w_out"][0]), A(inp["s5_w_glu"][0])
    col8 = lambda v: np.asarray(v, dtype=f).reshape(-1, 128).T
    d["gcols"] = A(np.concatenate([col8(inp["ffn1_norm"][0]), col8(inp["mix_norm"][0]), col8(inp["ffn2_norm"][0]),
                                   col8(inp["final_norm"]), col8(inp["ssm_out_norm"][0])], axis=1))
    d["grow_attn"] = A(np.asarray(inp["attn_out_norm"][0]).reshape(1, 512))
    d["gate_bias"] = A(np.asarray(inp["gate_bias"][0]).reshape(1, 24))
    d["cw1k"], d["cw1v"] = A(inp["cmp_k_w1"][0]), A(inp["cmp_v_w1"][0])
    d["cw2k"], d["cw2v"] = A(inp["cmp_k_w2"][0]), A(inp["cmp_v_w2"][0])
    d["cb1"] = A(np.concatenate([col8(inp["cmp_k_b1"][0]), col8(inp["cmp_v_b1"][0])], axis=1))
    d["posT"] = A(np.concatenate([np.asarray(inp["cmp_pos_k"][0]).T, np.asarray(inp["cmp_pos_v"][0]).T], axis=1))
    lr = np.asarray(inp["s5_lambda_re"][0], dtype=f)
    li = np.asarray(inp["s5_lambda_im"][0], dtype=f)
    ldt = np.repeat(np.asarray(inp["s5_log_dt"][0], dtype=f)[:, None], 64, axis=1)
    d["s5_rows"] = A(np.stack([lr.reshape(2048), li.reshape(2048), ldt.reshape(2048)]))
    colify = lambda a: a.reshape(16, 128).T
    d["s5_cols"] = A(np.concatenate([colify(lr), colify(li), colify(ldt)], axis=1))

    def rows_gh(a):
        return np.repeat(a.reshape(4, 8, 1, 64), 16, axis=2).transpose(1, 2, 0, 3).reshape(128, 4, 64)
    d["s5_rep"] = A(np.stack([rows_gh(lr), rows_gh(li), rows_gh(ldt)], axis=2).reshape(128, 4 * 3 * 64))

    def bt(a):
        return np.asarray(a, dtype=f).reshape(4, 8, 64, 16).transpose(1, 3, 0, 2).reshape(128, 4, 64)
    d["s5_bT"] = A(np.stack([bt(inp["s5_b_re"][0]), bt(inp["s5_b_im"][0])], axis=2).reshape(128, 4 * 2 * 64))

    def ct(a):
        return np.asarray(a, dtype=f).reshape(16, 2, 16, 64).transpose(1, 3, 0, 2).reshape(128, 16, 16)
    d["s5_c"] = A(np.stack([ct(inp["s5_c_re"][0]), ct(inp["s5_c_im"][0])], axis=2).reshape(128, 16 * 2 * 16))
    d["s5_dcol"] = A(np.asarray(inp["s5_d"][0], dtype=f).reshape(4, 128).T)
    d["bglu_col"] = A(np.asarray(inp["s5_b_glu"][0], dtype=f).reshape(4, 128).T)
    d.update(_consts())
    return d


def kernel(**inputs):
    x = np.asarray(inputs["x"], dtype=np.float32)
    shared = _prep_shared(inputs)
    nc = build(dbg=False)
    in_maps = []
    for b in range(NCORES):
        m = dict(shared)
        m["xT"] = np.ascontiguousarray(x[b].T)
        in_maps.append(m)
    res = run_bass_kernel_spmd(nc, in_maps, core_ids=list(range(NCORES)))
    out = np.empty((NCORES, T, D), dtype=np.float32)
    for b in range(NCORES):
        out[b] = np.asarray(res.results[b]["yT"], dtype=np.float32).T
    return out
```

```python
import contextlib
import numpy as np
import concourse.bass as bass
import concourse.mybir as mybir
from concourse.bass_utils import run_bass_kernel_spmd

F32 = mybir.dt.float32
BF16 = mybir.dt.bfloat16
ALU = mybir.AluOpType
AF = mybir.ActivationFunctionType

T = 4096
D = 1024
FF = 2816
NF = FF // 128
EPS = 1e-6
BIGM = 30000.0
NCORES = 8
GELU_C = 1.5957691216057308


class TR:
    def __init__(self, nc, es):
        self.nc, self.es = nc, es
        self.eng = dict(pe=nc.tensor, dve=nc.vector, act=nc.scalar, pool=nc.gpsimd, sp=nc.sync)
        self.sem, self.cnt = {}, {}
        self.seen = {e: {} for e in self.eng}
        for e in self.eng:
            self.sem[e] = es.enter_context(nc.semaphore("s_" + e))
            self.cnt[e] = 0
        self.bs = {}
        self.defer = None

    def _st(self, k):
        s = self.bs.get(k)
        if s is None:
            s = self.bs[k] = ({}, {})
        return s

    def _deps(self, r, w):
        d = {}
        for k in r:
            for sk, v in self._st(k)[0].items():
                if d.get(sk, 0) < v:
                    d[sk] = v
        for k in w:
            s = self._st(k)
            for dd in s:
                for sk, v in dd.items():
                    if d.get(sk, 0) < v:
                        d[sk] = v
        return d

    def _wait(self, e, d):
        for k, v in d.items():
            if k == e and e == 'pe':
                continue
            if self.seen[e].get(k, 0) >= v:
                continue
            self.eng[e].wait_ge(self.sem[k], v)
            self.seen[e][k] = v

    def _mark(self, src, v, r, w):
        for k in r:
            self._st(k)[1][src] = v
        for k in w:
            self._st(k)[0][src] = v

    def op(self, e, fn, r=(), w=()):
        if self.defer is not None:
            r, w = list(r), list(w)
            self.defer.append(lambda: self.op(e, fn, r, w))
            return
        self._wait(e, self._deps(r, w))
        ins = fn(self.eng[e])
        self.cnt[e] += 1
        ins.then_inc(self.sem[e], 1)
        self._mark(e, self.cnt[e], r, w)

    def dma(self, q, ch, out, in_, r=(), w=()):
        if self.defer is not None:
            r, w = list(r), list(w)
            self.defer.append(lambda: self.dma(q, ch, out, in_, r, w))
            return
        if ch not in self.sem:
            self.sem[ch] = self.es.enter_context(self.nc.semaphore("c_" + ch))
            self.cnt[ch] = 0
        self._wait(q, self._deps(r, w))
        ins = self.eng[q].dma_start(out=out, in_=in_)
        self.cnt[ch] += 16
        ins.then_inc(self.sem[ch], 16)
        self._mark(ch, self.cnt[ch], r, w)

    def barrier(self):
        allk = {k: v for k, v in self.cnt.items() if v > 0}
        for e in self.eng:
            self._wait(e, allk)


def build(dbg=False, stop=9):
    nc = bass.Bass("TRN2", target_bir_lowering=False)
    es = contextlib.ExitStack()
    with es:
        tr = TR(nc, es)

        def din(name, shape):
            return nc.dram_tensor(name, list(shape), F32, kind="ExternalInput").ap()

        def dscr(name, shape, dt):
            kind = "ExternalOutput" if dbg else "Internal"
            return nc.dram_tensor(name, list(shape), dt, kind=kind).ap()

        def sb(name, shape, dt):
            return es.enter_context(nc.sbuf_tensor(name, list(shape), dt))

        xT = din("xT", [D, T])
        wgs = [din("wg1", [D, FF]), din("wg2", [D, FF])]
        wus = [din("wu1", [D, FF]), din("wu2", [D, FF])]
        wds = [din("wd1", [FF, D]), din("wd2", [FF, D])]
        w_in = din("w_in", [D, 1816])
        w_out = din("w_out", [D, D])
        w_glu = din("w_glu", [512, 512])
        gcols = din("gcols", [128, 36])
        grow_attn = din("grow_attn", [1, 512])
        gate_bias = din("gate_bias", [1, 24])
        cw1 = [din("cw1k", [2048, 256]), din("cw1v", [2048, 256])]
        cw2 = [din("cw2k", [256, 64]), din("cw2v", [256, 64])]
        cb1 = din("cb1", [128, 4])
        posT = din("posT", [64, 64])
        s5_rows = din("s5_rows", [3, 2048])
        s5_cols = din("s5_cols", [128, 48])
        s5_rep = din("s5_rep", [128, 4 * 3 * 64])
        s5_bT = din("s5_bT", [128, 4 * 2 * 64])
        s5_c = din("s5_c", [128, 16 * 2 * 16])
        s5_dcol = din("s5_dcol", [128, 4])
        bglu_col = din("bglu_col", [128, 4])
        c_ident = din("c_ident", [128, 128])
        c_tri = din("c_tri", [128, 128])
        c_E0 = din("c_E0", [64, T])
        c_CB = din("c_CB", [128, 512])
        c_WB = din("c_WB", [128, 512])
        c_CM = din("c_CM", [128, 512])
        c_BD = din("c_BD", [128, 384])
        c_U0 = din("c_U0", [128, 128])
        c_L0 = din("c_L0", [128, 128])
        c_ovl = din("c_ovl", [128, 128])
        c_maskB = din("c_maskB", [128, 8])
        c_maskC = din("c_maskC", [128, 32])
        c_iota = din("c_iota", [128, 128])
        c_jcol = din("c_jcol", [128, 1])

        yT = nc.dram_tensor("yT", [D, T], F32, kind="ExternalOutput").ap()
        x1T = dscr("x1T", [D, T], F32)
        h2T = dscr("h2T", [D, T], BF16)
        qTd = dscr("qTd", [64, 32 * 8 * 128], BF16)
        uTd = dscr("uTd", [128, 4 * T], BF16)
        mixT = dscr("mixT", [D, T], BF16)

        GC = sb("GC", [128, 36], F32)
        ONES = sb("ONES", [128, 128], BF16)
        IDN = sb("IDN", [128, 128], BF16)
        EPSC = sb("EPSC", [128, 1], F32)
        tr.dma('sp', 'const', GC[:], gcols[:, :], w=['c'])
        tr.dma('pool', 'const', IDN[:], c_ident[:, :], w=['c'])
        tr.op('dve', lambda e: e.memset(ONES[:], 1.0), w=['c'])
        tr.op('dve', lambda e: e.memset(EPSC[:], 0.0), w=['c'])
        ONEC = sb("ONEC", [128, 1], F32)
        tr.op('dve', lambda e: e.memset(ONEC[:], 1.0), w=['c'])

        pb = [es.enter_context(nc.psum_tensor("pb%d" % i, [128, 512], F32)) for i in range(8)]

        def xview(dram, t0, n):
            return dram.rearrange("(k p) t -> p k t", p=128)[:, :, t0:t0 + n]

        def ffn_weights(ph, stk, load=True):
            WG = stk.enter_context(nc.sbuf_tensor("WG_%d" % ph, [128, 8, FF], BF16))
            WU = stk.enter_context(nc.sbuf_tensor("WU_%d" % ph, [128, 8, FF], BF16))
            WD = stk.enter_context(nc.sbuf_tensor("WD_%d" % ph, [128, NF, D], BF16))
            if load:
                ffn_wload(ph, (WG, WU, WD))
            return WG, WU, WD

        def ffn_wload(ph, wts, gu=True, wd=True):
            WG, WU, WD = wts
            wgd, wud, wdd = wgs[ph - 1], wus[ph - 1], wds[ph - 1]
            HF = FF // 2
            for hf, sfx in ((0, 'a'), (1, 'b')) if gu else ():
                for k in range(8):
                    tr.dma('pool', 'wg' + sfx, WG[:, k, hf * HF:(hf + 1) * HF],
                           wgd[k * 128:(k + 1) * 128, hf * HF:(hf + 1) * HF], w=['WG' + sfx])
                    tr.dma('pool', 'wu' + sfx, WU[:, k, hf * HF:(hf + 1) * HF],
                           wud[k * 128:(k + 1) * 128, hf * HF:(hf + 1) * HF], w=['WU' + sfx])
            for f in range(NF) if wd else ():
                tr.dma('pool', 'wd', WD[:, f, :], wdd[f * 128:(f + 1) * 128, :], w=['WD'])

        def ffn_phase(ph, wts=None):
            with contextlib.ExitStack() as fs:
                def fsb(name, shape, dt):
                    return fs.enter_context(nc.sbuf_tensor("%s_%d" % (name, ph), list(shape), dt))
                WG, WU, WD = wts if wts is not None else ffn_weights(ph, fs)
                if wts is not None:
                    ffn_wload(ph, wts, gu=False, wd=True)
                XT = [fsb("XT%d" % i, [128, 8, 512], F32) for i in range(2)]
                H = fsb("H", [128, 8, 512], BF16)
                AT = fsb("AT", [128, NF, 512], BF16)
                SG = [fsb("SG%d" % i, [128, 512], BF16) for i in range(2)]
                RS = [fsb("RS%d" % i, [128, 512], F32) for i in range(2)]
                src = xT if ph == 1 else x1T
                g0 = 0 if ph == 1 else 16
                hkeys = ['h%d' % k for k in range(8)]
                atk2 = ['at%d' % f for f in range(14, 22)]

                def xs(tt):
                    return XT[tt % 2], 'xt%d' % (tt % 2)

                def load(tt):
                    X, xk = xs(tt)
                    tr.dma('sp', 'xin%d' % (tt % 2), X[:], xview(src, tt * 512, 512), w=[xk])

                def norm_a(X, xk, HB, hk):
                    tr.op('act', lambda e: e.activation(out=HB, in_=X[:], func=AF.Square), r=[xk], w=hk)

                def norm_b(HBk, hk, PN, pnk):
                    for k in range(8):
                        tr.op('pe', lambda e, k=k: e.matmul(out=PN[:], lhsT=ONES[:], rhs=HBk(k),
                                                           start=(k == 0), stop=(k == 7)), r=[hk[k], 'c'], w=[pnk])

                def norm_c(X, xk, HBk, hk, PN, pnk, R, rk, goff, inplace=False):
                    tr.op('dve', lambda e: e.tensor_scalar(out=R[:], in0=PN[:], scalar1=1.0 / D, scalar2=EPS,
                                                          op0=ALU.mult, op1=ALU.add), r=[pnk], w=[rk])
                    tr.op('act', lambda e: e.activation(out=R[:], in_=R[:], func=AF.Sqrt), r=[rk], w=[rk])
                    tr.op('dve', lambda e: e.reciprocal(out=R[:], in_=R[:]), r=[rk], w=[rk])
                    for k in range(8):
                        dst = X[:, k, :] if inplace else HBk(k)
                        tr.op('dve', lambda e, k=k, dst=dst: e.scalar_tensor_tensor(
                            out=dst, in0=X[:, k, :], scalar=GC[:, goff + k:goff + k + 1], in1=R[:],
                            op0=ALU.mult, op1=ALU.mult), r=[xk, rk, 'c'], w=[xk if inplace else hk[k]])

                Hk = lambda k: H[:, k, :]
                A2k = lambda k: AT[:, 14 + k, :]

                def post_a(tt):
                    X, xk = xs(tt)
                    if ph == 1:
                        tr.dma('sp', 'xout%d' % (tt % 2), xview(x1T, tt * 512, 512), X[:], r=[xk], w=['x1T'])
                    norm_a(X, xk, AT[:, 14:22, :], atk2)

                def post_bc(tt):
                    X, xk = xs(tt)
                    norm_b(A2k, atk2, pb[7], 'pn2')
                    if ph == 1:
                        norm_c(X, xk, A2k, atk2, pb[7], 'pn2', RS[1], 'rs1', 8)
                        tr.dma('sp', 'hout', xview(h2T, tt * 512, 512), AT[:, 14:22, :], r=atk2, w=['h2T'])
                    else:
                        norm_c(X, xk, A2k, atk2, pb[7], 'pn2', RS[1], 'rs1', 24, inplace=True)
                        tr.dma('sp', 'yout%d' % (tt % 2), xview(yT, tt * 512, 512), X[:], r=[xk], w=['yT'])

                load(0)
                X0, xk0 = xs(0)
                norm_a(X0, xk0, H[:], hkeys)
                norm_b(Hk, hkeys, pb[6], 'pn')
                norm_c(X0, xk0, Hk, hkeys, pb[6], 'pn', RS[0], 'rs0', g0)
                for tt in range(8):
                    X, xk = xs(tt)
                    for f in range(NF):
                        b = f % 2
                        for k in range(8):
                            tr.op('pe', lambda e, k=k, f=f, b=b: e.matmul(
                                out=pb[b][:], lhsT=WG[:, k, f * 128:(f + 1) * 128], rhs=H[:, k, :],
                                start=(k == 0), stop=(k == 7)), r=['WG' + ('a' if f < NF // 2 else 'b'), hkeys[k]],
                                w=['pg%d' % b])
                        for k in range(8):
                            tr.op('pe', lambda e, k=k, f=f, b=b: e.matmul(
                                out=pb[2 + b][:], lhsT=WU[:, k, f * 128:(f + 1) * 128], rhs=H[:, k, :],
                                start=(k == 0), stop=(k == 7)), r=['WU' + ('a' if f < NF // 2 else 'b'), hkeys[k]],
                                w=['pu%d' % b])
                        tr.op('act', lambda e, b=b: e.activation(out=SG[b][:], in_=pb[b][:], func=AF.Silu),
                              r=['pg%d' % b], w=['sg%d' % b])
                        tr.op('dve', lambda e, b=b, f=f: e.tensor_tensor(out=AT[:, f, :], in0=SG[b][:],
                                                                         in1=pb[2 + b][:], op=ALU.mult),
                              r=['sg%d' % b, 'pu%d' % b], w=['at%d' % f])
                        if f == 2 and tt > 0:
                            post_bc(tt - 1)
                    if tt + 1 < 8:
                        load(tt + 1)
                        Xn, xkn = xs(tt + 1)
                        norm_a(Xn, xkn, H[:], hkeys)
                    for dk in range(8):
                        b = dk % 2
                        for f in range(NF):
                            tr.op('pe', lambda e, f=f, dk=dk, b=b: e.matmul(
                                out=pb[4 + b][:], lhsT=WD[:, f, dk * 128:(dk + 1) * 128], rhs=AT[:, f, :],
                                start=(f == 0), stop=(f == NF - 1)), r=['WD', 'at%d' % f], w=['pd%d' % b])
                        tr.op('dve', lambda e, dk=dk, b=b, X=X: e.scalar_tensor_tensor(
                            out=X[:, dk, :], in0=pb[4 + b][:], scalar=0.5, in1=X[:, dk, :],
                            op0=ALU.mult, op1=ALU.add), r=['pd%d' % b, xk], w=[xk])
                        if dk == 2 and tt + 1 < 8:
                            norm_b(Hk, hkeys, pb[6], 'pn')
                            norm_c(Xn, xkn, Hk, hkeys, pb[6], 'pn', RS[0], 'rs0', g0)
                    post_a(tt)
                post_bc(7)
            tr.barrier()

        def attention_phase():
            with contextlib.ExitStack() as a_:
                def asb(name, shape, dt):
                    return a_.enter_context(nc.sbuf_tensor(name, list(shape), dt))
                CBt = asb("CBt", [128, 512], BF16)
                WBt = asb("WBt", [128, 512], BF16)
                CMt = asb("CMt", [128, 512], BF16)
                BDt = asb("BDt", [128, 384], BF16)
                U0t = asb("U0t", [128, 128], F32)
                L0t = asb("L0t", [128, 128], F32)
                GAT = asb("GAT", [128, 512], F32)
                QN = [asb("QN%d" % i, [128, 512], BF16) for i in range(2)]
                ET = [asb("ET%d" % i, [128, 512], BF16) for i in range(3)]
                SCO = asb("SCO", [128, 64], F32)
                M8 = asb("M8", [128, 16], F32)
                WK = asb("WK", [128, 64], F32)
                SELW = asb("SELW", [128, 128], BF16)
                RZ = asb("RZ", [128, 4], F32)
                DEN = asb("DEN", [128, 12], F32)
                ATT = asb("ATT", [128, 512], F32)
                ATN = asb("ATN", [128, 512], BF16)
                JUNK = asb("JUNK", [128, 512], F32)
                SSQ = asb("SSQ", [128, 1], F32)
                MST = asb("MST", [128, 4, 128], BF16)
                tr.dma('pool', 'aconst', CBt[:], c_CB[:, :], w=['ac'])
                tr.dma('pool', 'aconst', WBt[:], c_WB[:, :], w=['ac'])
                tr.dma('pool', 'aconst', CMt[:], c_CM[:, :], w=['ac'])
                tr.dma('pool', 'aconst', BDt[:], c_BD[:, :], w=['ac'])
                tr.dma('sp', 'aconst', U0t[:], c_U0[:, :], w=['ac'])
                tr.dma('sp', 'aconst', L0t[:], c_L0[:, :], w=['ac'])
                tr.dma('sp', 'aconst', GAT[:], grow_attn.partition_broadcast(128), w=['ac'])
                tr.op('dve', lambda e: e.memset(SELW[:], 0.0), w=['selw'])
                OS = pb[3][:, 0:260].rearrange("p (r x) -> p r x", x=65)
                OW = pb[4][:, 0:260].rearrange("p (r x) -> p r x", x=65)
                OC = pb[5][:, 0:260].rearrange("p (r x) -> p r x", x=65)
                IM = pb[6][:, 0:256].rearrange("p (r x) -> p r x", x=64)
                PT = pb[5][:, 320:384].bitcast(BF16)
                PT4 = pb[6][:, 256:512].bitcast(BF16)
                PA = pb[7]
                ETA = [asb("ETA%d" % i, [128, 512], BF16) for i in range(2)]
                dq = []

                def pump(k):
                    for _ in range(min(k, len(dq))):
                        dq.pop(0)()

                def deferred(f, *a):
                    tr.defer = dq
                    f(*a)
                    tr.defer = None
                sring = [0]
                ering = [0]

                def nexts():
                    b = sring[0] % 3
                    sring[0] += 1
                    return pb[b], 'ps%d' % b

                def nexte():
                    b = ering[0] % 3
                    ering[0] += 1
                    return ET[b], 'et%d' % b

                qview = qTd.rearrange("p (c x) -> p c x", c=32)
                QN3 = [QN[0], QN[1], asb("QN2", [128, 512], BF16)]
                OCS = [asb("OCS%d" % i, [128, 4, 65], F32) for i in range(4)]
                TMPA = asb("TMPA", [128, 4, 64], F32)
                TMPB = asb("TMPB", [128, 4, 64], F32)
                OSS = [asb("OSS%d" % i, [128, 4, 65], F32) for i in range(2)]
                OWS = [asb("OWS%d" % i, [128, 4, 65], F32) for i in range(2)]
                ATN2 = [ATN, asb("ATN1", [128, 512], BF16)]
                NIT = 64
                mixv = mixT.rearrange("(k p) t -> p k t", p=128)
                pending = []

                def qload(it):
                    c, g = divmod(it, 2)
                    sl = it % 3
                    tr.dma('sp', 'qin%d' % sl, QN3[sl][0:64, :], qview[:, c, g * 512:(g + 1) * 512], r=['qTd'],
                           w=['qnq%d' % sl])

                def stageA(it):
                    c, g = divmod(it, 2)
                    sl = it % 3
                    Q = QN3[sl]
                    qq, qs = 'qnq%d' % sl, 'qns%d' % sl
                    nts = 1 if c < 16 else 2
                    tl = []
                    for nt in range(nts):
                        Mn = min(128, 8 * c + 7 - 128 * nt)
                        m = 8 * c - 128 * nt
                        need = m <= 192
                        PS, psk = PA, 'pa'
                        tr.op('pe', lambda e, PS=PS, Mn=Mn, nt=nt, need=need: e.matmul(
                            out=PS[0:Mn, :], lhsT=KC[g][:, 128 * nt:128 * nt + Mn], rhs=Q[0:64, :],
                            start=True, stop=not need), r=['KC%d' % g, qq], w=[psk])
                        if need:
                            tr.op('pe', lambda e, PS=PS, Mn=Mn, m=m: e.matmul(
                                out=PS[0:Mn, :], lhsT=BDt[:, 192 - m:192 - m + Mn], rhs=CMt[:],
                                start=False, stop=True), r=['ac'], w=[psk])
                        Et, ek = ETA[nt], 'eta%d' % nt
                        tr.op('act', lambda e, PS=PS, Et=Et, Mn=Mn: e.activation(
                            out=Et[0:Mn, :], in_=PS[0:Mn, :], func=AF.Exp, scale=0.125), r=[psk], w=[ek])
                        tl.append((nt, Mn, Et, ek))
                    first = True
                    for (nt, Mn, Et, ek) in tl:
                        for r in range(4):
                            tr.op('pe', lambda e, Et=Et, Mn=Mn, nt=nt, r=r, first=first: e.matmul(
                                out=OC[:, r, :], lhsT=Et[0:Mn, r * 128:(r + 1) * 128], rhs=VCX[g][0:Mn, nt, 0:65],
                                start=first, stop=(nt == nts - 1), skip_group_check=True),
                                r=[ek, 'VCX%d' % g], w=['oc'])
                            tr.op('pe', lambda e, Et=Et, Mn=Mn, nt=nt, r=r, first=first: e.matmul(
                                out=IM[:, r, :], lhsT=Et[0:Mn, r * 128:(r + 1) * 128], rhs=VCX[g][0:Mn, nt, 65:129],
                                start=first, stop=(nt == nts - 1), skip_group_check=True),
                                r=[ek, 'VCX%d' % g], w=['im'])
                            first = False
                    OCb, ock = OCS[it % 4], 'ocs%d' % (it % 4)
                    tr.op('dve', lambda e, OCb=OCb: e.tensor_copy(out=OCb[:], in_=OC), r=['oc'], w=[ock])
                    tr.op('dve', lambda e, OCb=OCb: e.tensor_scalar(out=RZ[:], in0=OCb[:, :, 64], scalar1=1e-30,
                                                                   scalar2=None, op0=ALU.max), r=[ock], w=['rz'])
                    tr.op('dve', lambda e: e.reciprocal(out=RZ[:], in_=RZ[:]), r=['rz'], w=['rz'])
                    tr.op('dve', lambda e: e.tensor_scalar(out=SCO[:], in0=IM[:, 0, :], scalar1=RZ[:, 0:1],
                                                          scalar2=None, op0=ALU.mult), r=['im', 'rz'], w=['sco'])
                    for r in range(1, 4):
                        tr.op('dve', lambda e, r=r: e.scalar_tensor_tensor(
                            out=SCO[:], in0=IM[:, r, :], scalar=RZ[:, r:r + 1], in1=SCO[:],
                            op0=ALU.mult, op1=ALU.add), r=['im', 'rz', 'sco'], w=['sco'])
                    lo = 62 - 2 * c
                    tr.op('dve', lambda e, lo=lo: e.tensor_tensor(out=SCO[:], in0=SCO[:], in1=L0t[:, lo:lo + 64],
                                                                 op=ALU.max), r=['sco', 'ac'], w=['sco'])
                    tr.op('dve', lambda e, lo=lo: e.tensor_tensor(out=SCO[:], in0=SCO[:], in1=U0t[:, lo:lo + 64],
                                                                 op=ALU.min), r=['sco', 'ac'], w=['sco'])
                    tr.op('dve', lambda e: e.memset(SCO[:, 0:1], 3e30), r=['sco'], w=['sco'])
                    tr.op('dve', lambda e: e.max(out=M8[:, 0:8], in_=SCO[:]), r=['sco'], w=['m8'])
                    tr.op('dve', lambda e: e.match_replace(out=WK[:], in_to_replace=M8[:, 0:8], in_values=SCO[:],
                                                          imm_value=-3e38), r=['sco', 'm8'], w=['wk'])
                    tr.op('dve', lambda e: e.max(out=M8[:, 8:16], in_=WK[:]), r=['wk'], w=['m8'])
                    tr.op('dve', lambda e: e.tensor_scalar(out=SELW[:, 64:128], in0=SCO[:], scalar1=M8[:, 15:16],
                                                          scalar2=None, op0=ALU.is_ge), r=['sco', 'm8'], w=['selw'])
                    tr.op('pe', lambda e: e.transpose(out=PT, in_=SELW[:], identity=IDN[:]), r=['selw', 'c'], w=['pt'])
                    tr.op('dve', lambda e, Q=Q: e.tensor_scalar(
                        out=Q[64:128, :].rearrange("p (r q) -> p r q", r=4),
                        in0=PT[64:128, :].unsqueeze(1).to_broadcast([64, 4, 128]),
                        scalar1=-1.0, scalar2=BIGM, op0=ALU.add, op1=ALU.mult), r=['pt'], w=[qs])

                def finish_pe(c):
                    AN = ATN2[c % 2]
                    for j in range(4):
                        tr.op('pe', lambda e, j=j, AN=AN: e.transpose(out=PT4[:, j * 128:(j + 1) * 128],
                                                                     in_=AN[:, j * 128:(j + 1) * 128], identity=IDN[:]),
                              r=['atn%d' % (c % 2), 'c'], w=['pt4'])
                    tr.op('dve', lambda e: e.tensor_copy(out=MST[:].rearrange("p a b -> p (a b)"), in_=PT4),
                          r=['pt4'], w=['mst'])
                    tr.dma('sp', 'mout', mixv[:, 0:4, c * 128:(c + 1) * 128], MST[:], r=['mst'], w=['mixT'])

                def stageB(it):
                    c, g = divmod(it, 2)
                    sl = it % 3
                    Q = QN3[sl]
                    qq, qs = 'qnq%d' % sl, 'qns%d' % sl
                    k0 = max(0, c - 4)
                    tiles = [('w', kt) for kt in range(k0, c + 1)] + [('s', kt) for kt in range(c + 1)]
                    n = len(tiles)
                    info = {}

                    def qk(i):
                        br, kt = tiles[i]
                        PS, psk = nexts()
                        extra = []
                        if kt == c:
                            extra.append(CBt)
                        if br == 'w' and kt == c - 4:
                            extra.append(WBt)
                        if br == 'w':
                            tr.op('pe', lambda e, PS=PS, kt=kt, ne=len(extra): e.matmul(
                                out=PS[:], lhsT=KW[g][:, kt * 128:(kt + 1) * 128], rhs=Q[0:64, :],
                                start=True, stop=(ne == 0)), r=['KW%d' % g, qq], w=[psk])
                        else:
                            tr.op('pe', lambda e, PS=PS, kt=kt, ne=len(extra): e.matmul(
                                out=PS[:], lhsT=KE[g][:, kt * 128:(kt + 1) * 128], rhs=Q[:, :],
                                start=True, stop=(ne == 0)), r=['KE%d' % g, qq, qs], w=[psk])
                        for j, bt in enumerate(extra):
                            tr.op('pe', lambda e, PS=PS, bt=bt, last=(j == len(extra) - 1): e.matmul(
                                out=PS[:], lhsT=IDN[:], rhs=bt[:], start=False, stop=last), r=['ac', 'c'], w=[psk])
                        Et, ek = nexte()
                        tr.op('act', lambda e, PS=PS, Et=Et: e.activation(
                            out=Et[:], in_=PS[:], func=AF.Exp, scale=0.125), r=[psk], w=[ek])
                        info[i] = (Et, ek)

                    started = {'w': False, 's': False}
                    lastw = max(i for i in range(n) if tiles[i][0] == 'w')

                    def pv(i):
                        br, kt = tiles[i]
                        Et, ek = info[i]
                        O, ok, V, vk = (OW, 'ow', VW1[g], 'VW1%d' % g) if br == 'w' else (OS, 'os', VS1[g], 'VS1%d' % g)
                        last = (i == lastw) if br == 'w' else (i == n - 1)
                        for r in range(4):
                            st = not started[br]
                            started[br] = True
                            tr.op('pe', lambda e, Et=Et, kt=kt, r=r, O=O, V=V, st=st, last=last: e.matmul(
                                out=O[:, r, :], lhsT=Et[:, r * 128:(r + 1) * 128], rhs=V[:, kt, :],
                                start=st, stop=last, skip_group_check=True), r=[ek, vk], w=[ok])

                    for i in range(min(2, n)):
                        qk(i)
                    for i in range(n):
                        if i + 2 < n:
                            qk(i + 2)
                        pv(i)
                        left = max(1, n - 1 - i)
                        pump((len(dq) + left - 1) // left)
                    pump(len(dq))
                    b2 = it % 2
                    tr.op('dve', lambda e: e.tensor_copy(out=OWS[b2][:], in_=OW), r=['ow'], w=['ows%d' % b2])
                    tr.op('dve', lambda e: e.tensor_copy(out=OSS[b2][:], in_=OS), r=['os'], w=['oss%d' % b2])

                def tailB(it):
                    c, g = divmod(it, 2)
                    b2 = it % 2
                    b4 = it % 4
                    srcs = ((OCS[b4], 'ocs%d' % b4), (OSS[b2], 'oss%d' % b2), (OWS[b2], 'ows%d' % b2))
                    for bi, (O, ok) in enumerate(srcs):
                        tr.op('dve', lambda e, O=O, bi=bi: e.tensor_scalar(
                            out=DEN[:, bi * 4:bi * 4 + 4], in0=O[:, :, 64], scalar1=1e-30, scalar2=None,
                            op0=ALU.max), r=[ok], w=['den'])
                    tr.op('dve', lambda e: e.reciprocal(out=DEN[:], in_=DEN[:]), r=['den'], w=['den'])
                    gv = GT[:, c, 12 * g:12 * g + 12].rearrange("p (r b) -> p b r", b=3)
                    tr.op('dve', lambda e, gv=gv: e.tensor_tensor(
                        out=DEN[:].rearrange("p (b r) -> p b r", b=3), in0=DEN[:].rearrange("p (b r) -> p b r", b=3),
                        in1=gv, op=ALU.mult), r=['den', 'GT'], w=['den'])
                    ATTg = ATT[:, g * 256:(g + 1) * 256].rearrange("p (r x) -> p r x", r=4)
                    denb = lambda bi: DEN[:, bi * 4:bi * 4 + 4].unsqueeze(2).to_broadcast([128, 4, 64])
                    tr.op('dve', lambda e: e.tensor_tensor(out=ATTg, in0=OCS[b4][:, :, 0:64], in1=denb(0), op=ALU.mult),
                          r=['ocs%d' % b4, 'den'], w=['att'])
                    tr.op('dve', lambda e: e.tensor_tensor(out=TMPA[:], in0=OSS[b2][:, :, 0:64], in1=denb(1), op=ALU.mult),
                          r=['oss%d' % b2, 'den'], w=['tmpa'])
                    tr.op('dve', lambda e: e.tensor_tensor(out=TMPB[:], in0=OWS[b2][:, :, 0:64], in1=denb(2), op=ALU.mult),
                          r=['ows%d' % b2, 'den'], w=['tmpb'])
                    tr.op('dve', lambda e: e.tensor_tensor(out=ATTg, in0=ATTg, in1=TMPA[:], op=ALU.add),
                          r=['att', 'tmpa'], w=['att'])
                    tr.op('dve', lambda e: e.tensor_tensor(out=ATTg, in0=ATTg, in1=TMPB[:], op=ALU.add),
                          r=['att', 'tmpb'], w=['att'])
                    if g == 1:
                        AN, ank = ATN2[c % 2], 'atn%d' % (c % 2)
                        tr.op('act', lambda e: e.activation(out=JUNK[:], in_=ATT[:], func=AF.Square, accum_out=SSQ[:]),
                              r=['att'], w=['junk', 'ssq'])
                        tr.op('dve', lambda e: e.tensor_scalar(out=SSQ[:], in0=SSQ[:], scalar1=1.0 / 512, scalar2=EPS,
                                                              op0=ALU.mult, op1=ALU.add), r=['ssq'], w=['ssq'])
                        tr.op('act', lambda e: e.activation(out=SSQ[:], in_=SSQ[:], func=AF.Ln), r=['ssq'], w=['ssq'])
                        tr.op('act', lambda e: e.activation(out=SSQ[:], in_=SSQ[:], func=AF.Exp, scale=-0.5),
                              r=['ssq'], w=['ssq'])
                        tr.op('dve', lambda e, AN=AN: e.scalar_tensor_tensor(out=AN[:], in0=ATT[:], scalar=SSQ[:, 0:1],
                                                                            in1=GAT[:], op0=ALU.mult, op1=ALU.mult),
                              r=['att', 'ssq', 'ac'], w=[ank])

                qload(0)
                qload(1)
                stageA(0)
                for it in range(NIT):
                    qa, qb = [], []
                    if it >= 1:
                        tr.defer = qa
                        tailB(it - 1)
                        tr.defer = None
                    if it + 1 < NIT:
                        tr.defer = qb
                        stageA(it + 1)
                        tr.defer = None
                    while qa or qb:
                        if qb:
                            dq.append(qb.pop(0))
                        if qa:
                            dq.append(qa.pop(0))
                    if it >= 1 and (it - 1) % 2 == 1:
                        deferred(finish_pe, (it - 1) // 2)
                    if it + 2 < NIT:
                        qload(it + 2)
                    stageB(it)
                tailB(NIT - 1)
                finish_pe((NIT - 1) // 2)
            tr.barrier()

        def s5_phase():
            PI = float(np.pi)
            with contextlib.ExitStack() as s_:
                def ssb(name, shape, dt):
                    return s_.enter_context(nc.sbuf_tensor(name, list(shape), dt))
                ROW = [ssb("ROW%d" % i, [128, 2048], F32) for i in range(3)]
                PRE_RE = ssb("PRE_RE", [128, 2048], F32)
                PRE_IM = ssb("PRE_IM", [128, 2048], F32)
                TH = ssb("TH", [128, 2048], F32)
                TMPI = ssb("TMPI", [128, 2048], mybir.dt.int32)
                JC = ssb("JC", [128, 1], F32)
                NJC = ssb("NJC", [128, 1], F32)
                NPI = ssb("NPI", [128, 1], F32)
                COL = ssb("COL", [128, 48], F32)
                IOT = ssb("IOT", [128, 128], F32)
                POST_RE = ssb("POST_RE", [128, 16, 128], F32)
                POST_IM = ssb("POST_IM", [128, 16, 128], F32)
                A128 = ssb("A128", [128, 2, 16], F32)
                SM = [ssb("SM%d" % i, [128, 16], F32) for i in range(3)]
                REP = ssb("REP", [128, 4, 3, 64], F32)
                BT = ssb("BT", [128, 4, 2, 64], F32)
                BB = ssb("BB", [128, 4, 2, 64], F32)
                RT = [ssb("RT%d" % i, [128, 4, 64], F32) for i in range(6)]
                BBD = ssb("BBD", [128, 4, 1024], BF16)
                MASKB = ssb("MASKB", [128, 8], F32)
                CC = ssb("CC", [128, 16, 2, 16], F32)
                MASKC = ssb("MASKC", [128, 4, 8], F32)
                CMAT = ssb("CMAT", [128, 16, 2, 128], BF16)
                DCOL = ssb("DCOL", [128, 4], F32)
                BGL = ssb("BGL", [128, 4], F32)
                WGL = ssb("WGL", [128, 4, 512], BF16)
                TRI = ssb("TRI", [128, 128], BF16)
                UT = [ssb("UT%d" % i, [128, 4, 512], BF16) for i in range(2)]
                T1B = ssb("T1B", [128, 8, 512], BF16)
                T2B = ssb("T2B", [128, 8, 512], BF16)
                NTRI = ssb("NTRI", [128, 128], BF16)
                NCM = ssb("NCM", [128, 16, 128], BF16)
                ZCF = ssb("ZCF", [128, 16, 2, 128], F32)
                CRE = ssb("CRE", [128, 16], F32)
                CIM = ssb("CIM", [128, 16], F32)
                TQ = [ssb("TQ%d" % i, [128, 16], F32) for i in range(4)]
                XU1 = ssb("XU1", [128, 16, 2, 128], BF16)
                XU2 = ssb("XU2", [128, 16, 2, 128], BF16)
                CARRY = ssb("CARRY", [128, 16, 2], F32)
                CT = [ssb("CT%d" % i, [128, 2, 2], F32) for i in range(2)]
                Y = ROW[0][:].rearrange("p (a b) -> p a b", a=4)
                YB = ROW[1][:].rearrange("p (a b) -> p a b", a=4)
                YG = ROW[2][:].rearrange("p (a b) -> p a b", a=4)
                YGB = ssb("YGB", [128, 4, 512], BF16)
                SGL = [ssb("SGL%d" % i, [128, 512], F32) for i in range(2)]
                S = TH[:].rearrange("p (a b) -> p a b", a=4)
                SQ = ssb("SQ", [128, 4, 512], BF16)
                RS5 = ssb("RS5", [128, 512], F32)
                OUT = ssb("OUT", [128, 4, 512], BF16)
                ck = ['s5c']
                for i in range(3):
                    tr.dma('sp', 's5const', ROW[i][:], s5_rows[i:i + 1, :].partition_broadcast(128), w=ck)
                tr.dma('sp', 's5const', JC[:], c_jcol[:, :], w=ck)
                tr.dma('sp', 's5const', COL[:], s5_cols[:, :], w=ck)
                tr.dma('sp', 's5const', IOT[:], c_iota[:, :], w=ck)
                tr.dma('sp', 's5const', REP[:].rearrange("p a b c -> p (a b c)"), s5_rep[:, :], w=ck)
                tr.dma('sp', 's5const', BT[:].rearrange("p a b c -> p (a b c)"), s5_bT[:, :], w=ck)
                tr.dma('sp', 's5const', MASKB[:], c_maskB[:, :], w=ck)
                tr.dma('sp', 's5const', CC[:].rearrange("p a b c -> p (a b c)"), s5_c[:, :], w=ck)
                tr.dma('sp', 's5const', MASKC[:].rearrange("p a b -> p (a b)"), c_maskC[:, :], w=ck)
                tr.dma('sp', 's5const', DCOL[:], s5_dcol[:, :], w=ck)
                tr.dma('sp', 's5const', BGL[:], bglu_col[:, :], w=ck)
                tr.dma('pool', 's5const', TRI[:], c_tri[:, :], w=ck)
                for k in range(4):
                    tr.dma('pool', 's5const', WGL[:, k, :], w_glu[k * 128:(k + 1) * 128, :], w=ck)
                V = lambda fn, r, w: tr.op('dve', fn, r=r, w=w)
                A = lambda fn, r, w: tr.op('act', fn, r=r, w=w)
                V(lambda e: e.memset(NPI[:], -PI), [], ck)
                V(lambda e: e.tensor_scalar(out=NJC[:], in0=JC[:], scalar1=-1.0, scalar2=None, op0=ALU.mult), ck, ck)
                V(lambda e: e.memset(CRE[:], 0.0), [], ['carry'])
                V(lambda e: e.memset(CIM[:], 0.0), [], ['carry'])

                def sincos(out_sin, out_cos, theta, tmpf, tmpi):
                    for out, shift in ((out_sin, 0.0), (out_cos, 0.5 * PI)):
                        V(lambda e, shift=shift: e.tensor_scalar(out=tmpf, in0=theta, scalar1=shift, scalar2=1.0 / (2 * PI),
                                                                 op0=ALU.add, op1=ALU.mult), ck, ck)
                        V(lambda e: e.tensor_copy(out=tmpi, in_=tmpf), ck, ck)
                        V(lambda e: e.tensor_copy(out=tmpf, in_=tmpi), ck, ck)
                        V(lambda e, out=out: e.scalar_tensor_tensor(out=out, in0=tmpf, scalar=-2 * PI, in1=theta,
                                                                    op0=ALU.mult, op1=ALU.add), ck, ck)
                        if shift:
                            V(lambda e, out=out, shift=shift: e.tensor_scalar(out=out, in0=out, scalar1=shift, scalar2=None,
                                                                              op0=ALU.add), ck, ck)
                        V(lambda e, out=out: e.tensor_scalar(out=tmpf, in0=out, scalar1=PI, scalar2=-2 * PI,
                                                             op0=ALU.is_gt, op1=ALU.mult), ck, ck)
                        V(lambda e, out=out: e.tensor_tensor(out=out, in0=out, in1=tmpf, op=ALU.add), ck, ck)
                        A(lambda e, out=out: e.activation(out=out, in_=out, func=AF.Sin), ck, ck)

                A(lambda e: e.activation(out=ROW[2][:], in_=ROW[2][:], func=AF.Exp), ck, ck)
                V(lambda e: e.tensor_tensor(out=ROW[0][:], in0=ROW[0][:], in1=ROW[2][:], op=ALU.mult), ck, ck)
                V(lambda e: e.tensor_tensor(out=ROW[1][:], in0=ROW[1][:], in1=ROW[2][:], op=ALU.mult), ck, ck)
                A(lambda e: e.activation(out=ROW[2][:], in_=ROW[0][:], func=AF.Exp, scale=NJC[:, 0:1]), ck, ck)
                V(lambda e: e.tensor_scalar(out=TH[:], in0=ROW[1][:], scalar1=JC[:, 0:1], scalar2=None, op0=ALU.mult),
                  ck, ck)
                sincos(PRE_IM[:], PRE_RE[:], TH[:], ROW[0][:], TMPI[:])
                V(lambda e: e.tensor_tensor(out=PRE_RE[:], in0=PRE_RE[:], in1=ROW[2][:], op=ALU.mult), ck, ck)
                V(lambda e: e.scalar_tensor_tensor(out=PRE_IM[:], in0=PRE_IM[:], scalar=-1.0, in1=ROW[2][:],
                                                   op0=ALU.mult, op1=ALU.mult), ck, ck)
                LRc, LIc, DTc = COL[:, 0:16], COL[:, 16:32], COL[:, 32:48]
                A(lambda e: e.activation(out=DTc, in_=DTc, func=AF.Exp), ck, ck)
                V(lambda e: e.tensor_tensor(out=LRc, in0=LRc, in1=DTc, op=ALU.mult), ck, ck)
                V(lambda e: e.tensor_tensor(out=LIc, in0=LIc, in1=DTc, op=ALU.mult), ck, ck)
                iob = IOT[:].unsqueeze(1).to_broadcast([128, 16, 128])
                THv = TH[:].rearrange("p (a b) -> p a b", a=16)
                R0v = ROW[0][:].rearrange("p (a b) -> p a b", a=16)
                R2v = ROW[2][:].rearrange("p (a b) -> p a b", a=16)
                V(lambda e: e.tensor_tensor(out=R2v, in0=iob, in1=LRc.unsqueeze(2).to_broadcast([128, 16, 128]),
                                            op=ALU.mult), ck, ck)
                A(lambda e: e.activation(out=ROW[2][:], in_=ROW[2][:], func=AF.Exp), ck, ck)
                V(lambda e: e.tensor_tensor(out=THv, in0=iob, in1=LIc.unsqueeze(2).to_broadcast([128, 16, 128]),
                                            op=ALU.mult), ck, ck)
                sincos(POST_IM[:].rearrange("p a b -> p (a b)"), POST_RE[:].rearrange("p a b -> p (a b)"), TH[:],
                       ROW[0][:], TMPI[:])
                V(lambda e: e.tensor_tensor(out=POST_RE[:], in0=POST_RE[:], in1=R2v, op=ALU.mult), ck, ck)
                V(lambda e: e.tensor_tensor(out=POST_IM[:], in0=POST_IM[:], in1=R2v, op=ALU.mult), ck, ck)
                V(lambda e: e.tensor_scalar(out=SM[0][:], in0=LRc, scalar1=128.0, scalar2=None, op0=ALU.mult), ck, ck)
                A(lambda e: e.activation(out=SM[0][:], in_=SM[0][:], func=AF.Exp), ck, ck)
                V(lambda e: e.tensor_scalar(out=SM[1][:], in0=LIc, scalar1=128.0, scalar2=None, op0=ALU.mult), ck, ck)
                sincos(A128[:, 1, :], A128[:, 0, :], SM[1][:], SM[2][:], TMPI[:, 0:16])
                V(lambda e: e.tensor_tensor(out=A128[:, 0, :], in0=A128[:, 0, :], in1=SM[0][:], op=ALU.mult), ck, ck)
                V(lambda e: e.tensor_tensor(out=A128[:, 1, :], in0=A128[:, 1, :], in1=SM[0][:], op=ALU.mult), ck, ck)
                lr, li, ldt = REP[:, :, 0, :], REP[:, :, 1, :], REP[:, :, 2, :]
                dtv, ldr, ldi, mag, sn, cs = [t[:] for t in RT]
                A(lambda e: e.activation(out=dtv, in_=ldt, func=AF.Exp), ck, ck)
                V(lambda e: e.tensor_tensor(out=ldr, in0=lr, in1=dtv, op=ALU.mult), ck, ck)
                V(lambda e: e.tensor_tensor(out=ldi, in0=li, in1=dtv, op=ALU.mult), ck, ck)
                A(lambda e: e.activation(out=mag, in_=ldr, func=AF.Exp), ck, ck)
                sincos(sn, cs, ldi, dtv, TMPI[:, 0:256].rearrange("p (a b) -> p a b", a=4))
                V(lambda e: e.tensor_tensor(out=cs, in0=cs, in1=mag, op=ALU.mult), ck, ck)
                V(lambda e: e.tensor_tensor(out=sn, in0=sn, in1=mag, op=ALU.mult), ck, ck)
                V(lambda e: e.tensor_scalar(out=cs, in0=cs, scalar1=-1.0, scalar2=None, op0=ALU.add), ck, ck)
                V(lambda e: e.tensor_tensor(out=dtv, in0=lr, in1=lr, op=ALU.mult), ck, ck)
                V(lambda e: e.tensor_tensor(out=mag, in0=li, in1=li, op=ALU.mult), ck, ck)
                V(lambda e: e.tensor_tensor(out=dtv, in0=dtv, in1=mag, op=ALU.add), ck, ck)
                V(lambda e: e.reciprocal(out=dtv, in_=dtv), ck, ck)
                V(lambda e: e.tensor_tensor(out=ldr, in0=cs, in1=lr, op=ALU.mult), ck, ck)
                V(lambda e: e.tensor_tensor(out=mag, in0=sn, in1=li, op=ALU.mult), ck, ck)
                V(lambda e: e.tensor_tensor(out=ldr, in0=ldr, in1=mag, op=ALU.add), ck, ck)
                V(lambda e: e.tensor_tensor(out=ldr, in0=ldr, in1=dtv, op=ALU.mult), ck, ck)
                V(lambda e: e.tensor_tensor(out=ldi, in0=sn, in1=lr, op=ALU.mult), ck, ck)
                V(lambda e: e.tensor_tensor(out=mag, in0=cs, in1=li, op=ALU.mult), ck, ck)
                V(lambda e: e.tensor_tensor(out=ldi, in0=ldi, in1=mag, op=ALU.subtract), ck, ck)
                V(lambda e: e.tensor_tensor(out=ldi, in0=ldi, in1=dtv, op=ALU.mult), ck, ck)
                br, bi_ = BT[:, :, 0, :], BT[:, :, 1, :]
                V(lambda e: e.tensor_tensor(out=BB[:, :, 0, :], in0=ldr, in1=br, op=ALU.mult), ck, ck)
                V(lambda e: e.tensor_tensor(out=mag, in0=ldi, in1=bi_, op=ALU.mult), ck, ck)
                V(lambda e: e.tensor_tensor(out=BB[:, :, 0, :], in0=BB[:, :, 0, :], in1=mag, op=ALU.subtract), ck, ck)
                V(lambda e: e.tensor_tensor(out=BB[:, :, 1, :], in0=ldr, in1=bi_, op=ALU.mult), ck, ck)
                V(lambda e: e.tensor_tensor(out=mag, in0=ldi, in1=br, op=ALU.mult), ck, ck)
                V(lambda e: e.tensor_tensor(out=BB[:, :, 1, :], in0=BB[:, :, 1, :], in1=mag, op=ALU.add), ck, ck)
                BBDv = BBD[:].rearrange("p k (a r g x) -> p k a r g x", a=4, r=2, g=2)
                for kt in range(4):
                    for a in range(4):
                        for ri in range(2):
                            V(lambda e, kt=kt, a=a, ri=ri: e.tensor_tensor(
                                out=BBDv[:, kt, a, ri, :, :],
                                in0=BB[:, kt, ri, :].unsqueeze(1).to_broadcast([128, 2, 64]),
                                in1=MASKB[:, 2 * a:2 * a + 2].unsqueeze(2).to_broadcast([128, 2, 64]),
                                op=ALU.mult), ck, ck)
                V(lambda e: e.tensor_scalar(out=NTRI[:], in0=TRI[:], scalar1=-1.0, scalar2=None, op0=ALU.mult), ck, ck)
                CMv = CMAT[:].rearrange("p a r (g h) -> p a r g h", g=8)
                NCMv = NCM[:].rearrange("p a (g h) -> p a g h", g=8)
                for pr in range(16):
                    in0 = CC[:, pr, 0, :].unsqueeze(1).to_broadcast([128, 8, 16])
                    in1 = MASKC[:, pr % 4, :].unsqueeze(2).to_broadcast([128, 8, 16])
                    V(lambda e, pr=pr, in0=in0, in1=in1: e.scalar_tensor_tensor(
                        out=NCMv[:, pr, :, :], in0=in0, scalar=-1.0, in1=in1, op0=ALU.mult, op1=ALU.mult), ck, ck)
                for pr in range(16):
                    for ri in range(2):
                        in0 = CC[:, pr, ri, :].unsqueeze(1).to_broadcast([128, 8, 16])
                        in1 = MASKC[:, pr % 4, :].unsqueeze(2).to_broadcast([128, 8, 16])
                        if ri == 0:
                            V(lambda e, pr=pr, in0=in0, in1=in1: e.tensor_tensor(out=CMv[:, pr, 0, :, :], in0=in0, in1=in1,
                                                                               op=ALU.mult), ck, ck)
                        else:
                            V(lambda e, pr=pr, in0=in0, in1=in1: e.scalar_tensor_tensor(
                                out=CMv[:, pr, 1, :, :], in0=in0, scalar=-1.0, in1=in1, op0=ALU.mult, op1=ALU.mult),
                                ck, ck)
                uview = uTd.rearrange("p (k t) -> p k t", k=4)
                mview = mixT.rearrange("(k p) t -> p k t", p=128)
                bring = [0]
                def uload(tt):
                    us = tt % 2
                    tr.dma('sp', 'uin%d' % us, UT[us][:], uview[:, :, tt * 512:tt * 512 + 512], r=['uTd'], w=['ut%d' % us])
                def pre(cc, jj):
                    tt, sub = divmod(cc, 4)
                    us = tt % 2; uk = 'ut%d' % us; U = UT[us]
                    tsl = slice(sub * 128, (sub + 1) * 128)
                    kt, half = divmod(jj, 2)
                    i = bring[0] % 2
                    bring[0] += 1
                    PB = pb[i]
                    pk = 'pbu%d' % i
                    pr0 = 4 * kt + 2 * half
                    tr.op('pe', lambda e: e.matmul(
                        out=PB[:], lhsT=U[:, kt, tsl], rhs=BBD[:, kt, half * 512:(half + 1) * 512],
                        start=True, stop=True), r=[uk, 's5c'], w=[pk])
                    PBv = PB[:].rearrange("p (a r x) -> p a r x", a=2, r=2)
                    pre_r = PRE_RE[:, pr0 * 128:pr0 * 128 + 256].rearrange("p (a x) -> p a x", a=2) \
                        .unsqueeze(2).to_broadcast([128, 2, 2, 128])
                    pre_i = PRE_IM[:, pr0 * 128:pr0 * 128 + 256].rearrange("p (a x) -> p a x", a=2) \
                        .unsqueeze(2).to_broadcast([128, 2, 2, 128])
                    t1 = T1B[:, jj, :].rearrange("p (a r x) -> p a r x", a=2, r=2)
                    t2 = T2B[:, jj, :].rearrange("p (a r x) -> p a r x", a=2, r=2)
                    tr.op('dve', lambda e: e.tensor_tensor(out=t1, in0=PBv, in1=pre_r, op=ALU.mult),
                          r=[pk, 's5c'], w=['t1_%d' % jj])
                    tr.op('dve', lambda e: e.tensor_tensor(out=t2, in0=PBv, in1=pre_i, op=ALU.mult),
                          r=[pk, 's5c'], w=['t2_%d' % jj])
                def post_a(cc, jj):
                    pg = jj
                    i = pg % 2
                    PZ = pb[2 + i]
                    zk = 'pz%d' % i
                    pr0 = 2 * pg
                    PZv = PZ[:].rearrange("p (a r x) -> p a r x", a=2, r=2)
                    t1 = T1B[:, jj, :].rearrange("p (a r x) -> p a r x", a=2, r=2)
                    t2 = T2B[:, jj, :].rearrange("p (a r x) -> p a r x", a=2, r=2)
                    rk = ['t1_%d' % jj, 't2_%d' % jj, 's5c']
                    for a in range(2):
                        tr.op('pe', lambda e, a=a: e.matmul(out=PZv[:, a, 0, :], lhsT=t1[:, a, 0, :], rhs=TRI[:],
                                                            start=True, stop=False, skip_group_check=True), r=rk, w=[zk])
                        tr.op('pe', lambda e, a=a: e.matmul(out=PZv[:, a, 0, :], lhsT=t2[:, a, 1, :], rhs=NTRI[:],
                                                            start=False, stop=True, skip_group_check=True), r=rk, w=[zk])
                        tr.op('pe', lambda e, a=a: e.matmul(out=PZv[:, a, 1, :], lhsT=t1[:, a, 1, :], rhs=TRI[:],
                                                            start=True, stop=False, skip_group_check=True), r=rk, w=[zk])
                        tr.op('pe', lambda e, a=a: e.matmul(out=PZv[:, a, 1, :], lhsT=t2[:, a, 0, :], rhs=TRI[:],
                                                            start=False, stop=True, skip_group_check=True), r=rk, w=[zk])
                    zck = 'zc%d' % pg
                    for a in range(2):
                        for ri in range(2):
                            CB_ = CRE if ri == 0 else CIM
                            tr.op('act', lambda e, a=a, ri=ri, CB_=CB_: e.activation(
                                out=ZCF[:, pr0 + a, ri, :], in_=PZv[:, a, ri, :], func=AF.Identity,
                                bias=CB_[:, pr0 + a:pr0 + a + 1]), r=[zk, 'carry'], w=[zck])
                def post_b(cc, jj):
                    pg = jj
                    pr0 = 2 * pg
                    zck = 'zc%d' % pg
                    Z = ZCF[:, pr0:pr0 + 2, :, :]
                    po_r = POST_RE[:, pr0:pr0 + 2, :].unsqueeze(2).to_broadcast([128, 2, 2, 128])
                    po_i = POST_IM[:, pr0:pr0 + 2, :].unsqueeze(2).to_broadcast([128, 2, 2, 128])
                    tr.op('dve', lambda e: e.tensor_tensor(out=XU1[:, pr0:pr0 + 2, :, :], in0=Z, in1=po_r, op=ALU.mult),
                          r=[zck, 's5c'], w=['x%d' % pg])
                    tr.op('dve', lambda e: e.tensor_tensor(out=XU2[:, pr0:pr0 + 2, :, :], in0=Z, in1=po_i, op=ALU.mult),
                          r=[zck, 's5c'], w=['x%d' % pg])
                def post_end(cc):
                    tt, sub = divmod(cc, 4)
                    us = tt % 2; uk = 'ut%d' % us; U = UT[us]
                    tsl = slice(sub * 128, (sub + 1) * 128)
                    zall = ['zc%d' % pg for pg in range(8)]
                    zr = ZCF[:, :, 0, 127]
                    zi = ZCF[:, :, 1, 127]
                    tr.op('dve', lambda e: e.tensor_tensor(out=TQ[0][:], in0=zr, in1=A128[:, 0, :], op=ALU.mult),
                          r=zall + ['s5c'], w=['tq'])
                    tr.op('dve', lambda e: e.tensor_tensor(out=TQ[1][:], in0=zi, in1=A128[:, 1, :], op=ALU.mult),
                          r=zall + ['s5c'], w=['tq'])
                    tr.op('dve', lambda e: e.tensor_tensor(out=TQ[2][:], in0=zr, in1=A128[:, 1, :], op=ALU.mult),
                          r=zall + ['s5c'], w=['tq'])
                    tr.op('dve', lambda e: e.tensor_tensor(out=TQ[3][:], in0=zi, in1=A128[:, 0, :], op=ALU.mult),
                          r=zall + ['s5c'], w=['tq'])
                    tr.op('dve', lambda e: e.tensor_tensor(out=CRE[:], in0=TQ[0][:], in1=TQ[1][:], op=ALU.subtract),
                          r=['tq'], w=['carry'])
                    tr.op('dve', lambda e: e.tensor_tensor(out=CIM[:], in0=TQ[2][:], in1=TQ[3][:], op=ALU.add),
                          r=['tq'], w=['carry'])
                def cmm(cc, kt):
                    i = kt % 2
                    PY = pb[4 + i]
                    yk = 'py%d' % i
                    n = 0
                    for a in range(4):
                        pr = 4 * kt + a
                        terms = ((CMAT[:, pr, 0, :], XU1[:, pr, 0, :]), (NCM[:, pr, :], XU2[:, pr, 1, :]),
                                 (CMAT[:, pr, 1, :], XU1[:, pr, 1, :]), (CMAT[:, pr, 1, :], XU2[:, pr, 0, :]))
                        for (cm, xx) in terms:
                            tr.op('pe', lambda e, PY=PY, cm=cm, xx=xx, n=n: e.matmul(
                                out=PY[:, 0:128], lhsT=cm, rhs=xx,
                                start=(n == 0), stop=(n == 15)), r=['s5c', 'x%d' % (pr // 2)], w=[yk])
                            n += 1
                def yop(cc, kt):
                    tt, sub = divmod(cc, 4)
                    us = tt % 2; uk = 'ut%d' % us; U = UT[us]
                    tsl = slice(sub * 128, (sub + 1) * 128)
                    PY = pb[4 + kt % 2]
                    tr.op('dve', lambda e: e.scalar_tensor_tensor(
                        out=Y[:, kt, tsl], in0=U[:, kt, tsl], scalar=DCOL[:, kt:kt + 1], in1=PY[:, 0:128],
                        op0=ALU.mult, op1=ALU.add), r=[uk, 'py%d' % (kt % 2), 's5c'], w=['y'])
                def tail(tt):
                    t0 = tt * 512
                    Yf = ROW[0][:]
                    YBf = ROW[1][:]
                    YGf = ROW[2][:]
                    tr.op('act', lambda e: e.activation(out=YBf, in_=Yf, func=AF.Square), r=['y'], w=['yb'])
                    tr.op('act', lambda e: e.activation(out=YBf, in_=YBf, func=AF.Identity, scale=0.044715,
                                                       bias=ONEC[:, 0:1]), r=['yb', 'c'], w=['yb'])
                    tr.op('dve', lambda e: e.tensor_tensor(out=YBf, in0=YBf, in1=Yf, op=ALU.mult), r=['yb', 'y'], w=['yb'])
                    tr.op('act', lambda e: e.activation(out=YBf, in_=YBf, func=AF.Sigmoid, scale=GELU_C), r=['yb'], w=['yb'])
                    tr.op('dve', lambda e: e.tensor_tensor(out=YGf, in0=Yf, in1=YBf, op=ALU.mult), r=['y', 'yb'], w=['yg'])
                    tr.op('act', lambda e: e.activation(out=YGB[:].rearrange("p a b -> p (a b)"), in_=YGf, func=AF.Copy),
                          r=['yg'], w=['ygb'])
                    for mt in range(4):
                        i = mt % 2
                        PG = pb[6 + i]
                        gk = 'pgl%d' % i
                        for kt in range(4):
                            tr.op('pe', lambda e, PG=PG, kt=kt, mt=mt: e.matmul(
                                out=PG[:], lhsT=WGL[:, kt, mt * 128:(mt + 1) * 128], rhs=YGB[:, kt, :],
                                start=(kt == 0), stop=(kt == 3)), r=['s5c', 'ygb'], w=[gk])
                        tr.op('act', lambda e, PG=PG, mt=mt, i=i: e.activation(
                            out=SGL[i][:], in_=PG[:], func=AF.Sigmoid, bias=BGL[:, mt:mt + 1]), r=[gk, 's5c'],
                            w=['sgl%d' % i])
                        tr.op('dve', lambda e, mt=mt, i=i: e.tensor_tensor(out=S[:, mt, :], in0=YG[:, mt, :],
                                                                          in1=SGL[i][:], op=ALU.mult),
                              r=['yg', 'sgl%d' % i], w=['s'])
                    tr.op('act', lambda e: e.activation(out=SQ[:].rearrange("p a b -> p (a b)"),
                                                       in_=TH[:], func=AF.Square),
                          r=['s'], w=['sq'])
                    PN = pb[6]
                    for mt in range(4):
                        tr.op('pe', lambda e, mt=mt: e.matmul(out=PN[:], lhsT=ONES[:], rhs=SQ[:, mt, :],
                                                             start=(mt == 0), stop=(mt == 3)), r=['sq', 'c'], w=['pgl0'])
                    tr.op('dve', lambda e: e.tensor_scalar(out=RS5[:], in0=PN[:], scalar1=1.0 / 512, scalar2=EPS,
                                                          op0=ALU.mult, op1=ALU.add), r=['pgl0'], w=['rs5'])
                    tr.op('act', lambda e: e.activation(out=RS5[:], in_=RS5[:], func=AF.Ln), r=['rs5'], w=['rs5'])
                    tr.op('act', lambda e: e.activation(out=RS5[:], in_=RS5[:], func=AF.Exp, scale=-0.5),
                          r=['rs5'], w=['rs5'])
                    for mt in range(4):
                        tr.op('dve', lambda e, mt=mt: e.scalar_tensor_tensor(
                            out=OUT[:, mt, :], in0=S[:, mt, :], scalar=GC[:, 32 + mt:33 + mt], in1=RS5[:],
                            op0=ALU.mult, op1=ALU.mult), r=['s', 'rs5', 'c'], w=['out5'])
                    tr.dma('sp', 'sout', mview[:, 4:8, t0:t0 + 512], OUT[:], r=['out5'], w=['mixT'])
                uload(0)
                uload(1)
                for jj in range(8):
                    pre(0, jj)
                for cc in range(32):
                    post_a(cc, 0)
                    for jj in range(8):
                        if jj + 1 < 8:
                            post_a(cc, jj + 1)
                        if cc + 1 < 32:
                            pre(cc + 1, jj)
                        post_b(cc, jj)
                        if jj % 2 == 1:
                            cmm(cc, jj // 2)
                            if jj >= 3:
                                yop(cc, jj // 2 - 1)
                    post_end(cc)
                    yop(cc, 3)
                    if cc % 4 == 3:
                        tail(cc // 4)
                        if cc // 4 + 2 < 8:
                            uload(cc // 4 + 2)
            tr.barrier()

        def wout_phase(after_wo=None):
            with contextlib.ExitStack() as w_:
                def wsb(name, shape, dt):
                    return w_.enter_context(nc.sbuf_tensor(name, list(shape), dt))
                WO = wsb("WO", [128, 8, D], BF16)
                XW = [wsb("XW%d" % i, [128, 8, 512], F32) for i in range(2)]
                MX = [wsb("MX%d" % i, [128, 8, 512], BF16) for i in range(2)]
                for k in range(8):
                    tr.dma('pool', 'wo', WO[:, k, :], w_out[k * 128:(k + 1) * 128, :], w=['WO'])
                if after_wo is not None:
                    after_wo()
                for tt in range(8):
                    s = tt % 2
                    t0 = tt * 512
                    xk, mk = 'xw%d' % s, 'mx%d' % s
                    tr.dma('sp', 'xwin%d' % s, XW[s][:], xview(x1T, t0, 512), r=['x1T_%d' % tt], w=[xk])
                    tr.dma('act', 'mxin%d' % s, MX[s][:], xview(mixT, t0, 512), r=['mixT'], w=[mk])
                    for dk in range(8):
                        b = dk % 4
                        for k in range(8):
                            tr.op('pe', lambda e, k=k, dk=dk, b=b, s=s: e.matmul(
                                out=pb[b][:], lhsT=WO[:, k, dk * 128:(dk + 1) * 128], rhs=MX[s][:, k, :],
                                start=(k == 0), stop=(k == 7)), r=['WO', mk], w=['pw%d' % b])
                        tr.op('dve', lambda e, dk=dk, b=b, s=s: e.tensor_tensor(
                            out=XW[s][:, dk, :], in0=pb[b][:], in1=XW[s][:, dk, :], op=ALU.add),
                            r=['pw%d' % b, xk], w=[xk])
                    tr.dma('act', 'xwout%d' % s, xview(x1T, t0, 512), XW[s][:], r=[xk], w=['x1T_%d' % tt])
            tr.barrier()

        ffn_phase(1)

        with contextlib.ExitStack() as ms:
            if stop < 1.2:
                return nc
            def msb(name, shape, dt):
                return ms.enter_context(nc.sbuf_tensor(name, list(shape), dt))
            KE = [msb("KE%d" % g, [128, T], BF16) for g in range(2)]
            KW = [msb("KW%d" % g, [64, T], BF16) for g in range(2)]
            VS1 = [msb("VS1%d" % g, [128, 32, 65], BF16) for g in range(2)]
            VW1 = [msb("VW1%d" % g, [128, 32, 65], BF16) for g in range(2)]
            GT = msb("GT", [128, 32, 24], F32)
            KC = [msb("KC%d" % g, [64, 256], BF16) for g in range(2)]
            VCX = [msb("VCX%d" % g, [128, 2, 129], BF16) for g in range(2)]
            for g in range(2):
                tr.dma('pool', 'const', KE[g][64:128, :], c_E0[:, :], w=['KE%d' % g])
                tr.op('dve', lambda e, g=g: e.memset(VS1[g][:, :, 64:65], 1.0), w=['VS1%d' % g])
                tr.op('dve', lambda e, g=g: e.memset(VW1[g][:, :, 64:65], 1.0), w=['VW1%d' % g])
                tr.op('dve', lambda e, g=g: e.memset(VCX[g][:, :, 64:65], 1.0), w=['VCX%d' % g])
                tr.dma('pool', 'const', VCX[g][:, :, 65:129], c_ovl.rearrange("p (a b) -> p a b", a=2),
                       w=['VCX%d' % g])

            with contextlib.ExitStack() as ps_:
                def psb(name, shape, dt):
                    return ps_.enter_context(nc.sbuf_tensor(name, list(shape), dt))
                WIN = psb("WIN", [128, 8, 1816], BF16)
                H2 = [psb("H2_%d" % i, [128, 8, 512], BF16) for i in range(2)]
                QST = psb("QST", [64, 4, 8, 128], BF16)
                UST = psb("UST", [128, 4, 512], BF16)
                KCin = [psb("KCin%d" % g, [64, 16, 256], BF16) for g in range(2)]
                VCin = [psb("VCin%d" % g, [64, 16, 256], BF16) for g in range(2)]
                GB = psb("GB", [128, 24], F32)
                W1 = [psb("W1_%d" % i, [64, 32, 256], BF16) for i in range(2)]
                W2 = [psb("W2_%d" % i, [128, 2, 64], BF16) for i in range(2)]
                CB1 = psb("CB1", [128, 4], F32)
                POST = psb("POST", [64, 64], BF16)
                HID = psb("HID", [128, 2, 256], BF16)
                GTMP = [psb("GTMP%d" % i, [128, 256], F32) for i in range(3)]
                PBIAS = psb("PBIAS", [128, 1], F32)
                for k in range(8):
                    tr.dma('pool', 'win', WIN[:, k, :], w_in[k * 128:(k + 1) * 128, :], w=['WIN'])
                tr.dma('sp', 'const', GB[:], gate_bias.partition_broadcast(128), w=['GB'])
                tr.dma('sp', 'const', CB1[:], cb1[:, :], w=['cmpw'])
                tr.dma('pool', 'const', POST[:], posT[:, :], w=['cmpw'])
                for i in range(2):
                    w1v = cw1[i].rearrange("(l d) h -> d l h", d=64)
                    for lc in range(8):
                        tr.dma('pool', 'const', W1[i][:, 4 * lc:4 * lc + 4, :], w1v[:, 4 * lc:4 * lc + 4, :], w=['cmpw'])
                    tr.dma('pool', 'const', W2[i][:], cw2[i].rearrange("(t p) d -> p t d", p=128), w=['cmpw'])
                if stop < 1.6:
                    tr.barrier()
                    return nc
                ring = [0]

                def nextbank():
                    b = ring[0] % 6
                    ring[0] += 1
                    return pb[b], 'pp%d' % b

                evq = [0]

                def evac(out, in_, rk, wk, force=None):
                    evq[0] += 1
                    if (evq[0] % 2 == 0 and force is None) or force == 'act':
                        tr.op('act', lambda e: e.activation(out=out, in_=in_, func=AF.Copy), r=[rk], w=[wk])
                    else:
                        tr.op('dve', lambda e: e.tensor_copy(out=out, in_=in_), r=[rk], w=[wk])

                for tt in range(8):
                    s = tt % 2
                    t0 = tt * 512
                    hk = 'h2_%d' % s
                    HH = H2[s]
                    tr.dma('sp', 'h2in%d' % s, HH[:], xview(h2T, t0, 512), r=['h2T'], w=[hk])
                    for h in range(8):
                        P, pk = nextbank()
                        for k in range(8):
                            tr.op('pe', lambda e, k=k, h=h, P=P: e.matmul(
                                out=P[0:64, :], lhsT=WIN[:, k, 64 * h:64 * h + 64], rhs=HH[:, k, :],
                                start=(k == 0), stop=(k == 7)), r=['WIN', hk], w=[pk])
                        evac(QST[:, :, h, :], P[0:64, :].rearrange("p (c q) -> p c q", q=128), pk, 'qst')
                    tr.dma('sp', 'qout', qTd.rearrange("p (c x) -> p c x", c=32)[:, 4 * tt:4 * tt + 4, :],
                           QST[:].rearrange("p c h q -> p c (h q)"), r=['qst'], w=['qTd'])
                    for (c0, dest, dn) in ((512, KCin, 'KCin'), (640, VCin, 'VCin'), (768, KE, 'KE'), (1024, KW, 'KW')):
                        for g in range(2):
                            P, pk = nextbank()
                            for k in range(8):
                                tr.op('pe', lambda e, k=k, P=P, cc=c0 + 64 * g: e.matmul(
                                    out=P[0:64, :], lhsT=WIN[:, k, cc:cc + 64], rhs=HH[:, k, :],
                                    start=(k == 0), stop=(k == 7)), r=['WIN', hk], w=[pk])
                            if dn in ('KCin', 'VCin'):
                                evac(dest[g][:, :, tt * 32:(tt + 1) * 32],
                                     P[0:64, :].rearrange("p (m r) -> p r m", r=16), pk, '%s%d' % (dn, g))
                            else:
                                evac(dest[g][0:64, t0:t0 + 512], P[0:64, :], pk, '%s%d' % (dn, g))
                    for kt in range(4):
                        P, pk = nextbank()
                        for k in range(8):
                            tr.op('pe', lambda e, k=k, P=P, cc=1304 + 128 * kt: e.matmul(
                                out=P[:, :], lhsT=WIN[:, k, cc:cc + 128], rhs=HH[:, k, :],
                                start=(k == 0), stop=(k == 7)), r=['WIN', hk], w=[pk])
                        evac(UST[:, kt, :], P[:, :], pk, 'ust')
                    tr.dma('sp', 'uout', uTd.rearrange("p (k t) -> p k t", k=4)[:, :, t0:t0 + 512], UST[:],
                           r=['ust'], w=['uTd'])
                    for sbk in range(4):
                        blk = tt * 4 + sbk
                        P, pk = nextbank()
                        for k in range(8):
                            tr.op('pe', lambda e, k=k, P=P, sbk=sbk: e.matmul(
                                out=P[:, 0:408], lhsT=HH[:, k, sbk * 128:(sbk + 1) * 128], rhs=WIN[:, k, 896:1304],
                                start=(k == 0), stop=(k == 7)), r=['WIN', hk], w=[pk])
                        for g in range(2):
                            evac(VS1[g][:, blk, 0:64], P[:, 64 * g:64 * g + 64], pk, 'VS1%d' % g, force='dve')
                            evac(VW1[g][:, blk, 0:64], P[:, 256 + 64 * g:256 + 64 * g + 64], pk, 'VW1%d' % g, force='dve')
                        tr.op('dve', lambda e, P=P, blk=blk: e.tensor_tensor(out=GT[:, blk, :], in0=P[:, 384:408],
                                                                            in1=GB[:], op=ALU.add),
                              r=[pk, 'GB'], w=['GT'])
                        tr.op('act', lambda e, blk=blk: e.activation(out=GT[:, blk, :], in_=GT[:, blk, :],
                                                                    func=AF.Sigmoid), r=['GT'], w=['GT'])

                if stop < 1.8:
                    tr.barrier()
                    return nc
                HIDS = [psb("HIDS%d" % g, [128, 2, 256], BF16) for g in range(2)]
                for g in range(2):
                    tr.op('dve', lambda e, g=g: e.memset(HIDS[g][:], 0.0), w=['hid%d_0' % g, 'hid%d_1' % g])
                A, B_, C_ = GTMP
                for kind in range(2):
                    srcs = KCin if kind == 0 else VCin
                    sname = 'KCin' if kind == 0 else 'VCin'
                    for hh in range(2):
                        P, pk = nextbank()
                        for l in range(32):
                            tr.op('pe', lambda e, l=l, P=P, hh=hh: e.matmul(
                                out=P[:, 0:1], lhsT=W1[kind][:, l, hh * 128:(hh + 1) * 128],
                                rhs=POST[:, kind * 32 + l:kind * 32 + l + 1], start=(l == 0), stop=(l == 31)),
                                r=['cmpw'], w=[pk])
                        tr.op('dve', lambda e, P=P, hh=hh: e.tensor_tensor(
                            out=PBIAS[:], in0=P[:, 0:1], in1=CB1[:, kind * 2 + hh:kind * 2 + hh + 1], op=ALU.add),
                            r=[pk, 'cmpw'], w=['pbias'])
                        for g in range(2):
                            P2, pk2 = nextbank()
                            for l in range(32):
                                rhs = srcs[g][:, l % 16, (l // 16):(l // 16) + 255]
                                tr.op('pe', lambda e, l=l, P2=P2, rhs=rhs, hh=hh: e.matmul(
                                    out=P2[:, 0:255], lhsT=W1[kind][:, l, hh * 128:(hh + 1) * 128], rhs=rhs,
                                    start=(l == 0), stop=(l == 31)), r=['cmpw', '%s%d' % (sname, g)], w=[pk2])
                            tr.op('dve', lambda e, P2=P2: e.tensor_scalar(out=A[:, 0:255], in0=P2[:, 0:255],
                                                                         scalar1=PBIAS[:, 0:1], scalar2=None,
                                                                         op0=ALU.add), r=[pk2, 'pbias'], w=['ga'])
                            tr.op('dve', lambda e: e.tensor_tensor(out=B_[:, 0:255], in0=A[:, 0:255], in1=A[:, 0:255],
                                                                  op=ALU.mult), r=['ga'], w=['gb'])
                            tr.op('dve', lambda e: e.tensor_scalar(out=B_[:, 0:255], in0=B_[:, 0:255], scalar1=0.044715,
                                                                  scalar2=1.0, op0=ALU.mult, op1=ALU.add),
                                  r=['gb'], w=['gb'])
                            tr.op('dve', lambda e: e.tensor_tensor(out=B_[:, 0:255], in0=B_[:, 0:255], in1=A[:, 0:255],
                                                                  op=ALU.mult), r=['gb', 'ga'], w=['gb'])
                            tr.op('act', lambda e: e.activation(out=C_[:, 0:255], in_=B_[:, 0:255], func=AF.Sigmoid,
                                                               scale=GELU_C), r=['gb'], w=['gc'])
                            tr.op('dve', lambda e, g=g, hh=hh: e.tensor_tensor(
                                out=HIDS[g][:, hh, 0:255], in0=A[:, 0:255], in1=C_[:, 0:255], op=ALU.mult),
                                r=['ga', 'gc'], w=['hid%d_%d' % (g, hh)])
                    for g in range(2):
                        hk2 = ['hid%d_0' % g, 'hid%d_1' % g]
                        if kind == 0:
                            P, pk = nextbank()
                            for hh in range(2):
                                tr.op('pe', lambda e, hh=hh, P=P, g=g: e.matmul(
                                    out=P[0:64, 0:256], lhsT=W2[0][:, hh, :], rhs=HIDS[g][:, hh, :],
                                    start=(hh == 0), stop=(hh == 1)), r=['cmpw'] + hk2, w=[pk])
                            evac(KC[g][:, :], P[0:64, 0:256], pk, 'KC%d' % g)
                        else:
                            for nt in range(2):
                                P, pk = nextbank()
                                for hh in range(2):
                                    tr.op('pe', lambda e, hh=hh, P=P, g=g, nt=nt: e.matmul(
                                        out=P[:, 0:64], lhsT=HIDS[g][:, hh, nt * 128:(nt + 1) * 128],
                                        rhs=W2[1][:, hh, :], start=(hh == 0), stop=(hh == 1)),
                                        r=['cmpw'] + hk2, w=[pk])
                                evac(VCX[g][:, nt, 0:64], P[:, 0:64], pk, 'VCX%d' % g)
            tr.barrier()
            if stop >= 3:
                attention_phase()
        if stop >= 4:
            s5_phase()
        with contextlib.ExitStack() as w2s:
            wts2 = ffn_weights(2, w2s, load=False) if stop >= 6 else None
            if stop >= 5:
                wout_phase((lambda: ffn_wload(2, wts2, gu=True, wd=False)) if stop >= 6 else None)
            if stop >= 6:
                ffn_phase(2, wts2)
    return nc


def _consts():
    f = np.float32
    c = {}
    c["c_ident"] = np.eye(128, dtype=f)
    j = np.arange(128)
    c["c_tri"] = (j[:, None] <= j[None, :]).astype(f)
    c["c_E0"] = (np.arange(T)[None, :] // 64 == np.arange(64)[:, None]).astype(f)
    cb = np.where(j[:, None] > j[None, :], -BIGM, 0.0).astype(f)
    wb = np.where(j[:, None] <= j[None, :], -BIGM, 0.0).astype(f)
    cm = np.where(16 * (j[:, None] - 64) > j[None, :] - 31, -BIGM, 0.0).astype(f)
    c["c_CB"] = np.tile(cb, (1, 4))
    c["c_WB"] = np.tile(wb, (1, 4))
    c["c_CM"] = np.tile(cm, (1, 4))
    c["c_BD"] = (np.arange(384)[None, :] == j[:, None] + 128).astype(f)
    hh = (j >= 64).astype(np.int64)[:, None]
    m = np.arange(128)[None, :]
    c["c_U0"] = np.where(m > 62 + hh, -1e30, 3e38).astype(f)
    l0 = np.full((128, 128), -3e38, dtype=f)
    l0[m == 62 + hh] = 2e30
    l0[m == 61 + hh] = 1e30
    c["c_L0"] = l0
    n = np.arange(256)[:, None]
    jj = np.arange(64)[None, :]
    ov = ((16 * n < 64 * jj + 64) & (16 * n + 32 > 64 * jj)).astype(f)
    ov[255] = 0
    c["c_ovl"] = np.ascontiguousarray(ov.reshape(2, 128, 64).transpose(1, 0, 2).reshape(128, 128))
    c["c_maskB"] = (np.arange(8)[None, :] == (j // 16)[:, None]).astype(f)
    gi = (j // 64)[:, None, None]
    p4 = np.arange(4)[None, :, None]
    g8 = np.arange(8)[None, None, :]
    c["c_maskC"] = (g8 == 2 * p4 + gi).astype(f).reshape(128, 32)
    c["c_iota"] = np.tile(np.arange(128, dtype=f)[None, :], (128, 1))
    c["c_jcol"] = np.arange(128, dtype=f)[:, None].copy()
    return c


def _prep_shared(inp):
    f = np.float32
    A = lambda a: np.ascontiguousarray(np.asarray(a, dtype=f))
    d = {}
    d["wg1"], d["wu1"], d["wd1"] = A(inp["ffn1_w_gate"][0]), A(inp["ffn1_w_up"][0]), A(inp["ffn1_w_down"][0])
    d["wg2"], d["wu2"], d["wd2"] = A(inp["ffn2_w_gate"][0]), A(inp["ffn2_w_up"][0]), A(inp["ffn2_w_down"][0])
    d["w_in"], d["w_out"], d["w_glu"] = A(inp["w_in"][0]), A(inp["w_out"][0]), A(inp["s5_w_glu"][0])
    col8 = lambda v: np.asarray(v, dtype=f).reshape(-1, 128).T
    d["gcols"] = A(np.concatenate([col8(inp["ffn1_norm"][0]), col8(inp["mix_norm"][0]), col8(inp["ffn2_norm"][0]),
                                   col8(inp["final_norm"]), col8(inp["ssm_out_norm"][0])], axis=1))
    d["grow_attn"] = A(np.asarray(inp["attn_out_norm"][0]).reshape(1, 512))
    d["gate_bias"] = A(np.asarray(inp["gate_bias"][0]).reshape(1, 24))
    d["cw1k"], d["cw1v"] = A(inp["cmp_k_w1"][0]), A(inp["cmp_v_w1"][0])
    d["cw2k"], d["cw2v"] = A(inp["cmp_k_w2"][0]), A(inp["cmp_v_w2"][0])
    d["cb1"] = A(np.concatenate([col8(inp["cmp_k_b1"][0]), col8(inp["cmp_v_b1"][0])], axis=1))
    d["posT"] = A(np.concatenate([np.asarray(inp["cmp_pos_k"][0]).T, np.asarray(inp["cmp_pos_v"][0]).T], axis=1))
    lr = np.asarray(inp["s5_lambda_re"][0], dtype=f)
    li = np.asarray(inp["s5_lambda_im"][0], dtype=f)
    ldt = np.repeat(np.asarray(inp["s5_log_dt"][0], dtype=f)[:, None], 64, axis=1)
    d["s5_rows"] = A(np.stack([lr.reshape(2048), li.reshape(2048), ldt.reshape(2048)]))
    colify = lambda a: a.reshape(16, 128).T
    d["s5_cols"] = A(np.concatenate([colify(lr), colify(li), colify(ldt)], axis=1))

    def rows_gh(a):
        return np.repeat(a.reshape(4, 8, 1, 64), 16, axis=2).transpose(1, 2, 0, 3).reshape(128, 4, 64)
    d["s5_rep"] = A(np.stack([rows_gh(lr), rows_gh(li), rows_gh(ldt)], axis=2).reshape(128, 4 * 3 * 64))

    def bt(a):
        return np.asarray(a, dtype=f).reshape(4, 8, 64, 16).transpose(1, 3, 0, 2).reshape(128, 4, 64)
    d["s5_bT"] = A(np.stack([bt(inp["s5_b_re"][0]), bt(inp["s5_b_im"][0])], axis=2).reshape(128, 4 * 2 * 64))

    def ct(a):
        return np.asarray(a, dtype=f).reshape(16, 2, 16, 64).transpose(1, 3, 0, 2).reshape(128, 16, 16)
    d["s5_c"] = A(np.stack([ct(inp["s5_c_re"][0]), ct(inp["s5_c_im"][0])], axis=2).reshape(128, 16 * 2 * 16))
    d["s5_dcol"] = A(np.asarray(inp["s5_d"][0], dtype=f).reshape(4, 128).T)
    d["bglu_col"] = A(np.asarray(inp["s5_b_glu"][0], dtype=f).reshape(4, 128).T)
    d.update(_consts())
    return d


def kernel(**inputs):
    x = np.asarray(inputs["x"], dtype=np.float32)
    shared = _prep_shared(inputs)
    nc = build(dbg=False)
    in_maps = []
    for b in range(NCORES):
        m = dict(shared)
        m["xT"] = np.ascontiguousarray(x[b].T)
        in_maps.append(m)
    res = run_bass_kernel_spmd(nc, in_maps, core_ids=list(range(NCORES)))
    out = np.empty((NCORES, T, D), dtype=np.float32)
    for b in range(NCORES):
        out[b] = np.asarray(res.results[b]["yT"], dtype=np.float32).T
    return out
```

```python
import contextlib
import numpy as np
import concourse.bass as bass
import concourse.mybir as mybir
from concourse.bass_utils import run_bass_kernel_spmd

F32 = mybir.dt.float32
BF16 = mybir.dt.bfloat16
ALU = mybir.AluOpType
AF = mybir.ActivationFunctionType

T = 4096
D = 1024
FF = 2816
NF = FF // 128
EPS = 1e-6
BIGM = 30000.0
NCORES = 8
GELU_C = 1.5957691216057308


class TR:
    def __init__(self, nc, es):
        self.nc, self.es = nc, es
        self.eng = dict(pe=nc.tensor, dve=nc.vector, act=nc.scalar, pool=nc.gpsimd, sp=nc.sync)
        self.sem, self.cnt = {}, {}
        self.seen = {e: {} for e in self.eng}
        for e in self.eng:
            self.sem[e] = es.enter_context(nc.semaphore("s_" + e))
            self.cnt[e] = 0
        self.bs = {}
        self.defer = None

    def _st(self, k):
        s = self.bs.get(k)
        if s is None:
            s = self.bs[k] = ({}, {})
        return s

    def _deps(self, r, w):
        d = {}
        for k in r:
            for sk, v in self._st(k)[0].items():
                if d.get(sk, 0) < v:
                    d[sk] = v
        for k in w:
            s = self._st(k)
            for dd in s:
                for sk, v in dd.items():
                    if d.get(sk, 0) < v:
                        d[sk] = v
        return d

    def _wait(self, e, d):
        for k, v in d.items():
            if k == e and e == 'pe':
                continue
            if self.seen[e].get(k, 0) >= v:
                continue
            self.eng[e].wait_ge(self.sem[k], v)
            self.seen[e][k] = v

    def _mark(self, src, v, r, w):
        for k in r:
            self._st(k)[1][src] = v
        for k in w:
            self._st(k)[0][src] = v

    def op(self, e, fn, r=(), w=()):
        if self.defer is not None:
            r, w = list(r), list(w)
            self.defer.append(lambda: self.op(e, fn, r, w))
            return
        self._wait(e, self._deps(r, w))
        ins = fn(self.eng[e])
        self.cnt[e] += 1
        ins.then_inc(self.sem[e], 1)
        self._mark(e, self.cnt[e], r, w)

    def dma(self, q, ch, out, in_, r=(), w=()):
        if self.defer is not None:
            r, w = list(r), list(w)
            self.defer.append(lambda: self.dma(q, ch, out, in_, r, w))
            return
        if ch not in self.sem:
            self.sem[ch] = self.es.enter_context(self.nc.semaphore("c_" + ch))
            self.cnt[ch] = 0
        self._wait(q, self._deps(r, w))
        ins = self.eng[q].dma_start(out=out, in_=in_)
        self.cnt[ch] += 16
        ins.then_inc(self.sem[ch], 16)
        self._mark(ch, self.cnt[ch], r, w)

    def barrier(self):
        allk = {k: v for k, v in self.cnt.items() if v > 0}
        for e in self.eng:
            self._wait(e, allk)


def build(dbg=False, stop=9):
    nc = bass.Bass("TRN2", target_bir_lowering=False)
    es = contextlib.ExitStack()
    with es:
        tr = TR(nc, es)

        def din(name, shape):
            return nc.dram_tensor(name, list(shape), F32, kind="ExternalInput").ap()

        def dscr(name, shape, dt):
            kind = "ExternalOutput" if dbg else "Internal"
            return nc.dram_tensor(name, list(shape), dt, kind=kind).ap()

        def sb(name, shape, dt):
            return es.enter_context(nc.sbuf_tensor(name, list(shape), dt))

        xT = din("xT", [D, T])
        wgs = [din("wg1", [D, FF]), din("wg2", [D, FF])]
        wus = [din("wu1", [D, FF]), din("wu2", [D, FF])]
        wds = [din("wd1", [FF, D]), din("wd2", [FF, D])]
        w_in = din("w_in", [D, 1816])
        w_out = din("w_out", [D, D])
        w_glu = din("w_glu", [512, 512])
        gcols = din("gcols", [128, 36])
        grow_attn = din("grow_attn", [1, 512])
        gate_bias = din("gate_bias", [1, 24])
        cw1 = [din("cw1k", [2048, 256]), din("cw1v", [2048, 256])]
        cw2 = [din("cw2k", [256, 64]), din("cw2v", [256, 64])]
        cb1 = din("cb1", [128, 4])
        posT = din("posT", [64, 64])
        s5_rows = din("s5_rows", [3, 2048])
        s5_cols = din("s5_cols", [128, 48])
        s5_rep = din("s5_rep", [128, 4 * 3 * 64])
        s5_bT = din("s5_bT", [128, 4 * 2 * 64])
        s5_c = din("s5_c", [128, 16 * 2 * 16])
        s5_dcol = din("s5_dcol", [128, 4])
        bglu_col = din("bglu_col", [128, 4])
        c_ident = din("c_ident", [128, 128])
        c_tri = din("c_tri", [128, 128])
        c_E0 = din("c_E0", [64, T])
        c_CB = din("c_CB", [128, 512])
        c_WB = din("c_WB", [128, 512])
        c_CM = din("c_CM", [128, 512])
        c_BD = din("c_BD", [128, 384])
        c_U0 = din("c_U0", [128, 128])
        c_L0 = din("c_L0", [128, 128])
        c_ovl = din("c_ovl", [128, 128])
        c_maskB = din("c_maskB", [128, 8])
        c_maskC = din("c_maskC", [128, 32])
        c_iota = din("c_iota", [128, 128])
        c_jcol = din("c_jcol", [128, 1])

        yT = nc.dram_tensor("yT", [D, T], F32, kind="ExternalOutput").ap()
        x1T = dscr("x1T", [D, T], F32)
        h2T = dscr("h2T", [D, T], BF16)
        qTd = dscr("qTd", [64, 32 * 8 * 128], BF16)
        uTd = dscr("uTd", [128, 4 * T], BF16)
        mixT = dscr("mixT", [D, T], BF16)

        GC = sb("GC", [128, 36], F32)
        ONES = sb("ONES", [128, 128], BF16)
        IDN = sb("IDN", [128, 128], BF16)
        EPSC = sb("EPSC", [128, 1], F32)
        tr.dma('sp', 'const', GC[:], gcols[:, :], w=['c'])
        tr.dma('pool', 'const', IDN[:], c_ident[:, :], w=['c'])
        tr.op('dve', lambda e: e.memset(ONES[:], 1.0), w=['c'])
        tr.op('dve', lambda e: e.memset(EPSC[:], 0.0), w=['c'])
        ONEC = sb("ONEC", [128, 1], F32)
        tr.op('dve', lambda e: e.memset(ONEC[:], 1.0), w=['c'])

        pb = [es.enter_context(nc.psum_tensor("pb%d" % i, [128, 512], F32)) for i in range(8)]

        def xview(dram, t0, n):
            return dram.rearrange("(k p) t -> p k t", p=128)[:, :, t0:t0 + n]

        def ffn_weights(ph, stk, load=True):
            WG = stk.enter_context(nc.sbuf_tensor("WG_%d" % ph, [128, 8, FF], BF16))
            WU = stk.enter_context(nc.sbuf_tensor("WU_%d" % ph, [128, 8, FF], BF16))
            WD = stk.enter_context(nc.sbuf_tensor("WD_%d" % ph, [128, NF, D], BF16))
            if load:
                ffn_wload(ph, (WG, WU, WD))
            return WG, WU, WD

        def ffn_wload(ph, wts, gu=True, wd=True):
            WG, WU, WD = wts
            wgd, wud, wdd = wgs[ph - 1], wus[ph - 1], wds[ph - 1]
            HF = FF // 2
            for hf, sfx in ((0, 'a'), (1, 'b')) if gu else ():
                for k in range(8):
                    tr.dma('pool', 'wg' + sfx, WG[:, k, hf * HF:(hf + 1) * HF],
                           wgd[k * 128:(k + 1) * 128, hf * HF:(hf + 1) * HF], w=['WG' + sfx])
                    tr.dma('pool', 'wu' + sfx, WU[:, k, hf * HF:(hf + 1) * HF],
                           wud[k * 128:(k + 1) * 128, hf * HF:(hf + 1) * HF], w=['WU' + sfx])
            for f in range(NF) if wd else ():
                tr.dma('pool', 'wd', WD[:, f, :], wdd[f * 128:(f + 1) * 128, :], w=['WD'])

        def ffn_phase(ph, wts=None):
            with contextlib.ExitStack() as fs:
                def fsb(name, shape, dt):
                    return fs.enter_context(nc.sbuf_tensor("%s_%d" % (name, ph), list(shape), dt))
                WG, WU, WD = wts if wts is not None else ffn_weights(ph, fs)
                if wts is not None:
                    ffn_wload(ph, wts, gu=False, wd=True)
                XT = [fsb("XT%d" % i, [128, 8, 512], F32) for i in range(2)]
                H = fsb("H", [128, 8, 512], BF16)
                AT = fsb("AT", [128, NF, 512], BF16)
                SG = [fsb("SG%d" % i, [128, 512], BF16) for i in range(2)]
                RS = [fsb("RS%d" % i, [128, 512], F32) for i in range(2)]
                src = xT if ph == 1 else x1T
                g0 = 0 if ph == 1 else 16
                hkeys = ['h%d' % k for k in range(8)]
                atk2 = ['at%d' % f for f in range(14, 22)]

                def xs(tt):
                    return XT[tt % 2], 'xt%d' % (tt % 2)

                def load(tt):
                    X, xk = xs(tt)
                    tr.dma('sp', 'xin%d' % (tt % 2), X[:], xview(src, tt * 512, 512), w=[xk])

                def norm_a(X, xk, HB, hk):
                    tr.op('act', lambda e: e.activation(out=HB, in_=X[:], func=AF.Square), r=[xk], w=hk)

                def norm_b(HBk, hk, PN, pnk):
                    for k in range(8):
                        tr.op('pe', lambda e, k=k: e.matmul(out=PN[:], lhsT=ONES[:], rhs=HBk(k),
                                                           start=(k == 0), stop=(k == 7)), r=[hk[k], 'c'], w=[pnk])

                def norm_c(X, xk, HBk, hk, PN, pnk, R, rk, goff, inplace=False):
                    tr.op('dve', lambda e: e.tensor_scalar(out=R[:], in0=PN[:], scalar1=1.0 / D, scalar2=EPS,
                                                          op0=ALU.mult, op1=ALU.add), r=[pnk], w=[rk])
                    tr.op('act', lambda e: e.activation(out=R[:], in_=R[:], func=AF.Sqrt), r=[rk], w=[rk])
                    tr.op('dve', lambda e: e.reciprocal(out=R[:], in_=R[:]), r=[rk], w=[rk])
                    for k in range(8):
                        dst = X[:, k, :] if inplace else HBk(k)
                        tr.op('dve', lambda e, k=k, dst=dst: e.scalar_tensor_tensor(
                            out=dst, in0=X[:, k, :], scalar=GC[:, goff + k:goff + k + 1], in1=R[:],
                            op0=ALU.mult, op1=ALU.mult), r=[xk, rk, 'c'], w=[xk if inplace else hk[k]])

                Hk = lambda k: H[:, k, :]
                A2k = lambda k: AT[:, 14 + k, :]

                def post_a(tt):
                    X, xk = xs(tt)
                    if ph == 1:
                        tr.dma('sp', 'xout%d' % (tt % 2), xview(x1T, tt * 512, 512), X[:], r=[xk], w=['x1T'])
                    norm_a(X, xk, AT[:, 14:22, :], atk2)

                def post_bc(tt):
                    X, xk = xs(tt)
                    norm_b(A2k, atk2, pb[7], 'pn2')
                    if ph == 1:
                        norm_c(X, xk, A2k, atk2, pb[7], 'pn2', RS[1], 'rs1', 8)
                        tr.dma('sp', 'hout', xview(h2T, tt * 512, 512), AT[:, 14:22, :], r=atk2, w=['h2T'])
                    else:
                        norm_c(X, xk, A2k, atk2, pb[7], 'pn2', RS[1], 'rs1', 24, inplace=True)
                        tr.dma('sp', 'yout%d' % (tt % 2), xview(yT, tt * 512, 512), X[:], r=[xk], w=['yT'])

                load(0)
                X0, xk0 = xs(0)
                norm_a(X0, xk0, H[:], hkeys)
                norm_b(Hk, hkeys, pb[6], 'pn')
                norm_c(X0, xk0, Hk, hkeys, pb[6], 'pn', RS[0], 'rs0', g0)
                for tt in range(8):
                    X, xk = xs(tt)
                    for f in range(NF):
                        b = f % 2
                        for k in range(8):
                            tr.op('pe', lambda e, k=k, f=f, b=b: e.matmul(
                                out=pb[b][:], lhsT=WG[:, k, f * 128:(f + 1) * 128], rhs=H[:, k, :],
                                start=(k == 0), stop=(k == 7)), r=['WG' + ('a' if f < NF // 2 else 'b'), hkeys[k]],
                                w=['pg%d' % b])
                        for k in range(8):
                            tr.op('pe', lambda e, k=k, f=f, b=b: e.matmul(
                                out=pb[2 + b][:], lhsT=WU[:, k, f * 128:(f + 1) * 128], rhs=H[:, k, :],
                                start=(k == 0), stop=(k == 7)), r=['WU' + ('a' if f < NF // 2 else 'b'), hkeys[k]],
                                w=['pu%d' % b])
                        tr.op('act', lambda e, b=b: e.activation(out=SG[b][:], in_=pb[b][:], func=AF.Silu),
                              r=['pg%d' % b], w=['sg%d' % b])
                        tr.op('dve', lambda e, b=b, f=f: e.tensor_tensor(out=AT[:, f, :], in0=SG[b][:],
                                                                         in1=pb[2 + b][:], op=ALU.mult),
                              r=['sg%d' % b, 'pu%d' % b], w=['at%d' % f])
                        if f == 2 and tt > 0:
                            post_bc(tt - 1)
                    if tt + 1 < 8:
                        load(tt + 1)
                        Xn, xkn = xs(tt + 1)
                        norm_a(Xn, xkn, H[:], hkeys)
                    for dk in range(8):
                        b = dk % 2
                        for f in range(NF):
                            tr.op('pe', lambda e, f=f, dk=dk, b=b: e.matmul(
                                out=pb[4 + b][:], lhsT=WD[:, f, dk * 128:(dk + 1) * 128], rhs=AT[:, f, :],
                                start=(f == 0), stop=(f == NF - 1)), r=['WD', 'at%d' % f], w=['pd%d' % b])
                        tr.op('dve', lambda e, dk=dk, b=b, X=X: e.scalar_tensor_tensor(
                            out=X[:, dk, :], in0=pb[4 + b][:], scalar=0.5, in1=X[:, dk, :],
                            op0=ALU.mult, op1=ALU.add), r=['pd%d' % b, xk], w=[xk])
                        if dk == 2 and tt + 1 < 8:
                            norm_b(Hk, hkeys, pb[6], 'pn')
                            norm_c(Xn, xkn, Hk, hkeys, pb[6], 'pn', RS[0], 'rs0', g0)
                    post_a(tt)
                post_bc(7)
            tr.barrier()

        def attention_phase():
            with contextlib.ExitStack() as a_:
                def asb(name, shape, dt):
                    return a_.enter_context(nc.sbuf_tensor(name, list(shape), dt))
                CBt = asb("CBt", [128, 512], BF16)
                WBt = asb("WBt", [128, 512], BF16)
                CMt = asb("CMt", [128, 512], BF16)
                BDt = asb("BDt", [128, 384], BF16)
                U0t = asb("U0t", [128, 128], F32)
                L0t = asb("L0t", [128, 128], F32)
                GAT = asb("GAT", [128, 512], F32)
                QN = [asb("QN%d" % i, [128, 512], BF16) for i in range(2)]
                ET = [asb("ET%d" % i, [128, 512], BF16) for i in range(3)]
                SCO = asb("SCO", [128, 64], F32)
                M8 = asb("M8", [128, 16], F32)
                WK = asb("WK", [128, 64], F32)
                SELW = asb("SELW", [128, 128], BF16)
                RZ = asb("RZ", [128, 4], F32)
                DEN = asb("DEN", [128, 12], F32)
                ATT = asb("ATT", [128, 512], F32)
                ATN = asb("ATN", [128, 512], BF16)
                JUNK = asb("JUNK", [128, 512], F32)
                SSQ = asb("SSQ", [128, 1], F32)
                MST = asb("MST", [128, 4, 128], BF16)
                tr.dma('pool', 'aconst', CBt[:], c_CB[:, :], w=['ac'])
                tr.dma('pool', 'aconst', WBt[:], c_WB[:, :], w=['ac'])
                tr.dma('pool', 'aconst', CMt[:], c_CM[:, :], w=['ac'])
                tr.dma('pool', 'aconst', BDt[:], c_BD[:, :], w=['ac'])
                tr.dma('sp', 'aconst', U0t[:], c_U0[:, :], w=['ac'])
                tr.dma('sp', 'aconst', L0t[:], c_L0[:, :], w=['ac'])
                tr.dma('sp', 'aconst', GAT[:], grow_attn.partition_broadcast(128), w=['ac'])
                tr.op('dve', lambda e: e.memset(SELW[:], 0.0), w=['selw'])
                OS = pb[3][:, 0:260].rearrange("p (r x) -> p r x", x=65)
                OW = pb[4][:, 0:260].rearrange("p (r x) -> p r x", x=65)
                OC = pb[5][:, 0:260].rearrange("p (r x) -> p r x", x=65)
                IM = pb[6][:, 0:256].rearrange("p (r x) -> p r x", x=64)
                PT = pb[5][:, 320:384].bitcast(BF16)
                PT4 = pb[6][:, 256:512].bitcast(BF16)
                PA = pb[7]
                ETA = [asb("ETA%d" % i, [128, 512], BF16) for i in range(2)]
                dq = []

                def pump(k):
                    for _ in range(min(k, len(dq))):
                        dq.pop(0)()

                def deferred(f, *a):
                    tr.defer = dq
                    f(*a)
                    tr.defer = None
                sring = [0]
                ering = [0]

                def nexts():
                    b = sring[0] % 3
                    sring[0] += 1
                    return pb[b], 'ps%d' % b

                def nexte():
                    b = ering[0] % 3
                    ering[0] += 1
                    return ET[b], 'et%d' % b

                qview = qTd.rearrange("p (c x) -> p c x", c=32)
                QN3 = [QN[0], QN[1], asb("QN2", [128, 512], BF16)]
                OCS = [asb("OCS%d" % i, [128, 4, 65], F32) for i in range(4)]
                TMPA = asb("TMPA", [128, 4, 64], F32)
                TMPB = asb("TMPB", [128, 4, 64], F32)
                OSS = [asb("OSS%d" % i, [128, 4, 65], F32) for i in range(2)]
                OWS = [asb("OWS%d" % i, [128, 4, 65], F32) for i in range(2)]
                ATN2 = [ATN, asb("ATN1", [128, 512], BF16)]
                NIT = 64
                mixv = mixT.rearrange("(k p) t -> p k t", p=128)
                pending = []

                def qload(it):
                    c, g = divmod(it, 2)
                    sl = it % 3
                    tr.dma('sp', 'qin%d' % sl, QN3[sl][0:64, :], qview[:, c, g * 512:(g + 1) * 512], r=['qTd'],
                           w=['qnq%d' % sl])

                def stageA(it):
                    c, g = divmod(it, 2)
                    sl = it % 3
                    Q = QN3[sl]
                    qq, qs = 'qnq%d' % sl, 'qns%d' % sl
                    nts = 1 if c < 16 else 2
                    tl = []
                    for nt in range(nts):
                        Mn = min(128, 8 * c + 7 - 128 * nt)
                        m = 8 * c - 128 * nt
                        need = m <= 192
                        PS, psk = PA, 'pa'
                        tr.op('pe', lambda e, PS=PS, Mn=Mn, nt=nt, need=need: e.matmul(
                            out=PS[0:Mn, :], lhsT=KC[g][:, 128 * nt:128 * nt + Mn], rhs=Q[0:64, :],
                            start=True, stop=not need), r=['KC%d' % g, qq], w=[psk])
                        if need:
                            tr.op('pe', lambda e, PS=PS, Mn=Mn, m=m: e.matmul(
                                out=PS[0:Mn, :], lhsT=BDt[:, 192 - m:192 - m + Mn], rhs=CMt[:],
                                start=False, stop=True), r=['ac'], w=[psk])
                        Et, ek = ETA[nt], 'eta%d' % nt
                        tr.op('act', lambda e, PS=PS, Et=Et, Mn=Mn: e.activation(
                            out=Et[0:Mn, :], in_=PS[0:Mn, :], func=AF.Exp, scale=0.125), r=[psk], w=[ek])
                        tl.append((nt, Mn, Et, ek))
                    first = True
                    for (nt, Mn, Et, ek) in tl:
                        for r in range(4):
                            tr.op('pe', lambda e, Et=Et, Mn=Mn, nt=nt, r=r, first=first: e.matmul(
                                out=OC[:, r, :], lhsT=Et[0:Mn, r * 128:(r + 1) * 128], rhs=VCX[g][0:Mn, nt, 0:65],
                                start=first, stop=(nt == nts - 1), skip_group_check=True),
                                r=[ek, 'VCX%d' % g], w=['oc'])
                            tr.op('pe', lambda e, Et=Et, Mn=Mn, nt=nt, r=r, first=first: e.matmul(
                                out=IM[:, r, :], lhsT=Et[0:Mn, r * 128:(r + 1) * 128], rhs=VCX[g][0:Mn, nt, 65:129],
                                start=first, stop=(nt == nts - 1), skip_group_check=True),
                                r=[ek, 'VCX%d' % g], w=['im'])
                            first = False
                    OCb, ock = OCS[it % 4], 'ocs%d' % (it % 4)
                    tr.op('dve', lambda e, OCb=OCb: e.tensor_copy(out=OCb[:], in_=OC), r=['oc'], w=[ock])
                    tr.op('dve', lambda e, OCb=OCb: e.tensor_scalar(out=RZ[:], in0=OCb[:, :, 64], scalar1=1e-30,
                                                                   scalar2=None, op0=ALU.max), r=[ock], w=['rz'])
                    tr.op('dve', lambda e: e.reciprocal(out=RZ[:], in_=RZ[:]), r=['rz'], w=['rz'])
                    tr.op('dve', lambda e: e.tensor_scalar(out=SCO[:], in0=IM[:, 0, :], scalar1=RZ[:, 0:1],
                                                          scalar2=None, op0=ALU.mult), r=['im', 'rz'], w=['sco'])
                    for r in range(1, 4):
                        tr.op('dve', lambda e, r=r: e.scalar_tensor_tensor(
                            out=SCO[:], in0=IM[:, r, :], scalar=RZ[:, r:r + 1], in1=SCO[:],
                            op0=ALU.mult, op1=ALU.add), r=['im', 'rz', 'sco'], w=['sco'])
                    lo = 62 - 2 * c
                    tr.op('dve', lambda e, lo=lo: e.tensor_tensor(out=SCO[:], in0=SCO[:], in1=L0t[:, lo:lo + 64],
                                                                 op=ALU.max), r=['sco', 'ac'], w=['sco'])
                    tr.op('dve', lambda e, lo=lo: e.tensor_tensor(out=SCO[:], in0=SCO[:], in1=U0t[:, lo:lo + 64],
                                                                 op=ALU.min), r=['sco', 'ac'], w=['sco'])
                    tr.op('dve', lambda e: e.memset(SCO[:, 0:1], 3e30), r=['sco'], w=['sco'])
                    tr.op('dve', lambda e: e.max(out=M8[:, 0:8], in_=SCO[:]), r=['sco'], w=['m8'])
                    tr.op('dve', lambda e: e.match_replace(out=WK[:], in_to_replace=M8[:, 0:8], in_values=SCO[:],
                                                          imm_value=-3e38), r=['sco', 'm8'], w=['wk'])
                    tr.op('dve', lambda e: e.max(out=M8[:, 8:16], in_=WK[:]), r=['wk'], w=['m8'])
                    tr.op('dve', lambda e: e.tensor_scalar(out=SELW[:, 64:128], in0=SCO[:], scalar1=M8[:, 15:16],
                                                          scalar2=None, op0=ALU.is_ge), r=['sco', 'm8'], w=['selw'])
                    tr.op('pe', lambda e: e.transpose(out=PT, in_=SELW[:], identity=IDN[:]), r=['selw', 'c'], w=['pt'])
                    tr.op('dve', lambda e, Q=Q: e.tensor_scalar(
                        out=Q[64:128, :].rearrange("p (r q) -> p r q", r=4),
                        in0=PT[64:128, :].unsqueeze(1).to_broadcast([64, 4, 128]),
                        scalar1=-1.0, scalar2=BIGM, op0=ALU.add, op1=ALU.mult), r=['pt'], w=[qs])

                def finish_pe(c):
                    AN = ATN2[c % 2]
                    for j in range(4):
                        tr.op('pe', lambda e, j=j, AN=AN: e.transpose(out=PT4[:, j * 128:(j + 1) * 128],
                                                                     in_=AN[:, j * 128:(j + 1) * 128], identity=IDN[:]),
                              r=['atn%d' % (c % 2), 'c'], w=['pt4'])
                    tr.op('dve', lambda e: e.tensor_copy(out=MST[:].rearrange("p a b -> p (a b)"), in_=PT4),
                          r=['pt4'], w=['mst'])
                    tr.dma('sp', 'mout', mixv[:, 0:4, c * 128:(c + 1) * 128], MST[:], r=['mst'], w=['mixT'])

                def stageB(it):
                    c, g = divmod(it, 2)
                    sl = it % 3
                    Q = QN3[sl]
                    qq, qs = 'qnq%d' % sl, 'qns%d' % sl
                    k0 = max(0, c - 4)
                    tiles = [('w', kt) for kt in range(k0, c + 1)] + [('s', kt) for kt in range(c + 1)]
                    n = len(tiles)
                    info = {}

                    def qk(i):
                        br, kt = tiles[i]
                        PS, psk = nexts()
                        extra = []
                        if kt == c:
                            extra.append(CBt)
                        if br == 'w' and kt == c - 4:
                            extra.append(WBt)
                        if br == 'w':
                            tr.op('pe', lambda e, PS=PS, kt=kt, ne=len(extra): e.matmul(
                                out=PS[:], lhsT=KW[g][:, kt * 128:(kt + 1) * 128], rhs=Q[0:64, :],
                                start=True, stop=(ne == 0)), r=['KW%d' % g, qq], w=[psk])
                        else:
                            tr.op('pe', lambda e, PS=PS, kt=kt, ne=len(extra): e.matmul(
                                out=PS[:], lhsT=KE[g][:, kt * 128:(kt + 1) * 128], rhs=Q[:, :],
                                start=True, stop=(ne == 0)), r=['KE%d' % g, qq, qs], w=[psk])
                        for j, bt in enumerate(extra):
                            tr.op('pe', lambda e, PS=PS, bt=bt, last=(j == len(extra) - 1): e.matmul(
                                out=PS[:], lhsT=IDN[:], rhs=bt[:], start=False, stop=last), r=['ac', 'c'], w=[psk])
                        Et, ek = nexte()
                        tr.op('act', lambda e, PS=PS, Et=Et: e.activation(
                            out=Et[:], in_=PS[:], func=AF.Exp, scale=0.125), r=[psk], w=[ek])
                        info[i] = (Et, ek)

                    started = {'w': False, 's': False}
                    lastw = max(i for i in range(n) if tiles[i][0] == 'w')

                    def pv(i):
                        br, kt = tiles[i]
                        Et, ek = info[i]
                        O, ok, V, vk = (OW, 'ow', VW1[g], 'VW1%d' % g) if br == 'w' else (OS, 'os', VS1[g], 'VS1%d' % g)
                        last = (i == lastw) if br == 'w' else (i == n - 1)
                        for r in range(4):
                            st = not started[br]
                            started[br] = True
                            tr.op('pe', lambda e, Et=Et, kt=kt, r=r, O=O, V=V, st=st, last=last: e.matmul(
                                out=O[:, r, :], lhsT=Et[:, r * 128:(r + 1) * 128], rhs=V[:, kt, :],
                                start=st, stop=last, skip_group_check=True), r=[ek, vk], w=[ok])

                    for i in range(min(2, n)):
                        qk(i)
                    for i in range(n):
                        if i + 2 < n:
                            qk(i + 2)
                        pv(i)
                        left = max(1, n - 1 - i)
                        pump((len(dq) + left - 1) // left)
                    pump(len(dq))
                    b2 = it % 2
                    tr.op('dve', lambda e: e.tensor_copy(out=OWS[b2][:], in_=OW), r=['ow'], w=['ows%d' % b2])
                    tr.op('dve', lambda e: e.tensor_copy(out=OSS[b2][:], in_=OS), r=['os'], w=['oss%d' % b2])

                def tailB(it):
                    c, g = divmod(it, 2)
                    b2 = it % 2
                    b4 = it % 4
                    srcs = ((OCS[b4], 'ocs%d' % b4), (OSS[b2], 'oss%d' % b2), (OWS[b2], 'ows%d' % b2))
                    for bi, (O, ok) in enumerate(srcs):
                        tr.op('dve', lambda e, O=O, bi=bi: e.tensor_scalar(
                            out=DEN[:, bi * 4:bi * 4 + 4], in0=O[:, :, 64], scalar1=1e-30, scalar2=None,
                            op0=ALU.max), r=[ok], w=['den'])
                    tr.op('dve', lambda e: e.reciprocal(out=DEN[:], in_=DEN[:]), r=['den'], w=['den'])
                    gv = GT[:, c, 12 * g:12 * g + 12].rearrange("p (r b) -> p b r", b=3)
                    tr.op('dve', lambda e, gv=gv: e.tensor_tensor(
                        out=DEN[:].rearrange("p (b r) -> p b r", b=3), in0=DEN[:].rearrange("p (b r) -> p b r", b=3),
                        in1=gv, op=ALU.mult), r=['den', 'GT'], w=['den'])
                    ATTg = ATT[:, g * 256:(g + 1) * 256].rearrange("p (r x) -> p r x", r=4)
                    denb = lambda bi: DEN[:, bi * 4:bi * 4 + 4].unsqueeze(2).to_broadcast([128, 4, 64])
                    tr.op('dve', lambda e: e.tensor_tensor(out=ATTg, in0=OCS[b4][:, :, 0:64], in1=denb(0), op=ALU.mult),
                          r=['ocs%d' % b4, 'den'], w=['att'])
                    tr.op('dve', lambda e: e.tensor_tensor(out=TMPA[:], in0=OSS[b2][:, :, 0:64], in1=denb(1), op=ALU.mult),
                          r=['oss%d' % b2, 'den'], w=['tmpa'])
                    tr.op('dve', lambda e: e.tensor_tensor(out=TMPB[:], in0=OWS[b2][:, :, 0:64], in1=denb(2), op=ALU.mult),
                          r=['ows%d' % b2, 'den'], w=['tmpb'])
                    tr.op('dve', lambda e: e.tensor_tensor(out=ATTg, in0=ATTg, in1=TMPA[:], op=ALU.add),
                          r=['att', 'tmpa'], w=['att'])
                    tr.op('dve', lambda e: e.tensor_tensor(out=ATTg, in0=ATTg, in1=TMPB[:], op=ALU.add),
                          r=['att', 'tmpb'], w=['att'])
                    if g == 1:
                        AN, ank = ATN2[c % 2], 'atn%d' % (c % 2)
                        tr.op('act', lambda e: e.activation(out=JUNK[:], in_=ATT[:], func=AF.Square, accum_out=SSQ[:]),
                              r=['att'], w=['junk', 'ssq'])
                        tr.op('dve', lambda e: e.tensor_scalar(out=SSQ[:], in0=SSQ[:], scalar1=1.0 / 512, scalar2=EPS,
                                                              op0=ALU.mult, op1=ALU.add), r=['ssq'], w=['ssq'])
                        tr.op('act', lambda e: e.activation(out=SSQ[:], in_=SSQ[:], func=AF.Ln), r=['ssq'], w=['ssq'])
                        tr.op('act', lambda e: e.activation(out=SSQ[:], in_=SSQ[:], func=AF.Exp, scale=-0.5),
                              r=['ssq'], w=['ssq'])
                        tr.op('dve', lambda e, AN=AN: e.scalar_tensor_tensor(out=AN[:], in0=ATT[:], scalar=SSQ[:, 0:1],
                                                                            in1=GAT[:], op0=ALU.mult, op1=ALU.mult),
                              r=['att', 'ssq', 'ac'], w=[ank])

                qload(0)
                qload(1)
                stageA(0)
                for it in range(NIT):
                    qa, qb = [], []
                    if it >= 1:
                        tr.defer = qa
                        tailB(it - 1)
                        tr.defer = None
                    if it + 1 < NIT:
                        tr.defer = qb
                        stageA(it + 1)
                        tr.defer = None
                    while qa or qb:
                        if qb:
                            dq.append(qb.pop(0))
                        if qa:
                            dq.append(qa.pop(0))
                    if it >= 1 and (it - 1) % 2 == 1:
                        deferred(finish_pe, (it - 1) // 2)
                    if it + 2 < NIT:
                        qload(it + 2)
                    stageB(it)
                tailB(NIT - 1)
                finish_pe((NIT - 1) // 2)
            tr.barrier()

        def s5_phase():
            PI = float(np.pi)
            with contextlib.ExitStack() as s_:
                def ssb(name, shape, dt):
                    return s_.enter_context(nc.sbuf_tensor(name, list(shape), dt))
                ROW = [ssb("ROW%d" % i, [128, 2048], F32) for i in range(3)]
                PRE_RE = ssb("PRE_RE", [128, 2048], F32)
                PRE_IM = ssb("PRE_IM", [128, 2048], F32)
                TH = ssb("TH", [128, 2048], F32)
                TMPI = ssb("TMPI", [128, 2048], mybir.dt.int32)
                JC = ssb("JC", [128, 1], F32)
                NJC = ssb("NJC", [128, 1], F32)
                NPI = ssb("NPI", [128, 1], F32)
                COL = ssb("COL", [128, 48], F32)
                IOT = ssb("IOT", [128, 128], F32)
                POST_RE = ssb("POST_RE", [128, 16, 128], F32)
                POST_IM = ssb("POST_IM", [128, 16, 128], F32)
                A128 = ssb("A128", [128, 2, 16], F32)
                SM = [ssb("SM%d" % i, [128, 16], F32) for i in range(3)]
                REP = ssb("REP", [128, 4, 3, 64], F32)
                BT = ssb("BT", [128, 4, 2, 64], F32)
                BB = ssb("BB", [128, 4, 2, 64], F32)
                RT = [ssb("RT%d" % i, [128, 4, 64], F32) for i in range(6)]
                BBD = ssb("BBD", [128, 4, 1024], BF16)
                MASKB = ssb("MASKB", [128, 8], F32)
                CC = ssb("CC", [128, 16, 2, 16], F32)
                MASKC = ssb("MASKC", [128, 4, 8], F32)
                CMAT = ssb("CMAT", [128, 16, 2, 128], BF16)
                DCOL = ssb("DCOL", [128, 4], F32)
                BGL = ssb("BGL", [128, 4], F32)
                WGL = ssb("WGL", [128, 4, 512], BF16)
                TRI = ssb("TRI", [128, 128], BF16)
                UT = [ssb("UT%d" % i, [128, 4, 512], BF16) for i in range(2)]
                T1B = ssb("T1B", [128, 8, 512], BF16)
                T2B = ssb("T2B", [128, 8, 512], BF16)
                NTRI = ssb("NTRI", [128, 128], BF16)
                NCM = ssb("NCM", [128, 16, 128], BF16)
                ZCF = ssb("ZCF", [128, 16, 2, 128], F32)
                CRE = ssb("CRE", [128, 16], F32)
                CIM = ssb("CIM", [128, 16], F32)
                TQ = [ssb("TQ%d" % i, [128, 16], F32) for i in range(4)]
                XU1 = ssb("XU1", [128, 16, 2, 128], BF16)
                XU2 = ssb("XU2", [128, 16, 2, 128], BF16)
                CARRY = ssb("CARRY", [128, 16, 2], F32)
                CT = [ssb("CT%d" % i, [128, 2, 2], F32) for i in range(2)]
                Y = ROW[0][:].rearrange("p (a b) -> p a b", a=4)
                YB = ROW[1][:].rearrange("p (a b) -> p a b", a=4)
                YG = ROW[2][:].rearrange("p (a b) -> p a b", a=4)
                YGB = ssb("YGB", [128, 4, 512], BF16)
                SGL = [ssb("SGL%d" % i, [128, 512], F32) for i in range(2)]
                S = TH[:].rearrange("p (a b) -> p a b", a=4)
                SQ = ssb("SQ", [128, 4, 512], BF16)
                RS5 = ssb("RS5", [128, 512], F32)
                OUT = ssb("OUT", [128, 4, 512], BF16)
                ck = ['s5c']
                for i in range(3):
                    tr.dma('sp', 's5const', ROW[i][:], s5_rows[i:i + 1, :].partition_broadcast(128), w=ck)
                tr.dma('sp', 's5const', JC[:], c_jcol[:, :], w=ck)
                tr.dma('sp', 's5const', COL[:], s5_cols[:, :], w=ck)
                tr.dma('sp', 's5const', IOT[:], c_iota[:, :], w=ck)
                tr.dma('sp', 's5const', REP[:].rearrange("p a b c -> p (a b c)"), s5_rep[:, :], w=ck)
                tr.dma('sp', 's5const', BT[:].rearrange("p a b c -> p (a b c)"), s5_bT[:, :], w=ck)
                tr.dma('sp', 's5const', MASKB[:], c_maskB[:, :], w=ck)
                tr.dma('sp', 's5const', CC[:].rearrange("p a b c -> p (a b c)"), s5_c[:, :], w=ck)
                tr.dma('sp', 's5const', MASKC[:].rearrange("p a b -> p (a b)"), c_maskC[:, :], w=ck)
                tr.dma('sp', 's5const', DCOL[:], s5_dcol[:, :], w=ck)
                tr.dma('sp', 's5const', BGL[:], bglu_col[:, :], w=ck)
                tr.dma('pool', 's5const', TRI[:], c_tri[:, :], w=ck)
                for k in range(4):
                    tr.dma('pool', 's5const', WGL[:, k, :], w_glu[k * 128:(k + 1) * 128, :], w=ck)
                V = lambda fn, r, w: tr.op('dve', fn, r=r, w=w)
                A = lambda fn, r, w: tr.op('act', fn, r=r, w=w)
                V(lambda e: e.memset(NPI[:], -PI), [], ck)
                V(lambda e: e.tensor_scalar(out=NJC[:], in0=JC[:], scalar1=-1.0, scalar2=None, op0=ALU.mult), ck, ck)
                V(lambda e: e.memset(CRE[:], 0.0), [], ['carry'])
                V(lambda e: e.memset(CIM[:], 0.0), [], ['carry'])

                def sincos(out_sin, out_cos, theta, tmpf, tmpi):
                    for out, shift in ((out_sin, 0.0), (out_cos, 0.5 * PI)):
                        V(lambda e, shift=shift: e.tensor_scalar(out=tmpf, in0=theta, scalar1=shift, scalar2=1.0 / (2 * PI),
                                                                 op0=ALU.add, op1=ALU.mult), ck, ck)
                        V(lambda e: e.tensor_copy(out=tmpi, in_=tmpf), ck, ck)
                        V(lambda e: e.tensor_copy(out=tmpf, in_=tmpi), ck, ck)
                        V(lambda e, out=out: e.scalar_tensor_tensor(out=out, in0=tmpf, scalar=-2 * PI, in1=theta,
                                                                    op0=ALU.mult, op1=ALU.add), ck, ck)
                        if shift:
                            V(lambda e, out=out, shift=shift: e.tensor_scalar(out=out, in0=out, scalar1=shift, scalar2=None,
                                                                              op0=ALU.add), ck, ck)
                        V(lambda e, out=out: e.tensor_scalar(out=tmpf, in0=out, scalar1=PI, scalar2=-2 * PI,
                                                             op0=ALU.is_gt, op1=ALU.mult), ck, ck)
                        V(lambda e, out=out: e.tensor_tensor(out=out, in0=out, in1=tmpf, op=ALU.add), ck, ck)
                        A(lambda e, out=out: e.activation(out=out, in_=out, func=AF.Sin), ck, ck)

                A(lambda e: e.activation(out=ROW[2][:], in_=ROW[2][:], func=AF.Exp), ck, ck)
                V(lambda e: e.tensor_tensor(out=ROW[0][:], in0=ROW[0][:], in1=ROW[2][:], op=ALU.mult), ck, ck)
                V(lambda e: e.tensor_tensor(out=ROW[1][:], in0=ROW[1][:], in1=ROW[2][:], op=ALU.mult), ck, ck)
                A(lambda e: e.activation(out=ROW[2][:], in_=ROW[0][:], func=AF.Exp, scale=NJC[:, 0:1]), ck, ck)
                V(lambda e: e.tensor_scalar(out=TH[:], in0=ROW[1][:], scalar1=JC[:, 0:1], scalar2=None, op0=ALU.mult),
                  ck, ck)
                sincos(PRE_IM[:], PRE_RE[:], TH[:], ROW[0][:], TMPI[:])
                V(lambda e: e.tensor_tensor(out=PRE_RE[:], in0=PRE_RE[:], in1=ROW[2][:], op=ALU.mult), ck, ck)
                V(lambda e: e.scalar_tensor_tensor(out=PRE_IM[:], in0=PRE_IM[:], scalar=-1.0, in1=ROW[2][:],
                                                   op0=ALU.mult, op1=ALU.mult), ck, ck)
                LRc, LIc, DTc = COL[:, 0:16], COL[:, 16:32], COL[:, 32:48]
                A(lambda e: e.activation(out=DTc, in_=DTc, func=AF.Exp), ck, ck)
                V(lambda e: e.tensor_tensor(out=LRc, in0=LRc, in1=DTc, op=ALU.mult), ck, ck)
                V(lambda e: e.tensor_tensor(out=LIc, in0=LIc, in1=DTc, op=ALU.mult), ck, ck)
                iob = IOT[:].unsqueeze(1).to_broadcast([128, 16, 128])
                THv = TH[:].rearrange("p (a b) -> p a b", a=16)
                R0v = ROW[0][:].rearrange("p (a b) -> p a b", a=16)
                R2v = ROW[2][:].rearrange("p (a b) -> p a b", a=16)
                V(lambda e: e.tensor_tensor(out=R2v, in0=iob, in1=LRc.unsqueeze(2).to_broadcast([128, 16, 128]),
                                            op=ALU.mult), ck, ck)
                A(lambda e: e.activation(out=ROW[2][:], in_=ROW[2][:], func=AF.Exp), ck, ck)
                V(lambda e: e.tensor_tensor(out=THv, in0=iob, in1=LIc.unsqueeze(2).to_broadcast([128, 16, 128]),
                                            op=ALU.mult), ck, ck)
                sincos(POST_IM[:].rearrange("p a b -> p (a b)"), POST_RE[:].rearrange("p a b -> p (a b)"), TH[:],
                       ROW[0][:], TMPI[:])
                V(lambda e: e.tensor_tensor(out=POST_RE[:], in0=POST_RE[:], in1=R2v, op=ALU.mult), ck, ck)
                V(lambda e: e.tensor_tensor(out=POST_IM[:], in0=POST_IM[:], in1=R2v, op=ALU.mult), ck, ck)
                V(lambda e: e.tensor_scalar(out=SM[0][:], in0=LRc, scalar1=128.0, scalar2=None, op0=ALU.mult), ck, ck)
                A(lambda e: e.activation(out=SM[0][:], in_=SM[0][:], func=AF.Exp), ck, ck)
                V(lambda e: e.tensor_scalar(out=SM[1][:], in0=LIc, scalar1=128.0, scalar2=None, op0=ALU.mult), ck, ck)
                sincos(A128[:, 1, :], A128[:, 0, :], SM[1][:], SM[2][:], TMPI[:, 0:16])
                V(lambda e: e.tensor_tensor(out=A128[:, 0, :], in0=A128[:, 0, :], in1=SM[0][:], op=ALU.mult), ck, ck)
                V(lambda e: e.tensor_tensor(out=A128[:, 1, :], in0=A128[:, 1, :], in1=SM[0][:], op=ALU.mult), ck, ck)
                lr, li, ldt = REP[:, :, 0, :], REP[:, :, 1, :], REP[:, :, 2, :]
                dtv, ldr, ldi, mag, sn, cs = [t[:] for t in RT]
                A(lambda e: e.activation(out=dtv, in_=ldt, func=AF.Exp), ck, ck)
                V(lambda e: e.tensor_tensor(out=ldr, in0=lr, in1=dtv, op=ALU.mult), ck, ck)
                V(lambda e: e.tensor_tensor(out=ldi, in0=li, in1=dtv, op=ALU.mult), ck, ck)
                A(lambda e: e.activation(out=mag, in_=ldr, func=AF.Exp), ck, ck)
                sincos(sn, cs, ldi, dtv, TMPI[:, 0:256].rearrange("p (a b) -> p a b", a=4))
                V(lambda e: e.tensor_tensor(out=cs, in0=cs, in1=mag, op=ALU.mult), ck, ck)
                V(lambda e: e.tensor_tensor(out=sn, in0=sn, in1=mag, op=ALU.mult), ck, ck)
                V(lambda e: e.tensor_scalar(out=cs, in0=cs, scalar1=-1.0, scalar2=None, op0=ALU.add), ck, ck)
                V(lambda e: e.tensor_tensor(out=dtv, in0=lr, in1=lr, op=ALU.mult), ck, ck)
                V(lambda e: e.tensor_tensor(out=mag, in0=li, in1=li, op=ALU.mult), ck, ck)
                V(lambda e: e.tensor_tensor(out=dtv, in0=dtv, in1=mag, op=ALU.add), ck, ck)
                V(lambda e: e.reciprocal(out=dtv, in_=dtv), ck, ck)
                V(lambda e: e.tensor_tensor(out=ldr, in0=cs, in1=lr, op=ALU.mult), ck, ck)
                V(lambda e: e.tensor_tensor(out=mag, in0=sn, in1=li, op=ALU.mult), ck, ck)
                V(lambda e: e.tensor_tensor(out=ldr, in0=ldr, in1=mag, op=ALU.add), ck, ck)
                V(lambda e: e.tensor_tensor(out=ldr, in0=ldr, in1=dtv, op=ALU.mult), ck, ck)
                V(lambda e: e.tensor_tensor(out=ldi, in0=sn, in1=lr, op=ALU.mult), ck, ck)
                V(lambda e: e.tensor_tensor(out=mag, in0=cs, in1=li, op=ALU.mult), ck, ck)
                V(lambda e: e.tensor_tensor(out=ldi, in0=ldi, in1=mag, op=ALU.subtract), ck, ck)
                V(lambda e: e.tensor_tensor(out=ldi, in0=ldi, in1=dtv, op=ALU.mult), ck, ck)
                br, bi_ = BT[:, :, 0, :], BT[:, :, 1, :]
                V(lambda e: e.tensor_tensor(out=BB[:, :, 0, :], in0=ldr, in1=br, op=ALU.mult), ck, ck)
                V(lambda e: e.tensor_tensor(out=mag, in0=ldi, in1=bi_, op=ALU.mult), ck, ck)
                V(lambda e: e.tensor_tensor(out=BB[:, :, 0, :], in0=BB[:, :, 0, :], in1=mag, op=ALU.subtract), ck, ck)
                V(lambda e: e.tensor_tensor(out=BB[:, :, 1, :], in0=ldr, in1=bi_, op=ALU.mult), ck, ck)
                V(lambda e: e.tensor_tensor(out=mag, in0=ldi, in1=br, op=ALU.mult), ck, ck)
                V(lambda e: e.tensor_tensor(out=BB[:, :, 1, :], in0=BB[:, :, 1, :], in1=mag, op=ALU.add), ck, ck)
                BBDv = BBD[:].rearrange("p k (a r g x) -> p k a r g x", a=4, r=2, g=2)
                for kt in range(4):
                    for a in range(4):
                        for ri in range(2):
                            V(lambda e, kt=kt, a=a, ri=ri: e.tensor_tensor(
                                out=BBDv[:, kt, a, ri, :, :],
                                in0=BB[:, kt, ri, :].unsqueeze(1).to_broadcast([128, 2, 64]),
                                in1=MASKB[:, 2 * a:2 * a + 2].unsqueeze(2).to_broadcast([128, 2, 64]),
                                op=ALU.mult), ck, ck)
                V(lambda e: e.tensor_scalar(out=NTRI[:], in0=TRI[:], scalar1=-1.0, scalar2=None, op0=ALU.mult), ck, ck)
                CMv = CMAT[:].rearrange("p a r (g h) -> p a r g h", g=8)
                NCMv = NCM[:].rearrange("p a (g h) -> p a g h", g=8)
                for pr in range(16):
                    in0 = CC[:, pr, 0, :].unsqueeze(1).to_broadcast([128, 8, 16])
                    in1 = MASKC[:, pr % 4, :].unsqueeze(2).to_broadcast([128, 8, 16])
                    V(lambda e, pr=pr, in0=in0, in1=in1: e.scalar_tensor_tensor(
                        out=NCMv[:, pr, :, :], in0=in0, scalar=-1.0, in1=in1, op0=ALU.mult, op1=ALU.mult), ck, ck)
                for pr in range(16):
                    for ri in range(2):
                        in0 = CC[:, pr, ri, :].unsqueeze(1).to_broadcast([128, 8, 16])
                        in1 = MASKC[:, pr % 4, :].unsqueeze(2).to_broadcast([128, 8, 16])
                        if ri == 0:
                            V(lambda e, pr=pr, in0=in0, in1=in1: e.tensor_tensor(out=CMv[:, pr, 0, :, :], in0=in0, in1=in1,
                                                                               op=ALU.mult), ck, ck)
                        else:
                            V(lambda e, pr=pr, in0=in0, in1=in1: e.scalar_tensor_tensor(
                                out=CMv[:, pr, 1, :, :], in0=in0, scalar=-1.0, in1=in1, op0=ALU.mult, op1=ALU.mult),
                                ck, ck)
                uview = uTd.rearrange("p (k t) -> p k t", k=4)
                mview = mixT.rearrange("(k p) t -> p k t", p=128)
                bring = [0]
                def uload(tt):
                    us = tt % 2
                    tr.dma('sp', 'uin%d' % us, UT[us][:], uview[:, :, tt * 512:tt * 512 + 512], r=['uTd'], w=['ut%d' % us])
                def pre(cc, jj):
                    tt, sub = divmod(cc, 4)
                    us = tt % 2; uk = 'ut%d' % us; U = UT[us]
                    tsl = slice(sub * 128, (sub + 1) * 128)
                    kt, half = divmod(jj, 2)
                    i = bring[0] % 2
                    bring[0] += 1
                    PB = pb[i]
                    pk = 'pbu%d' % i
                    pr0 = 4 * kt + 2 * half
                    tr.op('pe', lambda e: e.matmul(
                        out=PB[:], lhsT=U[:, kt, tsl], rhs=BBD[:, kt, half * 512:(half + 1) * 512],
                        start=True, stop=True), r=[uk, 's5c'], w=[pk])
                    PBv = PB[:].rearrange("p (a r x) -> p a r x", a=2, r=2)
                    pre_r = PRE_RE[:, pr0 * 128:pr0 * 128 + 256].rearrange("p (a x) -> p a x", a=2) \
                        .unsqueeze(2).to_broadcast([128, 2, 2, 128])
                    pre_i = PRE_IM[:, pr0 * 128:pr0 * 128 + 256].rearrange("p (a x) -> p a x", a=2) \
                        .unsqueeze(2).to_broadcast([128, 2, 2, 128])
                    t1 = T1B[:, jj, :].rearrange("p (a r x) -> p a r x", a=2, r=2)
                    t2 = T2B[:, jj, :].rearrange("p (a r x) -> p a r x", a=2, r=2)
                    tr.op('dve', lambda e: e.tensor_tensor(out=t1, in0=PBv, in1=pre_r, op=ALU.mult),
                          r=[pk, 's5c'], w=['t1_%d' % jj])
                    tr.op('dve', lambda e: e.tensor_tensor(out=t2, in0=PBv, in1=pre_i, op=ALU.mult),
                          r=[pk, 's5c'], w=['t2_%d' % jj])
                def post_a(cc, jj):
                    pg = jj
                    i = pg % 2
                    PZ = pb[2 + i]
                    zk = 'pz%d' % i
                    pr0 = 2 * pg
                    PZv = PZ[:].rearrange("p (a r x) -> p a r x", a=2, r=2)
                    t1 = T1B[:, jj, :].rearrange("p (a r x) -> p a r x", a=2, r=2)
                    t2 = T2B[:, jj, :].rearrange("p (a r x) -> p a r x", a=2, r=2)
                    rk = ['t1_%d' % jj, 't2_%d' % jj, 's5c']
                    for a in range(2):
                        tr.op('pe', lambda e, a=a: e.matmul(out=PZv[:, a, 0, :], lhsT=t1[:, a, 0, :], rhs=TRI[:],
                                                            start=True, stop=False, skip_group_check=True), r=rk, w=[zk])
                        tr.op('pe', lambda e, a=a: e.matmul(out=PZv[:, a, 0, :], lhsT=t2[:, a, 1, :], rhs=NTRI[:],
                                                            start=False, stop=True, skip_group_check=True), r=rk, w=[zk])
                        tr.op('pe', lambda e, a=a: e.matmul(out=PZv[:, a, 1, :], lhsT=t1[:, a, 1, :], rhs=TRI[:],
                                                            start=True, stop=False, skip_group_check=True), r=rk, w=[zk])
                        tr.op('pe', lambda e, a=a: e.matmul(out=PZv[:, a, 1, :], lhsT=t2[:, a, 0, :], rhs=TRI[:],
                                                            start=False, stop=True, skip_group_check=True), r=rk, w=[zk])
                    zck = 'zc%d' % pg
                    for a in range(2):
                        for ri in range(2):
                            CB_ = CRE if ri == 0 else CIM
                            tr.op('act', lambda e, a=a, ri=ri, CB_=CB_: e.activation(
                                out=ZCF[:, pr0 + a, ri, :], in_=PZv[:, a, ri, :], func=AF.Identity,
                                bias=CB_[:, pr0 + a:pr0 + a + 1]), r=[zk, 'carry'], w=[zck])
                def post_b(cc, jj):
                    pg = jj
                    pr0 = 2 * pg
                    zck = 'zc%d' % pg
                    Z = ZCF[:, pr0:pr0 + 2, :, :]
                    po_r = POST_RE[:, pr0:pr0 + 2, :].unsqueeze(2).to_broadcast([128, 2, 2, 128])
                    po_i = POST_IM[:, pr0:pr0 + 2, :].unsqueeze(2).to_broadcast([128, 2, 2, 128])
                    tr.op('dve', lambda e: e.tensor_tensor(out=XU1[:, pr0:pr0 + 2, :, :], in0=Z, in1=po_r, op=ALU.mult),
                          r=[zck, 's5c'], w=['x%d' % pg])
                    tr.op('dve', lambda e: e.tensor_tensor(out=XU2[:, pr0:pr0 + 2, :, :], in0=Z, in1=po_i, op=ALU.mult),
                          r=[zck, 's5c'], w=['x%d' % pg])
                def post_end(cc):
                    tt, sub = divmod(cc, 4)
                    us = tt % 2; uk = 'ut%d' % us; U = UT[us]
                    tsl = slice(sub * 128, (sub + 1) * 128)
                    zall = ['zc%d' % pg for pg in range(8)]
                    zr = ZCF[:, :, 0, 127]
                    zi = ZCF[:, :, 1, 127]
                    tr.op('dve', lambda e: e.tensor_tensor(out=TQ[0][:], in0=zr, in1=A128[:, 0, :], op=ALU.mult),
                          r=zall + ['s5c'], w=['tq'])
                    tr.op('dve', lambda e: e.tensor_tensor(out=TQ[1][:], in0=zi, in1=A128[:, 1, :], op=ALU.mult),
                          r=zall + ['s5c'], w=['tq'])
                    tr.op('dve', lambda e: e.tensor_tensor(out=TQ[2][:], in0=zr, in1=A128[:, 1, :], op=ALU.mult),
                          r=zall + ['s5c'], w=['tq'])
                    tr.op('dve', lambda e: e.tensor_tensor(out=TQ[3][:], in0=zi, in1=A128[:, 0, :], op=ALU.mult),
                          r=zall + ['s5c'], w=['tq'])
                    tr.op('dve', lambda e: e.tensor_tensor(out=CRE[:], in0=TQ[0][:], in1=TQ[1][:], op=ALU.subtract),
                          r=['tq'], w=['carry'])
                    tr.op('dve', lambda e: e.tensor_tensor(out=CIM[:], in0=TQ[2][:], in1=TQ[3][:], op=ALU.add),
                          r=['tq'], w=['carry'])
                def cmm(cc, kt):
                    i = kt % 2
                    PY = pb[4 + i]
                    yk = 'py%d' % i
                    n = 0
                    for a in range(4):
                        pr = 4 * kt + a
                        terms = ((CMAT[:, pr, 0, :], XU1[:, pr, 0, :]), (NCM[:, pr, :], XU2[:, pr, 1, :]),
                                 (CMAT[:, pr, 1, :], XU1[:, pr, 1, :]), (CMAT[:, pr, 1, :], XU2[:, pr, 0, :]))
                        for (cm, xx) in terms:
                            tr.op('pe', lambda e, PY=PY, cm=cm, xx=xx, n=n: e.matmul(
                                out=PY[:, 0:128], lhsT=cm, rhs=xx,
                                start=(n == 0), stop=(n == 15)), r=['s5c', 'x%d' % (pr // 2)], w=[yk])
                            n += 1
                def yop(cc, kt):
                    tt, sub = divmod(cc, 4)
                    us = tt % 2; uk = 'ut%d' % us; U = UT[us]
                    tsl = slice(sub * 128, (sub + 1) * 128)
                    PY = pb[4 + kt % 2]
                    tr.op('dve', lambda e: e.scalar_tensor_tensor(
                        out=Y[:, kt, tsl], in0=U[:, kt, tsl], scalar=DCOL[:, kt:kt + 1], in1=PY[:, 0:128],
                        op0=ALU.mult, op1=ALU.add), r=[uk, 'py%d' % (kt % 2), 's5c'], w=['y'])
                def tail(tt):
                    t0 = tt * 512
                    Yf = ROW[0][:]
                    YBf = ROW[1][:]
                    YGf = ROW[2][:]
                    tr.op('act', lambda e: e.activation(out=YBf, in_=Yf, func=AF.Square), r=['y'], w=['yb'])
                    tr.op('act', lambda e: e.activation(out=YBf, in_=YBf, func=AF.Identity, scale=0.044715,
                                                       bias=ONEC[:, 0:1]), r=['yb', 'c'], w=['yb'])
                    tr.op('dve', lambda e: e.tensor_tensor(out=YBf, in0=YBf, in1=Yf, op=ALU.mult), r=['yb', 'y'], w=['yb'])
                    tr.op('act', lambda e: e.activation(out=YBf, in_=YBf, func=AF.Sigmoid, scale=GELU_C), r=['yb'], w=['yb'])
                    tr.op('dve', lambda e: e.tensor_tensor(out=YGf, in0=Yf, in1=YBf, op=ALU.mult), r=['y', 'yb'], w=['yg'])
                    tr.op('act', lambda e: e.activation(out=YGB[:].rearrange("p a b -> p (a b)"), in_=YGf, func=AF.Copy),
                          r=['yg'], w=['ygb'])
                    for mt in range(4):
                        i = mt % 2
                        PG = pb[6 + i]
                        gk = 'pgl%d' % i
                        for kt in range(4):
                            tr.op('pe', lambda e, PG=PG, kt=kt, mt=mt: e.matmul(
                                out=PG[:], lhsT=WGL[:, kt, mt * 128:(mt + 1) * 128], rhs=YGB[:, kt, :],
                                start=(kt == 0), stop=(kt == 3)), r=['s5c', 'ygb'], w=[gk])
                        tr.op('act', lambda e, PG=PG, mt=mt, i=i: e.activation(
                            out=SGL[i][:], in_=PG[:], func=AF.Sigmoid, bias=BGL[:, mt:mt + 1]), r=[gk, 's5c'],
                            w=['sgl%d' % i])
                        tr.op('dve', lambda e, mt=mt, i=i: e.tensor_tensor(out=S[:, mt, :], in0=YG[:, mt, :],
                                                                          in1=SGL[i][:], op=ALU.mult),
                              r=['yg', 'sgl%d' % i], w=['s'])
                    tr.op('act', lambda e: e.activation(out=SQ[:].rearrange("p a b -> p (a b)"),
                                                       in_=TH[:], func=AF.Square),
                          r=['s'], w=['sq'])
                    PN = pb[6]
                    for mt in range(4):
                        tr.op('pe', lambda e, mt=mt: e.matmul(out=PN[:], lhsT=ONES[:], rhs=SQ[:, mt, :],
                                                             start=(mt == 0), stop=(mt == 3)), r=['sq', 'c'], w=['pgl0'])
                    tr.op('dve', lambda e: e.tensor_scalar(out=RS5[:], in0=PN[:], scalar1=1.0 / 512, scalar2=EPS,
                                                          op0=ALU.mult, op1=ALU.add), r=['pgl0'], w=['rs5'])
                    tr.op('act', lambda e: e.activation(out=RS5[:], in_=RS5[:], func=AF.Ln), r=['rs5'], w=['rs5'])
                    tr.op('act', lambda e: e.activation(out=RS5[:], in_=RS5[:], func=AF.Exp, scale=-0.5),
                          r=['rs5'], w=['rs5'])
                    for mt in range(4):
                        tr.op('dve', lambda e, mt=mt: e.scalar_tensor_tensor(
                            out=OUT[:, mt, :], in0=S[:, mt, :], scalar=GC[:, 32 + mt:33 + mt], in1=RS5[:],
                            op0=ALU.mult, op1=ALU.mult), r=['s', 'rs5', 'c'], w=['out5'])
                    tr.dma('sp', 'sout', mview[:, 4:8, t0:t0 + 512], OUT[:], r=['out5'], w=['mixT'])
                uload(0)
                uload(1)
                for jj in range(8):
                    pre(0, jj)
                for cc in range(32):
                    post_a(cc, 0)
                    for jj in range(8):
                        if jj + 1 < 8:
                            post_a(cc, jj + 1)
                        if cc + 1 < 32:
                            pre(cc + 1, jj)
                        post_b(cc, jj)
                        if jj % 2 == 1 and jj >= 3:
                            cmm(cc, jj // 2 - 1)
                            if jj >= 5:
                                yop(cc, jj // 2 - 2)
                    cmm(cc, 3)
                    post_end(cc)
                    yop(cc, 2)
                    yop(cc, 3)
                    if cc % 4 == 3:
                        tail(cc // 4)
                        if cc // 4 + 2 < 8:
                            uload(cc // 4 + 2)
            tr.barrier()

        def wout_phase(after_wo=None):
            with contextlib.ExitStack() as w_:
                def wsb(name, shape, dt):
                    return w_.enter_context(nc.sbuf_tensor(name, list(shape), dt))
                WO = wsb("WO", [128, 8, D], BF16)
                XW = [wsb("XW%d" % i, [128, 8, 512], F32) for i in range(2)]
                MX = [wsb("MX%d" % i, [128, 8, 512], BF16) for i in range(2)]
                for k in range(8):
                    tr.dma('pool', 'wo', WO[:, k, :], w_out[k * 128:(k + 1) * 128, :], w=['WO'])
                if after_wo is not None:
                    after_wo()
                for tt in range(8):
                    s = tt % 2
                    t0 = tt * 512
                    xk, mk = 'xw%d' % s, 'mx%d' % s
                    tr.dma('sp', 'xwin%d' % s, XW[s][:], xview(x1T, t0, 512), r=['x1T_%d' % tt], w=[xk])
                    tr.dma('act', 'mxin%d' % s, MX[s][:], xview(mixT, t0, 512), r=['mixT'], w=[mk])
                    for dk in range(8):
                        b = dk % 4
                        for k in range(8):
                            tr.op('pe', lambda e, k=k, dk=dk, b=b, s=s: e.matmul(
                                out=pb[b][:], lhsT=WO[:, k, dk * 128:(dk + 1) * 128], rhs=MX[s][:, k, :],
                                start=(k == 0), stop=(k == 7)), r=['WO', mk], w=['pw%d' % b])
                        tr.op('dve', lambda e, dk=dk, b=b, s=s: e.tensor_tensor(
                            out=XW[s][:, dk, :], in0=pb[b][:], in1=XW[s][:, dk, :], op=ALU.add),
                            r=['pw%d' % b, xk], w=[xk])
                    tr.dma('act', 'xwout%d' % s, xview(x1T, t0, 512), XW[s][:], r=[xk], w=['x1T_%d' % tt])
            tr.barrier()

        ffn_phase(1)

        with contextlib.ExitStack() as ms:
            if stop < 1.2:
                return nc
            def msb(name, shape, dt):
                return ms.enter_context(nc.sbuf_tensor(name, list(shape), dt))
            KE = [msb("KE%d" % g, [128, T], BF16) for g in range(2)]
            KW = [msb("KW%d" % g, [64, T], BF16) for g in range(2)]
            VS1 = [msb("VS1%d" % g, [128, 32, 65], BF16) for g in range(2)]
            VW1 = [msb("VW1%d" % g, [128, 32, 65], BF16) for g in range(2)]
            GT = msb("GT", [128, 32, 24], F32)
            KC = [msb("KC%d" % g, [64, 256], BF16) for g in range(2)]
            VCX = [msb("VCX%d" % g, [128, 2, 129], BF16) for g in range(2)]
            for g in range(2):
                tr.dma('pool', 'const', KE[g][64:128, :], c_E0[:, :], w=['KE%d' % g])
                tr.op('dve', lambda e, g=g: e.memset(VS1[g][:, :, 64:65], 1.0), w=['VS1%d' % g])
                tr.op('dve', lambda e, g=g: e.memset(VW1[g][:, :, 64:65], 1.0), w=['VW1%d' % g])
                tr.op('dve', lambda e, g=g: e.memset(VCX[g][:, :, 64:65], 1.0), w=['VCX%d' % g])
                tr.dma('pool', 'const', VCX[g][:, :, 65:129], c_ovl.rearrange("p (a b) -> p a b", a=2),
                       w=['VCX%d' % g])

            with contextlib.ExitStack() as ps_:
                def psb(name, shape, dt):
                    return ps_.enter_context(nc.sbuf_tensor(name, list(shape), dt))
                WIN = psb("WIN", [128, 8, 1816], BF16)
                H2 = [psb("H2_%d" % i, [128, 8, 512], BF16) for i in range(2)]
                QST = psb("QST", [64, 4, 8, 128], BF16)
                UST = psb("UST", [128, 4, 512], BF16)
                KCin = [psb("KCin%d" % g, [64, 16, 256], BF16) for g in range(2)]
                VCin = [psb("VCin%d" % g, [64, 16, 256], BF16) for g in range(2)]
                GB = psb("GB", [128, 24], F32)
                W1 = [psb("W1_%d" % i, [64, 32, 256], BF16) for i in range(2)]
                W2 = [psb("W2_%d" % i, [128, 2, 64], BF16) for i in range(2)]
                CB1 = psb("CB1", [128, 4], F32)
                POST = psb("POST", [64, 64], BF16)
                HID = psb("HID", [128, 2, 256], BF16)
                GTMP = [psb("GTMP%d" % i, [128, 256], F32) for i in range(3)]
                PBIAS = psb("PBIAS", [128, 1], F32)
                for k in range(8):
                    tr.dma('pool', 'win', WIN[:, k, :], w_in[k * 128:(k + 1) * 128, :], w=['WIN'])
                tr.dma('sp', 'const', GB[:], gate_bias.partition_broadcast(128), w=['GB'])
                tr.dma('sp', 'const', CB1[:], cb1[:, :], w=['cmpw'])
                tr.dma('pool', 'const', POST[:], posT[:, :], w=['cmpw'])
                for i in range(2):
                    w1v = cw1[i].rearrange("(l d) h -> d l h", d=64)
                    for lc in range(8):
                        tr.dma('pool', 'const', W1[i][:, 4 * lc:4 * lc + 4, :], w1v[:, 4 * lc:4 * lc + 4, :], w=['cmpw'])
                    tr.dma('pool', 'const', W2[i][:], cw2[i].rearrange("(t p) d -> p t d", p=128), w=['cmpw'])
                if stop < 1.6:
                    tr.barrier()
                    return nc
                ring = [0]

                def nextbank():
                    b = ring[0] % 6
                    ring[0] += 1
                    return pb[b], 'pp%d' % b

                evq = [0]

                def evac(out, in_, rk, wk, force=None):
                    evq[0] += 1
                    if (evq[0] % 2 == 0 and force is None) or force == 'act':
                        tr.op('act', lambda e: e.activation(out=out, in_=in_, func=AF.Copy), r=[rk], w=[wk])
                    else:
                        tr.op('dve', lambda e: e.tensor_copy(out=out, in_=in_), r=[rk], w=[wk])

                for tt in range(8):
                    s = tt % 2
                    t0 = tt * 512
                    hk = 'h2_%d' % s
                    HH = H2[s]
                    tr.dma('sp', 'h2in%d' % s, HH[:], xview(h2T, t0, 512), r=['h2T'], w=[hk])
                    for h in range(8):
                        P, pk = nextbank()
                        for k in range(8):
                            tr.op('pe', lambda e, k=k, h=h, P=P: e.matmul(
                                out=P[0:64, :], lhsT=WIN[:, k, 64 * h:64 * h + 64], rhs=HH[:, k, :],
                                start=(k == 0), stop=(k == 7)), r=['WIN', hk], w=[pk])
                        evac(QST[:, :, h, :], P[0:64, :].rearrange("p (c q) -> p c q", q=128), pk, 'qst')
                    tr.dma('sp', 'qout', qTd.rearrange("p (c x) -> p c x", c=32)[:, 4 * tt:4 * tt + 4, :],
                           QST[:].rearrange("p c h q -> p c (h q)"), r=['qst'], w=['qTd'])
                    for (c0, dest, dn) in ((512, KCin, 'KCin'), (640, VCin, 'VCin'), (768, KE, 'KE'), (1024, KW, 'KW')):
                        for g in range(2):
                            P, pk = nextbank()
                            for k in range(8):
                                tr.op('pe', lambda e, k=k, P=P, cc=c0 + 64 * g: e.matmul(
                                    out=P[0:64, :], lhsT=WIN[:, k, cc:cc + 64], rhs=HH[:, k, :],
                                    start=(k == 0), stop=(k == 7)), r=['WIN', hk], w=[pk])
                            if dn in ('KCin', 'VCin'):
                                evac(dest[g][:, :, tt * 32:(tt + 1) * 32],
                                     P[0:64, :].rearrange("p (m r) -> p r m", r=16), pk, '%s%d' % (dn, g))
                            else:
                                evac(dest[g][0:64, t0:t0 + 512], P[0:64, :], pk, '%s%d' % (dn, g))
                    for kt in range(4):
                        P, pk = nextbank()
                        for k in range(8):
                            tr.op('pe', lambda e, k=k, P=P, cc=1304 + 128 * kt: e.matmul(
                                out=P[:, :], lhsT=WIN[:, k, cc:cc + 128], rhs=HH[:, k, :],
                                start=(k == 0), stop=(k == 7)), r=['WIN', hk], w=[pk])
                        evac(UST[:, kt, :], P[:, :], pk, 'ust')
                    tr.dma('sp', 'uout', uTd.rearrange("p (k t) -> p k t", k=4)[:, :, t0:t0 + 512], UST[:],
                           r=['ust'], w=['uTd'])
                    for sbk in range(4):
                        blk = tt * 4 + sbk
                        P, pk = nextbank()
                        for k in range(8):
                            tr.op('pe', lambda e, k=k, P=P, sbk=sbk: e.matmul(
                                out=P[:, 0:408], lhsT=HH[:, k, sbk * 128:(sbk + 1) * 128], rhs=WIN[:, k, 896:1304],
                                start=(k == 0), stop=(k == 7)), r=['WIN', hk], w=[pk])
                        for g in range(2):
                            evac(VS1[g][:, blk, 0:64], P[:, 64 * g:64 * g + 64], pk, 'VS1%d' % g, force='dve')
                            evac(VW1[g][:, blk, 0:64], P[:, 256 + 64 * g:256 + 64 * g + 64], pk, 'VW1%d' % g, force='dve')
                        tr.op('dve', lambda e, P=P, blk=blk: e.tensor_tensor(out=GT[:, blk, :], in0=P[:, 384:408],
                                                                            in1=GB[:], op=ALU.add),
                              r=[pk, 'GB'], w=['GT'])
                        tr.op('act', lambda e, blk=blk: e.activation(out=GT[:, blk, :], in_=GT[:, blk, :],
                                                                    func=AF.Sigmoid), r=['GT'], w=['GT'])

                if stop < 1.8:
                    tr.barrier()
                    return nc
                HIDS = [psb("HIDS%d" % g, [128, 2, 256], BF16) for g in range(2)]
                for g in range(2):
                    tr.op('dve', lambda e, g=g: e.memset(HIDS[g][:], 0.0), w=['hid%d_0' % g, 'hid%d_1' % g])
                A, B_, C_ = GTMP
                for kind in range(2):
                    srcs = KCin if kind == 0 else VCin
                    sname = 'KCin' if kind == 0 else 'VCin'
                    for hh in range(2):
                        P, pk = nextbank()
                        for l in range(32):
                            tr.op('pe', lambda e, l=l, P=P, hh=hh: e.matmul(
                                out=P[:, 0:1], lhsT=W1[kind][:, l, hh * 128:(hh + 1) * 128],
                                rhs=POST[:, kind * 32 + l:kind * 32 + l + 1], start=(l == 0), stop=(l == 31)),
                                r=['cmpw'], w=[pk])
                        tr.op('dve', lambda e, P=P, hh=hh: e.tensor_tensor(
                            out=PBIAS[:], in0=P[:, 0:1], in1=CB1[:, kind * 2 + hh:kind * 2 + hh + 1], op=ALU.add),
                            r=[pk, 'cmpw'], w=['pbias'])
                        for g in range(2):
                            P2, pk2 = nextbank()
                            for l in range(32):
                                rhs = srcs[g][:, l % 16, (l // 16):(l // 16) + 255]
                                tr.op('pe', lambda e, l=l, P2=P2, rhs=rhs, hh=hh: e.matmul(
                                    out=P2[:, 0:255], lhsT=W1[kind][:, l, hh * 128:(hh + 1) * 128], rhs=rhs,
                                    start=(l == 0), stop=(l == 31)), r=['cmpw', '%s%d' % (sname, g)], w=[pk2])
                            tr.op('dve', lambda e, P2=P2: e.tensor_scalar(out=A[:, 0:255], in0=P2[:, 0:255],
                                                                         scalar1=PBIAS[:, 0:1], scalar2=None,
                                                                         op0=ALU.add), r=[pk2, 'pbias'], w=['ga'])
                            tr.op('dve', lambda e: e.tensor_tensor(out=B_[:, 0:255], in0=A[:, 0:255], in1=A[:, 0:255],
                                                                  op=ALU.mult), r=['ga'], w=['gb'])
                            tr.op('dve', lambda e: e.tensor_scalar(out=B_[:, 0:255], in0=B_[:, 0:255], scalar1=0.044715,
                                                                  scalar2=1.0, op0=ALU.mult, op1=ALU.add),
                                  r=['gb'], w=['gb'])
                            tr.op('dve', lambda e: e.tensor_tensor(out=B_[:, 0:255], in0=B_[:, 0:255], in1=A[:, 0:255],
                                                                  op=ALU.mult), r=['gb', 'ga'], w=['gb'])
                            tr.op('act', lambda e: e.activation(out=C_[:, 0:255], in_=B_[:, 0:255], func=AF.Sigmoid,
                                                               scale=GELU_C), r=['gb'], w=['gc'])
                            tr.op('dve', lambda e, g=g, hh=hh: e.tensor_tensor(
                                out=HIDS[g][:, hh, 0:255], in0=A[:, 0:255], in1=C_[:, 0:255], op=ALU.mult),
                                r=['ga', 'gc'], w=['hid%d_%d' % (g, hh)])
                    for g in range(2):
                        hk2 = ['hid%d_0' % g, 'hid%d_1' % g]
                        if kind == 0:
                            P, pk = nextbank()
                            for hh in range(2):
                                tr.op('pe', lambda e, hh=hh, P=P, g=g: e.matmul(
                                    out=P[0:64, 0:256], lhsT=W2[0][:, hh, :], rhs=HIDS[g][:, hh, :],
                                    start=(hh == 0), stop=(hh == 1)), r=['cmpw'] + hk2, w=[pk])
                            evac(KC[g][:, :], P[0:64, 0:256], pk, 'KC%d' % g)
                        else:
                            for nt in range(2):
                                P, pk = nextbank()
                                for hh in range(2):
                                    tr.op('pe', lambda e, hh=hh, P=P, g=g, nt=nt: e.matmul(
                                        out=P[:, 0:64], lhsT=HIDS[g][:, hh, nt * 128:(nt + 1) * 128],
                                        rhs=W2[1][:, hh, :], start=(hh == 0), stop=(hh == 1)),
                                        r=['cmpw'] + hk2, w=[pk])
                                evac(VCX[g][:, nt, 0:64], P[:, 0:64], pk, 'VCX%d' % g)
            tr.barrier()
            if stop >= 3:
                attention_phase()
        if stop >= 4:
            s5_phase()
        with contextlib.ExitStack() as w2s:
            wts2 = ffn_weights(2, w2s, load=False) if stop >= 6 else None
            if stop >= 5:
                wout_phase((lambda: ffn_wload(2, wts2, gu=True, wd=False)) if stop >= 6 else None)
            if stop >= 6:
                ffn_phase(2, wts2)
    return nc


def _consts():
    f = np.float32
    c = {}
    c["c_ident"] = np.eye(128, dtype=f)
    j = np.arange(128)
    c["c_tri"] = (j[:, None] <= j[None, :]).astype(f)
    c["c_E0"] = (np.arange(T)[None, :] // 64 == np.arange(64)[:, None]).astype(f)
    cb = np.where(j[:, None] > j[None, :], -BIGM, 0.0).astype(f)
    wb = np.where(j[:, None] <= j[None, :], -BIGM, 0.0).astype(f)
    cm = np.where(16 * (j[:, None] - 64) > j[None, :] - 31, -BIGM, 0.0).astype(f)
    c["c_CB"] = np.tile(cb, (1, 4))
    c["c_WB"] = np.tile(wb, (1, 4))
    c["c_CM"] = np.tile(cm, (1, 4))
    c["c_BD"] = (np.arange(384)[None, :] == j[:, None] + 128).astype(f)
    hh = (j >= 64).astype(np.int64)[:, None]
    m = np.arange(128)[None, :]
    c["c_U0"] = np.where(m > 62 + hh, -1e30, 3e38).astype(f)
    l0 = np.full((128, 128), -3e38, dtype=f)
    l0[m == 62 + hh] = 2e30
    l0[m == 61 + hh] = 1e30
    c["c_L0"] = l0
    n = np.arange(256)[:, None]
    jj = np.arange(64)[None, :]
    ov = ((16 * n < 64 * jj + 64) & (16 * n + 32 > 64 * jj)).astype(f)
    ov[255] = 0
    c["c_ovl"] = np.ascontiguousarray(ov.reshape(2, 128, 64).transpose(1, 0, 2).reshape(128, 128))
    c["c_maskB"] = (np.arange(8)[None, :] == (j // 16)[:, None]).astype(f)
    gi = (j // 64)[:, None, None]
    p4 = np.arange(4)[None, :, None]
    g8 = np.arange(8)[None, None, :]
    c["c_maskC"] = (g8 == 2 * p4 + gi).astype(f).reshape(128, 32)
    c["c_iota"] = np.tile(np.arange(128, dtype=f)[None, :], (128, 1))
    c["c_jcol"] = np.arange(128, dtype=f)[:, None].copy()
    return c


def _prep_shared(inp):
    f = np.float32
    A = lambda a: np.ascontiguousarray(np.asarray(a, dtype=f))
    d = {}
    d["wg1"], d["wu1"], d["wd1"] = A(inp["ffn1_w_gate"][0]), A(inp["ffn1_w_up"][0]), A(inp["ffn1_w_down"][0])
    d["wg2"], d["wu2"], d["wd2"] = A(inp["ffn2_w_gate"][0]), A(inp["ffn2_w_up"][0]), A(inp["ffn2_w_down"][0])
    d["w_in"], d["w_out"], d["w_glu"] = A(inp["w_in"][0]), A(inp["w_out"][0]), A(inp["s5_w_glu"][0])
    col8 = lambda v: np.asarray(v, dtype=f).reshape(-1, 128).T
    d["gcols"] = A(np.concatenate([col8(inp["ffn1_norm"][0]), col8(inp["mix_norm"][0]), col8(inp["ffn2_norm"][0]),
                                   col8(inp["final_norm"]), col8(inp["ssm_out_norm"][0])], axis=1))
    d["grow_attn"] = A(np.asarray(inp["attn_out_norm"][0]).reshape(1, 512))
    d["gate_bias"] = A(np.asarray(inp["gate_bias"][0]).reshape(1, 24))
    d["cw1k"], d["cw1v"] = A(inp["cmp_k_w1"][0]), A(inp["cmp_v_w1"][0])
    d["cw2k"], d["cw2v"] = A(inp["cmp_k_w2"][0]), A(inp["cmp_v_w2"][0])
    d["cb1"] = A(np.concatenate([col8(inp["cmp_k_b1"][0]), col8(inp["cmp_v_b1"][0])], axis=1))
    d["posT"] = A(np.concatenate([np.asarray(inp["cmp_pos_k"][0]).T, np.asarray(inp["cmp_pos_v"][0]).T], axis=1))
    lr = np.asarray(inp["s5_lambda_re"][0], dtype=f)
    li = np.asarray(inp["s5_lambda_im"][0], dtype=f)
    ldt = np.repeat(np.asarray(inp["s5_log_dt"][0], dtype=f)[:, None], 64, axis=1)
    d["s5_rows"] = A(np.stack([lr.reshape(2048), li.reshape(2048), ldt.reshape(2048)]))
    colify = lambda a: a.reshape(16, 128).T
    d["s5_cols"] = A(np.concatenate([colify(lr), colify(li), colify(ldt)], axis=1))

    def rows_gh(a):
        return np.repeat(a.reshape(4, 8, 1, 64), 16, axis=2).transpose(1, 2, 0, 3).reshape(128, 4, 64)
    d["s5_rep"] = A(np.stack([rows_gh(lr), rows_gh(li), rows_gh(ldt)], axis=2).reshape(128, 4 * 3 * 64))

    def bt(a):
        return np.asarray(a, dtype=f).reshape(4, 8, 64, 16).transpose(1, 3, 0, 2).reshape(128, 4, 64)
    d["s5_bT"] = A(np.stack([bt(inp["s5_b_re"][0]), bt(inp["s5_b_im"][0])], axis=2).reshape(128, 4 * 2 * 64))

    def ct(a):
        return np.asarray(a, dtype=f).reshape(16, 2, 16, 64).transpose(1, 3, 0, 2).reshape(128, 16, 16)
    d["s5_c"] = A(np.stack([ct(inp["s5_c_re"][0]), ct(inp["s5_c_im"][0])], axis=2).reshape(128, 16 * 2 * 16))
    d["s5_dcol"] = A(np.asarray(inp["s5_d"][0], dtype=f).reshape(4, 128).T)
    d["bglu_col"] = A(np.asarray(inp["s5_b_glu"][0], dtype=f).reshape(4, 128).T)
    d.update(_consts())
    return d


def kernel(**inputs):
    x = np.asarray(inputs["x"], dtype=np.float32)
    shared = _prep_shared(inputs)
    nc = build(dbg=False)
    in_maps = []
    for b in range(NCORES):
        m = dict(shared)
        m["xT"] = np.ascontiguousarray(x[b].T)
        in_maps.append(m)
    res = run_bass_kernel_spmd(nc, in_maps, core_ids=list(range(NCORES)))
    out = np.empty((NCORES, T, D), dtype=np.float32)
    for b in range(NCORES):
        out[b] = np.asarray(res.results[b]["yT"], dtype=np.float32).T
    return out
```
